# Optimizing a Trainium2 kernel written in Bass

```python
import numpy as np
import jax
import jax.numpy as jnp
from jax import lax

D_MODEL = 2048
BATCH = 4
SEQ = 4096
DEPTH = 1

POOL_WINDOWS = (2, 4, 8, 16)
POOL_GROUPS = 4
POOL_WIDTH = D_MODEL // 2
POOL_GROUP_DIM = POOL_WIDTH // POOL_GROUPS

HEAD_DIM = 64
N_HEADS = (D_MODEL // 2) // HEAD_DIM
N_KV_GROUPS = 4
HEADS_PER_GROUP = N_HEADS // N_KV_GROUPS
Q_WIDTH = N_HEADS * HEAD_DIM
KV_WIDTH = N_KV_GROUPS * HEAD_DIM
CMP_BLOCK = 32
CMP_STRIDE = 16
CMP_HIDDEN = 256
SEL_BLOCK = 64
SEL_TOP_N = 16
WINDOW = 512
WIN_Q_BLOCK = 128
SEL_Q_BLOCK = 64
ROPE_THETA = 10000.0
FORCE_SCORE = 1e6

N_EXPERT_GROUPS = 8
EXPERTS_PER_GROUP = 8
N_EXPERTS = N_EXPERT_GROUPS * EXPERTS_PER_GROUP
EXPERT_HIDDEN = D_MODEL // 4
TOP_K = 2
EXPERT_ROW_BLOCK = 128

DN_ALPHA = (2.0 * DEPTH) ** 0.25
DN_BETA = (8.0 * DEPTH) ** -0.25
LN_EPS = 1e-5

IN_SPLIT_SIZES = (POOL_WIDTH, Q_WIDTH, KV_WIDTH, KV_WIDTH, KV_WIDTH, KV_WIDTH, KV_WIDTH, KV_WIDTH, 3 * N_HEADS, 2 * D_MODEL)
IN_COLS = POOL_WIDTH + Q_WIDTH + 6 * KV_WIDTH + 3 * N_HEADS + 2 * D_MODEL

kernel_name = "hybrid_pool_nsa_hmoe_deepnorm"


def layer_norm(x, g, b):
    xf = x.astype(jnp.float32)
    mu = jnp.mean(xf, axis=-1, keepdims=True)
    var = jnp.mean(jnp.square(xf - mu), axis=-1, keepdims=True)
    y = (xf - mu) * lax.rsqrt(var + LN_EPS) * g.astype(jnp.float32) + b.astype(jnp.float32)
    return y.astype(x.dtype)


def rope(x, pos):
    half = HEAD_DIM // 2
    inv_freq = ROPE_THETA ** (-2.0 * jnp.arange(half, dtype=jnp.float32) / HEAD_DIM)
    ang = pos.astype(jnp.float32)[:, None] * inv_freq[None, :]
    cos = jnp.cos(ang).astype(x.dtype)
    sin = jnp.sin(ang).astype(x.dtype)
    x1, x2 = x[..., :half], x[..., half:]
    return jnp.concatenate([x1 * cos - x2 * sin, x2 * cos + x1 * sin], axis=-1)


def masked_softmax(s, mask):
    s = s.astype(jnp.float32)
    m = jnp.max(jnp.where(mask, s, -jnp.inf), axis=-1, keepdims=True)
    e = jnp.exp(jnp.where(mask, s - m, -jnp.inf))
    return e / jnp.maximum(jnp.sum(e, axis=-1, keepdims=True), 1e-30)


def to_heads(t, n):
    b, s, _ = t.shape
    return t.reshape(b, s, n, HEAD_DIM).transpose(0, 2, 1, 3)


def pool_mixer(u, pool_mix, pool_scale):
    b, s, _ = u.shape
    ug = u.reshape(b, s, POOL_GROUPS, POOL_GROUP_DIM)
    cs = jnp.pad(jnp.cumsum(ug.astype(jnp.float32), axis=1), ((0, 0), (1, 0), (0, 0), (0, 0)))
    t = jnp.arange(s)
    means = []
    for g, w in enumerate(POOL_WINDOWS):
        cg = cs[:, :, g]
        lo = jnp.maximum(t + 1 - w, 0)
        cnt = jnp.minimum(t + 1, w).astype(jnp.float32)
        means.append((cg[:, 1:] - cg[:, lo]) / cnt[None, :, None])
    pooled = jnp.stack(means, axis=2).astype(u.dtype) - ug
    mixed = jnp.einsum('bsgc,gcd->bsgd', pooled, pool_mix)
    return mixed.reshape(b, s, POOL_WIDTH) * pool_scale


def compress_blocks(blocks, pos_emb, w1, w2):
    z = blocks + pos_emb
    z = z.reshape(z.shape[:-2] + (CMP_BLOCK * HEAD_DIM,))
    return jax.nn.gelu(z @ w1) @ w2


def native_sparse_attention(q, kc, vc, ks, vs, kw, vw, g_nsa, cmp_pos_k, cmp_pos_v,
                            cmp_k_w1, cmp_k_w2, cmp_v_w1, cmp_v_w2):
    b, s, _ = q.shape
    G, HG = N_KV_GROUPS, HEADS_PER_GROUP
    scale = HEAD_DIM ** -0.5
    pos = jnp.arange(s)
    qh = rope(to_heads(q, N_HEADS), pos).reshape(b, G, HG, s, HEAD_DIM)

    n_cmp = (s - CMP_BLOCK) // CMP_STRIDE + 1
    blk_idx = np.arange(n_cmp)[:, None] * CMP_STRIDE + np.arange(CMP_BLOCK)[None, :]
    cmp_end = blk_idx[:, -1]
    k_cmp = compress_blocks(to_heads(kc, G)[:, :, blk_idx], cmp_pos_k, cmp_k_w1, cmp_k_w2)
    k_cmp = rope(k_cmp, jnp.asarray(cmp_end))
    v_cmp = compress_blocks(to_heads(vc, G)[:, :, blk_idx], cmp_pos_v, cmp_v_w1, cmp_v_w2)
    s_cmp = jnp.einsum('bghqd,bgcd->bghqc', qh, k_cmp) * scale
    cmp_mask = jnp.asarray(cmp_end)[None, :] <= pos[:, None]
    p_cmp = masked_softmax(s_cmp, cmp_mask)
    o_cmp = jnp.einsum('bghqc,bgcd->bghqd', p_cmp.astype(v_cmp.dtype), v_cmp)

    n_sel = s // SEL_BLOCK
    top_n = min(SEL_TOP_N, n_sel)
    c_start = np.arange(n_cmp) * CMP_STRIDE
    s_start = np.arange(n_sel) * SEL_BLOCK
    overlap = ((c_start[:, None] + CMP_BLOCK - 1 >= s_start[None, :]) &
               (c_start[:, None] <= s_start[None, :] + SEL_BLOCK - 1)).astype(np.float32)
    imp = jnp.einsum('bghqc,cj->bgqj', p_cmp, jnp.asarray(overlap))
    cur = pos // SEL_BLOCK
    j = jnp.arange(n_sel)
    forced = (j[None, :] == 0) | (j[None, :] == cur[:, None]) | (j[None, :] == cur[:, None] - 1)
    future = j[None, :] > cur[:, None]
    imp = jnp.where(forced, FORCE_SCORE, jnp.where(future, -FORCE_SCORE, imp))
    _, sel_idx = lax.top_k(imp, top_n)

    k_sb = rope(to_heads(ks, G), pos).reshape(b, G, n_sel, SEL_BLOCK, HEAD_DIM)
    v_sb = to_heads(vs, G).reshape(b, G, n_sel, SEL_BLOCK, HEAD_DIM)
    nqb = s // SEL_Q_BLOCK
    q_blk = qh.reshape(b, G, HG, nqb, SEL_Q_BLOCK, HEAD_DIM).transpose(3, 0, 1, 2, 4, 5)
    idx_blk = sel_idx.reshape(b, G, nqb, SEL_Q_BLOCK, top_n).transpose(2, 0, 1, 3, 4)
    bi = jnp.arange(b)[:, None, None, None]
    gi = jnp.arange(G)[None, :, None, None]

    def sel_step(args):
        qb, ib, blk = args
        kg = k_sb[bi, gi, ib]
        vg = v_sb[bi, gi, ib]
        sc = jnp.einsum('bghqd,bgqnkd->bghqnk', qb, kg) * scale
        tq = blk * SEL_Q_BLOCK + jnp.arange(SEL_Q_BLOCK)
        kpos = ib[..., None] * SEL_BLOCK + jnp.arange(SEL_BLOCK)
        mask = (kpos <= tq[:, None, None]).reshape(b, G, 1, SEL_Q_BLOCK, top_n * SEL_BLOCK)
        p = masked_softmax(sc.reshape(b, G, HG, SEL_Q_BLOCK, top_n * SEL_BLOCK), mask)
        p = p.reshape(sc.shape).astype(vg.dtype)
        return jnp.einsum('bghqnk,bgqnkd->bghqd', p, vg)

    o_slc = lax.map(sel_step, (q_blk, idx_blk, jnp.arange(nqb)))
    o_slc = o_slc.transpose(1, 2, 3, 0, 4, 5).reshape(b, G, HG, s, HEAD_DIM)

    k_w = rope(to_heads(kw, G), pos)
    v_w = to_heads(vw, G)
    kpad = jnp.pad(k_w, ((0, 0), (0, 0), (WINDOW, 0), (0, 0)))
    vpad = jnp.pad(v_w, ((0, 0), (0, 0), (WINDOW, 0), (0, 0)))
    span = WINDOW + WIN_Q_BLOCK
    nwb = s // WIN_Q_BLOCK
    q_wblk = qh.reshape(b, G, HG, nwb, WIN_Q_BLOCK, HEAD_DIM).transpose(3, 0, 1, 2, 4, 5)

    def win_step(args):
        qb, blk = args
        start = blk * WIN_Q_BLOCK
        kb = lax.dynamic_slice_in_dim(kpad, start, span, axis=2)
        vb = lax.dynamic_slice_in_dim(vpad, start, span, axis=2)
        sc = jnp.einsum('bghqd,bgkd->bghqk', qb, kb) * scale
        tq = start + jnp.arange(WIN_Q_BLOCK)
        kpos = start - WINDOW + jnp.arange(span)
        mask = ((kpos[None, :] <= tq[:, None]) & (kpos[None, :] > tq[:, None] - WINDOW)
                & (kpos[None, :] >= 0))
        p = masked_softmax(sc, mask).astype(vb.dtype)
        return jnp.einsum('bghqk,bgkd->bghqd', p, vb)

    o_win = lax.map(win_step, (q_wblk, jnp.arange(nwb)))
    o_win = o_win.transpose(1, 2, 3, 0, 4, 5).reshape(b, G, HG, s, HEAD_DIM)

    g = jax.nn.sigmoid(g_nsa.astype(jnp.float32)).astype(q.dtype)
    g = g.reshape(b, s, 3, G, HG).transpose(2, 0, 3, 4, 1)[..., None]
    o = g[0] * o_cmp + g[1] * o_slc + g[2] * o_win
    return o.reshape(b, N_HEADS, s, HEAD_DIM).transpose(0, 2, 1, 3).reshape(b, s, Q_WIDTH)


def token_mixer(h, w_in, pool_mix, pool_scale, w_pool_proj, w_nsa_proj, cmp_pos_k, cmp_pos_v,
                cmp_k_w1, cmp_k_w2, cmp_v_w1, cmp_v_w2, w_out):
    proj = h @ w_in
    points = [int(v) for v in np.cumsum(IN_SPLIT_SIZES)[:-1]]
    u_pool, q, kc, vc, ks, vs, kw, vw, g_nsa, g_merge = jnp.split(proj, points, axis=-1)
    y_pool = pool_mixer(u_pool, pool_mix, pool_scale) @ w_pool_proj
    y_attn = native_sparse_attention(q, kc, vc, ks, vs, kw, vw, g_nsa, cmp_pos_k, cmp_pos_v,
                                     cmp_k_w1, cmp_k_w2, cmp_v_w1, cmp_v_w2) @ w_nsa_proj
    gates = jax.nn.sigmoid(g_merge.astype(jnp.float32)).astype(h.dtype)
    g_pool, g_attn = gates[..., :D_MODEL], gates[..., D_MODEL:]
    return (g_pool * y_pool + g_attn * y_attn) @ w_out


def hierarchical_moe(h, router_group_w, router_group_b, router_expert_w, router_expert_b,
                     w_gate, w_up, w_down):
    b, s, d = h.shape
    n = b * s
    t = h.reshape(n, d)
    gl = (t @ router_group_w + router_group_b).astype(jnp.float32)
    gp = jax.nn.softmax(gl, axis=-1)
    grp = jnp.argmax(gl, axis=-1)
    g_gate = jnp.take_along_axis(gp, grp[:, None], axis=1)[:, 0]
    el = jnp.einsum('nd,gde->nge', t, router_expert_w) + router_expert_b
    el = jnp.take_along_axis(el, grp[:, None, None], axis=1)[:, 0].astype(jnp.float32)
    top_v, top_i = lax.top_k(el, TOP_K)
    wts = jax.nn.softmax(top_v, axis=-1) * g_gate[:, None]
    eid = grp[:, None] * EXPERTS_PER_GROUP + top_i
    m = n * TOP_K
    flat_e = eid.reshape(-1)
    order = jnp.argsort(flat_e)
    sorted_e = flat_e[order]
    tok = order // TOP_K
    w_sorted = wts.reshape(-1)[order]
    sizes = jnp.bincount(flat_e, length=N_EXPERTS)
    padded = (sizes + EXPERT_ROW_BLOCK - 1) // EXPERT_ROW_BLOCK * EXPERT_ROW_BLOCK
    ends = jnp.cumsum(padded)
    group_start = jnp.cumsum(sizes) - sizes
    dest = (ends - padded)[sorted_e] + (jnp.arange(m) - group_start[sorted_e])
    n_blk = -(-(m + N_EXPERTS * (EXPERT_ROW_BLOCK - 1)) // EXPERT_ROW_BLOCK)
    rows = n_blk * EXPERT_ROW_BLOCK
    xbuf = jnp.zeros((rows, d), t.dtype).at[dest].set(t[tok])
    blk_e = jnp.minimum(jnp.searchsorted(ends, jnp.arange(n_blk) * EXPERT_ROW_BLOCK, side='right'),
                        N_EXPERTS - 1)

    def expert_block(args):
        xb, e = args
        return (jax.nn.silu(xb @ w_gate[e]) * (xb @ w_up[e])) @ w_down[e]

    ybuf = lax.map(expert_block, (xbuf.reshape(n_blk, EXPERT_ROW_BLOCK, d), blk_e)).reshape(rows, d)
    contrib = ybuf[dest] * w_sorted[:, None].astype(t.dtype)
    y = jnp.zeros_like(t).at[tok].add(contrib)
    return y.reshape(b, s, d)


def setup_inputs(seed: int = 0) -> dict:
    key = jax.random.key(seed)
    ks = jax.random.split(key, 26)
    L, D = DEPTH, D_MODEL

    def nrm(k, shape, scale):
        return jax.random.normal(k, shape, jnp.float32) * scale

    return {
        "x": nrm(ks[0], (BATCH, SEQ, D), 1.0),
        "w_in": nrm(ks[1], (L, D, IN_COLS), D ** -0.5),
        "pool_mix": nrm(ks[2], (L, POOL_GROUPS, POOL_GROUP_DIM, POOL_GROUP_DIM), POOL_GROUP_DIM ** -0.5),
        "pool_scale": 1.0 + nrm(ks[3], (L, POOL_WIDTH), 0.1),
        "w_pool_proj": nrm(ks[4], (L, POOL_WIDTH, D), POOL_WIDTH ** -0.5),
        "w_nsa_proj": nrm(ks[5], (L, Q_WIDTH, D), Q_WIDTH ** -0.5),
        "cmp_pos_k": nrm(ks[6], (L, CMP_BLOCK, HEAD_DIM), 0.1),
        "cmp_pos_v": nrm(ks[7], (L, CMP_BLOCK, HEAD_DIM), 0.1),
        "cmp_k_w1": nrm(ks[8], (L, CMP_BLOCK * HEAD_DIM, CMP_HIDDEN), (CMP_BLOCK * HEAD_DIM) ** -0.5),
        "cmp_k_w2": nrm(ks[9], (L, CMP_HIDDEN, HEAD_DIM), CMP_HIDDEN ** -0.5),
        "cmp_v_w1": nrm(ks[10], (L, CMP_BLOCK * HEAD_DIM, CMP_HIDDEN), (CMP_BLOCK * HEAD_DIM) ** -0.5),
        "cmp_v_w2": nrm(ks[11], (L, CMP_HIDDEN, HEAD_DIM), CMP_HIDDEN ** -0.5),
        "w_out": nrm(ks[12], (L, D, D), D ** -0.5 * DN_BETA),
        "ln1_g": 1.0 + nrm(ks[13], (L, D), 0.05),
        "ln1_b": nrm(ks[14], (L, D), 0.02),
        "router_group_w": nrm(ks[15], (L, D, N_EXPERT_GROUPS), D ** -0.5),
        "router_group_b": nrm(ks[16], (L, N_EXPERT_GROUPS), 0.01),
        "router_expert_w": nrm(ks[17], (L, N_EXPERT_GROUPS, D, EXPERTS_PER_GROUP), D ** -0.5),
        "router_expert_b": nrm(ks[18], (L, N_EXPERT_GROUPS, EXPERTS_PER_GROUP), 0.01),
        "w_gate": nrm(ks[19], (L, N_EXPERTS, D, EXPERT_HIDDEN), D ** -0.5),
        "w_up": nrm(ks[20], (L, N_EXPERTS, D, EXPERT_HIDDEN), D ** -0.5),
        "w_down": nrm(ks[21], (L, N_EXPERTS, EXPERT_HIDDEN, D), EXPERT_HIDDEN ** -0.5 * DN_BETA),
        "ln2_g": 1.0 + nrm(ks[22], (L, D), 0.05),
        "ln2_b": nrm(ks[23], (L, D), 0.02),
    }


def reference(x, w_in, pool_mix, pool_scale, w_pool_proj, w_nsa_proj, cmp_pos_k, cmp_pos_v,
              cmp_k_w1, cmp_k_w2, cmp_v_w1, cmp_v_w2, w_out, ln1_g, ln1_b,
              router_group_w, router_group_b, router_expert_w, router_expert_b,
              w_gate, w_up, w_down, ln2_g, ln2_b):
    h = x
    for l in range(DEPTH):
        y = token_mixer(h, w_in[l], pool_mix[l], pool_scale[l], w_pool_proj[l], w_nsa_proj[l],
                        cmp_pos_k[l], cmp_pos_v[l], cmp_k_w1[l], cmp_k_w2[l], cmp_v_w1[l],
                        cmp_v_w2[l], w_out[l])
        h = layer_norm(DN_ALPHA * h + y, ln1_g[l], ln1_b[l])
        y = hierarchical_moe(h, router_group_w[l], router_group_b[l], router_expert_w[l],
                             router_expert_b[l], w_gate[l], w_up[l], w_down[l])
        h = layer_norm(DN_ALPHA * h + y, ln2_g[l], ln2_b[l])
    return h
```

```python
import numpy as np
import concourse.bass as bass
import concourse.mybir as mybir
from concourse.bass_utils import run_bass_kernel_spmd
from contextlib import ExitStack

F32 = mybir.dt.float32
BF16 = mybir.dt.bfloat16
I32 = mybir.dt.int32
U32 = mybir.dt.uint32
AF = mybir.ActivationFunctionType
ALU = mybir.AluOpType
AX = mybir.AxisListType

D = 2048
S = 4096
NT = 2048
HD = 64
NH = 16
NG = 4
NCMP = 255
NEG = -30000.0
OWN = ([0, 3, 4, 7], [1, 2, 5, 6])
DN_ALPHA = 2.0 ** 0.25
LN_EPS = 1e-5
NEXP = 64
CAP = 128

DEBUG = []
STOP_AFTER = None
GROUPS = None
PARTS = None


def _on(name):
    return PARTS is None or name in PARTS


class _Op:
    __slots__ = ("eng", "fn", "dma", "deps", "idx", "marked", "count", "sem", "semval", "gid")


class Phase:
    ENGS = ("pe", "act", "dve", "pool", "sp")
    NDMA = 6

    def __init__(self, nc, name):
        self.nc = nc
        self.name = name
        self.ops = {e: [] for e in self.ENGS}
        self.bufs = {}
        self.excl = set()
        self.nops = 0

    def _buf(self, k):
        b = self.bufs.get(k)
        if b is None:
            b = [[], []]
            self.bufs[k] = b
        return b

    def add(self, eng, fn, reads=(), writes=(), dma=False):
        op = _Op()
        op.eng, op.fn, op.dma = eng, fn, dma
        op.idx = len(self.ops[eng])
        op.marked = False
        op.gid = self.nops
        self.nops += 1
        deps = set()
        for k in reads:
            b = self._buf(k)
            deps.update(b[0])
            if k in self.excl:
                for r in b[1]:
                    if r.eng != eng:
                        deps.add(r)
            b[1].append(op)
        for k in writes:
            b = self._buf(k)
            if dma and b[0] and not b[1] and all(w.dma for w in b[0]):
                b[0].append(op)
            else:
                deps.update(b[0])
                deps.update(b[1])
                b[1] = []
                b[0] = [op]
        deps.discard(op)
        op.deps = deps
        self.ops[eng].append(op)
        return op

    def mm(self, out, lhsT, rhs, start, stop, reads, writes, skip=False):
        if skip:
            return self.add("pe", lambda e: e.matmul(out, lhsT, rhs, start=start, stop=stop, skip_group_check=True), reads, writes)
        return self.add("pe", lambda e: e.matmul(out, lhsT, rhs, start=start, stop=stop), reads, writes)

    def tr(self, out, in_, ident, reads, writes):
        return self.add("pe", lambda e: e.transpose(out, in_, ident), reads, writes)

    def dma(self, q, out, in_, reads, writes):
        return self.add(q, lambda e: e.dma_start(out=out, in_=in_), reads, writes, dma=True)

    def emit(self, stack):
        nc = self.nc
        engs = {"pe": nc.tensor, "act": nc.scalar, "dve": nc.vector, "pool": nc.gpsimd, "sp": nc.sync}
        for e in self.ENGS:
            for op in self.ops[e]:
                for d in op.deps:
                    if d.dma:
                        continue
                    if d.eng == "pe" and op.eng == "pe" and not op.dma:
                        continue
                    d.marked = True
        esem = {e: nc.alloc_semaphore(name=f"{self.name}_{e}") for e in self.ENGS}
        dsem = {}
        for e in self.ENGS:
            cnt = 0
            nd = 0
            for op in self.ops[e]:
                if op.dma:
                    if e not in dsem:
                        dsem[e] = [nc.alloc_semaphore(name=f"{self.name}_{e}_d{i}") for i in range(self.NDMA)]
                    op.sem = dsem[e][nd % self.NDMA]
                    op.semval = 16 * (nd // self.NDMA + 1)
                    nd += 1
                elif op.marked:
                    cnt += 1
                    op.count = cnt
            assert cnt < 60000, (self.name, e, cnt)
        block = stack.enter_context(nc.Block())

        def body(ename):
            def _(eng):
                seen = {}

                def wait(sem, val):
                    k = id(sem)
                    if seen.get(k, 0) >= val:
                        return
                    seen[k] = val
                    eng.wait_ge(sem, val)

                for op in self.ops[ename]:
                    for d in sorted(op.deps, key=lambda o: o.gid):
                        if d.dma:
                            wait(d.sem, d.semval)
                        elif d.eng == "pe" and ename == "pe" and not op.dma:
                            continue
                        else:
                            wait(esem[d.eng], d.count)
                    if op.dma:
                        if op.semval > 16:
                            wait(op.sem, op.semval - 16)
                        op.fn(eng).then_inc(op.sem, 16)
                    else:
                        ins = op.fn(eng)
                        if op.marked:
                            ins.then_inc(esem[ename], 1)
                last = {}
                for op in self.ops[ename]:
                    if op.dma:
                        last[id(op.sem)] = (op.sem, op.semval)
                for sem, val in last.values():
                    wait(sem, val)
            return _

        for ename, reg in (("pe", block.tensor), ("act", block.scalar), ("dve", block.vector),
                           ("pool", block.gpsimd), ("sp", block.sync)):
            if self.ops[ename]:
                reg(body(ename))


class Ctx:
    def __init__(self, nc, stack, name):
        self.nc, self.stack, self.name = nc, stack, name
        self.P = Phase(nc, name)

    def sb(self, name, shape, dt):
        return self.stack.enter_context(self.nc.sbuf_tensor(f"{self.name}_{name}", list(shape), dt))

    def ps(self, name, shape, dt):
        self.P.excl.add(name)
        return self.stack.enter_context(self.nc.psum_tensor(f"{self.name}_{name}", list(shape), dt))


def _dram(nc, name, shape, dt, kind):
    return nc.dram_tensor(name, list(shape), dt, kind=kind).ap()


def _scratch(nc, name, shape, dt):
    kind = "ExternalOutput" if name in DEBUG else "Internal"
    return _dram(nc, name, shape, dt, kind)


def _rope(P, dst, src, cos, sin, nh, tmp, rkeys, wkeys, tag):
    s3 = src.rearrange("p (h d) -> p h d", h=nh)
    d3 = dst.rearrange("p (h d) -> p h d", h=nh)
    cb = cos.unsqueeze(1).broadcast_to([128, nh, 32])
    sn = sin.unsqueeze(1).broadcast_to([128, nh, 32])
    t1 = tmp[:, 0, 0:nh, :]
    t2 = tmp[:, 1, 0:nh, :]
    q1, q2 = s3[:, :, 0:32], s3[:, :, 32:64]
    tk = [tag + "_t1", tag + "_t2"]
    P.add("dve", lambda e: e.tensor_tensor(t1, q1, cb, ALU.mult), rkeys, [tk[0]])
    P.add("dve", lambda e: e.tensor_tensor(t2, q2, sn, ALU.mult), rkeys, [tk[1]])
    P.add("dve", lambda e: e.tensor_tensor(d3[:, :, 0:32], t1, t2, ALU.subtract), tk, wkeys)
    P.add("dve", lambda e: e.tensor_tensor(t1, q2, cb, ALU.mult), rkeys, [tk[0]])
    P.add("dve", lambda e: e.tensor_tensor(t2, q1, sn, ALU.mult), rkeys, [tk[1]])
    P.add("dve", lambda e: e.tensor_tensor(d3[:, :, 32:64], t1, t2, ALU.add), tk, wkeys)


def phase_A(nc, io, sc):
    with ExitStack() as st:
        C = Ctx(nc, st, "A")
        P = C.P
        ident = C.sb("ident", [128, 128], BF16)
        P.dma("sp", ident[:, :], io["ident"], [], ["ident"])
        wk = [C.sb(f"w{i}", [128, 16, 512], BF16) for i in range(2)]
        xt = [C.sb(f"xt{i}", [128, 16, 512], BF16) for i in range(2)]
        xo = C.sb("xo", [128, 16, NT], BF16)
        xh = C.sb("xh", [128, 16, 64], BF16)
        cosg = C.sb("cosg", [128, 32, 32], F32)
        sing = C.sb("sing", [128, 32, 32], F32)
        cosp = C.sb("cosp", [128, 16, 32], F32)
        sinp = C.sb("sinp", [128, 16, 32], F32)
        coso = C.sb("coso", [128, 16, 32], F32)
        sino = C.sb("sino", [128, 16, 32], F32)
        cosq = C.sb("cosq", [128, 16, 32], F32)
        sinq = C.sb("sinq", [128, 16, 32], F32)
        for t, n in ((cosg, "cosg"), (sing, "sing"), (cosp, "cosp"), (sinp, "sinp"), (coso, "coso"),
                     (sino, "sino"), (cosq, "cosq"), (sinq, "sinq")):
            P.dma("sp", t[:, :, :], io[n], [], [n])
        rtmp = C.sb("rtmp", [128, 2, 8, 32], F32)
        ksr = [C.sb(f"ksr{i}", [128, 512], BF16) for i in range(2)]
        kcv = [C.sb(f"kcv{i}", [128, 512], BF16) for i in range(2)]
        stT = [C.sb(f"stT{i}", [128, 6, 512], BF16) for i in range(2)]
        vst = [C.sb(f"vst{i}", [128, 4, 256], BF16) for i in range(2)]
        pm = [C.ps(f"pm{i}", [128, 512], F32) for i in range(4)]
        pT = [C.ps(f"pT{i}", [128, 8, 128], BF16) for i in range(2)]

        wq = ["pool", "sp"]
        nw = [0]
        nx = [0]
        npm = [0]
        npt = [0]

        def load_w(src_ap, ncols=512):
            i = nw[0] % 2
            nw[0] += 1
            P.dma("pool", wk[i][:, :, 0:ncols], src_ap.rearrange("(k p) n -> p k n", p=128), [], [f"w{i}"])
            return i

        def load_x(src_ap):
            i = nx[0] % 2
            nx[0] += 1
            P.dma("pool", xt[i][:, :, :], src_ap.rearrange("(k p) n -> p k n", p=128), [], [f"xt{i}"])
            return i

        def proj(xtile_ap, xkey, wi, ncols=512, c0=0):
            b = npm[0] % 4
            npm[0] += 1
            for k in range(16):
                P.mm(pm[b][:, 0:ncols], xtile_ap(k), wk[wi][:, k, c0:c0 + ncols], k == 0, k == 15,
                     [xkey, f"w{wi}"], [f"pm{b}"])
            return b

        def transposes(srcs, dst, dstkey, tcol):
            b = npt[0] % 2
            npt[0] += 1
            n = len(srcs)
            for i, (ap, key) in enumerate(srcs):
                P.tr(pT[b][:, i, :], ap, ident[:, :], [key, "ident"], [f"pT{b}"])
            P.add("act", lambda e: e.copy(dst[:, 0:n, tcol * 128:(tcol + 1) * 128], pT[b][:, 0:n, :]),
                  [f"pT{b}"], [dstkey])

        w_ks = load_w(io["w_A1"][:, 0:512])
        w_kc = load_w(io["w_A1"][:, 512:1024])
        for ch in (range(8) if _on("A1") else []):
            xi = load_x(io["xTg"][:, ch * 512:(ch + 1) * 512])
            si = ch % 2
            for j in range(4):
                tt = ch * 4 + j
                xa = (lambda xi, j: (lambda k: xt[xi][:, k, j * 128:(j + 1) * 128]))(xi, j)
                b0 = proj(xa, f"xt{xi}", w_ks)
                b1 = proj(xa, f"xt{xi}", w_kc)
                r = tt % 2
                _rope(P, ksr[r][:, 0:256], pm[b0][:, 0:256], cosg[:, tt, :], sing[:, tt, :], 4, rtmp,
                      [f"pm{b0}", "cosg", "sing"], [f"ksr{r}"], "rA")
                P.add("act", lambda e, si=si, j=j, b0=b0: e.copy(vst[si][:, j, :], pm[b0][:, 256:512]),
                      [f"pm{b0}"], [f"vst{si}"])
                P.add("act", lambda e, r=r, b1=b1: e.copy(kcv[r][:, :], pm[b1][:, :]), [f"pm{b1}"], [f"kcv{r}"])
                srcs = [(ksr[r][:, 0:128], f"ksr{r}"), (ksr[r][:, 128:256], f"ksr{r}")]
                srcs += [(kcv[r][:, i * 128:(i + 1) * 128], f"kcv{r}") for i in range(4)]
                transposes(srcs, stT[si], f"stT{si}", j)
            P.dma("sp", sc["KT_A1"].rearrange("i p t -> p i t")[:, :, ch * 512:(ch + 1) * 512], stT[si][:, :, :],
                  [f"stT{si}"], ["KT_A1"])
            P.dma("sp", sc["Vs"].rearrange("(c j p) n -> c p j n", j=4, p=128)[ch], vst[si][:, :, :],
                  [f"vst{si}"], ["Vs"])

        w_kw = load_w(io["w_A2"])
        for ch in range(4):
            P.dma("pool", xo[:, :, ch * 512:(ch + 1) * 512],
                  io["xTo"][:, ch * 512:(ch + 1) * 512].rearrange("(k p) n -> p k n", p=128), [], ["xo"])
        for lc in range(4):
            P.dma("pool", xh[:, :, lc * 16:(lc + 1) * 16],
                  io["xTp"][:, lc * 512 + 496:lc * 512 + 512].rearrange("(k p) t -> p k t", p=128), [], ["xh"])
        for which in (("prev", "own") if _on("A2") else ()):
            cosw, sinw = (cosp, sinp) if which == "prev" else (coso, sino)
            cn, sn_ = ("cosp", "sinp") if which == "prev" else ("coso", "sino")
            for ch in range(4):
                si = ch % 2
                if which == "prev":
                    xi = load_x(io["xTp"][:, ch * 512:(ch + 1) * 512])
                for j in range(4):
                    tt = ch * 4 + j
                    if which == "prev":
                        xa = (lambda xi, j: (lambda k: xt[xi][:, k, j * 128:(j + 1) * 128]))(xi, j)
                        xkey = f"xt{xi}"
                    else:
                        xa = (lambda tt: (lambda k: xo[:, k, tt * 128:(tt + 1) * 128]))(tt)
                        xkey = "xo"
                    b0 = proj(xa, xkey, w_kw)
                    r = tt % 2
                    _rope(P, ksr[r][:, 0:256], pm[b0][:, 0:256], cosw[:, tt, :], sinw[:, tt, :], 4, rtmp,
                          [f"pm{b0}", cn, sn_], [f"ksr{r}"], "rA")
                    P.add("act", lambda e, si=si, j=j, b0=b0: e.copy(vst[si][:, j, :], pm[b0][:, 256:512]),
                          [f"pm{b0}"], [f"vst{si}"])
                    srcs = [(ksr[r][:, 0:128], f"ksr{r}"), (ksr[r][:, 128:256], f"ksr{r}")]
                    transposes(srcs, stT[si], f"stT{si}", j)
                P.dma("sp", sc["KwT_" + which].rearrange("i p t -> p i t")[:, :, ch * 512:(ch + 1) * 512],
                      stT[si][:, 0:2, :], [f"stT{si}"], ["KwT_" + which])
                P.dma("sp", sc["Vw_" + which].rearrange("(c j p) n -> c p j n", j=4, p=128)[ch], vst[si][:, :, :],
                      [f"vst{si}"], ["Vw_" + which])

        for qc in (range(2) if _on("Q") else []):
            wi = load_w(io["w_q"][:, qc * 512:(qc + 1) * 512])
            for ch in range(4):
                si = ch % 2
                for j in range(4):
                    tt = ch * 4 + j
                    xa = (lambda tt: (lambda k: xo[:, k, tt * 128:(tt + 1) * 128]))(tt)
                    b0 = proj(xa, "xo", wi)
                    r = tt % 2
                    _rope(P, ksr[r][:, :], pm[b0][:, :], cosq[:, tt, :], sinq[:, tt, :], 8, rtmp,
                          [f"pm{b0}", "cosq", "sinq"], [f"ksr{r}"], "rA")
                    srcs = [(ksr[r][:, i * 128:(i + 1) * 128], f"ksr{r}") for i in range(4)]
                    transposes(srcs, stT[si], f"stT{si}", j)
                P.dma("sp", sc["QT"][qc * 4:(qc + 1) * 4].rearrange("i p t -> p i t")[:, :, ch * 512:(ch + 1) * 512],
                      stT[si][:, 0:4, :], [f"stT{si}"], ["QT"])

        gst = C.sb("gst", [128, 16, 48], F32)
        wi = load_w(io["w_gn"], 48)
        for tt in (range(16) if _on("GN") else []):
            xa = (lambda tt: (lambda k: xo[:, k, tt * 128:(tt + 1) * 128]))(tt)
            b0 = proj(xa, "xo", wi, 48)
            P.add("act", lambda e, tt=tt, b0=b0: e.activation(gst[:, tt, :], pm[b0][:, 0:48], AF.Sigmoid),
                  [f"pm{b0}"], ["gst"])
        P.dma("sp", sc["Gn"].rearrange("(t p) n -> p t n", p=128), gst[:, :, :], ["gst"], ["Gn"])

        gms = [C.sb(f"gms{i}", [128, 512], BF16) for i in range(2)]
        ng = 0
        for gc in (range(8) if _on("GM") else []):
            wi = load_w(io["w_gm"][:, gc * 512:(gc + 1) * 512])
            for tt in range(16):
                xa = (lambda tt: (lambda k: xo[:, k, tt * 128:(tt + 1) * 128]))(tt)
                b0 = proj(xa, "xo", wi)
                gi = ng % 2
                ng += 1
                P.add("act", lambda e, gi=gi, b0=b0: e.activation(gms[gi][:, :], pm[b0][:, :], AF.Sigmoid),
                      [f"pm{b0}"], [f"gms{gi}"])
                P.dma("sp", sc["Gm"][tt * 128:(tt + 1) * 128, gc * 512:(gc + 1) * 512], gms[gi][:, :],
                      [f"gms{gi}"], ["Gm"])

        pmix = C.sb("pmix", [128, 4, 2, 256], BF16)
        P.dma("pool", pmix[:, :, :, :], io["pool_mix"].rearrange("g (c p) d -> p g c d", p=128), [], ["pmix"])
        pscale = C.sb("pscale", [128, 8], F32)
        P.dma("sp", pscale[:, :], io["pool_scale"], [], ["pscale"])
        rc16 = C.sb("rc16", [128, 4, 4, 16], F32)
        P.dma("sp", rc16[:, :, :, :], io["rc16"], [], ["rc16"])
        U = [C.sb(f"U{i}", [128, 528], F32) for i in range(2)]
        Wa = C.sb("Wa", [128, 528], F32)
        Wb = C.sb("Wb", [128, 528], F32)
        pld = [C.sb(f"pld{i}", [128, 2, 512], BF16) for i in range(2)]
        mxs = [C.sb(f"mxs{i}", [128, 512], BF16) for i in range(2)]
        phalo = C.ps("phalo", [128, 16], F32)
        nu = 0
        nmx = 0
        for g in (range(4) if _on("POOL") else []):
            win = (2, 4, 8, 16)[g]
            wis = [load_w(io["w_pool"][:, (2 * g + c2) * 128:(2 * g + c2 + 1) * 128], 128) for c2 in range(2)]
            for lc in range(4):
                pi = (g * 4 + lc) % 2
                for c2 in range(2):
                    wi = wis[c2]
                    b = npm[0] % 4
                    npm[0] += 1
                    for k in range(16):
                        P.mm(pm[b][:, :], wk[wi][:, k, 0:128], xo[:, k, lc * 512:(lc + 1) * 512], k == 0, k == 15,
                             ["xo", f"w{wi}"], [f"pm{b}"])
                    for k in range(16):
                        P.mm(phalo[:, :], wk[wi][:, k, 0:128], xh[:, k, lc * 16:(lc + 1) * 16], k == 0, k == 15,
                             ["xh", f"w{wi}"], ["phalo"])
                    ui = nu % 2
                    nu += 1
                    Ut = U[ui]
                    uk = f"U{ui}"
                    P.add("act", lambda e, Ut=Ut, b=b: e.copy(Ut[:, 16:528], pm[b][:, :]), [f"pm{b}"], [uk])
                    P.add("act", lambda e, Ut=Ut: e.copy(Ut[:, 0:16], phalo[:, :]), ["phalo"], [uk])
                    src, sk = Ut, uk
                    step = 1
                    dsts = [(Wa, "Wa"), (Wb, "Wb")]
                    di = 0
                    while step < win:
                        dt_, dk = dsts[di % 2]
                        di += 1
                        lo = 2 * step - 1
                        P.add("dve", lambda e, dt_=dt_, src=src, lo=lo, step=step: e.tensor_tensor(
                            dt_[:, lo:528], src[:, lo:528], src[:, lo - step:528 - step], ALU.add), [sk], [dk])
                        src, sk = dt_, dk
                        step *= 2
                    P.add("dve", lambda e, src=src, Ut=Ut, pi=pi, c2=c2, win=win: e.scalar_tensor_tensor(
                        pld[pi][:, c2, 16:512], src[:, 32:528], 1.0 / win, Ut[:, 32:528], ALU.mult, ALU.subtract),
                        [sk, uk], [f"pld{pi}"])
                    P.add("dve", lambda e, src=src, lc=lc, g=g: e.tensor_tensor(
                        Wa[:, 0:16] if src is not Wa else Wb[:, 0:16], src[:, 16:32], rc16[:, lc, g, :], ALU.mult),
                        [sk, "rc16"], ["Wa" if src is not Wa else "Wb"])
                    P.add("dve", lambda e, src=src, Ut=Ut, pi=pi, c2=c2: e.tensor_tensor(
                        pld[pi][:, c2, 0:16], Wa[:, 0:16] if src is not Wa else Wb[:, 0:16], Ut[:, 16:32], ALU.subtract),
                        ["Wa" if src is not Wa else "Wb", uk], [f"pld{pi}"])
                for d2 in range(2):
                    b = npm[0] % 4
                    npm[0] += 1
                    for c2 in range(2):
                        P.mm(pm[b][:, :], pmix[:, g, c2, d2 * 128:(d2 + 1) * 128], pld[pi][:, c2, :], c2 == 0, c2 == 1,
                             ["pmix", f"pld{pi}"], [f"pm{b}"])
                    mi = nmx % 2
                    nmx += 1
                    ct = 2 * g + d2
                    P.add("dve", lambda e, mi=mi, b=b, ct=ct: e.tensor_scalar(
                        mxs[mi][:, :], pm[b][:, :], pscale[:, ct:ct + 1], None, ALU.mult), [f"pm{b}", "pscale"], [f"mxs{mi}"])
                    P.dma("sp", sc["MixT"][ct * 128:(ct + 1) * 128, lc * 512:(lc + 1) * 512], mxs[mi][:, :],
                          [f"mxs{mi}"], ["MixT"])
        P.emit(st)


def _bf16(a):
    import ml_dtypes
    return np.asarray(a, dtype=np.float32).astype(ml_dtypes.bfloat16)


def _rope_tab(pos, scale=1.0):
    half = HD // 2
    inv = (10000.0 ** (-2.0 * np.arange(half, dtype=np.float32) / HD)).astype(np.float32)
    ang = pos.astype(np.float32)[:, None] * inv[None, :]
    c = (np.cos(ang).astype(np.float32) * np.float32(scale)).astype(np.float32)
    s = (np.sin(ang).astype(np.float32) * np.float32(scale)).astype(np.float32)
    n = pos.shape[0] // 128
    c = np.ascontiguousarray(c.reshape(n, 128, half).transpose(1, 0, 2))
    s = np.ascontiguousarray(s.reshape(n, 128, half).transpose(1, 0, 2))
    return c, s


def _core_tables(p):
    t = {}
    own_pos = np.concatenate([np.arange(512 * g, 512 * g + 512) for g in OWN[p]])
    prev_pos = np.concatenate([np.arange(512 * (g - 1), 512 * g) if g > 0 else np.zeros(512, np.int64) for g in OWN[p]])
    prev_valid = np.concatenate([np.full(512, 1.0 if g > 0 else 0.0, np.float32) for g in OWN[p]])
    t["own_pos"], t["prev_pos"], t["prev_valid"] = own_pos, prev_pos, prev_valid
    t["cosg"], t["sing"] = _rope_tab(np.arange(S))
    t["cosp"], t["sinp"] = _rope_tab(prev_pos)
    t["coso"], t["sino"] = _rope_tab(own_pos)
    t["cosq"], t["sinq"] = _rope_tab(own_pos, HD ** -0.5)
    rc = np.zeros((128, 4, 4, 16), np.float32)
    for lc, g in enumerate(OWN[p]):
        for gi, w in enumerate((2, 4, 8, 16)):
            for tt in range(16):
                rc[:, lc, gi, tt] = 1.0 / (min(tt + 1, w) if g == 0 else w)
    t["rc16"] = rc
    return t


_IN_A = {
    "ident": ([128, 128], BF16), "xTg": ([D, S], F32), "xTo": ([D, NT], F32), "xTp": ([D, NT], F32),
    "w_A1": ([D, 1024], F32), "w_A2": ([D, 512], F32), "w_q": ([D, 1024], F32), "w_gn": ([D, 48], F32),
    "w_pool": ([D, 1024], F32), "w_gm": ([D, 4096], F32), "pool_mix": ([4, 256, 256], F32),
    "pool_scale": ([128, 8], F32), "rc16": ([128, 4, 4, 16], F32),
    "cosg": ([128, 32, 32], F32), "sing": ([128, 32, 32], F32), "cosp": ([128, 16, 32], F32),
    "sinp": ([128, 16, 32], F32), "coso": ([128, 16, 32], F32), "sino": ([128, 16, 32], F32),
    "cosq": ([128, 16, 32], F32), "sinq": ([128, 16, 32], F32),
}
_SC_A = {
    "KT_A1": ([6, 128, S], BF16), "Vs": ([S, 256], BF16), "KwT_prev": ([2, 128, NT], BF16),
    "KwT_own": ([2, 128, NT], BF16), "Vw_prev": ([NT, 256], BF16), "Vw_own": ([NT, 256], BF16),
    "QT": ([8, 128, NT], BF16), "Gn": ([NT, 48], F32), "Gm": ([NT, 4096], BF16), "MixT": ([1024, NT], BF16),
    "OT": ([8, 128, NT], BF16),
}


def build_nc(in_specs, sc_specs, phases, final_out=None):
    nc = bass.Bass("TRN2", target_bir_lowering=False)
    io = {n: _dram(nc, n, shp, dt, "ExternalInput") for n, (shp, dt) in in_specs.items()}
    sc = {n: _scratch(nc, n, shp, dt) for n, (shp, dt) in sc_specs.items()}
    if final_out is not None:
        n, shp, dt = final_out
        sc[n] = _dram(nc, n, shp, dt, "ExternalOutput")
    for ph in phases:
        snap = nc.snapshot_sems()
        ph(nc, io, sc)
        nc.clear_and_free_semaphores(nc.allocated_since(snap))
        nc.all_engine_barrier()
    return nc


def _ts(P, out, in0, s1, s2, op0, op1, reads, writes, eng="dve"):
    if op1 is None:
        return P.add(eng, lambda e: e.tensor_scalar(out, in0, s1, None, op0), reads, writes)
    return P.add(eng, lambda e: e.tensor_scalar(out, in0, s1, s2, op0, op1), reads, writes)


def _tt(P, out, in0, in1, op, reads, writes, eng="dve"):
    return P.add(eng, lambda e: e.tensor_tensor(out, in0, in1, op), reads, writes)


def _stt(P, out, in0, sc_, in1, op0, op1, reads, writes):
    return P.add("dve", lambda e: e.scalar_tensor_tensor(out, in0, sc_, in1, op0, op1), reads, writes)


def _act(P, out, in_, func, reads, writes, bias=None, scale=None):
    kw = {}
    if bias is not None:
        kw["bias"] = bias
    if scale is not None:
        kw["scale"] = scale
    return P.add("act", lambda e: e.activation(out, in_, func, **kw), reads, writes)


def _bg_convert(P, io, sc, experts):
    for e_ in experts:
        P.dma("pool", sc["WgB"][e_].rearrange("p (k n) -> p k n", k=16),
              io["w_gate"][e_].rearrange("(k p) n -> p k n", p=128), [], [f"WgB{e_}"])
        P.dma("pool", sc["WuB"][e_].rearrange("p (k n) -> p k n", k=16),
              io["w_up"][e_].rearrange("(k p) n -> p k n", p=128), [], [f"WuB{e_}"])
        P.dma("pool", sc["WdB"][e_].rearrange("p (k n) -> p k n", k=4),
              io["w_down"][e_].rearrange("(k p) n -> p k n", p=128), [], [f"WdB{e_}"])


def phase_B(nc, io, sc):
    with ExitStack() as st:
        C = Ctx(nc, st, "B")
        P = C.P
        ident = C.sb("ident", [128, 128], BF16)
        P.dma("sp", ident[:, :], io["ident"], [], ["ident"])
        Ksp = C.sb("Ksp", [128, S], BF16)
        P.dma("sp", Ksp[64:128, :], io["Eoh"], [], ["Ksp_e"])
        Qp = C.sb("Qp", [128, 4, NT], BF16)
        Vsp = C.sb("Vsp", [128, 32, 65], BF16)
        Kwp = C.sb("Kwp", [64, 4, 8, 128], BF16)
        Vwp = C.sb("Vwp", [128, 4, 8, 65], BF16)
        pvo = C.sb("pvo", [128, 16], BF16)
        P.dma("sp", pvo[:, :], io["pvones"], [], ["pvo"])
        P.add("pool", lambda e: e.memset(Vsp[:, :, 64:65], 1.0), [], ["Vsp_1"])
        P.add("pool", lambda e: e.memset(Vwp[:, :, 4:8, 64:65], 1.0), [], ["Vwp_1"])
        P.add("pool", lambda e: e.tensor_copy(Vwp[:, :, 0:4, 64:65].rearrange("p a b c -> p a (b c)"),
                                              pvo[:, :].rearrange("p (a b) -> p a b", a=4)), ["pvo"], ["Vwp_1"])
        KcT = C.sb("KcT", [64, S], BF16)
        VcT = C.sb("VcT", [64, S], BF16)
        w1 = [C.sb(f"w1_{i}", [64, 32, 256], BF16) for i in range(2)]
        w2 = C.sb("w2", [128, 3, 2, 64], BF16)
        posT = C.sb("posT", [64, 2, 32], BF16)
        P.dma("pool", w1[0][:, :, :], io["cmp_k_w1"].rearrange("(l d) j -> d l j", d=64), [], ["w1_0"])
        P.dma("pool", w1[1][:, :, :], io["cmp_v_w1"].rearrange("(l d) j -> d l j", d=64), [], ["w1_1"])
        for i, n in enumerate(("cmp_k_w2", "cmp_k_w2s", "cmp_v_w2")):
            P.dma("pool", w2[:, i, :, :], io[n].rearrange("(t p) d -> p t d", p=128), [], ["w2"])
        P.dma("pool", posT[:, 0, :], io["cmp_pos_kT"], [], ["posT"])
        P.dma("pool", posT[:, 1, :], io["cmp_pos_vT"], [], ["posT"])
        ccos = C.sb("ccos", [64, 256], F32)
        csin = C.sb("csin", [64, 256], F32)
        P.dma("sp", ccos[:, :], io["ccos"], [], ["ccos"])
        P.dma("sp", csin[:, :], io["csin"], [], ["csin"])
        cbias = C.sb("cbias", [128, 2, 2, 512], BF16)
        tritab = C.sb("tritab", [128, 4, 8, 128], BF16)
        P.dma("sp", tritab[:, :, :, :], io["tritab"], [], ["tritab"])
        tri2 = C.sb("tri2", [128, 2, 128], BF16)
        P.dma("sp", tri2[:, :, :], io["tri2"], [], ["tri2"])
        Vcp = C.sb("Vcp", [128, 2, 129], BF16)
        P.add("pool", lambda e: e.memset(Vcp[:, :, 0:65], 0.0), [], ["Vcp"])
        P.add("pool", lambda e: e.memset(Vcp[:, :, 64:65], 1.0), [], ["Vcp"])
        P.dma("sp", Vcp[:, :, 65:129], io["ovl"], [], ["Vcp_o"])
        KcmpT = C.sb("KcmpT", [64, 256], BF16)
        P.add("pool", lambda e: e.memset(KcmpT[:, :], 0.0), [], ["KcmpT"])
        Mk = C.sb("Mk", [128, 16, 64], F32)
        Ad = C.sb("Ad", [128, 16, 64], F32)
        Fm = C.sb("Fm", [128, 16, 64], F32)
        for t, n in ((Mk, "Mk"), (Ad, "Ad"), (Fm, "Fm")):
            P.dma("sp", t[:, :, :], io[n], [], [n])
        Gn = C.sb("Gn", [128, 16, 48], F32)
        P.dma("sp", Gn[:, :, :], sc["Gn"].rearrange("(t p) n -> p t n", p=128), [], ["Gn"])
        O = C.sb("O", [128, 16, 256], F32)
        Ob = [C.sb(f"Ob{i}", [128, 256], BF16) for i in range(2)]
        Os = [C.sb(f"Os{i}", [128, 2, 128], BF16) for i in range(2)]
        Pt = [C.sb(f"Pt{i}", [128, 512], BF16) for i in range(4)]
        Pc = [C.sb(f"Pc{i}", [128, 2, 512], BF16) for i in range(2)]
        Pw = [C.sb(f"Pw{i}", [128, 5, 128], BF16) for i in range(3)]
        hb = C.sb("hb", [128, 256], F32)
        sq = C.sb("sq", [128, 256], F32)
        uu = C.sb("uu", [128, 256], F32)
        sg = C.sb("sg", [128, 256], F32)
        gT = C.sb("gT", [128, 2, 2, 256], BF16)
        cb = C.sb("cb", [128, 4], F32)
        impb = C.sb("impb", [128, 4, 64], F32)
        impm = C.sb("impm", [128, 64], F32)
        imp2 = C.sb("imp2", [128, 64], F32)
        m8 = C.sb("m8", [128, 16], F32)
        nsel = C.sb("nsel", [128, 64], F32)
        NegT = C.sb("NegT", [128, 4, 128], BF16)
        P.add("pool", lambda e: e.memset(NegT[:, :, :], 0.0), [], [f"NegT{i}" for i in range(4)])
        sm = C.sb("sm", [128, 16, 4], F32)
        nsm = [0]
        t1 = C.sb("t1", [64, 256], F32)
        t2 = C.sb("t2", [64, 256], F32)
        pS = [C.ps(f"pS{i}", [128, 512], F32) for i in range(4)]
        pA = [C.ps(f"pA{i}", [128, 512], F32) for i in range(2)]
        pX = C.ps("pX", [128, 512], F32)
        pTb = C.ps("pTb", [128, 8, 128], BF16)

        _bg_convert(P, io, sc, BG_SPLIT[0])
        for kv in range(2):
            for jt in range(2):
                for l in range(32):
                    P.mm(pX[:, 0:1], w1[kv][:, l, jt * 128:(jt + 1) * 128], posT[:, kv, l:l + 1], l == 0, l == 31,
                         [f"w1_{kv}", "posT"], ["pX"])
                i = kv * 2 + jt
                P.add("dve", lambda e, i=i: e.tensor_copy(cb[:, i:i + 1], pX[:, 0:1]), ["pX"], ["cb"])

        npt = [0]
        npc = [0]
        npw = [0]
        nps = [0]

        for g in (range(4) if GROUPS is None else GROUPS):
            pi, hf = g // 2, g % 2
            rows = slice(hf * 64, hf * 64 + 64)
            P.dma("sp", Ksp[0:64, :], sc["KT_A1"][pi, rows, :], ["KT_A1"], ["Ksp_k"])
            P.dma("sp", KcT[:, :], sc["KT_A1"][2 + pi, rows, :], ["KT_A1"], ["KcT"])
            P.dma("sp", VcT[:, :], sc["KT_A1"][4 + pi, rows, :], ["KT_A1"], ["VcT"])
            P.dma("sp", Vsp[:, :, 0:64], sc["Vs"][:, g * 64:(g + 1) * 64].rearrange("(t p) d -> p t d", p=128),
                  ["Vs"], ["Vsp_v"])
            for lc in range(4):
                P.dma("sp", Kwp[:, lc, 0:4, :], sc["KwT_prev"][pi, rows, lc * 512:(lc + 1) * 512], ["KwT_prev"], ["Kwp"])
                P.dma("sp", Kwp[:, lc, 4:8, :], sc["KwT_own"][pi, rows, lc * 512:(lc + 1) * 512], ["KwT_own"], ["Kwp"])
                P.dma("sp", Vwp[:, lc, 0:4, 0:64],
                      sc["Vw_prev"][lc * 512:(lc + 1) * 512, g * 64:(g + 1) * 64].rearrange("(t p) d -> p t d", p=128),
                      ["Vw_prev"], ["Vwp_v"])
                P.dma("sp", Vwp[:, lc, 4:8, 0:64],
                      sc["Vw_own"][lc * 512:(lc + 1) * 512, g * 64:(g + 1) * 64].rearrange("(t p) d -> p t d", p=128),
                      ["Vw_own"], ["Vwp_v"])
            for hh in range(4):
                h = 4 * g + hh
                P.dma("sp", Qp[0:64, hh, :], sc["QT"][h // 2, (h % 2) * 64:(h % 2) * 64 + 64, :], ["QT"], ["Qp_q"])

            for kv, src, skey in ((0, KcT, "KcT"), (1, VcT, "VcT")):
                for jt in range(2):
                    for l in range(32):
                        P.mm(pX[:, 0:255], w1[kv][:, l, jt * 128:(jt + 1) * 128], src[:, l:l + 16 * 254 + 1:16],
                             l == 0, l == 31, [f"w1_{kv}", skey], ["pX"])
                    i = kv * 2 + jt
                    _act(P, sq[:, 0:255], pX[:, 0:255], AF.Square, ["pX", "cb"], ["sq"], bias=cb[:, i:i + 1])
                    _ts(P, hb[:, 0:255], pX[:, 0:255], cb[:, i:i + 1], None, ALU.add, None, ["pX", "cb"], ["hb"])
                    _ts(P, uu[:, 0:255], sq[:, 0:255], 0.044715, 1.0, ALU.mult, ALU.add, ["sq"], ["uu"])
                    _tt(P, uu[:, 0:255], uu[:, 0:255], hb[:, 0:255], ALU.mult, ["uu", "hb"], ["uu"])
                    _act(P, sg[:, 0:255], uu[:, 0:255], AF.Sigmoid, ["uu"], ["sg"], scale=1.5957691216057308)
                    _tt(P, gT[:, kv, jt, 0:255], hb[:, 0:255], sg[:, 0:255], ALU.mult, ["hb", "sg"], ["gT"])
            for jt in range(2):
                P.mm(pX[0:64, 0:255], w2[:, 0, jt, :], gT[:, 0, jt, 0:255], jt == 0, jt == 1, ["w2", "gT"], ["pX"])
            _tt(P, t1[:, 0:255], pX[0:64, 0:255], ccos[:, 0:255], ALU.mult, ["pX", "ccos"], ["t1"])
            for jt in range(2):
                P.mm(pX[0:64, 0:255], w2[:, 1, jt, :], gT[:, 0, jt, 0:255], jt == 0, jt == 1, ["w2", "gT"], ["pX"])
            _tt(P, t2[:, 0:255], pX[0:64, 0:255], csin[:, 0:255], ALU.mult, ["pX", "csin"], ["t2"])
            _tt(P, KcmpT[:, 0:255], t1[:, 0:255], t2[:, 0:255], ALU.add, ["t1", "t2"], ["KcmpT"])
            for ct in range(2):
                n = 128 if ct == 0 else 127
                for jt in range(2):
                    P.mm(pX[0:n, 0:64], gT[:, 1, jt, ct * 128:ct * 128 + n], w2[:, 2, jt, :], jt == 0, jt == 1,
                         ["gT", "w2"], ["pX"])
                P.add("dve", lambda e, ct=ct, n=n: e.tensor_copy(Vcp[0:n, ct, 0:64], pX[0:n, 0:64]), ["pX"], ["Vcp"])

            for lc in range(4):
                cbi = lc % 2
                P.dma("sp", cbias[:, :, cbi, :], io["cmpbias"][:, :, lc * 512:(lc + 1) * 512], [], [f"cbias{cbi}"])
                qsl = slice(lc * 512, (lc + 1) * 512)

                def norm(acc_ap, acc_key, tt, hh, gcol, first):
                    si = nsm[0] % 16
                    nsm[0] += 1
                    sk = f"sm{si}"
                    _ts(P, sm[:, si, 0:1], acc_ap[:, 64:65], 1e-30, None, ALU.max, None, [acc_key], [sk])
                    P.add("dve", lambda e: e.reciprocal(sm[:, si, 1:2], sm[:, si, 0:1]), [sk], [sk])
                    _tt(P, sm[:, si, 2:3], sm[:, si, 1:2], Gn[:, tt, gcol:gcol + 1], ALU.mult, [sk, "Gn"], [sk])
                    osl = O[:, tt, hh * 64:(hh + 1) * 64]
                    if first:
                        _ts(P, osl, acc_ap[:, 0:64], sm[:, si, 2:3], None, ALU.mult, None, [acc_key, sk], [f"O{tt}"])
                    else:
                        _stt(P, osl, acc_ap[:, 0:64], sm[:, si, 2:3], osl, ALU.mult, ALU.add, [acc_key, sk, f"O{tt}"], [f"O{tt}"])
                    return sm[:, si, 1:2], sk

                b1banks = {}

                def b1_qk(hh):
                    bs = []
                    for ct in range(2):
                        b = nps[0] % 2
                        nps[0] += 1
                        P.mm(pS[b][:, :], KcmpT[:, ct * 128:(ct + 1) * 128], Qp[0:64, hh, qsl], True, False,
                             ["KcmpT", "Qp_q"], [f"pS{b}"])
                        P.mm(pS[b][:, :], ident[:, :], cbias[:, ct, cbi, :], False, True,
                             ["ident", f"cbias{cbi}"], [f"pS{b}"])
                        bs.append(b)
                    b1banks[hh] = bs

                def b1_rest(hh):
                    h = 4 * g + hh
                    pci = npc[0] % 2
                    npc[0] += 1
                    for ct in range(2):
                        b = b1banks[hh][ct]
                        _act(P, Pc[pci][:, ct, :], pS[b][:, :], AF.Exp, [f"pS{b}"], [f"Pc{pci}"])
                    accs = (pA if hh % 2 == 0 else pS[2:4])
                    akeys = (["pA0", "pA1"] if hh % 2 == 0 else ["pS2", "pS3"])
                    for qs in range(4):
                        acc = accs[qs // 2][:, (qs % 2) * 256:(qs % 2) * 256 + 129]
                        ak = akeys[qs // 2]
                        for ct in range(2):
                            P.mm(acc, Pc[pci][:, ct, qs * 128:(qs + 1) * 128], Vcp[:, ct, :], ct == 0, ct == 1,
                                 [f"Pc{pci}", "Vcp", "Vcp_o"], [ak])
                    for qs in range(4):
                        tt = lc * 4 + qs
                        acc = accs[qs // 2][:, (qs % 2) * 256:(qs % 2) * 256 + 129]
                        ak = akeys[qs // 2]
                        rz, sk = norm(acc, ak, tt, hh, h, True)
                        if hh == 0:
                            _ts(P, impb[:, qs, :], acc[:, 65:129], rz, None, ALU.mult, None, [ak, sk], [f"impb{qs}"])
                        else:
                            _stt(P, impb[:, qs, :], acc[:, 65:129], rz, impb[:, qs, :], ALU.mult, ALU.add,
                                 [ak, sk, f"impb{qs}"], [f"impb{qs}"])

                for hh in range(4):
                    b1_qk(hh)
                    b1_rest(hh)

                for qs in range(4):
                    tt = lc * 4 + qs
                    iq = impb[:, qs, :]
                    _tt(P, impm[:, :], iq, Mk[:, tt, :], ALU.mult, [f"impb{qs}", "Mk"], ["impm"])
                    _tt(P, impm[:, :], impm[:, :], Ad[:, tt, :], ALU.add, ["impm", "Ad"], ["impm"])
                    P.add("dve", lambda e: e.max(m8[:, 0:8], impm[:, :]), ["impm"], ["m8"])
                    P.add("dve", lambda e: e.match_replace(imp2[:, :], m8[:, 0:8], impm[:, :], -3.0e6), ["impm", "m8"], ["imp2"])
                    P.add("dve", lambda e: e.max(m8[:, 8:16], imp2[:, :]), ["imp2"], ["m8"])
                    _ts(P, nsel[:, :], impm[:, :], m8[:, 15:16], -NEG, ALU.is_ge, ALU.mult, ["impm", "m8"], ["nsel"])
                    _stt(P, NegT[:, qs, 64:128], nsel[:, :], NEG, Fm[:, tt, :], ALU.add, ALU.add, ["nsel", "Fm"], [f"NegT{qs}"])

                wunits = [(hh, qs) for hh in range(4) for qs in range(4)]
                wbanks = {}

                def b3_qk(u):
                    hh, qs = wunits[u]
                    tt = lc * 4 + qs
                    b = nps[0] % 4
                    b2 = (nps[0] + 1) % 4
                    nps[0] += 2
                    qap = Qp[0:64, hh, tt * 128:(tt + 1) * 128]
                    for r in range(qs, qs + 4):
                        o = pS[b][:, (r - qs) * 128:(r - qs + 1) * 128]
                        P.mm(o, Kwp[:, lc, r, :], qap, True, r != qs, ["Kwp", "Qp_q"], [f"pS{b}"])
                        if r == qs:
                            P.mm(o, ident[:, :], tri2[:, 0, :], False, True, ["ident", "tri2"], [f"pS{b}"])
                    P.mm(pS[b2][:, 0:128], Kwp[:, lc, qs + 4, :], qap, True, False, ["Kwp", "Qp_q"], [f"pS{b2}"])
                    P.mm(pS[b2][:, 0:128], ident[:, :], tri2[:, 1, :], False, True, ["ident", "tri2"], [f"pS{b2}"])
                    wbanks[u] = (b, b2)

                def b3_rest(u):
                    hh, qs = wunits[u]
                    h = 4 * g + hh
                    tt = lc * 4 + qs
                    b, b2 = wbanks[u]
                    pwi = npw[0] % 3
                    npw[0] += 1
                    _act(P, Pw[pwi][:, 0:4, :], pS[b][:, :].rearrange("p (a b) -> p a b", a=4), AF.Exp, [f"pS{b}"], [f"Pw{pwi}"])
                    _act(P, Pw[pwi][:, 4, :], pS[b2][:, 0:128], AF.Exp, [f"pS{b2}"], [f"Pw{pwi}"])
                    ai = u % 2
                    acc = pA[ai][:, 0:65]
                    for r in range(5):
                        P.mm(acc, Pw[pwi][:, r, :], Vwp[:, lc, qs + r, :], r == 0, r == 4,
                             [f"Pw{pwi}", "Vwp_v", "Vwp_1"], [f"pA{ai}"])
                    norm(acc, f"pA{ai}", tt, hh, 32 + h, False)

                b3_qk(0)
                for u in range(16):
                    if u + 1 < 16:
                        b3_qk(u + 1)
                    b3_rest(u)

                for qs in range(4):
                    tt = lc * 4 + qs
                    P.tr(pTb[:, 4 + qs, :], NegT[:, qs, :], ident[:, :], [f"NegT{qs}", "ident"], ["pTb"])
                    P.add("act", lambda e, tt=tt, qs=qs: e.copy(
                        Qp[64:128, :, tt * 128:(tt + 1) * 128],
                        pTb[64:128, 4 + qs:5 + qs, :].broadcast_to([64, 4, 128])), ["pTb"], ["Qp_m"])

                E = 8 * (lc + 1)
                sunits = [(hh, kt) for hh in range(4) for kt in range(E)]
                sbanks = {}

                def b2_qk(u):
                    hh, kt = sunits[u]
                    b = nps[0] % 4
                    nps[0] += 1
                    s = kt - (E - 8)
                    P.mm(pS[b][:, :], Ksp[:, kt * 128:(kt + 1) * 128], Qp[:, hh, qsl], True, s < 0,
                         ["Ksp_k", "Ksp_e", "Qp_q", "Qp_m"], [f"pS{b}"])
                    if s >= 0:
                        qd = s % 4
                        P.mm(pS[b][:, qd * 128:(qd + 1) * 128], ident[:, :], tritab[:, lc, s, :], False, True,
                             ["ident", "tritab"], [f"pS{b}"])
                    sbanks[u] = b

                def b2_rest(u):
                    hh, kt = sunits[u]
                    h = 4 * g + hh
                    b = sbanks[u]
                    pti = npt[0] % 4
                    npt[0] += 1
                    _act(P, Pt[pti][:, :], pS[b][:, :], AF.Exp, [f"pS{b}"], [f"Pt{pti}"])
                    ai = hh % 2
                    for qs in range(4):
                        P.mm(pA[ai][:, qs * 128:qs * 128 + 65], Pt[pti][:, qs * 128:(qs + 1) * 128], Vsp[:, kt, :],
                             kt == 0 and qs == 0, kt == E - 1, [f"Pt{pti}", "Vsp_v", "Vsp_1"], [f"pA{ai}"], skip=True)
                    if kt == E - 1:
                        for qs in range(4):
                            norm(pA[ai][:, qs * 128:qs * 128 + 65], f"pA{ai}", lc * 4 + qs, hh, 16 + h, False)

                LA = 2
                nsu = len(sunits)
                for u in range(min(LA, nsu)):
                    b2_qk(u)
                for u in range(nsu):
                    if u + LA < nsu:
                        b2_qk(u + LA)
                    b2_rest(u)

            for tt in range(16):
                oi = tt % 2
                P.add("act", lambda e, oi=oi, tt=tt: e.copy(Ob[oi][:, :], O[:, tt, :]), [f"O{tt}"], [f"Ob{oi}"])
                for i in range(2):
                    P.tr(pTb[:, 2 + i, :], Ob[oi][:, i * 128:(i + 1) * 128], ident[:, :], [f"Ob{oi}", "ident"], ["pTb"])
                P.add("dve", lambda e, oi=oi: e.tensor_copy(Os[oi][:, :, :], pTb[:, 2:4, :]), ["pTb"], [f"Os{oi}"])
                P.dma("sp", sc["OT"][2 * g:2 * g + 2].rearrange("i p t -> p i t")[:, :, tt * 128:(tt + 1) * 128],
                      Os[oi][:, :, :], [f"Os{oi}"], ["OT"])
        P.emit(st)


def _core_tables_B(p, t):
    own_pos = t["own_pos"]
    c = np.arange(256)
    cend = 16 * c + 31
    valid = (c[:, None] <= 254) & (cend[:, None] <= own_pos[None, :])
    cb = np.where(valid, 0.0, NEG).astype(np.float32).reshape(2, 128, NT).transpose(1, 0, 2)
    t["cmpbias"] = _bf16(np.ascontiguousarray(cb))
    k = np.arange(128)
    tri = np.where(k[:, None] > k[None, :], NEG, 0.0).astype(np.float32)
    tt = np.zeros((128, 4, 8, 128), np.float32)
    for lc, gc in enumerate(OWN[p]):
        E = 8 * (lc + 1)
        for s in range(8):
            kt = E - 8 + s
            if 4 * gc <= kt < 4 * gc + 4:
                assert (kt - 4 * gc) == s % 4
                tt[:, lc, s, :] = tri
    t["tritab"] = _bf16(tt)
    tri2 = np.zeros((128, 2, 128), np.float32)
    tri2[:, 0, :] = np.where(k[:, None] <= k[None, :], NEG, 0.0)
    tri2[:, 1, :] = np.where(k[:, None] > k[None, :], NEG, 0.0)
    t["tri2"] = _bf16(tri2)
    cs = np.arange(256) * 16
    ss = np.arange(64) * 64
    ov = ((cs[:, None] + 31 >= ss[None, :]) & (cs[:, None] <= ss[None, :] + 63) & (c[:, None] <= 254)).astype(np.float32)
    t["ovl"] = _bf16(np.ascontiguousarray(ov.reshape(2, 128, 64).transpose(1, 0, 2)))
    cur = own_pos // 64
    j = np.arange(64)
    forced = (j[None, :] == 0) | (j[None, :] == cur[:, None]) | (j[None, :] == cur[:, None] - 1)
    future = j[None, :] > cur[:, None]
    mk = (~(forced | future)).astype(np.float32)
    ad = np.where(forced, 1e6, np.where(future, -1e6, 0.0)).astype(np.float32)
    fm = np.where(future, NEG, 0.0).astype(np.float32)
    lay = lambda a: np.ascontiguousarray(a.reshape(16, 128, 64).transpose(1, 0, 2))
    t["Mk"], t["Ad"], t["Fm"] = lay(mk), lay(ad), lay(fm)
    t["pvones"] = _bf16(np.ascontiguousarray(t["prev_valid"].reshape(16, 128).T))
    t["Eoh"] = _bf16((np.arange(S)[None, :] // 64 == j[:, None]).astype(np.float32))
    half = HD // 2
    inv = (10000.0 ** (-2.0 * np.arange(half, dtype=np.float32) / HD)).astype(np.float32)
    ang = cend.astype(np.float32)[None, :] * np.concatenate([inv, inv])[:, None]
    t["ccos"] = np.cos(ang).astype(np.float32)
    sn = np.sin(ang).astype(np.float32)
    sn[:half] *= -1.0
    t["csin"] = sn
    return t


_IN_B = {
    "Eoh": ([64, S], BF16), "pvones": ([128, 16], BF16), "cmp_k_w1": ([2048, 256], F32), "cmp_v_w1": ([2048, 256], F32),
    "cmp_k_w2": ([256, 64], F32), "cmp_k_w2s": ([256, 64], F32), "cmp_v_w2": ([256, 64], F32),
    "cmp_pos_kT": ([64, 32], F32), "cmp_pos_vT": ([64, 32], F32), "ccos": ([64, 256], F32), "csin": ([64, 256], F32),
    "tritab": ([128, 4, 8, 128], BF16), "tri2": ([128, 2, 128], BF16), "ovl": ([128, 2, 64], BF16),
    "cmpbias": ([128, 2, NT], BF16), "Mk": ([128, 16, 64], F32), "Ad": ([128, 16, 64], F32), "Fm": ([128, 16, 64], F32),
}


def phase_C1(nc, io, sc):
    with ExitStack() as st:
        C = Ctx(nc, st, "C1")
        P = C.P
        ident = C.sb("ident", [128, 128], BF16)
        P.dma("sp", ident[:, :], io["ident"], [], ["ident"])
        wn = C.sb("wn", [128, 8, 2048], BF16)
        wp = C.sb("wp", [128, 8, 2048], BF16)
        for c in range(4):
            cs = slice(c * 512, (c + 1) * 512)
            P.dma("pool", wn[:, :, cs], io["w_nsa_proj"][:, cs].rearrange("(k p) n -> p k n", p=128), [], ["wn"])
            P.dma("pool", wp[:, :, cs], io["w_pool_proj"][:, cs].rearrange("(k p) n -> p k n", p=128), [], ["wp"])
        _bg_convert(P, io, sc, BG_SPLIT[1])
        oT = [C.sb(f"oT{i}", [128, 8, 128], BF16) for i in range(2)]
        mT = [C.sb(f"mT{i}", [128, 8, 128], BF16) for i in range(2)]
        gm = [C.sb(f"gm{i}", [128, 4096], BF16) for i in range(2)]
        ta = [C.sb(f"ta{i}", [128, 512], F32) for i in range(2)]
        tb = [C.sb(f"tb{i}", [128, 512], F32) for i in range(2)]
        z = [C.sb(f"z{i}", [128, 2048], BF16) for i in range(2)]
        zT = [C.sb(f"zT{i}", [128, 16, 128], BF16) for i in range(2)]
        pa = [C.ps(f"pa{i}", [128, 512], F32) for i in range(2)]
        pb = [C.ps(f"pb{i}", [128, 512], F32) for i in range(2)]
        pT = [C.ps(f"pT{i}", [128, 8, 128], BF16) for i in range(2)]
        n = 0
        for tt in range(16):
            i = tt % 2
            ts_ = slice(tt * 128, (tt + 1) * 128)
            P.dma("sp", oT[i][:, :, :], sc["OT"].rearrange("k p t -> p k t")[:, :, ts_], ["OT"], [f"oT{i}"])
            P.dma("sp", mT[i][:, :, :], sc["MixT"].rearrange("(k p) t -> p k t", p=128)[:, :, ts_], ["MixT"], [f"mT{i}"])
            P.dma("sp", gm[i][:, :], sc["Gm"][ts_, :], ["Gm"], [f"gm{i}"])
            for cc in range(4):
                j = n % 2
                n += 1
                cs = slice(cc * 512, (cc + 1) * 512)
                for k in range(8):
                    P.mm(pa[j][:, :], oT[i][:, k, :], wn[:, k, cs], k == 0, k == 7, [f"oT{i}", "wn"], [f"pa{j}"])
                for k in range(8):
                    P.mm(pb[j][:, :], mT[i][:, k, :], wp[:, k, cs], k == 0, k == 7, [f"mT{i}", "wp"], [f"pb{j}"])
                _tt(P, ta[j][:, :], pa[j][:, :], gm[i][:, 2048 + cc * 512:2048 + (cc + 1) * 512], ALU.mult,
                    [f"pa{j}", f"gm{i}"], [f"ta{j}"])
                _tt(P, tb[j][:, :], pb[j][:, :], gm[i][:, cs], ALU.mult, [f"pb{j}", f"gm{i}"], [f"tb{j}"])
                _tt(P, z[i][:, cs], ta[j][:, :], tb[j][:, :], ALU.add, [f"ta{j}", f"tb{j}"], [f"z{i}"], eng="pool")
            for hh in range(2):
                for k in range(8):
                    kk = hh * 8 + k
                    P.tr(pT[hh][:, k, :], z[i][:, kk * 128:(kk + 1) * 128], ident[:, :], [f"z{i}", "ident"], [f"pT{hh}"])
                P.add("act", lambda e, i=i, hh=hh: e.copy(zT[i][:, hh * 8:(hh + 1) * 8, :], pT[hh][:, :, :]),
                      [f"pT{hh}"], [f"zT{i}"])
            P.dma("sp", sc["ZT"].rearrange("k p t -> p k t")[:, :, ts_], zT[i][:, :, :], [f"zT{i}"], ["ZT"])
        P.emit(st)


def _layer_norm(P, dst, src, skey, dkey, g_bc, b_bc, gkeys, st6, mv, tmp, tkeys):
    for c in range(4):
        P.add("dve", lambda e, c=c: e.bn_stats(st6[:, c * 6:(c + 1) * 6], src[:, c * 512:(c + 1) * 512]), [skey], [tkeys[0]])
    P.add("dve", lambda e: e.bn_aggr(mv[:, 0:2], st6[:, 0:24]), [tkeys[0]], [tkeys[1]])
    _act(P, mv[:, 2:3], mv[:, 1:2], AF.Sqrt, [tkeys[1]], [tkeys[1]], bias=mv[:, 4:5])
    P.add("dve", lambda e: e.reciprocal(mv[:, 3:4], mv[:, 2:3]), [tkeys[1]], [tkeys[1]])
    _ts(P, tmp[:, :], src[:, :], mv[:, 0:1], mv[:, 3:4], ALU.subtract, ALU.mult, [skey, tkeys[1]], [tkeys[2]])
    _tt(P, tmp[:, :], tmp[:, :], g_bc[:, :], ALU.mult, [tkeys[2], gkeys[0]], [tkeys[2]], eng="pool")
    _tt(P, dst[:, :], tmp[:, :], b_bc[:, :], ALU.add, [tkeys[2], gkeys[1]], [dkey])


def _breg(eng, cache):
    if "r" not in cache:
        cache["r"] = eng.to_reg(NEXP * CAP - 1)
    return cache["r"]


def phase_C2(nc, io, sc):
    with ExitStack() as st:
        C = Ctx(nc, st, "C2")
        P = C.P
        breg = {}
        identf = C.sb("identf", [128, 128], F32)
        P.dma("sp", identf[:, :], io["identf"], [], ["identf"])
        wo = C.sb("wo", [128, 16, 2048], BF16)
        for c in range(4):
            cs = slice(c * 512, (c + 1) * 512)
            P.dma("pool", wo[:, :, cs], io["w_out"][:, cs].rearrange("(k p) n -> p k n", p=128), [], ["wo"])
        _bg_convert(P, io, sc, BG_SPLIT[2])
        wr = C.sb("wr", [128, 16, 72], F32)
        P.dma("sp", wr[:, :, :], io["w_router"].rearrange("(k p) n -> p k n", p=128), [], ["wr"])
        br = C.sb("br", [128, 72], F32)
        P.dma("sp", br[:, :], io["b_router"], [], ["br"])
        eid64 = C.sb("eid64", [128, 64], F32)
        P.dma("sp", eid64[:, :], io["eid64"], [], ["eid64"])
        g1 = C.sb("g1", [128, 2048], F32)
        b1 = C.sb("b1", [128, 2048], F32)
        P.dma("sp", g1[:, :], io["ln1_g"], [], ["g1"])
        P.dma("sp", b1[:, :], io["ln1_b"], [], ["b1"])
        Ut = C.sb("Ut", [128, 128], BF16)
        P.dma("sp", Ut[:, :], io["utri"], [], ["Ut"])
        ones = C.sb("ones", [128, 128], BF16)
        P.add("pool", lambda e: e.memset(ones[:, :], 1.0), [], ["ones"])
        zeros = C.sb("zeros", [128, 2048], BF16)
        P.add("pool", lambda e: e.memset(zeros[:, :], 0.0), [], ["zeros"])
        for e_ in range(NEXP):
            P.dma("sp", sc["Xg"][e_ * 128:(e_ + 1) * 128, :], zeros[:, :], ["zeros"], ["Xg"])
        accind = C.sb("accind", [128, 64], F32)
        P.add("pool", lambda e: e.memset(accind[:, :], 0.0), [], ["accind"])
        accb = C.sb("accb", [128, 64], BF16)
        zT = [C.sb(f"zT{i}", [128, 16, 128], BF16) for i in range(2)]
        xt = [C.sb(f"xt{i}", [128, 2048], F32) for i in range(2)]
        r = C.sb("r", [128, 2048], F32)
        tmp = C.sb("tmp", [128, 2048], F32)
        h1 = [C.sb(f"h1_{i}", [128, 2048], F32) for i in range(2)]
        h1b = [C.sb(f"h1b{i}", [128, 2048], BF16) for i in range(2)]
        h1T = C.sb("h1T", [128, 16, 128], F32)
        st6 = C.sb("st6", [128, 24], F32)
        mv = C.sb("mv", [128, 8], F32)
        P.add("pool", lambda e: e.memset(mv[:, 4:5], LN_EPS), [], ["mv"])
        lg = C.sb("lg", [128, 72], F32)
        rt = C.sb("rt", [128, 64], F32)
        e3 = C.sb("e3", [128, 8, 8], F32)
        E1 = C.sb("E1", [128, 8, 8], F32)
        E2 = C.sb("E2", [128, 8, 8], F32)
        indb = C.sb("indb", [128, 64], BF16)
        posf = C.sb("posf", [128, 64], F32)
        ridx = [C.sb(f"ridx{i}", [128, 2], I32) for i in range(2)]
        rw = [C.sb(f"rw{i}", [128, 2], F32) for i in range(2)]
        py = [C.ps(f"py{i}", [128, 512], F32) for i in range(2)]
        pt = [C.ps(f"pt{i}", [128, 4, 128], F32) for i in range(2)]
        pl = C.ps("pl", [128, 72], F32)
        pp = C.ps("pp", [128, 64], F32)
        n = 0
        npt = 0
        for tt in range(16):
            i = tt % 2
            ts_ = slice(tt * 128, (tt + 1) * 128)
            P.dma("sp", zT[i][:, :, :], sc["ZT"].rearrange("k p t -> p k t")[:, :, ts_], ["ZT"], [f"zT{i}"])
            P.dma("sp", xt[i][:, :], io["x_own"][ts_, :], [], [f"xt{i}"])
            for cc in range(4):
                j = n % 2
                n += 1
                cs = slice(cc * 512, (cc + 1) * 512)
                for k in range(16):
                    P.mm(py[j][:, :], zT[i][:, k, :], wo[:, k, cs], k == 0, k == 15, [f"zT{i}", "wo"], [f"py{j}"])
                _stt(P, r[:, cs], xt[i][:, cs], DN_ALPHA, py[j][:, :], ALU.mult, ALU.add, [f"xt{i}", f"py{j}"], ["r"])
            _layer_norm(P, h1[i], r, "r", f"h1_{i}", g1, b1, ["g1", "b1"], st6, mv, tmp, ["st6", "mv", "tmp"])
            P.dma("sp", sc["H1"][ts_, :], h1[i][:, :], [f"h1_{i}"], ["H1"])
            P.add("act", lambda e, i=i: e.copy(h1b[i][:, :], h1[i][:, :]), [f"h1_{i}"], [f"h1b{i}"])
            for k4 in range(4):
                j = npt % 2
                npt += 1
                for k in range(4):
                    kk = k4 * 4 + k
                    P.tr(pt[j][:, k, :], h1[i][:, kk * 128:(kk + 1) * 128], identf[:, :], [f"h1_{i}", "identf"], [f"pt{j}"])
                P.add("act", lambda e, j=j, k4=k4: e.copy(h1T[:, k4 * 4:(k4 + 1) * 4, :], pt[j][:, :, :]), [f"pt{j}"], ["h1T"])
            for k in range(16):
                P.mm(pl[:, :], h1T[:, k, :], wr[:, k, :], k == 0, k == 15, ["h1T", "wr"], ["pl"])
            _tt(P, lg[:, :], pl[:, :], br[:, :], ALU.add, ["pl", "br"], ["lg"])
            R = ["rt"]
            P.add("dve", lambda e: e.tensor_reduce(rt[:, 0:1], lg[:, 0:8], AX.X, ALU.max), ["lg"], R)
            _ts(P, rt[:, 8:16], lg[:, 0:8], rt[:, 0:1], None, ALU.is_equal, None, ["lg"] + R, R)
            _ts(P, rt[:, 1:2], rt[:, 0:1], -1.0, None, ALU.mult, None, R, R)
            P.add("act", lambda e: e.activation(rt[:, 16:24], lg[:, 0:8], AF.Exp, bias=rt[:, 1:2], accum_out=rt[:, 2:3]), ["lg"] + R, R)
            P.add("dve", lambda e: e.reciprocal(rt[:, 3:4], rt[:, 2:3]), R, R)
            _tt(P, e3[:, :, :], lg[:, 8:72].rearrange("p (g e) -> p g e", g=8),
                rt[:, 8:16].unsqueeze(2).broadcast_to([128, 8, 8]), ALU.mult, ["lg"] + R, ["e3"])
            P.add("dve", lambda e: e.tensor_reduce(rt[:, 24:32], e3[:, :, :].rearrange("p g e -> p e g"), AX.X, ALU.add), ["e3"], R)
            P.add("dve", lambda e: e.max(rt[:, 32:40], rt[:, 24:32]), R, R)
            _ts(P, rt[:, 40:48], rt[:, 24:32], rt[:, 32:33], None, ALU.is_equal, None, R, R)
            _ts(P, rt[:, 48:56], rt[:, 24:32], rt[:, 33:34], None, ALU.is_equal, None, R, R)
            _tt(P, rt[:, 4:5], rt[:, 32:33], rt[:, 33:34], ALU.subtract, R, R)
            _act(P, rt[:, 5:6], rt[:, 4:5], AF.Sigmoid, R, R)
            _tt(P, rw[i][:, 0:1], rt[:, 5:6], rt[:, 3:4], ALU.mult, R, [f"rw{i}"])
            _tt(P, rw[i][:, 1:2], rt[:, 3:4], rw[i][:, 0:1], ALU.subtract, R + [f"rw{i}"], [f"rw{i}"])
            gb = rt[:, 8:16].unsqueeze(2).broadcast_to([128, 8, 8])
            _tt(P, E1[:, :, :], gb, rt[:, 40:48].unsqueeze(1).broadcast_to([128, 8, 8]), ALU.mult, R, ["E1"])
            _tt(P, E2[:, :, :], gb, rt[:, 48:56].unsqueeze(1).broadcast_to([128, 8, 8]), ALU.mult, R, ["E2"])
            E1f = E1[:, :, :].rearrange("p g e -> p (g e)")
            E2f = E2[:, :, :].rearrange("p g e -> p (g e)")
            _tt(P, posf[:, :], E1f, E2f, ALU.add, ["E1", "E2"], ["posf"])
            P.add("dve", lambda e: e.tensor_copy(indb[:, :], posf[:, :]), ["posf"], ["indb"])
            P.add("dve", lambda e: e.tensor_copy(accb[:, :], accind[:, :]), ["accind"], ["accb"])
            P.mm(pp[:, :], Ut[:, :], indb[:, :], True, False, ["Ut", "indb"], ["pp"])
            P.mm(pp[:, :], ones[:, :], accb[:, :], False, True, ["ones", "accb"], ["pp"])
            _tt(P, accind[:, :], accind[:, :], posf[:, :], ALU.add, ["accind", "posf"], ["accind"])
            P.add("dve", lambda e: e.tensor_copy(posf[:, :], pp[:, :]), ["pp"], ["posf"])
            for kk, (Ef, ek) in enumerate(((E1f, "E1"), (E2f, "E2"))):
                o0 = 56 + kk * 4
                _tt(P, e3[:, :, :].rearrange("p g e -> p (g e)"), Ef, posf[:, :], ALU.mult, [ek, "posf"], ["e3"])
                P.add("dve", lambda e, o0=o0: e.tensor_reduce(rt[:, o0:o0 + 1], e3[:, :, :].rearrange("p g e -> p (g e)"), AX.X, ALU.add), ["e3"], R)
                _tt(P, e3[:, :, :].rearrange("p g e -> p (g e)"), Ef, eid64[:, :], ALU.mult, [ek, "eid64"], ["e3"])
                P.add("dve", lambda e, o0=o0: e.tensor_reduce(rt[:, o0 + 1:o0 + 2], e3[:, :, :].rearrange("p g e -> p (g e)"), AX.X, ALU.add), ["e3"], R)
                _ts(P, rt[:, o0 + 2:o0 + 3], rt[:, o0:o0 + 1], float(CAP), 1.0e6, ALU.is_ge, ALU.mult, R, R)
                _stt(P, rt[:, o0 + 3:o0 + 4], rt[:, o0 + 1:o0 + 2], float(CAP), rt[:, o0:o0 + 1], ALU.mult, ALU.add, R, R)
                _tt(P, rt[:, o0 + 3:o0 + 4], rt[:, o0 + 3:o0 + 4], rt[:, o0 + 2:o0 + 3], ALU.add, R, R)
                P.add("dve", lambda e, i=i, kk=kk, o0=o0: e.tensor_copy(ridx[i][:, kk:kk + 1], rt[:, o0 + 3:o0 + 4]), R, [f"ridx{i}"])
            P.dma("sp", sc["Ridx"][ts_, :], ridx[i][:, :], [f"ridx{i}"], ["Ridx"])
            P.dma("sp", sc["Rw"][ts_, :], rw[i][:, :], [f"rw{i}"], ["Rw"])
            for kk in range(2):
                P.add("pool", lambda e, i=i, kk=kk: e.indirect_dma_start(
                    out=sc["Xg"][:, :], out_offset=bass.IndirectOffsetOnAxis(ap=ridx[i][:, kk:kk + 1], axis=0),
                    in_=h1b[i][:, :], in_offset=None, bounds_check=_breg(e, breg), oob_is_err=False),
                    [f"h1b{i}", f"ridx{i}", "Xg"], ["Xg_s"], dma=True)
        P.emit(st)


def phase_D(nc, io, sc):
    with ExitStack() as st:
        C = Ctx(nc, st, "D")
        P = C.P
        ident = C.sb("ident", [128, 128], BF16)
        P.dma("sp", ident[:, :], io["ident"], [], ["ident"])
        wg = [C.sb(f"wg{i}", [128, 16, 512], BF16) for i in range(2)]
        wu = [C.sb(f"wu{i}", [128, 16, 512], BF16) for i in range(2)]
        wd = [C.sb(f"wd{i}", [128, 4, 2048], BF16) for i in range(2)]
        xe = [C.sb(f"xe{i}", [128, 2048], BF16) for i in range(2)]
        xT = [C.sb(f"xT{i}", [128, 16, 128], BF16) for i in range(2)]
        sg = C.sb("sg", [128, 512], F32)
        hm = C.sb("hm", [128, 512], BF16)
        hT = C.sb("hT", [128, 4, 128], BF16)
        ye = [C.sb(f"ye{i}", [128, 2048], F32) for i in range(2)]
        pg = C.ps("pg", [128, 512], F32)
        pu = C.ps("pu", [128, 512], F32)
        py = [C.ps(f"py{i}", [128, 512], F32) for i in range(2)]
        pT = [C.ps(f"pT{i}", [128, 8, 128], BF16) for i in range(2)]
        n = 0
        for e_ in (range(NEXP) if EXPERTS is None else EXPERTS):
            i = e_ % 2
            P.dma("sp", wg[i][:, :, :], sc["WgB"][e_].rearrange("p (k n) -> p k n", k=16), ["WgB"], [f"wg{i}"])
            P.dma("sp", wu[i][:, :, :], sc["WuB"][e_].rearrange("p (k n) -> p k n", k=16), ["WuB"], [f"wu{i}"])
            P.dma("sp", wd[i][:, :, :], sc["WdB"][e_].rearrange("p (k n) -> p k n", k=4), ["WdB"], [f"wd{i}"])
            P.dma("sp", xe[i][:, :], sc["Xg"][e_ * 128:(e_ + 1) * 128, :], ["Xg"], [f"xe{i}"])
            for hh in range(2):
                for k in range(8):
                    kk = hh * 8 + k
                    P.tr(pT[hh][:, k, :], xe[i][:, kk * 128:(kk + 1) * 128], ident[:, :], [f"xe{i}", "ident"], [f"pT{hh}"])
                if hh == 0:
                    P.add("act", lambda e, i=i: e.copy(xT[i][:, 0:8, :], pT[0][:, :, :]), ["pT0"], [f"xT{i}"])
                else:
                    P.add("dve", lambda e, i=i: e.tensor_copy(xT[i][:, 8:16, :], pT[1][:, :, :]), ["pT1"], [f"xT{i}"])
            for k in range(16):
                P.mm(pg[:, :], xT[i][:, k, :], wg[i][:, k, :], k == 0, k == 15, [f"xT{i}", f"wg{i}"], ["pg"])
            for k in range(16):
                P.mm(pu[:, :], xT[i][:, k, :], wu[i][:, k, :], k == 0, k == 15, [f"xT{i}", f"wu{i}"], ["pu"])
            _act(P, sg[:, :], pg[:, :], AF.Silu, ["pg"], ["sg"])
            _tt(P, hm[:, :], sg[:, :], pu[:, :], ALU.mult, ["sg", "pu"], ["hm"])
            for k in range(4):
                P.tr(pT[0][:, k, :], hm[:, k * 128:(k + 1) * 128], ident[:, :], ["hm", "ident"], ["pT0"])
            P.add("act", lambda e: e.copy(hT[:, :, :], pT[0][:, 0:4, :]), ["pT0"], ["hT"])
            for cc in range(4):
                j = n % 2
                n += 1
                cs = slice(cc * 512, (cc + 1) * 512)
                for k in range(4):
                    P.mm(py[j][:, :], hT[:, k, :], wd[i][:, k, cs], k == 0, k == 3, ["hT", f"wd{i}"], [f"py{j}"])
                if cc % 2 == 0:
                    P.add("act", lambda e, i=i, j=j, cs=cs: e.copy(ye[i][:, cs], py[j][:, :]), [f"py{j}"], [f"ye{i}"])
                else:
                    P.add("dve", lambda e, i=i, j=j, cs=cs: e.tensor_copy(ye[i][:, cs], py[j][:, :]), [f"py{j}"], [f"ye{i}"])
            P.dma("sp", sc["Yg"][e_ * 128:(e_ + 1) * 128, :], ye[i][:, :], [f"ye{i}"], ["Yg"])
        P.emit(st)


def phase_E(nc, io, sc):
    with ExitStack() as st:
        C = Ctx(nc, st, "E")
        P = C.P
        breg = {}
        g2 = C.sb("g2", [128, 2048], F32)
        b2 = C.sb("b2", [128, 2048], F32)
        P.dma("sp", g2[:, :], io["ln2_g"], [], ["g2"])
        P.dma("sp", b2[:, :], io["ln2_b"], [], ["b2"])
        y1 = [C.sb(f"y1_{i}", [128, 2048], F32) for i in range(2)]
        y2 = [C.sb(f"y2_{i}", [128, 2048], F32) for i in range(2)]
        h1 = [C.sb(f"h1_{i}", [128, 2048], F32) for i in range(2)]
        ridx = [C.sb(f"ridx{i}", [128, 2], I32) for i in range(2)]
        rw = [C.sb(f"rw{i}", [128, 2], F32) for i in range(2)]
        r = C.sb("r", [128, 2048], F32)
        tmp = C.sb("tmp", [128, 2048], F32)
        ot = [C.sb(f"ot{i}", [128, 2048], F32) for i in range(2)]
        st6 = C.sb("st6", [128, 24], F32)
        mv = C.sb("mv", [128, 8], F32)
        P.add("pool", lambda e: e.memset(mv[:, 4:5], LN_EPS), [], ["mv"])
        for tt in range(16):
            i = tt % 2
            ts_ = slice(tt * 128, (tt + 1) * 128)
            P.dma("sp", ridx[i][:, :], sc["Ridx"][ts_, :], ["Ridx"], [f"ridx{i}"])
            P.dma("sp", rw[i][:, :], sc["Rw"][ts_, :], ["Rw"], [f"rw{i}"])
            P.dma("sp", h1[i][:, :], sc["H1"][ts_, :], ["H1"], [f"h1_{i}"])
            P.add("pool", lambda e, i=i: e.memset(y1[i][:, :], 0.0), [], [f"y1_{i}"])
            P.add("pool", lambda e, i=i: e.memset(y2[i][:, :], 0.0), [], [f"y2_{i}"])
            for kk, yb, yk in ((0, y1[i], f"y1_{i}"), (1, y2[i], f"y2_{i}")):
                P.add("pool", lambda e, i=i, kk=kk, yb=yb: e.indirect_dma_start(
                    out=yb[:, :], out_offset=None, in_=sc["Yg"][:, :],
                    in_offset=bass.IndirectOffsetOnAxis(ap=ridx[i][:, kk:kk + 1], axis=0),
                    bounds_check=_breg(e, breg), oob_is_err=False), [f"ridx{i}", "Yg", yk], [yk + "g"], dma=True)
            _ts(P, tmp[:, :], y1[i][:, :], rw[i][:, 0:1], None, ALU.mult, None, [f"y1_{i}", f"y1_{i}g", f"rw{i}"], ["tmp"])
            _stt(P, tmp[:, :], y2[i][:, :], rw[i][:, 1:2], tmp[:, :], ALU.mult, ALU.add, [f"y2_{i}", f"y2_{i}g", f"rw{i}", "tmp"], ["tmp"])
            _stt(P, r[:, :], h1[i][:, :], DN_ALPHA, tmp[:, :], ALU.mult, ALU.add, [f"h1_{i}", "tmp"], ["r"])
            _layer_norm(P, ot[i], r, "r", f"ot{i}", g2, b2, ["g2", "b2"], st6, mv, tmp, ["st6", "mv", "tmp"])
            P.dma("sp", sc["out"][ts_, :], ot[i][:, :], [f"ot{i}"], ["out"])
        P.emit(st)


EXPERTS = None
BG_SPLIT = (range(0, 36), range(36, 48), range(48, 64))
_IN_C = {
    "identf": ([128, 128], F32), "w_nsa_proj": ([1024, 2048], F32), "w_pool_proj": ([1024, 2048], F32),
    "w_out": ([2048, 2048], F32), "w_router": ([2048, 72], F32), "b_router": ([128, 72], F32),
    "eid64": ([128, 64], F32), "utri": ([128, 128], BF16), "ln1_g": ([128, 2048], F32), "ln1_b": ([128, 2048], F32),
    "ln2_g": ([128, 2048], F32), "ln2_b": ([128, 2048], F32), "x_own": ([NT, 2048], F32),
    "w_gate": ([NEXP, 2048, 512], F32), "w_up": ([NEXP, 2048, 512], F32), "w_down": ([NEXP, 512, 2048], F32),
}
_SC_C = {
    "ZT": ([16, 128, NT], BF16), "H1": ([NT, 2048], F32), "Xg": ([NEXP * CAP, 2048], BF16),
    "Yg": ([NEXP * CAP, 2048], F32), "Ridx": ([NT, 2], I32), "Rw": ([NT, 2], F32),
    "WgB": ([NEXP, 128, 16 * 512], BF16), "WuB": ([NEXP, 128, 16 * 512], BF16), "WdB": ([NEXP, 128, 4 * 2048], BF16),
}


def _shared_inputs(inp):
    w_in = inp["w_in"][0]
    ca = np.ascontiguousarray
    m = {
        "ident": _bf16(np.eye(128)), "identf": np.eye(128, dtype=np.float32),
        "w_A1": ca(np.concatenate([w_in[:, 2560:3072], w_in[:, 2048:2560]], 1)),
        "w_A2": ca(w_in[:, 3072:3584]), "w_q": ca(w_in[:, 1024:2048]), "w_gn": ca(w_in[:, 3584:3632]),
        "w_pool": ca(w_in[:, 0:1024]), "w_gm": ca(w_in[:, 3632:7728]),
        "pool_mix": ca(inp["pool_mix"][0]), "pool_scale": ca(inp["pool_scale"][0].reshape(8, 128).T),
        "cmp_k_w1": ca(inp["cmp_k_w1"][0]), "cmp_v_w1": ca(inp["cmp_v_w1"][0]),
        "cmp_k_w2": ca(inp["cmp_k_w2"][0]), "cmp_v_w2": ca(inp["cmp_v_w2"][0]),
        "cmp_k_w2s": ca(np.concatenate([inp["cmp_k_w2"][0][:, 32:], inp["cmp_k_w2"][0][:, :32]], 1)),
        "cmp_pos_kT": ca(inp["cmp_pos_k"][0].T), "cmp_pos_vT": ca(inp["cmp_pos_v"][0].T),
        "w_nsa_proj": ca(inp["w_nsa_proj"][0]), "w_pool_proj": ca(inp["w_pool_proj"][0]), "w_out": ca(inp["w_out"][0]),
        "w_router": ca(np.concatenate([inp["router_group_w"][0],
                                       inp["router_expert_w"][0].transpose(1, 0, 2).reshape(D, 64)], 1)),
        "b_router": ca(np.broadcast_to(np.concatenate([inp["router_group_b"][0], inp["router_expert_b"][0].reshape(64)])[None, :], (128, 72))),
        "eid64": ca(np.broadcast_to(np.arange(64, dtype=np.float32)[None, :], (128, 64))),
        "utri": _bf16((np.arange(128)[:, None] < np.arange(128)[None, :]).astype(np.float32)),
        "w_gate": ca(inp["w_gate"][0]), "w_up": ca(inp["w_up"][0]), "w_down": ca(inp["w_down"][0]),
    }
    for n in ("ln1_g", "ln1_b", "ln2_g", "ln2_b"):
        m[n] = ca(np.broadcast_to(inp[n][0][None, :], (128, D)))
    return m


def _core_inputs(inp, shared, core, tabs):
    b, p = core // 2, core % 2
    t = tabs[p]
    x0 = inp["x"][b]
    xo = x0[t["own_pos"]]
    xp = x0[t["prev_pos"]] * t["prev_valid"][:, None]
    m = dict(shared)
    m["xTg"] = np.ascontiguousarray(x0.T)
    m["xTo"] = np.ascontiguousarray(xo.T)
    m["xTp"] = np.ascontiguousarray(xp.T)
    m["x_own"] = np.ascontiguousarray(xo)
    for k in ALL_IN:
        if k not in m:
            m[k] = t[k]
    return m


ALL_IN = {}
ALL_IN.update(_IN_A)
ALL_IN.update(_IN_B)
ALL_IN.update(_IN_C)
ALL_SC = {}
ALL_SC.update(_SC_A)
ALL_SC.update(_SC_C)
PHASES = [phase_A, phase_B, phase_C1, phase_C2, phase_D, phase_E]


def build_full():
    return build_nc(ALL_IN, ALL_SC, PHASES, final_out=("out", [NT, D], F32))


def kernel(**inputs):
    inp = {k: np.asarray(v) for k, v in inputs.items()}
    tabs = []
    for p in range(2):
        t = _core_tables(p)
        tabs.append(_core_tables_B(p, t))
    shared = _shared_inputs(inp)
    in_maps = [_core_inputs(inp, shared, c, tabs) for c in range(8)]
    nc = build_full()
    res = run_bass_kernel_spmd(nc, in_maps, core_ids=list(range(8)))
    out = np.zeros((4, S, D), np.float32)
    for c in range(8):
        b, p = c // 2, c % 2
        out[b, tabs[p]["own_pos"]] = np.asarray(res.results[c]["out"]).astype(np.float32)
    return out
```

```python
import numpy as np
import concourse.bass as bass
import concourse.mybir as mybir
from concourse.bass_utils import run_bass_kernel_spmd
from contextlib import ExitStack

F32 = mybir.dt.float32
BF16 = mybir.dt.bfloat16
I32 = mybir.dt.int32
U32 = mybir.dt.uint32
AF = mybir.ActivationFunctionType
ALU = mybir.AluOpType
AX = mybir.AxisListType

D = 2048
S = 4096
NT = 2048
HD = 64
NH = 16
NG = 4
NCMP = 255
NEG = -30000.0
OWN = ([0, 3, 4, 7], [1, 2, 5, 6])
DN_ALPHA = 2.0 ** 0.25
LN_EPS = 1e-5
NEXP = 64
CAP = 128

DEBUG = []
STOP_AFTER = None
GROUPS = None
PARTS = None


def _on(name):
    return PARTS is None or name in PARTS


class _Op:
    __slots__ = ("eng", "fn", "dma", "deps", "idx", "marked", "count", "sem", "semval", "gid")


class Phase:
    ENGS = ("pe", "act", "dve", "pool", "sp")
    NDMA = 6

    def __init__(self, nc, name):
        self.nc = nc
        self.name = name
        self.ops = {e: [] for e in self.ENGS}
        self.bufs = {}
        self.excl = set()
        self.nops = 0

    def _buf(self, k):
        b = self.bufs.get(k)
        if b is None:
            b = [[], []]
            self.bufs[k] = b
        return b

    def add(self, eng, fn, reads=(), writes=(), dma=False):
        op = _Op()
        op.eng, op.fn, op.dma = eng, fn, dma
        op.idx = len(self.ops[eng])
        op.marked = False
        op.gid = self.nops
        self.nops += 1
        deps = set()
        for k in reads:
            b = self._buf(k)
            deps.update(b[0])
            if k in self.excl:
                for r in b[1]:
                    if r.eng != eng:
                        deps.add(r)
            b[1].append(op)
        for k in writes:
            b = self._buf(k)
            if dma and b[0] and not b[1] and all(w.dma for w in b[0]):
                b[0].append(op)
            else:
                deps.update(b[0])
                deps.update(b[1])
                b[1] = []
                b[0] = [op]
        deps.discard(op)
        op.deps = deps
        self.ops[eng].append(op)
        return op

    def mm(self, out, lhsT, rhs, start, stop, reads, writes, skip=False):
        if skip:
            return self.add("pe", lambda e: e.matmul(out, lhsT, rhs, start=start, stop=stop, skip_group_check=True), reads, writes)
        return self.add("pe", lambda e: e.matmul(out, lhsT, rhs, start=start, stop=stop), reads, writes)

    def tr(self, out, in_, ident, reads, writes):
        return self.add("pe", lambda e: e.transpose(out, in_, ident), reads, writes)

    def dma(self, q, out, in_, reads, writes):
        return self.add(q, lambda e: e.dma_start(out=out, in_=in_), reads, writes, dma=True)

    def emit(self, stack):
        nc = self.nc
        engs = {"pe": nc.tensor, "act": nc.scalar, "dve": nc.vector, "pool": nc.gpsimd, "sp": nc.sync}
        for e in self.ENGS:
            for op in self.ops[e]:
                for d in op.deps:
                    if d.dma:
                        continue
                    if d.eng == "pe" and op.eng == "pe" and not op.dma:
                        continue
                    d.marked = True
        esem = {e: nc.alloc_semaphore(name=f"{self.name}_{e}") for e in self.ENGS}
        dsem = {}
        for e in self.ENGS:
            cnt = 0
            nd = 0
            for op in self.ops[e]:
                if op.dma:
                    if e not in dsem:
                        dsem[e] = [nc.alloc_semaphore(name=f"{self.name}_{e}_d{i}") for i in range(self.NDMA)]
                    op.sem = dsem[e][nd % self.NDMA]
                    op.semval = 16 * (nd // self.NDMA + 1)
                    nd += 1
                elif op.marked:
                    cnt += 1
                    op.count = cnt
            assert cnt < 60000, (self.name, e, cnt)
        block = stack.enter_context(nc.Block())

        def body(ename):
            def _(eng):
                seen = {}

                def wait(sem, val):
                    k = id(sem)
                    if seen.get(k, 0) >= val:
                        return
                    seen[k] = val
                    eng.wait_ge(sem, val)

                for op in self.ops[ename]:
                    for d in sorted(op.deps, key=lambda o: o.gid):
                        if d.dma:
                            wait(d.sem, d.semval)
                        elif d.eng == "pe" and ename == "pe" and not op.dma:
                            continue
                        else:
                            wait(esem[d.eng], d.count)
                    if op.dma:
                        if op.semval > 16:
                            wait(op.sem, op.semval - 16)
                        op.fn(eng).then_inc(op.sem, 16)
                    else:
                        ins = op.fn(eng)
                        if op.marked:
                            ins.then_inc(esem[ename], 1)
                last = {}
                for op in self.ops[ename]:
                    if op.dma:
                        last[id(op.sem)] = (op.sem, op.semval)
                for sem, val in last.values():
                    wait(sem, val)
            return _

        for ename, reg in (("pe", block.tensor), ("act", block.scalar), ("dve", block.vector),
                           ("pool", block.gpsimd), ("sp", block.sync)):
            if self.ops[ename]:
                reg(body(ename))


class Ctx:
    def __init__(self, nc, stack, name):
        self.nc, self.stack, self.name = nc, stack, name
        self.P = Phase(nc, name)

    def sb(self, name, shape, dt):
        return self.stack.enter_context(self.nc.sbuf_tensor(f"{self.name}_{name}", list(shape), dt))

    def ps(self, name, shape, dt):
        self.P.excl.add(name)
        return self.stack.enter_context(self.nc.psum_tensor(f"{self.name}_{name}", list(shape), dt))


def _dram(nc, name, shape, dt, kind):
    return nc.dram_tensor(name, list(shape), dt, kind=kind).ap()


def _scratch(nc, name, shape, dt):
    kind = "ExternalOutput" if name in DEBUG else "Internal"
    return _dram(nc, name, shape, dt, kind)


def _rope(P, dst, src, cos, sin, nh, tmp, rkeys, wkeys, tag):
    s3 = src.rearrange("p (h d) -> p h d", h=nh)
    d3 = dst.rearrange("p (h d) -> p h d", h=nh)
    cb = cos.unsqueeze(1).broadcast_to([128, nh, 32])
    sn = sin.unsqueeze(1).broadcast_to([128, nh, 32])
    t1 = tmp[:, 0, 0:nh, :]
    t2 = tmp[:, 1, 0:nh, :]
    q1, q2 = s3[:, :, 0:32], s3[:, :, 32:64]
    tk = [tag + "_t1", tag + "_t2"]
    P.add("dve", lambda e: e.tensor_tensor(t1, q1, cb, ALU.mult), rkeys, [tk[0]])
    P.add("dve", lambda e: e.tensor_tensor(t2, q2, sn, ALU.mult), rkeys, [tk[1]])
    P.add("dve", lambda e: e.tensor_tensor(d3[:, :, 0:32], t1, t2, ALU.subtract), tk, wkeys)
    P.add("dve", lambda e: e.tensor_tensor(t1, q2, cb, ALU.mult), rkeys, [tk[0]])
    P.add("dve", lambda e: e.tensor_tensor(t2, q1, sn, ALU.mult), rkeys, [tk[1]])
    P.add("dve", lambda e: e.tensor_tensor(d3[:, :, 32:64], t1, t2, ALU.add), tk, wkeys)


def phase_A(nc, io, sc):
    with ExitStack() as st:
        C = Ctx(nc, st, "A")
        P = C.P
        ident = C.sb("ident", [128, 128], BF16)
        P.dma("sp", ident[:, :], io["ident"], [], ["ident"])
        wk = [C.sb(f"w{i}", [128, 16, 512], BF16) for i in range(2)]
        xt = [C.sb(f"xt{i}", [128, 16, 512], BF16) for i in range(2)]
        xo = C.sb("xo", [128, 16, NT], BF16)
        xh = C.sb("xh", [128, 16, 64], BF16)
        cosg = C.sb("cosg", [128, 32, 32], F32)
        sing = C.sb("sing", [128, 32, 32], F32)
        cosp = C.sb("cosp", [128, 16, 32], F32)
        sinp = C.sb("sinp", [128, 16, 32], F32)
        coso = C.sb("coso", [128, 16, 32], F32)
        sino = C.sb("sino", [128, 16, 32], F32)
        cosq = C.sb("cosq", [128, 16, 32], F32)
        sinq = C.sb("sinq", [128, 16, 32], F32)
        for t, n in ((cosg, "cosg"), (sing, "sing"), (cosp, "cosp"), (sinp, "sinp"), (coso, "coso"),
                     (sino, "sino"), (cosq, "cosq"), (sinq, "sinq")):
            P.dma("sp", t[:, :, :], io[n], [], [n])
        rtmp = C.sb("rtmp", [128, 2, 8, 32], F32)
        ksr = [C.sb(f"ksr{i}", [128, 512], BF16) for i in range(2)]
        kcv = [C.sb(f"kcv{i}", [128, 512], BF16) for i in range(2)]
        stT = [C.sb(f"stT{i}", [128, 6, 512], BF16) for i in range(2)]
        vst = [C.sb(f"vst{i}", [128, 4, 256], BF16) for i in range(2)]
        pm = [C.ps(f"pm{i}", [128, 512], F32) for i in range(4)]
        pT = [C.ps(f"pT{i}", [128, 8, 128], BF16) for i in range(2)]

        wq = ["pool", "sp"]
        nw = [0]
        nx = [0]
        npm = [0]
        npt = [0]

        def load_w(src_ap, ncols=512):
            i = nw[0] % 2
            nw[0] += 1
            P.dma("pool", wk[i][:, :, 0:ncols], src_ap.rearrange("(k p) n -> p k n", p=128), [], [f"w{i}"])
            return i

        def load_x(src_ap):
            i = nx[0] % 2
            nx[0] += 1
            P.dma("pool", xt[i][:, :, :], src_ap.rearrange("(k p) n -> p k n", p=128), [], [f"xt{i}"])
            return i

        def proj(xtile_ap, xkey, wi, ncols=512, c0=0):
            b = npm[0] % 4
            npm[0] += 1
            for k in range(16):
                P.mm(pm[b][:, 0:ncols], xtile_ap(k), wk[wi][:, k, c0:c0 + ncols], k == 0, k == 15,
                     [xkey, f"w{wi}"], [f"pm{b}"])
            return b

        def transposes(srcs, dst, dstkey, tcol):
            b = npt[0] % 2
            npt[0] += 1
            n = len(srcs)
            for i, (ap, key) in enumerate(srcs):
                P.tr(pT[b][:, i, :], ap, ident[:, :], [key, "ident"], [f"pT{b}"])
            P.add("act", lambda e: e.copy(dst[:, 0:n, tcol * 128:(tcol + 1) * 128], pT[b][:, 0:n, :]),
                  [f"pT{b}"], [dstkey])

        w_ks = load_w(io["w_A1"][:, 0:512])
        w_kc = load_w(io["w_A1"][:, 512:1024])
        for ch in (range(8) if _on("A1") else []):
            xi = load_x(io["xTg"][:, ch * 512:(ch + 1) * 512])
            si = ch % 2
            for j in range(4):
                tt = ch * 4 + j
                xa = (lambda xi, j: (lambda k: xt[xi][:, k, j * 128:(j + 1) * 128]))(xi, j)
                b0 = proj(xa, f"xt{xi}", w_ks)
                b1 = proj(xa, f"xt{xi}", w_kc)
                r = tt % 2
                _rope(P, ksr[r][:, 0:256], pm[b0][:, 0:256], cosg[:, tt, :], sing[:, tt, :], 4, rtmp,
                      [f"pm{b0}", "cosg", "sing"], [f"ksr{r}"], "rA")
                P.add("act", lambda e, si=si, j=j, b0=b0: e.copy(vst[si][:, j, :], pm[b0][:, 256:512]),
                      [f"pm{b0}"], [f"vst{si}"])
                P.add("act", lambda e, r=r, b1=b1: e.copy(kcv[r][:, :], pm[b1][:, :]), [f"pm{b1}"], [f"kcv{r}"])
                srcs = [(ksr[r][:, 0:128], f"ksr{r}"), (ksr[r][:, 128:256], f"ksr{r}")]
                srcs += [(kcv[r][:, i * 128:(i + 1) * 128], f"kcv{r}") for i in range(4)]
                transposes(srcs, stT[si], f"stT{si}", j)
            P.dma("sp", sc["KT_A1"].rearrange("i p t -> p i t")[:, :, ch * 512:(ch + 1) * 512], stT[si][:, :, :],
                  [f"stT{si}"], ["KT_A1"])
            P.dma("sp", sc["Vs"].rearrange("(c j p) n -> c p j n", j=4, p=128)[ch], vst[si][:, :, :],
                  [f"vst{si}"], ["Vs"])

        w_kw = load_w(io["w_A2"])
        for ch in range(4):
            P.dma("pool", xo[:, :, ch * 512:(ch + 1) * 512],
                  io["xTo"][:, ch * 512:(ch + 1) * 512].rearrange("(k p) n -> p k n", p=128), [], ["xo"])
        for lc in range(4):
            P.dma("pool", xh[:, :, lc * 16:(lc + 1) * 16],
                  io["xTp"][:, lc * 512 + 496:lc * 512 + 512].rearrange("(k p) t -> p k t", p=128), [], ["xh"])
        for which in (("prev", "own") if _on("A2") else ()):
            cosw, sinw = (cosp, sinp) if which == "prev" else (coso, sino)
            cn, sn_ = ("cosp", "sinp") if which == "prev" else ("coso", "sino")
            for ch in range(4):
                si = ch % 2
                if which == "prev":
                    xi = load_x(io["xTp"][:, ch * 512:(ch + 1) * 512])
                for j in range(4):
                    tt = ch * 4 + j
                    if which == "prev":
                        xa = (lambda xi, j: (lambda k: xt[xi][:, k, j * 128:(j + 1) * 128]))(xi, j)
                        xkey = f"xt{xi}"
                    else:
                        xa = (lambda tt: (lambda k: xo[:, k, tt * 128:(tt + 1) * 128]))(tt)
                        xkey = "xo"
                    b0 = proj(xa, xkey, w_kw)
                    r = tt % 2
                    _rope(P, ksr[r][:, 0:256], pm[b0][:, 0:256], cosw[:, tt, :], sinw[:, tt, :], 4, rtmp,
                          [f"pm{b0}", cn, sn_], [f"ksr{r}"], "rA")
                    P.add("act", lambda e, si=si, j=j, b0=b0: e.copy(vst[si][:, j, :], pm[b0][:, 256:512]),
                          [f"pm{b0}"], [f"vst{si}"])
                    srcs = [(ksr[r][:, 0:128], f"ksr{r}"), (ksr[r][:, 128:256], f"ksr{r}")]
                    transposes(srcs, stT[si], f"stT{si}", j)
                P.dma("sp", sc["KwT_" + which].rearrange("i p t -> p i t")[:, :, ch * 512:(ch + 1) * 512],
                      stT[si][:, 0:2, :], [f"stT{si}"], ["KwT_" + which])
                P.dma("sp", sc["Vw_" + which].rearrange("(c j p) n -> c p j n", j=4, p=128)[ch], vst[si][:, :, :],
                      [f"vst{si}"], ["Vw_" + which])

        for qc in (range(2) if _on("Q") else []):
            wi = load_w(io["w_q"][:, qc * 512:(qc + 1) * 512])
            for ch in range(4):
                si = ch % 2
                for j in range(4):
                    tt = ch * 4 + j
                    xa = (lambda tt: (lambda k: xo[:, k, tt * 128:(tt + 1) * 128]))(tt)
                    b0 = proj(xa, "xo", wi)
                    r = tt % 2
                    _rope(P, ksr[r][:, :], pm[b0][:, :], cosq[:, tt, :], sinq[:, tt, :], 8, rtmp,
                          [f"pm{b0}", "cosq", "sinq"], [f"ksr{r}"], "rA")
                    srcs = [(ksr[r][:, i * 128:(i + 1) * 128], f"ksr{r}") for i in range(4)]
                    transposes(srcs, stT[si], f"stT{si}", j)
                P.dma("sp", sc["QT"][qc * 4:(qc + 1) * 4].rearrange("i p t -> p i t")[:, :, ch * 512:(ch + 1) * 512],
                      stT[si][:, 0:4, :], [f"stT{si}"], ["QT"])

        gst = C.sb("gst", [128, 16, 48], F32)
        wi = load_w(io["w_gn"], 48)
        for tt in (range(16) if _on("GN") else []):
            xa = (lambda tt: (lambda k: xo[:, k, tt * 128:(tt + 1) * 128]))(tt)
            b0 = proj(xa, "xo", wi, 48)
            P.add("act", lambda e, tt=tt, b0=b0: e.activation(gst[:, tt, :], pm[b0][:, 0:48], AF.Sigmoid),
                  [f"pm{b0}"], ["gst"])
        P.dma("sp", sc["Gn"].rearrange("(t p) n -> p t n", p=128), gst[:, :, :], ["gst"], ["Gn"])

        gms = [C.sb(f"gms{i}", [128, 512], BF16) for i in range(2)]
        ng = 0
        for gc in (range(8) if _on("GM") else []):
            wi = load_w(io["w_gm"][:, gc * 512:(gc + 1) * 512])
            for tt in range(16):
                xa = (lambda tt: (lambda k: xo[:, k, tt * 128:(tt + 1) * 128]))(tt)
                b0 = proj(xa, "xo", wi)
                gi = ng % 2
                ng += 1
                P.add("act", lambda e, gi=gi, b0=b0: e.activation(gms[gi][:, :], pm[b0][:, :], AF.Sigmoid),
                      [f"pm{b0}"], [f"gms{gi}"])
                P.dma("sp", sc["Gm"][tt * 128:(tt + 1) * 128, gc * 512:(gc + 1) * 512], gms[gi][:, :],
                      [f"gms{gi}"], ["Gm"])

        pmix = C.sb("pmix", [128, 4, 2, 256], BF16)
        P.dma("pool", pmix[:, :, :, :], io["pool_mix"].rearrange("g (c p) d -> p g c d", p=128), [], ["pmix"])
        pscale = C.sb("pscale", [128, 8], F32)
        P.dma("sp", pscale[:, :], io["pool_scale"], [], ["pscale"])
        rc16 = C.sb("rc16", [128, 4, 4, 16], F32)
        P.dma("sp", rc16[:, :, :, :], io["rc16"], [], ["rc16"])
        U = [C.sb(f"U{i}", [128, 528], F32) for i in range(2)]
        Wa = C.sb("Wa", [128, 528], F32)
        Wb = C.sb("Wb", [128, 528], F32)
        pld = [C.sb(f"pld{i}", [128, 2, 512], BF16) for i in range(2)]
        mxs = [C.sb(f"mxs{i}", [128, 512], BF16) for i in range(2)]
        phalo = C.ps("phalo", [128, 16], F32)
        nu = 0
        nmx = 0
        for g in (range(4) if _on("POOL") else []):
            win = (2, 4, 8, 16)[g]
            wis = [load_w(io["w_pool"][:, (2 * g + c2) * 128:(2 * g + c2 + 1) * 128], 128) for c2 in range(2)]
            for lc in range(4):
                pi = (g * 4 + lc) % 2
                for c2 in range(2):
                    wi = wis[c2]
                    b = npm[0] % 4
                    npm[0] += 1
                    for k in range(16):
                        P.mm(pm[b][:, :], wk[wi][:, k, 0:128], xo[:, k, lc * 512:(lc + 1) * 512], k == 0, k == 15,
                             ["xo", f"w{wi}"], [f"pm{b}"])
                    for k in range(16):
                        P.mm(phalo[:, :], wk[wi][:, k, 0:128], xh[:, k, lc * 16:(lc + 1) * 16], k == 0, k == 15,
                             ["xh", f"w{wi}"], ["phalo"])
                    ui = nu % 2
                    nu += 1
                    Ut = U[ui]
                    uk = f"U{ui}"
                    P.add("act", lambda e, Ut=Ut, b=b: e.copy(Ut[:, 16:528], pm[b][:, :]), [f"pm{b}"], [uk])
                    P.add("act", lambda e, Ut=Ut: e.copy(Ut[:, 0:16], phalo[:, :]), ["phalo"], [uk])
                    src, sk = Ut, uk
                    step = 1
                    dsts = [(Wa, "Wa"), (Wb, "Wb")]
                    di = 0
                    while step < win:
                        dt_, dk = dsts[di % 2]
                        di += 1
                        lo = 2 * step - 1
                        P.add("dve", lambda e, dt_=dt_, src=src, lo=lo, step=step: e.tensor_tensor(
                            dt_[:, lo:528], src[:, lo:528], src[:, lo - step:528 - step], ALU.add), [sk], [dk])
                        src, sk = dt_, dk
                        step *= 2
                    P.add("dve", lambda e, src=src, Ut=Ut, pi=pi, c2=c2, win=win: e.scalar_tensor_tensor(
                        pld[pi][:, c2, 16:512], src[:, 32:528], 1.0 / win, Ut[:, 32:528], ALU.mult, ALU.subtract),
                        [sk, uk], [f"pld{pi}"])
                    P.add("dve", lambda e, src=src, lc=lc, g=g: e.tensor_tensor(
                        Wa[:, 0:16] if src is not Wa else Wb[:, 0:16], src[:, 16:32], rc16[:, lc, g, :], ALU.mult),
                        [sk, "rc16"], ["Wa" if src is not Wa else "Wb"])
                    P.add("dve", lambda e, src=src, Ut=Ut, pi=pi, c2=c2: e.tensor_tensor(
                        pld[pi][:, c2, 0:16], Wa[:, 0:16] if src is not Wa else Wb[:, 0:16], Ut[:, 16:32], ALU.subtract),
                        ["Wa" if src is not Wa else "Wb", uk], [f"pld{pi}"])
                for d2 in range(2):
                    b = npm[0] % 4
                    npm[0] += 1
                    for c2 in range(2):
                        P.mm(pm[b][:, :], pmix[:, g, c2, d2 * 128:(d2 + 1) * 128], pld[pi][:, c2, :], c2 == 0, c2 == 1,
                             ["pmix", f"pld{pi}"], [f"pm{b}"])
                    mi = nmx % 2
                    nmx += 1
                    ct = 2 * g + d2
                    P.add("dve", lambda e, mi=mi, b=b, ct=ct: e.tensor_scalar(
                        mxs[mi][:, :], pm[b][:, :], pscale[:, ct:ct + 1], None, ALU.mult), [f"pm{b}", "pscale"], [f"mxs{mi}"])
                    P.dma("sp", sc["MixT"][ct * 128:(ct + 1) * 128, lc * 512:(lc + 1) * 512], mxs[mi][:, :],
                          [f"mxs{mi}"], ["MixT"])
        P.emit(st)


def _bf16(a):
    import ml_dtypes
    return np.asarray(a, dtype=np.float32).astype(ml_dtypes.bfloat16)


def _rope_tab(pos, scale=1.0):
    half = HD // 2
    inv = (10000.0 ** (-2.0 * np.arange(half, dtype=np.float32) / HD)).astype(np.float32)
    ang = pos.astype(np.float32)[:, None] * inv[None, :]
    c = (np.cos(ang).astype(np.float32) * np.float32(scale)).astype(np.float32)
    s = (np.sin(ang).astype(np.float32) * np.float32(scale)).astype(np.float32)
    n = pos.shape[0] // 128
    c = np.ascontiguousarray(c.reshape(n, 128, half).transpose(1, 0, 2))
    s = np.ascontiguousarray(s.reshape(n, 128, half).transpose(1, 0, 2))
    return c, s


def _core_tables(p):
    t = {}
    own_pos = np.concatenate([np.arange(512 * g, 512 * g + 512) for g in OWN[p]])
    prev_pos = np.concatenate([np.arange(512 * (g - 1), 512 * g) if g > 0 else np.zeros(512, np.int64) for g in OWN[p]])
    prev_valid = np.concatenate([np.full(512, 1.0 if g > 0 else 0.0, np.float32) for g in OWN[p]])
    t["own_pos"], t["prev_pos"], t["prev_valid"] = own_pos, prev_pos, prev_valid
    t["cosg"], t["sing"] = _rope_tab(np.arange(S))
    t["cosp"], t["sinp"] = _rope_tab(prev_pos)
    t["coso"], t["sino"] = _rope_tab(own_pos)
    t["cosq"], t["sinq"] = _rope_tab(own_pos, HD ** -0.5)
    rc = np.zeros((128, 4, 4, 16), np.float32)
    for lc, g in enumerate(OWN[p]):
        for gi, w in enumerate((2, 4, 8, 16)):
            for tt in range(16):
                rc[:, lc, gi, tt] = 1.0 / (min(tt + 1, w) if g == 0 else w)
    t["rc16"] = rc
    return t


_IN_A = {
    "ident": ([128, 128], BF16), "xTg": ([D, S], F32), "xTo": ([D, NT], F32), "xTp": ([D, NT], F32),
    "w_A1": ([D, 1024], F32), "w_A2": ([D, 512], F32), "w_q": ([D, 1024], F32), "w_gn": ([D, 48], F32),
    "w_pool": ([D, 1024], F32), "w_gm": ([D, 4096], F32), "pool_mix": ([4, 256, 256], F32),
    "pool_scale": ([128, 8], F32), "rc16": ([128, 4, 4, 16], F32),
    "cosg": ([128, 32, 32], F32), "sing": ([128, 32, 32], F32), "cosp": ([128, 16, 32], F32),
    "sinp": ([128, 16, 32], F32), "coso": ([128, 16, 32], F32), "sino": ([128, 16, 32], F32),
    "cosq": ([128, 16, 32], F32), "sinq": ([128, 16, 32], F32),
}
_SC_A = {
    "KT_A1": ([6, 128, S], BF16), "Vs": ([S, 256], BF16), "KwT_prev": ([2, 128, NT], BF16),
    "KwT_own": ([2, 128, NT], BF16), "Vw_prev": ([NT, 256], BF16), "Vw_own": ([NT, 256], BF16),
    "QT": ([8, 128, NT], BF16), "Gn": ([NT, 48], F32), "Gm": ([NT, 4096], BF16), "MixT": ([1024, NT], BF16),
    "OT": ([8, 128, NT], BF16),
}


def build_nc(in_specs, sc_specs, phases, final_out=None):
    nc = bass.Bass("TRN2", target_bir_lowering=False)
    io = {n: _dram(nc, n, shp, dt, "ExternalInput") for n, (shp, dt) in in_specs.items()}
    sc = {n: _scratch(nc, n, shp, dt) for n, (shp, dt) in sc_specs.items()}
    if final_out is not None:
        n, shp, dt = final_out
        sc[n] = _dram(nc, n, shp, dt, "ExternalOutput")
    for ph in phases:
        snap = nc.snapshot_sems()
        ph(nc, io, sc)
        nc.clear_and_free_semaphores(nc.allocated_since(snap))
        nc.all_engine_barrier()
    return nc


def _ts(P, out, in0, s1, s2, op0, op1, reads, writes, eng="dve"):
    if op1 is None:
        return P.add(eng, lambda e: e.tensor_scalar(out, in0, s1, None, op0), reads, writes)
    return P.add(eng, lambda e: e.tensor_scalar(out, in0, s1, s2, op0, op1), reads, writes)


def _tt(P, out, in0, in1, op, reads, writes, eng="dve"):
    return P.add(eng, lambda e: e.tensor_tensor(out, in0, in1, op), reads, writes)


def _stt(P, out, in0, sc_, in1, op0, op1, reads, writes):
    return P.add("dve", lambda e: e.scalar_tensor_tensor(out, in0, sc_, in1, op0, op1), reads, writes)


def _act(P, out, in_, func, reads, writes, bias=None, scale=None):
    kw = {}
    if bias is not None:
        kw["bias"] = bias
    if scale is not None:
        kw["scale"] = scale
    return P.add("act", lambda e: e.activation(out, in_, func, **kw), reads, writes)


def phase_B(nc, io, sc):
    with ExitStack() as st:
        C = Ctx(nc, st, "B")
        P = C.P
        ident = C.sb("ident", [128, 128], BF16)
        P.dma("sp", ident[:, :], io["ident"], [], ["ident"])
        Ksp = C.sb("Ksp", [128, S], BF16)
        P.dma("sp", Ksp[64:128, :], io["Eoh"], [], ["Ksp_e"])
        Qp = C.sb("Qp", [128, 4, NT], BF16)
        Vsp = C.sb("Vsp", [128, 32, 65], BF16)
        Kwp = C.sb("Kwp", [64, 4, 8, 128], BF16)
        Vwp = C.sb("Vwp", [128, 4, 8, 65], BF16)
        pvo = C.sb("pvo", [128, 16], BF16)
        P.dma("sp", pvo[:, :], io["pvones"], [], ["pvo"])
        P.add("pool", lambda e: e.memset(Vsp[:, :, 64:65], 1.0), [], ["Vsp_1"])
        P.add("pool", lambda e: e.memset(Vwp[:, :, 4:8, 64:65], 1.0), [], ["Vwp_1"])
        P.add("pool", lambda e: e.tensor_copy(Vwp[:, :, 0:4, 64:65].rearrange("p a b c -> p a (b c)"),
                                              pvo[:, :].rearrange("p (a b) -> p a b", a=4)), ["pvo"], ["Vwp_1"])
        KcT = C.sb("KcT", [64, S], BF16)
        VcT = C.sb("VcT", [64, S], BF16)
        zeros = C.sb("zeros", [128, 2048], BF16)
        P.add("pool", lambda e: e.memset(zeros[:, :], 0.0), [], ["zeros"])
        for e_ in range(NEXP):
            P.dma("pool", sc["Xg"][e_ * 128:(e_ + 1) * 128, :], zeros[:, :], ["zeros"], ["Xg"])
        w1 = [C.sb(f"w1_{i}", [64, 32, 256], BF16) for i in range(2)]
        w2 = C.sb("w2", [128, 3, 2, 64], BF16)
        posT = C.sb("posT", [64, 2, 32], BF16)
        P.dma("pool", w1[0][:, :, :], io["cmp_k_w1"].rearrange("(l d) j -> d l j", d=64), [], ["w1_0"])
        P.dma("pool", w1[1][:, :, :], io["cmp_v_w1"].rearrange("(l d) j -> d l j", d=64), [], ["w1_1"])
        for i, n in enumerate(("cmp_k_w2", "cmp_k_w2s", "cmp_v_w2")):
            P.dma("pool", w2[:, i, :, :], io[n].rearrange("(t p) d -> p t d", p=128), [], ["w2"])
        P.dma("pool", posT[:, 0, :], io["cmp_pos_kT"], [], ["posT"])
        P.dma("pool", posT[:, 1, :], io["cmp_pos_vT"], [], ["posT"])
        ccos = C.sb("ccos", [64, 256], F32)
        csin = C.sb("csin", [64, 256], F32)
        P.dma("sp", ccos[:, :], io["ccos"], [], ["ccos"])
        P.dma("sp", csin[:, :], io["csin"], [], ["csin"])
        cbias = C.sb("cbias", [128, 2, 2, 512], BF16)
        tritab = C.sb("tritab", [128, 4, 8, 128], BF16)
        P.dma("sp", tritab[:, :, :, :], io["tritab"], [], ["tritab"])
        tri2 = C.sb("tri2", [128, 2, 128], BF16)
        P.dma("sp", tri2[:, :, :], io["tri2"], [], ["tri2"])
        Vcp = C.sb("Vcp", [128, 2, 129], BF16)
        P.add("pool", lambda e: e.memset(Vcp[:, :, 0:65], 0.0), [], ["Vcp"])
        P.add("pool", lambda e: e.memset(Vcp[:, :, 64:65], 1.0), [], ["Vcp"])
        P.dma("sp", Vcp[:, :, 65:129], io["ovl"], [], ["Vcp_o"])
        KcmpT = C.sb("KcmpT", [64, 256], BF16)
        P.add("pool", lambda e: e.memset(KcmpT[:, :], 0.0), [], ["KcmpT"])
        Mk = C.sb("Mk", [128, 16, 64], F32)
        Ad = C.sb("Ad", [128, 16, 64], F32)
        Fm = C.sb("Fm", [128, 16, 64], F32)
        for t, n in ((Mk, "Mk"), (Ad, "Ad"), (Fm, "Fm")):
            P.dma("sp", t[:, :, :], io[n], [], [n])
        Gn = C.sb("Gn", [128, 16, 48], F32)
        P.dma("sp", Gn[:, :, :], sc["Gn"].rearrange("(t p) n -> p t n", p=128), [], ["Gn"])
        O = C.sb("O", [128, 16, 256], F32)
        Ob = [C.sb(f"Ob{i}", [128, 256], BF16) for i in range(2)]
        Os = [C.sb(f"Os{i}", [128, 2, 128], BF16) for i in range(2)]
        Pt = [C.sb(f"Pt{i}", [128, 512], BF16) for i in range(4)]
        Pc = [C.sb(f"Pc{i}", [128, 2, 512], BF16) for i in range(2)]
        Pw = [C.sb(f"Pw{i}", [128, 5, 128], BF16) for i in range(3)]
        hb = C.sb("hb", [128, 256], F32)
        sq = C.sb("sq", [128, 256], F32)
        uu = C.sb("uu", [128, 256], F32)
        sg = C.sb("sg", [128, 256], F32)
        gT = C.sb("gT", [128, 2, 2, 256], BF16)
        cb = C.sb("cb", [128, 4], F32)
        impb = C.sb("impb", [128, 4, 64], F32)
        impm = C.sb("impm", [128, 64], F32)
        imp2 = C.sb("imp2", [128, 64], F32)
        m8 = C.sb("m8", [128, 16], F32)
        nsel = C.sb("nsel", [128, 64], F32)
        NegT = C.sb("NegT", [128, 4, 128], BF16)
        P.add("pool", lambda e: e.memset(NegT[:, :, :], 0.0), [], [f"NegT{i}" for i in range(4)])
        sm = C.sb("sm", [128, 16, 4], F32)
        nsm = [0]
        t1 = C.sb("t1", [64, 256], F32)
        t2 = C.sb("t2", [64, 256], F32)
        pS = [C.ps(f"pS{i}", [128, 512], F32) for i in range(4)]
        pA = [C.ps(f"pA{i}", [128, 512], F32) for i in range(2)]
        pX = C.ps("pX", [128, 512], F32)
        pTb = C.ps("pTb", [128, 8, 128], BF16)

        for kv in range(2):
            for jt in range(2):
                for l in range(32):
                    P.mm(pX[:, 0:1], w1[kv][:, l, jt * 128:(jt + 1) * 128], posT[:, kv, l:l + 1], l == 0, l == 31,
                         [f"w1_{kv}", "posT"], ["pX"])
                i = kv * 2 + jt
                P.add("dve", lambda e, i=i: e.tensor_copy(cb[:, i:i + 1], pX[:, 0:1]), ["pX"], ["cb"])

        npt = [0]
        npc = [0]
        npw = [0]
        nps = [0]

        for g in (range(4) if GROUPS is None else GROUPS):
            pi, hf = g // 2, g % 2
            rows = slice(hf * 64, hf * 64 + 64)
            P.dma("sp", Ksp[0:64, :], sc["KT_A1"][pi, rows, :], ["KT_A1"], ["Ksp_k"])
            P.dma("sp", KcT[:, :], sc["KT_A1"][2 + pi, rows, :], ["KT_A1"], ["KcT"])
            P.dma("sp", VcT[:, :], sc["KT_A1"][4 + pi, rows, :], ["KT_A1"], ["VcT"])
            P.dma("sp", Vsp[:, :, 0:64], sc["Vs"][:, g * 64:(g + 1) * 64].rearrange("(t p) d -> p t d", p=128),
                  ["Vs"], ["Vsp_v"])
            for lc in range(4):
                P.dma("sp", Kwp[:, lc, 0:4, :], sc["KwT_prev"][pi, rows, lc * 512:(lc + 1) * 512], ["KwT_prev"], ["Kwp"])
                P.dma("sp", Kwp[:, lc, 4:8, :], sc["KwT_own"][pi, rows, lc * 512:(lc + 1) * 512], ["KwT_own"], ["Kwp"])
                P.dma("sp", Vwp[:, lc, 0:4, 0:64],
                      sc["Vw_prev"][lc * 512:(lc + 1) * 512, g * 64:(g + 1) * 64].rearrange("(t p) d -> p t d", p=128),
                      ["Vw_prev"], ["Vwp_v"])
                P.dma("sp", Vwp[:, lc, 4:8, 0:64],
                      sc["Vw_own"][lc * 512:(lc + 1) * 512, g * 64:(g + 1) * 64].rearrange("(t p) d -> p t d", p=128),
                      ["Vw_own"], ["Vwp_v"])
            for hh in range(4):
                h = 4 * g + hh
                P.dma("sp", Qp[0:64, hh, :], sc["QT"][h // 2, (h % 2) * 64:(h % 2) * 64 + 64, :], ["QT"], ["Qp_q"])

            for kv, src, skey in ((0, KcT, "KcT"), (1, VcT, "VcT")):
                for jt in range(2):
                    for l in range(32):
                        P.mm(pX[:, 0:255], w1[kv][:, l, jt * 128:(jt + 1) * 128], src[:, l:l + 16 * 254 + 1:16],
                             l == 0, l == 31, [f"w1_{kv}", skey], ["pX"])
                    i = kv * 2 + jt
                    _act(P, sq[:, 0:255], pX[:, 0:255], AF.Square, ["pX", "cb"], ["sq"], bias=cb[:, i:i + 1])
                    _ts(P, hb[:, 0:255], pX[:, 0:255], cb[:, i:i + 1], None, ALU.add, None, ["pX", "cb"], ["hb"])
                    _ts(P, uu[:, 0:255], sq[:, 0:255], 0.044715, 1.0, ALU.mult, ALU.add, ["sq"], ["uu"])
                    _tt(P, uu[:, 0:255], uu[:, 0:255], hb[:, 0:255], ALU.mult, ["uu", "hb"], ["uu"])
                    _act(P, sg[:, 0:255], uu[:, 0:255], AF.Sigmoid, ["uu"], ["sg"], scale=1.5957691216057308)
                    _tt(P, gT[:, kv, jt, 0:255], hb[:, 0:255], sg[:, 0:255], ALU.mult, ["hb", "sg"], ["gT"])
            for jt in range(2):
                P.mm(pX[0:64, 0:255], w2[:, 0, jt, :], gT[:, 0, jt, 0:255], jt == 0, jt == 1, ["w2", "gT"], ["pX"])
            _tt(P, t1[:, 0:255], pX[0:64, 0:255], ccos[:, 0:255], ALU.mult, ["pX", "ccos"], ["t1"])
            for jt in range(2):
                P.mm(pX[0:64, 0:255], w2[:, 1, jt, :], gT[:, 0, jt, 0:255], jt == 0, jt == 1, ["w2", "gT"], ["pX"])
            _tt(P, t2[:, 0:255], pX[0:64, 0:255], csin[:, 0:255], ALU.mult, ["pX", "csin"], ["t2"])
            _tt(P, KcmpT[:, 0:255], t1[:, 0:255], t2[:, 0:255], ALU.add, ["t1", "t2"], ["KcmpT"])
            for ct in range(2):
                n = 128 if ct == 0 else 127
                for jt in range(2):
                    P.mm(pX[0:n, 0:64], gT[:, 1, jt, ct * 128:ct * 128 + n], w2[:, 2, jt, :], jt == 0, jt == 1,
                         ["gT", "w2"], ["pX"])
                P.add("dve", lambda e, ct=ct, n=n: e.tensor_copy(Vcp[0:n, ct, 0:64], pX[0:n, 0:64]), ["pX"], ["Vcp"])

            for lc in range(4):
                cbi = lc % 2
                P.dma("sp", cbias[:, :, cbi, :], io["cmpbias"][:, :, lc * 512:(lc + 1) * 512], [], [f"cbias{cbi}"])
                qsl = slice(lc * 512, (lc + 1) * 512)

                def norm(acc_ap, acc_key, tt, hh, gcol, first):
                    si = nsm[0] % 16
                    nsm[0] += 1
                    sk = f"sm{si}"
                    _ts(P, sm[:, si, 0:1], acc_ap[:, 64:65], 1e-30, None, ALU.max, None, [acc_key], [sk])
                    P.add("dve", lambda e: e.reciprocal(sm[:, si, 1:2], sm[:, si, 0:1]), [sk], [sk])
                    _tt(P, sm[:, si, 2:3], sm[:, si, 1:2], Gn[:, tt, gcol:gcol + 1], ALU.mult, [sk, "Gn"], [sk])
                    osl = O[:, tt, hh * 64:(hh + 1) * 64]
                    if first:
                        _ts(P, osl, acc_ap[:, 0:64], sm[:, si, 2:3], None, ALU.mult, None, [acc_key, sk], [f"O{tt}"])
                    else:
                        _stt(P, osl, acc_ap[:, 0:64], sm[:, si, 2:3], osl, ALU.mult, ALU.add, [acc_key, sk, f"O{tt}"], [f"O{tt}"])
                    return sm[:, si, 1:2], sk

                b1banks = {}

                def b1_qk(hh):
                    bs = []
                    for ct in range(2):
                        b = nps[0] % 2
                        nps[0] += 1
                        P.mm(pS[b][:, :], KcmpT[:, ct * 128:(ct + 1) * 128], Qp[0:64, hh, qsl], True, False,
                             ["KcmpT", "Qp_q"], [f"pS{b}"])
                        P.mm(pS[b][:, :], ident[:, :], cbias[:, ct, cbi, :], False, True,
                             ["ident", f"cbias{cbi}"], [f"pS{b}"])
                        bs.append(b)
                    b1banks[hh] = bs

                def b1_rest(hh):
                    h = 4 * g + hh
                    pci = npc[0] % 2
                    npc[0] += 1
                    for ct in range(2):
                        b = b1banks[hh][ct]
                        _act(P, Pc[pci][:, ct, :], pS[b][:, :], AF.Exp, [f"pS{b}"], [f"Pc{pci}"])
                    accs = (pA if hh % 2 == 0 else pS[2:4])
                    akeys = (["pA0", "pA1"] if hh % 2 == 0 else ["pS2", "pS3"])
                    for qs in range(4):
                        acc = accs[qs // 2][:, (qs % 2) * 256:(qs % 2) * 256 + 129]
                        ak = akeys[qs // 2]
                        for ct in range(2):
                            P.mm(acc, Pc[pci][:, ct, qs * 128:(qs + 1) * 128], Vcp[:, ct, :], ct == 0, ct == 1,
                                 [f"Pc{pci}", "Vcp", "Vcp_o"], [ak])
                    for qs in range(4):
                        tt = lc * 4 + qs
                        acc = accs[qs // 2][:, (qs % 2) * 256:(qs % 2) * 256 + 129]
                        ak = akeys[qs // 2]
                        rz, sk = norm(acc, ak, tt, hh, h, True)
                        if hh == 0:
                            _ts(P, impb[:, qs, :], acc[:, 65:129], rz, None, ALU.mult, None, [ak, sk], [f"impb{qs}"])
                        else:
                            _stt(P, impb[:, qs, :], acc[:, 65:129], rz, impb[:, qs, :], ALU.mult, ALU.add,
                                 [ak, sk, f"impb{qs}"], [f"impb{qs}"])

                for hh in range(4):
                    b1_qk(hh)
                    b1_rest(hh)

                for qs in range(4):
                    tt = lc * 4 + qs
                    iq = impb[:, qs, :]
                    _tt(P, impm[:, :], iq, Mk[:, tt, :], ALU.mult, [f"impb{qs}", "Mk"], ["impm"])
                    _tt(P, impm[:, :], impm[:, :], Ad[:, tt, :], ALU.add, ["impm", "Ad"], ["impm"])
                    P.add("dve", lambda e: e.max(m8[:, 0:8], impm[:, :]), ["impm"], ["m8"])
                    P.add("dve", lambda e: e.match_replace(imp2[:, :], m8[:, 0:8], impm[:, :], -3.0e6), ["impm", "m8"], ["imp2"])
                    P.add("dve", lambda e: e.max(m8[:, 8:16], imp2[:, :]), ["imp2"], ["m8"])
                    _ts(P, nsel[:, :], impm[:, :], m8[:, 15:16], -NEG, ALU.is_ge, ALU.mult, ["impm", "m8"], ["nsel"])
                    _stt(P, NegT[:, qs, 64:128], nsel[:, :], NEG, Fm[:, tt, :], ALU.add, ALU.add, ["nsel", "Fm"], [f"NegT{qs}"])

                wunits = [(hh, qs) for hh in range(4) for qs in range(4)]
                wbanks = {}

                def b3_qk(u):
                    hh, qs = wunits[u]
                    tt = lc * 4 + qs
                    b = nps[0] % 4
                    b2 = (nps[0] + 1) % 4
                    nps[0] += 2
                    qap = Qp[0:64, hh, tt * 128:(tt + 1) * 128]
                    for r in range(qs, qs + 4):
                        o = pS[b][:, (r - qs) * 128:(r - qs + 1) * 128]
                        P.mm(o, Kwp[:, lc, r, :], qap, True, r != qs, ["Kwp", "Qp_q"], [f"pS{b}"])
                        if r == qs:
                            P.mm(o, ident[:, :], tri2[:, 0, :], False, True, ["ident", "tri2"], [f"pS{b}"])
                    P.mm(pS[b2][:, 0:128], Kwp[:, lc, qs + 4, :], qap, True, False, ["Kwp", "Qp_q"], [f"pS{b2}"])
                    P.mm(pS[b2][:, 0:128], ident[:, :], tri2[:, 1, :], False, True, ["ident", "tri2"], [f"pS{b2}"])
                    wbanks[u] = (b, b2)

                def b3_rest(u):
                    hh, qs = wunits[u]
                    h = 4 * g + hh
                    tt = lc * 4 + qs
                    b, b2 = wbanks[u]
                    pwi = npw[0] % 3
                    npw[0] += 1
                    _act(P, Pw[pwi][:, 0:4, :], pS[b][:, :].rearrange("p (a b) -> p a b", a=4), AF.Exp, [f"pS{b}"], [f"Pw{pwi}"])
                    _act(P, Pw[pwi][:, 4, :], pS[b2][:, 0:128], AF.Exp, [f"pS{b2}"], [f"Pw{pwi}"])
                    ai = u % 2
                    acc = pA[ai][:, 0:65]
                    for r in range(5):
                        P.mm(acc, Pw[pwi][:, r, :], Vwp[:, lc, qs + r, :], r == 0, r == 4,
                             [f"Pw{pwi}", "Vwp_v", "Vwp_1"], [f"pA{ai}"])
                    norm(acc, f"pA{ai}", tt, hh, 32 + h, False)

                b3_qk(0)
                for u in range(16):
                    if u + 1 < 16:
                        b3_qk(u + 1)
                    b3_rest(u)

                for qs in range(4):
                    tt = lc * 4 + qs
                    P.tr(pTb[:, 4 + qs, :], NegT[:, qs, :], ident[:, :], [f"NegT{qs}", "ident"], ["pTb"])
                    P.add("act", lambda e, tt=tt, qs=qs: e.copy(
                        Qp[64:128, :, tt * 128:(tt + 1) * 128],
                        pTb[64:128, 4 + qs:5 + qs, :].broadcast_to([64, 4, 128])), ["pTb"], ["Qp_m"])

                E = 8 * (lc + 1)
                sunits = [(hh, kt) for hh in range(4) for kt in range(E)]
                sbanks = {}

                def b2_qk(u):
                    hh, kt = sunits[u]
                    b = nps[0] % 4
                    nps[0] += 1
                    s = kt - (E - 8)
                    P.mm(pS[b][:, :], Ksp[:, kt * 128:(kt + 1) * 128], Qp[:, hh, qsl], True, s < 0,
                         ["Ksp_k", "Ksp_e", "Qp_q", "Qp_m"], [f"pS{b}"])
                    if s >= 0:
                        qd = s % 4
                        P.mm(pS[b][:, qd * 128:(qd + 1) * 128], ident[:, :], tritab[:, lc, s, :], False, True,
                             ["ident", "tritab"], [f"pS{b}"])
                    sbanks[u] = b

                def b2_rest(u):
                    hh, kt = sunits[u]
                    h = 4 * g + hh
                    b = sbanks[u]
                    pti = npt[0] % 4
                    npt[0] += 1
                    _act(P, Pt[pti][:, :], pS[b][:, :], AF.Exp, [f"pS{b}"], [f"Pt{pti}"])
                    ai = hh % 2
                    for qs in range(4):
                        P.mm(pA[ai][:, qs * 128:qs * 128 + 65], Pt[pti][:, qs * 128:(qs + 1) * 128], Vsp[:, kt, :],
                             kt == 0 and qs == 0, kt == E - 1, [f"Pt{pti}", "Vsp_v", "Vsp_1"], [f"pA{ai}"], skip=True)
                    if kt == E - 1:
                        for qs in range(4):
                            norm(pA[ai][:, qs * 128:qs * 128 + 65], f"pA{ai}", lc * 4 + qs, hh, 16 + h, False)

                LA = 2
                nsu = len(sunits)
                for u in range(min(LA, nsu)):
                    b2_qk(u)
                for u in range(nsu):
                    if u + LA < nsu:
                        b2_qk(u + LA)
                    b2_rest(u)

            for tt in range(16):
                oi = tt % 2
                P.add("act", lambda e, oi=oi, tt=tt: e.copy(Ob[oi][:, :], O[:, tt, :]), [f"O{tt}"], [f"Ob{oi}"])
                for i in range(2):
                    P.tr(pTb[:, 2 + i, :], Ob[oi][:, i * 128:(i + 1) * 128], ident[:, :], [f"Ob{oi}", "ident"], ["pTb"])
                P.add("dve", lambda e, oi=oi: e.tensor_copy(Os[oi][:, :, :], pTb[:, 2:4, :]), ["pTb"], [f"Os{oi}"])
                P.dma("sp", sc["OT"][2 * g:2 * g + 2].rearrange("i p t -> p i t")[:, :, tt * 128:(tt + 1) * 128],
                      Os[oi][:, :, :], [f"Os{oi}"], ["OT"])
        P.emit(st)


def _core_tables_B(p, t):
    own_pos = t["own_pos"]
    c = np.arange(256)
    cend = 16 * c + 31
    valid = (c[:, None] <= 254) & (cend[:, None] <= own_pos[None, :])
    cb = np.where(valid, 0.0, NEG).astype(np.float32).reshape(2, 128, NT).transpose(1, 0, 2)
    t["cmpbias"] = _bf16(np.ascontiguousarray(cb))
    k = np.arange(128)
    tri = np.where(k[:, None] > k[None, :], NEG, 0.0).astype(np.float32)
    tt = np.zeros((128, 4, 8, 128), np.float32)
    for lc, gc in enumerate(OWN[p]):
        E = 8 * (lc + 1)
        for s in range(8):
            kt = E - 8 + s
            if 4 * gc <= kt < 4 * gc + 4:
                assert (kt - 4 * gc) == s % 4
                tt[:, lc, s, :] = tri
    t["tritab"] = _bf16(tt)
    tri2 = np.zeros((128, 2, 128), np.float32)
    tri2[:, 0, :] = np.where(k[:, None] <= k[None, :], NEG, 0.0)
    tri2[:, 1, :] = np.where(k[:, None] > k[None, :], NEG, 0.0)
    t["tri2"] = _bf16(tri2)
    cs = np.arange(256) * 16
    ss = np.arange(64) * 64
    ov = ((cs[:, None] + 31 >= ss[None, :]) & (cs[:, None] <= ss[None, :] + 63) & (c[:, None] <= 254)).astype(np.float32)
    t["ovl"] = _bf16(np.ascontiguousarray(ov.reshape(2, 128, 64).transpose(1, 0, 2)))
    cur = own_pos // 64
    j = np.arange(64)
    forced = (j[None, :] == 0) | (j[None, :] == cur[:, None]) | (j[None, :] == cur[:, None] - 1)
    future = j[None, :] > cur[:, None]
    mk = (~(forced | future)).astype(np.float32)
    ad = np.where(forced, 1e6, np.where(future, -1e6, 0.0)).astype(np.float32)
    fm = np.where(future, NEG, 0.0).astype(np.float32)
    lay = lambda a: np.ascontiguousarray(a.reshape(16, 128, 64).transpose(1, 0, 2))
    t["Mk"], t["Ad"], t["Fm"] = lay(mk), lay(ad), lay(fm)
    t["pvones"] = _bf16(np.ascontiguousarray(t["prev_valid"].reshape(16, 128).T))
    t["Eoh"] = _bf16((np.arange(S)[None, :] // 64 == j[:, None]).astype(np.float32))
    half = HD // 2
    inv = (10000.0 ** (-2.0 * np.arange(half, dtype=np.float32) / HD)).astype(np.float32)
    ang = cend.astype(np.float32)[None, :] * np.concatenate([inv, inv])[:, None]
    t["ccos"] = np.cos(ang).astype(np.float32)
    sn = np.sin(ang).astype(np.float32)
    sn[:half] *= -1.0
    t["csin"] = sn
    return t


_IN_B = {
    "Eoh": ([64, S], BF16), "pvones": ([128, 16], BF16), "cmp_k_w1": ([2048, 256], F32), "cmp_v_w1": ([2048, 256], F32),
    "cmp_k_w2": ([256, 64], F32), "cmp_k_w2s": ([256, 64], F32), "cmp_v_w2": ([256, 64], F32),
    "cmp_pos_kT": ([64, 32], F32), "cmp_pos_vT": ([64, 32], F32), "ccos": ([64, 256], F32), "csin": ([64, 256], F32),
    "tritab": ([128, 4, 8, 128], BF16), "tri2": ([128, 2, 128], BF16), "ovl": ([128, 2, 64], BF16),
    "cmpbias": ([128, 2, NT], BF16), "Mk": ([128, 16, 64], F32), "Ad": ([128, 16, 64], F32), "Fm": ([128, 16, 64], F32),
}


def phase_C1(nc, io, sc):
    with ExitStack() as st:
        C = Ctx(nc, st, "C1")
        P = C.P
        ident = C.sb("ident", [128, 128], BF16)
        P.dma("sp", ident[:, :], io["ident"], [], ["ident"])
        wn = C.sb("wn", [128, 8, 2048], BF16)
        wp = C.sb("wp", [128, 8, 2048], BF16)
        for c in range(4):
            cs = slice(c * 512, (c + 1) * 512)
            P.dma("pool", wn[:, :, cs], io["w_nsa_proj"][:, cs].rearrange("(k p) n -> p k n", p=128), [], ["wn"])
            P.dma("pool", wp[:, :, cs], io["w_pool_proj"][:, cs].rearrange("(k p) n -> p k n", p=128), [], ["wp"])
        oT = [C.sb(f"oT{i}", [128, 8, 128], BF16) for i in range(2)]
        mT = [C.sb(f"mT{i}", [128, 8, 128], BF16) for i in range(2)]
        gm = [C.sb(f"gm{i}", [128, 4096], BF16) for i in range(2)]
        ta = [C.sb(f"ta{i}", [128, 512], F32) for i in range(2)]
        tb = [C.sb(f"tb{i}", [128, 512], F32) for i in range(2)]
        z = [C.sb(f"z{i}", [128, 2048], BF16) for i in range(2)]
        zT = [C.sb(f"zT{i}", [128, 16, 128], BF16) for i in range(2)]
        pa = [C.ps(f"pa{i}", [128, 512], F32) for i in range(2)]
        pb = [C.ps(f"pb{i}", [128, 512], F32) for i in range(2)]
        pT = [C.ps(f"pT{i}", [128, 8, 128], BF16) for i in range(2)]
        n = [0]

        def c1_stage1(tt):
            i = tt % 2
            ts_ = slice(tt * 128, (tt + 1) * 128)
            P.dma("sp", oT[i][:, :, :], sc["OT"].rearrange("k p t -> p k t")[:, :, ts_], ["OT"], [f"oT{i}"])
            P.dma("sp", mT[i][:, :, :], sc["MixT"].rearrange("(k p) t -> p k t", p=128)[:, :, ts_], ["MixT"], [f"mT{i}"])
            P.dma("sp", gm[i][:, :], sc["Gm"][ts_, :], ["Gm"], [f"gm{i}"])
            for cc in range(4):
                j = n[0] % 2
                n[0] += 1
                cs = slice(cc * 512, (cc + 1) * 512)
                for k in range(8):
                    P.mm(pa[j][:, :], oT[i][:, k, :], wn[:, k, cs], k == 0, k == 7, [f"oT{i}", "wn"], [f"pa{j}"])
                for k in range(8):
                    P.mm(pb[j][:, :], mT[i][:, k, :], wp[:, k, cs], k == 0, k == 7, [f"mT{i}", "wp"], [f"pb{j}"])
                _tt(P, ta[j][:, :], pa[j][:, :], gm[i][:, 2048 + cc * 512:2048 + (cc + 1) * 512], ALU.mult,
                    [f"pa{j}", f"gm{i}"], [f"ta{j}"])
                _tt(P, tb[j][:, :], pb[j][:, :], gm[i][:, cs], ALU.mult, [f"pb{j}", f"gm{i}"], [f"tb{j}"])
                _tt(P, z[i][:, cs], ta[j][:, :], tb[j][:, :], ALU.add, [f"ta{j}", f"tb{j}"], [f"z{i}"], eng="pool")

        def c1_stage2(tt):
            i = tt % 2
            ts_ = slice(tt * 128, (tt + 1) * 128)
            for hh in range(2):
                for k in range(8):
                    kk = hh * 8 + k
                    P.tr(pT[hh][:, k, :], z[i][:, kk * 128:(kk + 1) * 128], ident[:, :], [f"z{i}", "ident"], [f"pT{hh}"])
                P.add("act", lambda e, hh=hh: e.copy(zT[i][:, hh * 8:(hh + 1) * 8, :], pT[hh][:, :, :]),
                      [f"pT{hh}"], [f"zT{i}"])
            P.dma("sp", sc["ZT"].rearrange("k p t -> p k t")[:, :, ts_], zT[i][:, :, :], [f"zT{i}"], ["ZT"])

        c1_stage1(0)
        for tt in range(16):
            if tt + 1 < 16:
                c1_stage1(tt + 1)
            c1_stage2(tt)
        P.emit(st)


def _layer_norm(P, dst, src, skey, dkey, g_bc, b_bc, gkeys, st6, mv, tmp, tkeys):
    for c in range(4):
        P.add("dve", lambda e, c=c: e.bn_stats(st6[:, c * 6:(c + 1) * 6], src[:, c * 512:(c + 1) * 512]), [skey], [tkeys[0]])
    P.add("dve", lambda e: e.bn_aggr(mv[:, 0:2], st6[:, 0:24]), [tkeys[0]], [tkeys[1]])
    _act(P, mv[:, 2:3], mv[:, 1:2], AF.Sqrt, [tkeys[1]], [tkeys[1]], bias=mv[:, 4:5])
    P.add("dve", lambda e: e.reciprocal(mv[:, 3:4], mv[:, 2:3]), [tkeys[1]], [tkeys[1]])
    _ts(P, tmp[:, :], src[:, :], mv[:, 0:1], mv[:, 3:4], ALU.subtract, ALU.mult, [skey, tkeys[1]], [tkeys[2]])
    _tt(P, tmp[:, :], tmp[:, :], g_bc[:, :], ALU.mult, [tkeys[2], gkeys[0]], [tkeys[2]], eng="pool")
    _tt(P, dst[:, :], tmp[:, :], b_bc[:, :], ALU.add, [tkeys[2], gkeys[1]], [dkey])


def _breg(eng, cache):
    if "r" not in cache:
        cache["r"] = eng.to_reg(NEXP * CAP - 1)
    return cache["r"]


def phase_C2(nc, io, sc):
    with ExitStack() as st:
        C = Ctx(nc, st, "C2")
        P = C.P
        breg = {}
        identf = C.sb("identf", [128, 128], F32)
        P.dma("sp", identf[:, :], io["identf"], [], ["identf"])
        wo = C.sb("wo", [128, 16, 2048], BF16)
        for c in range(4):
            cs = slice(c * 512, (c + 1) * 512)
            P.dma("pool", wo[:, :, cs], io["w_out"][:, cs].rearrange("(k p) n -> p k n", p=128), [], ["wo"])
        wr = C.sb("wr", [128, 16, 72], F32)
        P.dma("sp", wr[:, :, :], io["w_router"].rearrange("(k p) n -> p k n", p=128), [], ["wr"])
        br = C.sb("br", [128, 72], F32)
        P.dma("sp", br[:, :], io["b_router"], [], ["br"])
        eid64 = C.sb("eid64", [128, 64], F32)
        P.dma("sp", eid64[:, :], io["eid64"], [], ["eid64"])
        g1 = C.sb("g1", [128, 2048], F32)
        b1 = C.sb("b1", [128, 2048], F32)
        P.dma("sp", g1[:, :], io["ln1_g"], [], ["g1"])
        P.dma("sp", b1[:, :], io["ln1_b"], [], ["b1"])
        Ut = C.sb("Ut", [128, 128], BF16)
        P.dma("sp", Ut[:, :], io["utri"], [], ["Ut"])
        ones = C.sb("ones", [128, 128], BF16)
        P.add("pool", lambda e: e.memset(ones[:, :], 1.0), [], ["ones"])
        accind = C.sb("accind", [128, 64], F32)
        P.add("pool", lambda e: e.memset(accind[:, :], 0.0), [], ["accind"])
        zT = [C.sb(f"zT{i}", [128, 16, 128], BF16) for i in range(2)]
        xt = [C.sb(f"xt{i}", [128, 2048], F32) for i in range(2)]
        r = [C.sb(f"r{i}", [128, 2048], F32) for i in range(2)]
        tmp = C.sb("tmp", [128, 2048], F32)
        h1 = [C.sb(f"h1_{i}", [128, 2048], F32) for i in range(2)]
        h1b = [C.sb(f"h1b{i}", [128, 2048], BF16) for i in range(2)]
        h1T = C.sb("h1T", [128, 16, 128], F32)
        st6 = C.sb("st6", [128, 24], F32)
        mv = C.sb("mv", [128, 8], F32)
        P.add("pool", lambda e: e.memset(mv[:, 4:5], LN_EPS), [], ["mv"])
        lg = C.sb("lg", [128, 72], F32)
        rt = [C.sb(f"rt{i}", [128, 64], F32) for i in range(2)]
        e3 = C.sb("e3", [128, 8, 8], F32)
        E1 = [C.sb(f"E1_{i}", [128, 8, 8], F32) for i in range(2)]
        E2 = [C.sb(f"E2_{i}", [128, 8, 8], F32) for i in range(2)]
        indb = [C.sb(f"indb{i}", [128, 64], BF16) for i in range(2)]
        accb = [C.sb(f"accb{i}", [128, 64], BF16) for i in range(2)]
        posf = C.sb("posf", [128, 64], F32)
        ridx = [C.sb(f"ridx{i}", [128, 2], I32) for i in range(2)]
        rw = [C.sb(f"rw{i}", [128, 2], F32) for i in range(2)]
        py = [C.ps(f"py{i}", [128, 512], F32) for i in range(4)]
        pt = [C.ps(f"pt{i}", [128, 4, 128], F32) for i in range(2)]
        pl = C.ps("pl", [128, 72], F32)
        pp = C.ps("pp", [128, 64], F32)
        npt = [0]

        def stage1(tt):
            i = tt % 2
            ts_ = slice(tt * 128, (tt + 1) * 128)
            P.dma("sp", zT[i][:, :, :], sc["ZT"].rearrange("k p t -> p k t")[:, :, ts_], ["ZT"], [f"zT{i}"])
            P.dma("sp", xt[i][:, :], io["x_own"][ts_, :], [], [f"xt{i}"])
            for cc in range(4):
                cs = slice(cc * 512, (cc + 1) * 512)
                for k in range(16):
                    P.mm(py[cc][:, :], zT[i][:, k, :], wo[:, k, cs], k == 0, k == 15, [f"zT{i}", "wo"], [f"py{cc}"])

        def stage1b(tt):
            i = tt % 2
            for cc in range(4):
                cs = slice(cc * 512, (cc + 1) * 512)
                _stt(P, r[i][:, cs], xt[i][:, cs], DN_ALPHA, py[cc][:, :], ALU.mult, ALU.add, [f"xt{i}", f"py{cc}"], [f"r{i}"])

        def stage2a(tt):
            i = tt % 2
            ts_ = slice(tt * 128, (tt + 1) * 128)
            _layer_norm(P, h1[i], r[i], f"r{i}", f"h1_{i}", g1, b1, ["g1", "b1"], st6, mv, tmp, ["st6", "mv", "tmp"])
            P.dma("sp", sc["H1"][ts_, :], h1[i][:, :], [f"h1_{i}"], ["H1"])
            P.add("act", lambda e: e.copy(h1b[i][:, :], h1[i][:, :]), [f"h1_{i}"], [f"h1b{i}"])

        def stage2b(tt):
            i = tt % 2
            R = [f"rt{i}"]
            rt_ = rt[i]
            for k4 in range(4):
                j = npt[0] % 2
                npt[0] += 1
                for k in range(4):
                    kk = k4 * 4 + k
                    P.tr(pt[j][:, k, :], h1[i][:, kk * 128:(kk + 1) * 128], identf[:, :], [f"h1_{i}", "identf"], [f"pt{j}"])
                P.add("act", lambda e, j=j, k4=k4: e.copy(h1T[:, k4 * 4:(k4 + 1) * 4, :], pt[j][:, :, :]), [f"pt{j}"], ["h1T"])
            for k in range(16):
                P.mm(pl[:, :], h1T[:, k, :], wr[:, k, :], k == 0, k == 15, ["h1T", "wr"], ["pl"])
            _tt(P, lg[:, :], pl[:, :], br[:, :], ALU.add, ["pl", "br"], ["lg"])
            P.add("dve", lambda e: e.tensor_reduce(rt_[:, 0:1], lg[:, 0:8], AX.X, ALU.max), ["lg"], R)
            _ts(P, rt_[:, 8:16], lg[:, 0:8], rt_[:, 0:1], None, ALU.is_equal, None, ["lg"] + R, R)
            _ts(P, rt_[:, 1:2], rt_[:, 0:1], -1.0, None, ALU.mult, None, R, R)
            P.add("act", lambda e: e.activation(rt_[:, 16:24], lg[:, 0:8], AF.Exp, bias=rt_[:, 1:2], accum_out=rt_[:, 2:3]), ["lg"] + R, R)
            P.add("dve", lambda e: e.reciprocal(rt_[:, 3:4], rt_[:, 2:3]), R, R)
            _tt(P, e3[:, :, :], lg[:, 8:72].rearrange("p (g e) -> p g e", g=8),
                rt_[:, 8:16].unsqueeze(2).broadcast_to([128, 8, 8]), ALU.mult, ["lg"] + R, ["e3"])
            P.add("dve", lambda e: e.tensor_reduce(rt_[:, 24:32], e3[:, :, :].rearrange("p g e -> p e g"), AX.X, ALU.add), ["e3"], R)
            P.add("dve", lambda e: e.max(rt_[:, 32:40], rt_[:, 24:32]), R, R)
            _ts(P, rt_[:, 40:48], rt_[:, 24:32], rt_[:, 32:33], None, ALU.is_equal, None, R, R)
            _ts(P, rt_[:, 48:56], rt_[:, 24:32], rt_[:, 33:34], None, ALU.is_equal, None, R, R)
            _tt(P, rt_[:, 4:5], rt_[:, 32:33], rt_[:, 33:34], ALU.subtract, R, R)
            _act(P, rt_[:, 5:6], rt_[:, 4:5], AF.Sigmoid, R, R)
            _tt(P, rw[i][:, 0:1], rt_[:, 5:6], rt_[:, 3:4], ALU.mult, R, [f"rw{i}"])
            _tt(P, rw[i][:, 1:2], rt_[:, 3:4], rw[i][:, 0:1], ALU.subtract, R + [f"rw{i}"], [f"rw{i}"])
            gb = rt_[:, 8:16].unsqueeze(2).broadcast_to([128, 8, 8])
            _tt(P, E1[i][:, :, :], gb, rt_[:, 40:48].unsqueeze(1).broadcast_to([128, 8, 8]), ALU.mult, R, [f"E1_{i}"])
            _tt(P, E2[i][:, :, :], gb, rt_[:, 48:56].unsqueeze(1).broadcast_to([128, 8, 8]), ALU.mult, R, [f"E2_{i}"])
            E1f = E1[i][:, :, :].rearrange("p g e -> p (g e)")
            E2f = E2[i][:, :, :].rearrange("p g e -> p (g e)")
            _tt(P, posf[:, :], E1f, E2f, ALU.add, [f"E1_{i}", f"E2_{i}"], ["posf"])
            P.add("dve", lambda e: e.tensor_copy(indb[i][:, :], posf[:, :]), ["posf"], [f"indb{i}"])
            P.add("dve", lambda e: e.tensor_copy(accb[i][:, :], accind[:, :]), ["accind"], [f"accb{i}"])
            _tt(P, accind[:, :], accind[:, :], posf[:, :], ALU.add, ["accind", "posf"], ["accind"])

        def stage3(tt):
            i = tt % 2
            ts_ = slice(tt * 128, (tt + 1) * 128)
            R = [f"rt{i}"]
            rt_ = rt[i]
            P.mm(pp[:, :], Ut[:, :], indb[i][:, :], True, False, ["Ut", f"indb{i}"], ["pp"])
            P.mm(pp[:, :], ones[:, :], accb[i][:, :], False, True, ["ones", f"accb{i}"], ["pp"])
            P.add("dve", lambda e: e.tensor_copy(posf[:, :], pp[:, :]), ["pp"], ["posf"])
            e3f = e3[:, :, :].rearrange("p g e -> p (g e)")
            for kk, (Eb, ek) in enumerate(((E1[i], f"E1_{i}"), (E2[i], f"E2_{i}"))):
                Ef = Eb[:, :, :].rearrange("p g e -> p (g e)")
                o0 = 56 + kk * 4
                _tt(P, e3f, Ef, posf[:, :], ALU.mult, [ek, "posf"], ["e3"])
                P.add("dve", lambda e, o0=o0: e.tensor_reduce(rt_[:, o0:o0 + 1], e3f, AX.X, ALU.add), ["e3"], R)
                _tt(P, e3f, Ef, eid64[:, :], ALU.mult, [ek, "eid64"], ["e3"])
                P.add("dve", lambda e, o0=o0: e.tensor_reduce(rt_[:, o0 + 1:o0 + 2], e3f, AX.X, ALU.add), ["e3"], R)
                _ts(P, rt_[:, o0 + 2:o0 + 3], rt_[:, o0:o0 + 1], float(CAP), 1.0e6, ALU.is_ge, ALU.mult, R, R)
                _stt(P, rt_[:, o0 + 3:o0 + 4], rt_[:, o0 + 1:o0 + 2], float(CAP), rt_[:, o0:o0 + 1], ALU.mult, ALU.add, R, R)
                _tt(P, rt_[:, o0 + 3:o0 + 4], rt_[:, o0 + 3:o0 + 4], rt_[:, o0 + 2:o0 + 3], ALU.add, R, R)
                P.add("dve", lambda e, kk=kk, o0=o0: e.tensor_copy(ridx[i][:, kk:kk + 1], rt_[:, o0 + 3:o0 + 4]), R, [f"ridx{i}"])
            P.dma("sp", sc["Ridx"][ts_, :], ridx[i][:, :], [f"ridx{i}"], ["Ridx"])
            P.dma("sp", sc["Rw"][ts_, :], rw[i][:, :], [f"rw{i}"], ["Rw"])
            for kk in range(2):
                P.add("pool", lambda e, kk=kk: e.indirect_dma_start(
                    out=sc["Xg"][:, :], out_offset=bass.IndirectOffsetOnAxis(ap=ridx[i][:, kk:kk + 1], axis=0),
                    in_=h1b[i][:, :], in_offset=None, bounds_check=_breg(e, breg), oob_is_err=False),
                    [f"h1b{i}", f"ridx{i}"], ["Xg_s"], dma=True)

        stage1(0)
        stage1b(0)
        for tt in range(16):
            if tt + 1 < 16:
                stage1(tt + 1)
            stage2a(tt)
            if tt + 1 < 16:
                stage1b(tt + 1)
            stage2b(tt)
            if tt >= 1:
                stage3(tt - 1)
        stage3(15)
        P.emit(st)


def phase_D(nc, io, sc):
    with ExitStack() as st:
        C = Ctx(nc, st, "D")
        P = C.P
        ident = C.sb("ident", [128, 128], BF16)
        P.dma("sp", ident[:, :], io["ident"], [], ["ident"])
        wg = [C.sb(f"wg{i}", [128, 16, 512], BF16) for i in range(2)]
        wu = [C.sb(f"wu{i}", [128, 16, 512], BF16) for i in range(2)]
        wd = [C.sb(f"wd{i}", [128, 4, 2048], BF16) for i in range(2)]
        xe = [C.sb(f"xe{i}", [128, 2048], BF16) for i in range(2)]
        xT = [C.sb(f"xT{i}", [128, 16, 128], BF16) for i in range(2)]
        sg = C.sb("sg", [128, 512], F32)
        hm = C.sb("hm", [128, 512], BF16)
        hT = C.sb("hT", [128, 4, 128], BF16)
        ye = [C.sb(f"ye{i}", [128, 2048], F32) for i in range(2)]
        pg = C.ps("pg", [128, 512], F32)
        pu = C.ps("pu", [128, 512], F32)
        py = [C.ps(f"py{i}", [128, 512], F32) for i in range(2)]
        pT = [C.ps(f"pT{i}", [128, 8, 128], BF16) for i in range(2)]
        n = 0
        for e_ in (range(NEXP) if EXPERTS is None else EXPERTS):
            i = e_ % 2
            P.dma("pool", wg[i][:, :, :], io["w_gate"][e_].rearrange("(k p) n -> p k n", p=128), [], [f"wg{i}"])
            P.dma("pool", wu[i][:, :, :], io["w_up"][e_].rearrange("(k p) n -> p k n", p=128), [], [f"wu{i}"])
            for c in range(4):
                P.dma("pool", wd[i][:, :, c * 512:(c + 1) * 512],
                      io["w_down"][e_][:, c * 512:(c + 1) * 512].rearrange("(k p) n -> p k n", p=128), [], [f"wd{i}"])
            P.dma("sp", xe[i][:, :], sc["Xg"][e_ * 128:(e_ + 1) * 128, :], ["Xg"], [f"xe{i}"])
            for hh in range(2):
                for k in range(8):
                    kk = hh * 8 + k
                    P.tr(pT[hh][:, k, :], xe[i][:, kk * 128:(kk + 1) * 128], ident[:, :], [f"xe{i}", "ident"], [f"pT{hh}"])
                if hh == 0:
                    P.add("act", lambda e, i=i: e.copy(xT[i][:, 0:8, :], pT[0][:, :, :]), ["pT0"], [f"xT{i}"])
                else:
                    P.add("dve", lambda e, i=i: e.tensor_copy(xT[i][:, 8:16, :], pT[1][:, :, :]), ["pT1"], [f"xT{i}"])
            for k in range(16):
                P.mm(pg[:, :], xT[i][:, k, :], wg[i][:, k, :], k == 0, k == 15, [f"xT{i}", f"wg{i}"], ["pg"])
            for k in range(16):
                P.mm(pu[:, :], xT[i][:, k, :], wu[i][:, k, :], k == 0, k == 15, [f"xT{i}", f"wu{i}"], ["pu"])
            _act(P, sg[:, :], pg[:, :], AF.Silu, ["pg"], ["sg"])
            _tt(P, hm[:, :], sg[:, :], pu[:, :], ALU.mult, ["sg", "pu"], ["hm"])
            for k in range(4):
                P.tr(pT[0][:, k, :], hm[:, k * 128:(k + 1) * 128], ident[:, :], ["hm", "ident"], ["pT0"])
            P.add("act", lambda e: e.copy(hT[:, :, :], pT[0][:, 0:4, :]), ["pT0"], ["hT"])
            for cc in range(4):
                j = n % 2
                n += 1
                cs = slice(cc * 512, (cc + 1) * 512)
                for k in range(4):
                    P.mm(py[j][:, :], hT[:, k, :], wd[i][:, k, cs], k == 0, k == 3, ["hT", f"wd{i}"], [f"py{j}"])
                if cc % 2 == 0:
                    P.add("act", lambda e, i=i, j=j, cs=cs: e.copy(ye[i][:, cs], py[j][:, :]), [f"py{j}"], [f"ye{i}"])
                else:
                    P.add("dve", lambda e, i=i, j=j, cs=cs: e.tensor_copy(ye[i][:, cs], py[j][:, :]), [f"py{j}"], [f"ye{i}"])
            P.dma("sp", sc["Yg"][e_ * 128:(e_ + 1) * 128, :], ye[i][:, :], [f"ye{i}"], ["Yg"])
        P.emit(st)


def phase_E(nc, io, sc):
    with ExitStack() as st:
        C = Ctx(nc, st, "E")
        P = C.P
        breg = {}
        g2 = C.sb("g2", [128, 2048], F32)
        b2 = C.sb("b2", [128, 2048], F32)
        P.dma("sp", g2[:, :], io["ln2_g"], [], ["g2"])
        P.dma("sp", b2[:, :], io["ln2_b"], [], ["b2"])
        y1 = [C.sb(f"y1_{i}", [128, 2048], F32) for i in range(2)]
        y2 = [C.sb(f"y2_{i}", [128, 2048], F32) for i in range(2)]
        h1 = [C.sb(f"h1_{i}", [128, 2048], F32) for i in range(2)]
        ridx = [C.sb(f"ridx{i}", [128, 2], I32) for i in range(2)]
        rw = [C.sb(f"rw{i}", [128, 2], F32) for i in range(2)]
        st6 = [C.sb(f"st6_{i}", [128, 24], F32) for i in range(2)]
        mv = [C.sb(f"mv{i}", [128, 8], F32) for i in range(2)]
        for i in range(2):
            P.add("pool", lambda e, i=i: e.memset(mv[i][:, 4:5], LN_EPS), [], [f"mv{i}"])

        def part1(tt):
            i = tt % 2
            ts_ = slice(tt * 128, (tt + 1) * 128)
            P.dma("sp", ridx[i][:, :], sc["Ridx"][ts_, :], ["Ridx"], [f"ridx{i}"])
            P.dma("sp", rw[i][:, :], sc["Rw"][ts_, :], ["Rw"], [f"rw{i}"])
            P.dma("sp", h1[i][:, :], sc["H1"][ts_, :], ["H1"], [f"h1_{i}"])
            P.add("pool", lambda e: e.memset(y1[i][:, :], 0.0), [], [f"y1_{i}"])
            P.add("pool", lambda e: e.memset(y2[i][:, :], 0.0), [], [f"y2_{i}"])
            for kk, yb, yk in ((0, y1[i], f"y1_{i}"), (1, y2[i], f"y2_{i}")):
                P.add("pool", lambda e, kk=kk, yb=yb: e.indirect_dma_start(
                    out=yb[:, :], out_offset=None, in_=sc["Yg"][:, :],
                    in_offset=bass.IndirectOffsetOnAxis(ap=ridx[i][:, kk:kk + 1], axis=0),
                    bounds_check=_breg(e, breg), oob_is_err=False), [f"ridx{i}", "Yg", yk], [yk + "g"], dma=True)
            y1k = [f"y1_{i}", f"y1_{i}g"]
            y2k = [f"y2_{i}", f"y2_{i}g"]
            _ts(P, y1[i][:, :], y1[i][:, :], rw[i][:, 0:1], None, ALU.mult, None, y1k + [f"rw{i}"], [f"y1_{i}"])
            _stt(P, y1[i][:, :], y2[i][:, :], rw[i][:, 1:2], y1[i][:, :], ALU.mult, ALU.add, y1k + y2k + [f"rw{i}"], [f"y1_{i}"])
            _stt(P, h1[i][:, :], h1[i][:, :], DN_ALPHA, y1[i][:, :], ALU.mult, ALU.add, [f"h1_{i}"] + y1k, [f"h1_{i}"])
            for c in range(4):
                P.add("dve", lambda e, c=c: e.bn_stats(st6[i][:, c * 6:(c + 1) * 6], h1[i][:, c * 512:(c + 1) * 512]),
                      [f"h1_{i}"], [f"st6_{i}"])
            P.add("dve", lambda e: e.bn_aggr(mv[i][:, 0:2], st6[i][:, 0:24]), [f"st6_{i}"], [f"mv{i}"])
            _act(P, mv[i][:, 2:3], mv[i][:, 1:2], AF.Sqrt, [f"mv{i}"], [f"mv{i}"], bias=mv[i][:, 4:5])
            P.add("dve", lambda e: e.reciprocal(mv[i][:, 3:4], mv[i][:, 2:3]), [f"mv{i}"], [f"mv{i}"])

        def part2(tt):
            i = tt % 2
            ts_ = slice(tt * 128, (tt + 1) * 128)
            _ts(P, y2[i][:, :], h1[i][:, :], mv[i][:, 0:1], mv[i][:, 3:4], ALU.subtract, ALU.mult,
                [f"h1_{i}", f"mv{i}", f"y2_{i}g"], [f"y2_{i}"])
            _tt(P, y2[i][:, :], y2[i][:, :], g2[:, :], ALU.mult, [f"y2_{i}", "g2"], [f"y2_{i}"], eng="pool")
            _tt(P, y1[i][:, :], y2[i][:, :], b2[:, :], ALU.add, [f"y2_{i}", "b2", f"y1_{i}g"], [f"y1_{i}"])
            P.dma("sp", sc["out"][ts_, :], y1[i][:, :], [f"y1_{i}"], ["out"])

        part1(0)
        for tt in range(16):
            if tt + 1 < 16:
                part1(tt + 1)
            part2(tt)
        P.emit(st)


EXPERTS = None
_IN_C = {
    "identf": ([128, 128], F32), "w_nsa_proj": ([1024, 2048], F32), "w_pool_proj": ([1024, 2048], F32),
    "w_out": ([2048, 2048], F32), "w_router": ([2048, 72], F32), "b_router": ([128, 72], F32),
    "eid64": ([128, 64], F32), "utri": ([128, 128], BF16), "ln1_g": ([128, 2048], F32), "ln1_b": ([128, 2048], F32),
    "ln2_g": ([128, 2048], F32), "ln2_b": ([128, 2048], F32), "x_own": ([NT, 2048], F32),
    "w_gate": ([NEXP, 2048, 512], F32), "w_up": ([NEXP, 2048, 512], F32), "w_down": ([NEXP, 512, 2048], F32),
}
_SC_C = {
    "ZT": ([16, 128, NT], BF16), "H1": ([NT, 2048], F32), "Xg": ([NEXP * CAP, 2048], BF16),
    "Yg": ([NEXP * CAP, 2048], F32), "Ridx": ([NT, 2], I32), "Rw": ([NT, 2], F32),
}


def _shared_inputs(inp):
    w_in = inp["w_in"][0]
    ca = np.ascontiguousarray
    m = {
        "ident": _bf16(np.eye(128)), "identf": np.eye(128, dtype=np.float32),
        "w_A1": ca(np.concatenate([w_in[:, 2560:3072], w_in[:, 2048:2560]], 1)),
        "w_A2": ca(w_in[:, 3072:3584]), "w_q": ca(w_in[:, 1024:2048]), "w_gn": ca(w_in[:, 3584:3632]),
        "w_pool": ca(w_in[:, 0:1024]), "w_gm": ca(w_in[:, 3632:7728]),
        "pool_mix": ca(inp["pool_mix"][0]), "pool_scale": ca(inp["pool_scale"][0].reshape(8, 128).T),
        "cmp_k_w1": ca(inp["cmp_k_w1"][0]), "cmp_v_w1": ca(inp["cmp_v_w1"][0]),
        "cmp_k_w2": ca(inp["cmp_k_w2"][0]), "cmp_v_w2": ca(inp["cmp_v_w2"][0]),
        "cmp_k_w2s": ca(np.concatenate([inp["cmp_k_w2"][0][:, 32:], inp["cmp_k_w2"][0][:, :32]], 1)),
        "cmp_pos_kT": ca(inp["cmp_pos_k"][0].T), "cmp_pos_vT": ca(inp["cmp_pos_v"][0].T),
        "w_nsa_proj": ca(inp["w_nsa_proj"][0]), "w_pool_proj": ca(inp["w_pool_proj"][0]), "w_out": ca(inp["w_out"][0]),
        "w_router": ca(np.concatenate([inp["router_group_w"][0],
                                       inp["router_expert_w"][0].transpose(1, 0, 2).reshape(D, 64)], 1)),
        "b_router": ca(np.broadcast_to(np.concatenate([inp["router_group_b"][0], inp["router_expert_b"][0].reshape(64)])[None, :], (128, 72))),
        "eid64": ca(np.broadcast_to(np.arange(64, dtype=np.float32)[None, :], (128, 64))),
        "utri": _bf16((np.arange(128)[:, None] < np.arange(128)[None, :]).astype(np.float32)),
        "w_gate": ca(inp["w_gate"][0]), "w_up": ca(inp["w_up"][0]), "w_down": ca(inp["w_down"][0]),
    }
    for n in ("ln1_g", "ln1_b", "ln2_g", "ln2_b"):
        m[n] = ca(np.broadcast_to(inp[n][0][None, :], (128, D)))
    return m


def _core_inputs(inp, shared, core, tabs):
    b, p = core // 2, core % 2
    t = tabs[p]
    x0 = inp["x"][b]
    xo = x0[t["own_pos"]]
    xp = x0[t["prev_pos"]] * t["prev_valid"][:, None]
    m = dict(shared)
    m["xTg"] = np.ascontiguousarray(x0.T)
    m["xTo"] = np.ascontiguousarray(xo.T)
    m["xTp"] = np.ascontiguousarray(xp.T)
    m["x_own"] = np.ascontiguousarray(xo)
    for k in ALL_IN:
        if k not in m:
            m[k] = t[k]
    return m


ALL_IN = {}
ALL_IN.update(_IN_A)
ALL_IN.update(_IN_B)
ALL_IN.update(_IN_C)
ALL_SC = {}
ALL_SC.update(_SC_A)
ALL_SC.update(_SC_C)
PHASES = [phase_A, phase_B, phase_C1, phase_C2, phase_D, phase_E]


def build_full():
    return build_nc(ALL_IN, ALL_SC, PHASES, final_out=("out", [NT, D], F32))


def kernel(**inputs):
    inp = {k: np.asarray(v) for k, v in inputs.items()}
    tabs = []
    for p in range(2):
        t = _core_tables(p)
        tabs.append(_core_tables_B(p, t))
    shared = _shared_inputs(inp)
    in_maps = [_core_inputs(inp, shared, c, tabs) for c in range(8)]
    nc = build_full()
    res = run_bass_kernel_spmd(nc, in_maps, core_ids=list(range(8)))
    out = np.zeros((4, S, D), np.float32)
    for c in range(8):
        b, p = c // 2, c % 2
        out[b, tabs[p]["own_pos"]] = np.asarray(res.results[c]["out"]).astype(np.float32)
    return out
```

```python
import numpy as np
import concourse.bass as bass
import concourse.mybir as mybir
from concourse.bass_utils import run_bass_kernel_spmd
from contextlib import ExitStack

F32 = mybir.dt.float32
BF16 = mybir.dt.bfloat16
I32 = mybir.dt.int32
U32 = mybir.dt.uint32
AF = mybir.ActivationFunctionType
ALU = mybir.AluOpType
AX = mybir.AxisListType

D = 2048
S = 4096
NT = 2048
HD = 64
NH = 16
NG = 4
NCMP = 255
NEG = -30000.0
OWN = ([0, 3, 4, 7], [1, 2, 5, 6])
DN_ALPHA = 2.0 ** 0.25
LN_EPS = 1e-5
NEXP = 64
CAP = 128

DEBUG = []
STOP_AFTER = None
GROUPS = None
PARTS = None


def _on(name):
    return PARTS is None or name in PARTS


class _Op:
    __slots__ = ("eng", "fn", "dma", "deps", "idx", "marked", "count", "sem", "semval", "gid")


class Phase:
    ENGS = ("pe", "act", "dve", "pool", "sp")
    NDMA = 6

    def __init__(self, nc, name):
        self.nc = nc
        self.name = name
        self.ops = {e: [] for e in self.ENGS}
        self.bufs = {}
        self.excl = set()
        self.nops = 0

    def _buf(self, k):
        b = self.bufs.get(k)
        if b is None:
            b = [[], []]
            self.bufs[k] = b
        return b

    def add(self, eng, fn, reads=(), writes=(), dma=False):
        op = _Op()
        op.eng, op.fn, op.dma = eng, fn, dma
        op.idx = len(self.ops[eng])
        op.marked = False
        op.gid = self.nops
        self.nops += 1
        deps = set()
        for k in reads:
            b = self._buf(k)
            deps.update(b[0])
            if k in self.excl:
                for r in b[1]:
                    if r.eng != eng:
                        deps.add(r)
            b[1].append(op)
        for k in writes:
            b = self._buf(k)
            if dma and b[0] and not b[1] and all(w.dma for w in b[0]):
                b[0].append(op)
            else:
                deps.update(b[0])
                deps.update(b[1])
                b[1] = []
                b[0] = [op]
        deps.discard(op)
        op.deps = deps
        self.ops[eng].append(op)
        return op

    def mm(self, out, lhsT, rhs, start, stop, reads, writes, skip=False):
        if skip:
            return self.add("pe", lambda e: e.matmul(out, lhsT, rhs, start=start, stop=stop, skip_group_check=True), reads, writes)
        return self.add("pe", lambda e: e.matmul(out, lhsT, rhs, start=start, stop=stop), reads, writes)

    def tr(self, out, in_, ident, reads, writes):
        return self.add("pe", lambda e: e.transpose(out, in_, ident), reads, writes)

    def dma(self, q, out, in_, reads, writes):
        return self.add(q, lambda e: e.dma_start(out=out, in_=in_), reads, writes, dma=True)

    def emit(self, stack):
        nc = self.nc
        engs = {"pe": nc.tensor, "act": nc.scalar, "dve": nc.vector, "pool": nc.gpsimd, "sp": nc.sync}
        for e in self.ENGS:
            for op in self.ops[e]:
                for d in op.deps:
                    if d.dma:
                        continue
                    if d.eng == "pe" and op.eng == "pe" and not op.dma:
                        continue
                    d.marked = True
        esem = {e: nc.alloc_semaphore(name=f"{self.name}_{e}") for e in self.ENGS}
        dsem = {}
        for e in self.ENGS:
            cnt = 0
            nd = 0
            for op in self.ops[e]:
                if op.dma:
                    if e not in dsem:
                        dsem[e] = [nc.alloc_semaphore(name=f"{self.name}_{e}_d{i}") for i in range(self.NDMA)]
                    op.sem = dsem[e][nd % self.NDMA]
                    op.semval = 16 * (nd // self.NDMA + 1)
                    nd += 1
                elif op.marked:
                    cnt += 1
                    op.count = cnt
            assert cnt < 60000, (self.name, e, cnt)
        block = stack.enter_context(nc.Block())

        def body(ename):
            def _(eng):
                seen = {}

                def wait(sem, val):
                    k = id(sem)
                    if seen.get(k, 0) >= val:
                        return
                    seen[k] = val
                    eng.wait_ge(sem, val)

                for op in self.ops[ename]:
                    for d in sorted(op.deps, key=lambda o: o.gid):
                        if d.dma:
                            wait(d.sem, d.semval)
                        elif d.eng == "pe" and ename == "pe" and not op.dma:
                            continue
                        else:
                            wait(esem[d.eng], d.count)
                    if op.dma:
                        if op.semval > 16:
                            wait(op.sem, op.semval - 16)
                        op.fn(eng).then_inc(op.sem, 16)
                    else:
                        ins = op.fn(eng)
                        if op.marked:
                            ins.then_inc(esem[ename], 1)
                last = {}
                for op in self.ops[ename]:
                    if op.dma:
                        last[id(op.sem)] = (op.sem, op.semval)
                for sem, val in last.values():
                    wait(sem, val)
            return _

        for ename, reg in (("pe", block.tensor), ("act", block.scalar), ("dve", block.vector),
                           ("pool", block.gpsimd), ("sp", block.sync)):
            if self.ops[ename]:
                reg(body(ename))


class Ctx:
    def __init__(self, nc, stack, name):
        self.nc, self.stack, self.name = nc, stack, name
        self.P = Phase(nc, name)

    def sb(self, name, shape, dt):
        return self.stack.enter_context(self.nc.sbuf_tensor(f"{self.name}_{name}", list(shape), dt))

    def ps(self, name, shape, dt):
        self.P.excl.add(name)
        return self.stack.enter_context(self.nc.psum_tensor(f"{self.name}_{name}", list(shape), dt))


def _dram(nc, name, shape, dt, kind):
    return nc.dram_tensor(name, list(shape), dt, kind=kind).ap()


def _scratch(nc, name, shape, dt):
    kind = "ExternalOutput" if name in DEBUG else "Internal"
    return _dram(nc, name, shape, dt, kind)


def _rope(P, dst, src, cos, sin, nh, tmp, rkeys, wkeys, tag):
    s3 = src.rearrange("p (h d) -> p h d", h=nh)
    d3 = dst.rearrange("p (h d) -> p h d", h=nh)
    cb = cos.unsqueeze(1).broadcast_to([128, nh, 32])
    sn = sin.unsqueeze(1).broadcast_to([128, nh, 32])
    t1 = tmp[:, 0, 0:nh, :]
    t2 = tmp[:, 1, 0:nh, :]
    q1, q2 = s3[:, :, 0:32], s3[:, :, 32:64]
    tk = [tag + "_t1", tag + "_t2"]
    P.add("dve", lambda e: e.tensor_tensor(t1, q1, cb, ALU.mult), rkeys, [tk[0]])
    P.add("dve", lambda e: e.tensor_tensor(t2, q2, sn, ALU.mult), rkeys, [tk[1]])
    P.add("dve", lambda e: e.tensor_tensor(d3[:, :, 0:32], t1, t2, ALU.subtract), tk, wkeys)
    P.add("dve", lambda e: e.tensor_tensor(t1, q2, cb, ALU.mult), rkeys, [tk[0]])
    P.add("dve", lambda e: e.tensor_tensor(t2, q1, sn, ALU.mult), rkeys, [tk[1]])
    P.add("dve", lambda e: e.tensor_tensor(d3[:, :, 32:64], t1, t2, ALU.add), tk, wkeys)


def phase_A(nc, io, sc):
    with ExitStack() as st:
        C = Ctx(nc, st, "A")
        P = C.P
        ident = C.sb("ident", [128, 128], BF16)
        P.dma("sp", ident[:, :], io["ident"], [], ["ident"])
        wk = [C.sb(f"w{i}", [128, 16, 512], BF16) for i in range(2)]
        xt = [C.sb(f"xt{i}", [128, 16, 512], BF16) for i in range(2)]
        xo = C.sb("xo", [128, 16, NT], BF16)
        xh = C.sb("xh", [128, 16, 64], BF16)
        cosg = C.sb("cosg", [128, 32, 32], F32)
        sing = C.sb("sing", [128, 32, 32], F32)
        cosp = C.sb("cosp", [128, 16, 32], F32)
        sinp = C.sb("sinp", [128, 16, 32], F32)
        coso = C.sb("coso", [128, 16, 32], F32)
        sino = C.sb("sino", [128, 16, 32], F32)
        cosq = C.sb("cosq", [128, 16, 32], F32)
        sinq = C.sb("sinq", [128, 16, 32], F32)
        for t, n in ((cosg, "cosg"), (sing, "sing"), (cosp, "cosp"), (sinp, "sinp"), (coso, "coso"),
                     (sino, "sino"), (cosq, "cosq"), (sinq, "sinq")):
            P.dma("sp", t[:, :, :], io[n], [], [n])
        rtmp = C.sb("rtmp", [128, 2, 8, 32], F32)
        ksr = [C.sb(f"ksr{i}", [128, 512], BF16) for i in range(2)]
        kcv = [C.sb(f"kcv{i}", [128, 512], BF16) for i in range(2)]
        stT = [C.sb(f"stT{i}", [128, 6, 512], BF16) for i in range(2)]
        vst = [C.sb(f"vst{i}", [128, 4, 256], BF16) for i in range(2)]
        pm = [C.ps(f"pm{i}", [128, 512], F32) for i in range(4)]
        pT = [C.ps(f"pT{i}", [128, 8, 128], BF16) for i in range(2)]

        wq = ["pool", "sp"]
        nw = [0]
        nx = [0]
        npm = [0]
        npt = [0]

        def load_w(src_ap, ncols=512):
            i = nw[0] % 2
            nw[0] += 1
            P.dma("pool", wk[i][:, :, 0:ncols], src_ap.rearrange("(k p) n -> p k n", p=128), [], [f"w{i}"])
            return i

        def load_x(src_ap):
            i = nx[0] % 2
            nx[0] += 1
            P.dma("pool", xt[i][:, :, :], src_ap.rearrange("(k p) n -> p k n", p=128), [], [f"xt{i}"])
            return i

        def proj(xtile_ap, xkey, wi, ncols=512, c0=0):
            b = npm[0] % 4
            npm[0] += 1
            for k in range(16):
                P.mm(pm[b][:, 0:ncols], xtile_ap(k), wk[wi][:, k, c0:c0 + ncols], k == 0, k == 15,
                     [xkey, f"w{wi}"], [f"pm{b}"])
            return b

        def transposes(srcs, dst, dstkey, tcol):
            b = npt[0] % 2
            npt[0] += 1
            n = len(srcs)
            for i, (ap, key) in enumerate(srcs):
                P.tr(pT[b][:, i, :], ap, ident[:, :], [key, "ident"], [f"pT{b}"])
            P.add("act", lambda e: e.copy(dst[:, 0:n, tcol * 128:(tcol + 1) * 128], pT[b][:, 0:n, :]),
                  [f"pT{b}"], [dstkey])

        pending = []

        def flush():
            for f in pending:
                f()
            del pending[:]

        w_ks = load_w(io["w_A1"][:, 0:512])
        w_kc = load_w(io["w_A1"][:, 512:1024])
        for ch in (range(8) if _on("A1") else []):
            xi = load_x(io["xTg"][:, ch * 512:(ch + 1) * 512])
            si = ch % 2
            for j in range(4):
                tt = ch * 4 + j
                xa = (lambda xi, j: (lambda k: xt[xi][:, k, j * 128:(j + 1) * 128]))(xi, j)
                b0 = proj(xa, f"xt{xi}", w_ks)
                b1 = proj(xa, f"xt{xi}", w_kc)
                flush()
                r = tt % 2
                _rope(P, ksr[r][:, 0:256], pm[b0][:, 0:256], cosg[:, tt, :], sing[:, tt, :], 4, rtmp,
                      [f"pm{b0}", "cosg", "sing"], [f"ksr{r}"], "rA")
                P.add("act", lambda e, si=si, j=j, b0=b0: e.copy(vst[si][:, j, :], pm[b0][:, 256:512]),
                      [f"pm{b0}"], [f"vst{si}"])
                P.add("act", lambda e, r=r, b1=b1: e.copy(kcv[r][:, :], pm[b1][:, :]), [f"pm{b1}"], [f"kcv{r}"])
                srcs = [(ksr[r][:, 0:128], f"ksr{r}"), (ksr[r][:, 128:256], f"ksr{r}")]
                srcs += [(kcv[r][:, i * 128:(i + 1) * 128], f"kcv{r}") for i in range(4)]
                pending.append(lambda srcs=srcs, si=si, j=j: transposes(srcs, stT[si], f"stT{si}", j))
            pending.append(lambda si=si, ch=ch: P.dma(
                "sp", sc["KT_A1"].rearrange("i p t -> p i t")[:, :, ch * 512:(ch + 1) * 512], stT[si][:, :, :],
                [f"stT{si}"], ["KT_A1"]))
            pending.append(lambda si=si, ch=ch: P.dma(
                "sp", sc["Vs"].rearrange("(c j p) n -> c p j n", j=4, p=128)[ch], vst[si][:, :, :],
                [f"vst{si}"], ["Vs"]))
        flush()

        w_kw = load_w(io["w_A2"])
        def load_own():
            for ch in range(4):
                P.dma("pool", xo[:, :, ch * 512:(ch + 1) * 512],
                      io["xTo"][:, ch * 512:(ch + 1) * 512].rearrange("(k p) n -> p k n", p=128), [], ["xo"])
            for lc in range(4):
                P.dma("pool", xh[:, :, lc * 16:(lc + 1) * 16],
                      io["xTp"][:, lc * 512 + 496:lc * 512 + 512].rearrange("(k p) t -> p k t", p=128), [], ["xh"])

        for which in (("prev", "own") if _on("A2") else ()):
            cosw, sinw = (cosp, sinp) if which == "prev" else (coso, sino)
            cn, sn_ = ("cosp", "sinp") if which == "prev" else ("coso", "sino")
            for ch in range(4):
                si = ch % 2
                if which == "prev":
                    xi = load_x(io["xTp"][:, ch * 512:(ch + 1) * 512])
                    if ch == 0:
                        load_own()
                for j in range(4):
                    tt = ch * 4 + j
                    if which == "prev":
                        xa = (lambda xi, j: (lambda k: xt[xi][:, k, j * 128:(j + 1) * 128]))(xi, j)
                        xkey = f"xt{xi}"
                    else:
                        xa = (lambda tt: (lambda k: xo[:, k, tt * 128:(tt + 1) * 128]))(tt)
                        xkey = "xo"
                    b0 = proj(xa, xkey, w_kw)
                    flush()
                    r = tt % 2
                    _rope(P, ksr[r][:, 0:256], pm[b0][:, 0:256], cosw[:, tt, :], sinw[:, tt, :], 4, rtmp,
                          [f"pm{b0}", cn, sn_], [f"ksr{r}"], "rA")
                    P.add("act", lambda e, si=si, j=j, b0=b0: e.copy(vst[si][:, j, :], pm[b0][:, 256:512]),
                          [f"pm{b0}"], [f"vst{si}"])
                    srcs = [(ksr[r][:, 0:128], f"ksr{r}"), (ksr[r][:, 128:256], f"ksr{r}")]
                    pending.append(lambda srcs=srcs, si=si, j=j: transposes(srcs, stT[si], f"stT{si}", j))
                pending.append(lambda si=si, ch=ch, which=which: P.dma(
                    "sp", sc["KwT_" + which].rearrange("i p t -> p i t")[:, :, ch * 512:(ch + 1) * 512],
                    stT[si][:, 0:2, :], [f"stT{si}"], ["KwT_" + which]))
                pending.append(lambda si=si, ch=ch, which=which: P.dma(
                    "sp", sc["Vw_" + which].rearrange("(c j p) n -> c p j n", j=4, p=128)[ch], vst[si][:, :, :],
                    [f"vst{si}"], ["Vw_" + which]))
        flush()

        for qc in (range(2) if _on("Q") else []):
            wi = load_w(io["w_q"][:, qc * 512:(qc + 1) * 512])
            for ch in range(4):
                si = ch % 2
                for j in range(4):
                    tt = ch * 4 + j
                    xa = (lambda tt: (lambda k: xo[:, k, tt * 128:(tt + 1) * 128]))(tt)
                    b0 = proj(xa, "xo", wi)
                    flush()
                    r = tt % 2
                    _rope(P, ksr[r][:, :], pm[b0][:, :], cosq[:, tt, :], sinq[:, tt, :], 8, rtmp,
                          [f"pm{b0}", "cosq", "sinq"], [f"ksr{r}"], "rA")
                    srcs = [(ksr[r][:, i * 128:(i + 1) * 128], f"ksr{r}") for i in range(4)]
                    pending.append(lambda srcs=srcs, si=si, j=j: transposes(srcs, stT[si], f"stT{si}", j))
                pending.append(lambda si=si, ch=ch, qc=qc: P.dma(
                    "sp", sc["QT"][qc * 4:(qc + 1) * 4].rearrange("i p t -> p i t")[:, :, ch * 512:(ch + 1) * 512],
                    stT[si][:, 0:4, :], [f"stT{si}"], ["QT"]))
        flush()

        gst = C.sb("gst", [128, 16, 48], F32)
        wi = load_w(io["w_gn"], 48)
        for tt in (range(16) if _on("GN") else []):
            xa = (lambda tt: (lambda k: xo[:, k, tt * 128:(tt + 1) * 128]))(tt)
            b0 = proj(xa, "xo", wi, 48)
            P.add("act", lambda e, tt=tt, b0=b0: e.activation(gst[:, tt, :], pm[b0][:, 0:48], AF.Sigmoid),
                  [f"pm{b0}"], ["gst"])
        P.dma("sp", sc["Gn"].rearrange("(t p) n -> p t n", p=128), gst[:, :, :], ["gst"], ["Gn"])

        gms = [C.sb(f"gms{i}", [128, 512], BF16) for i in range(2)]
        ng = 0
        for gc in (range(8) if _on("GM") else []):
            wi = load_w(io["w_gm"][:, gc * 512:(gc + 1) * 512])
            for tt in range(16):
                xa = (lambda tt: (lambda k: xo[:, k, tt * 128:(tt + 1) * 128]))(tt)
                b0 = proj(xa, "xo", wi)
                gi = ng % 2
                ng += 1
                P.add("act", lambda e, gi=gi, b0=b0: e.activation(gms[gi][:, :], pm[b0][:, :], AF.Sigmoid),
                      [f"pm{b0}"], [f"gms{gi}"])
                P.dma("sp", sc["Gm"][tt * 128:(tt + 1) * 128, gc * 512:(gc + 1) * 512], gms[gi][:, :],
                      [f"gms{gi}"], ["Gm"])

        pmix = C.sb("pmix", [128, 4, 2, 256], BF16)
        P.dma("pool", pmix[:, :, :, :], io["pool_mix"].rearrange("g (c p) d -> p g c d", p=128), [], ["pmix"])
        pscale = C.sb("pscale", [128, 8], F32)
        P.dma("sp", pscale[:, :], io["pool_scale"], [], ["pscale"])
        rc16 = C.sb("rc16", [128, 4, 4, 16], F32)
        P.dma("sp", rc16[:, :, :, :], io["rc16"], [], ["rc16"])
        U = [C.sb(f"U{i}", [128, 528], F32) for i in range(2)]
        Wa = C.sb("Wa", [128, 528], F32)
        Wb = C.sb("Wb", [128, 528], F32)
        pld = [C.sb(f"pld{i}", [128, 2, 512], BF16) for i in range(2)]
        mxs = [C.sb(f"mxs{i}", [128, 512], BF16) for i in range(2)]
        phalo = C.ps("phalo", [128, 16], F32)
        nu = 0
        nmx = 0
        for g in (range(4) if _on("POOL") else []):
            win = (2, 4, 8, 16)[g]
            wis = [load_w(io["w_pool"][:, (2 * g + c2) * 128:(2 * g + c2 + 1) * 128], 128) for c2 in range(2)]
            for lc in range(4):
                pi = (g * 4 + lc) % 2
                for c2 in range(2):
                    wi = wis[c2]
                    b = npm[0] % 4
                    npm[0] += 1
                    for k in range(16):
                        P.mm(pm[b][:, :], wk[wi][:, k, 0:128], xo[:, k, lc * 512:(lc + 1) * 512], k == 0, k == 15,
                             ["xo", f"w{wi}"], [f"pm{b}"])
                    for k in range(16):
                        P.mm(phalo[:, :], wk[wi][:, k, 0:128], xh[:, k, lc * 16:(lc + 1) * 16], k == 0, k == 15,
                             ["xh", f"w{wi}"], ["phalo"])
                    ui = nu % 2
                    nu += 1
                    Ut = U[ui]
                    uk = f"U{ui}"
                    P.add("act", lambda e, Ut=Ut, b=b: e.copy(Ut[:, 16:528], pm[b][:, :]), [f"pm{b}"], [uk])
                    P.add("act", lambda e, Ut=Ut: e.copy(Ut[:, 0:16], phalo[:, :]), ["phalo"], [uk])
                    src, sk = Ut, uk
                    step = 1
                    dsts = [(Wa, "Wa"), (Wb, "Wb")]
                    di = 0
                    while step < win:
                        dt_, dk = dsts[di % 2]
                        di += 1
                        lo = 2 * step - 1
                        P.add("dve", lambda e, dt_=dt_, src=src, lo=lo, step=step: e.tensor_tensor(
                            dt_[:, lo:528], src[:, lo:528], src[:, lo - step:528 - step], ALU.add), [sk], [dk])
                        src, sk = dt_, dk
                        step *= 2
                    P.add("dve", lambda e, src=src, Ut=Ut, pi=pi, c2=c2, win=win: e.scalar_tensor_tensor(
                        pld[pi][:, c2, 16:512], src[:, 32:528], 1.0 / win, Ut[:, 32:528], ALU.mult, ALU.subtract),
                        [sk, uk], [f"pld{pi}"])
                    P.add("dve", lambda e, src=src, lc=lc, g=g: e.tensor_tensor(
                        Wa[:, 0:16] if src is not Wa else Wb[:, 0:16], src[:, 16:32], rc16[:, lc, g, :], ALU.mult),
                        [sk, "rc16"], ["Wa" if src is not Wa else "Wb"])
                    P.add("dve", lambda e, src=src, Ut=Ut, pi=pi, c2=c2: e.tensor_tensor(
                        pld[pi][:, c2, 0:16], Wa[:, 0:16] if src is not Wa else Wb[:, 0:16], Ut[:, 16:32], ALU.subtract),
                        ["Wa" if src is not Wa else "Wb", uk], [f"pld{pi}"])
                for d2 in range(2):
                    b = npm[0] % 4
                    npm[0] += 1
                    for c2 in range(2):
                        P.mm(pm[b][:, :], pmix[:, g, c2, d2 * 128:(d2 + 1) * 128], pld[pi][:, c2, :], c2 == 0, c2 == 1,
                             ["pmix", f"pld{pi}"], [f"pm{b}"])
                    mi = nmx % 2
                    nmx += 1
                    ct = 2 * g + d2
                    P.add("dve", lambda e, mi=mi, b=b, ct=ct: e.tensor_scalar(
                        mxs[mi][:, :], pm[b][:, :], pscale[:, ct:ct + 1], None, ALU.mult), [f"pm{b}", "pscale"], [f"mxs{mi}"])
                    P.dma("sp", sc["MixT"][ct * 128:(ct + 1) * 128, lc * 512:(lc + 1) * 512], mxs[mi][:, :],
                          [f"mxs{mi}"], ["MixT"])
        P.emit(st)


def _bf16(a):
    import ml_dtypes
    return np.asarray(a, dtype=np.float32).astype(ml_dtypes.bfloat16)


def _rope_tab(pos, scale=1.0):
    half = HD // 2
    inv = (10000.0 ** (-2.0 * np.arange(half, dtype=np.float32) / HD)).astype(np.float32)
    ang = pos.astype(np.float32)[:, None] * inv[None, :]
    c = (np.cos(ang).astype(np.float32) * np.float32(scale)).astype(np.float32)
    s = (np.sin(ang).astype(np.float32) * np.float32(scale)).astype(np.float32)
    n = pos.shape[0] // 128
    c = np.ascontiguousarray(c.reshape(n, 128, half).transpose(1, 0, 2))
    s = np.ascontiguousarray(s.reshape(n, 128, half).transpose(1, 0, 2))
    return c, s


def _core_tables(p):
    t = {}
    own_pos = np.concatenate([np.arange(512 * g, 512 * g + 512) for g in OWN[p]])
    prev_pos = np.concatenate([np.arange(512 * (g - 1), 512 * g) if g > 0 else np.zeros(512, np.int64) for g in OWN[p]])
    prev_valid = np.concatenate([np.full(512, 1.0 if g > 0 else 0.0, np.float32) for g in OWN[p]])
    t["own_pos"], t["prev_pos"], t["prev_valid"] = own_pos, prev_pos, prev_valid
    t["cosg"], t["sing"] = _rope_tab(np.arange(S))
    t["cosp"], t["sinp"] = _rope_tab(prev_pos)
    t["coso"], t["sino"] = _rope_tab(own_pos)
    t["cosq"], t["sinq"] = _rope_tab(own_pos, HD ** -0.5)
    rc = np.zeros((128, 4, 4, 16), np.float32)
    for lc, g in enumerate(OWN[p]):
        for gi, w in enumerate((2, 4, 8, 16)):
            for tt in range(16):
                rc[:, lc, gi, tt] = 1.0 / (min(tt + 1, w) if g == 0 else w)
    t["rc16"] = rc
    return t


_IN_A = {
    "ident": ([128, 128], BF16), "xTg": ([D, S], F32), "xTo": ([D, NT], F32), "xTp": ([D, NT], F32),
    "w_A1": ([D, 1024], F32), "w_A2": ([D, 512], F32), "w_q": ([D, 1024], F32), "w_gn": ([D, 48], F32),
    "w_pool": ([D, 1024], F32), "w_gm": ([D, 4096], F32), "pool_mix": ([4, 256, 256], F32),
    "pool_scale": ([128, 8], F32), "rc16": ([128, 4, 4, 16], F32),
    "cosg": ([128, 32, 32], F32), "sing": ([128, 32, 32], F32), "cosp": ([128, 16, 32], F32),
    "sinp": ([128, 16, 32], F32), "coso": ([128, 16, 32], F32), "sino": ([128, 16, 32], F32),
    "cosq": ([128, 16, 32], F32), "sinq": ([128, 16, 32], F32),
}
_SC_A = {
    "KT_A1": ([6, 128, S], BF16), "Vs": ([S, 256], BF16), "KwT_prev": ([2, 128, NT], BF16),
    "KwT_own": ([2, 128, NT], BF16), "Vw_prev": ([NT, 256], BF16), "Vw_own": ([NT, 256], BF16),
    "QT": ([8, 128, NT], BF16), "Gn": ([NT, 48], F32), "Gm": ([NT, 4096], BF16), "MixT": ([1024, NT], BF16),
    "OT": ([8, 128, NT], BF16),
}


def build_nc(in_specs, sc_specs, phases, final_out=None):
    nc = bass.Bass("TRN2", target_bir_lowering=False)
    io = {n: _dram(nc, n, shp, dt, "ExternalInput") for n, (shp, dt) in in_specs.items()}
    sc = {n: _scratch(nc, n, shp, dt) for n, (shp, dt) in sc_specs.items()}
    if final_out is not None:
        n, shp, dt = final_out
        sc[n] = _dram(nc, n, shp, dt, "ExternalOutput")
    for ph in phases:
        snap = nc.snapshot_sems()
        ph(nc, io, sc)
        nc.clear_and_free_semaphores(nc.allocated_since(snap))
        nc.all_engine_barrier()
    return nc


def _ts(P, out, in0, s1, s2, op0, op1, reads, writes, eng="dve"):
    if op1 is None:
        return P.add(eng, lambda e: e.tensor_scalar(out, in0, s1, None, op0), reads, writes)
    return P.add(eng, lambda e: e.tensor_scalar(out, in0, s1, s2, op0, op1), reads, writes)


def _tt(P, out, in0, in1, op, reads, writes, eng="dve"):
    return P.add(eng, lambda e: e.tensor_tensor(out, in0, in1, op), reads, writes)


def _stt(P, out, in0, sc_, in1, op0, op1, reads, writes):
    return P.add("dve", lambda e: e.scalar_tensor_tensor(out, in0, sc_, in1, op0, op1), reads, writes)


def _act(P, out, in_, func, reads, writes, bias=None, scale=None):
    kw = {}
    if bias is not None:
        kw["bias"] = bias
    if scale is not None:
        kw["scale"] = scale
    return P.add("act", lambda e: e.activation(out, in_, func, **kw), reads, writes)


def phase_B(nc, io, sc):
    with ExitStack() as st:
        C = Ctx(nc, st, "B")
        P = C.P
        ident = C.sb("ident", [128, 128], BF16)
        P.dma("sp", ident[:, :], io["ident"], [], ["ident"])
        Ksp = C.sb("Ksp", [128, S], BF16)
        P.dma("sp", Ksp[64:128, :], io["Eoh"], [], ["Ksp_e"])
        Qp = C.sb("Qp", [128, 4, NT], BF16)
        Vsp = C.sb("Vsp", [128, 32, 65], BF16)
        Kwp = C.sb("Kwp", [64, 4, 8, 128], BF16)
        Vwp = C.sb("Vwp", [128, 4, 8, 65], BF16)
        pvo = C.sb("pvo", [128, 16], BF16)
        P.dma("sp", pvo[:, :], io["pvones"], [], ["pvo"])
        P.add("pool", lambda e: e.memset(Vsp[:, :, 64:65], 1.0), [], ["Vsp_1"])
        P.add("pool", lambda e: e.memset(Vwp[:, :, 4:8, 64:65], 1.0), [], ["Vwp_1"])
        P.add("pool", lambda e: e.tensor_copy(Vwp[:, :, 0:4, 64:65].rearrange("p a b c -> p a (b c)"),
                                              pvo[:, :].rearrange("p (a b) -> p a b", a=4)), ["pvo"], ["Vwp_1"])
        KcT = C.sb("KcT", [64, S], BF16)
        VcT = C.sb("VcT", [64, S], BF16)
        w1 = [C.sb(f"w1_{i}", [64, 32, 256], BF16) for i in range(2)]
        w2 = C.sb("w2", [128, 3, 2, 64], BF16)
        posT = C.sb("posT", [64, 2, 32], BF16)
        P.dma("pool", w1[0][:, :, :], io["cmp_k_w1"].rearrange("(l d) j -> d l j", d=64), [], ["w1_0"])
        P.dma("pool", w1[1][:, :, :], io["cmp_v_w1"].rearrange("(l d) j -> d l j", d=64), [], ["w1_1"])
        for i, n in enumerate(("cmp_k_w2", "cmp_k_w2s", "cmp_v_w2")):
            P.dma("pool", w2[:, i, :, :], io[n].rearrange("(t p) d -> p t d", p=128), [], ["w2"])
        P.dma("pool", posT[:, 0, :], io["cmp_pos_kT"], [], ["posT"])
        P.dma("pool", posT[:, 1, :], io["cmp_pos_vT"], [], ["posT"])
        ccos = C.sb("ccos", [64, 256], F32)
        csin = C.sb("csin", [64, 256], F32)
        P.dma("sp", ccos[:, :], io["ccos"], [], ["ccos"])
        P.dma("sp", csin[:, :], io["csin"], [], ["csin"])
        cbias = C.sb("cbias", [128, 2, 2, 512], BF16)
        tritab = C.sb("tritab", [128, 4, 8, 128], BF16)
        P.dma("sp", tritab[:, :, :, :], io["tritab"], [], ["tritab"])
        tri2 = C.sb("tri2", [128, 2, 128], BF16)
        P.dma("sp", tri2[:, :, :], io["tri2"], [], ["tri2"])
        Vcp = C.sb("Vcp", [128, 2, 129], BF16)
        P.add("pool", lambda e: e.memset(Vcp[:, :, 0:65], 0.0), [], ["Vcp"])
        P.add("pool", lambda e: e.memset(Vcp[:, :, 64:65], 1.0), [], ["Vcp"])
        P.dma("sp", Vcp[:, :, 65:129], io["ovl"], [], ["Vcp_o"])
        KcmpT = C.sb("KcmpT", [64, 256], BF16)
        P.add("pool", lambda e: e.memset(KcmpT[:, :], 0.0), [], ["KcmpT"])
        Mk = C.sb("Mk", [128, 16, 64], F32)
        Ad = C.sb("Ad", [128, 16, 64], F32)
        Fm = C.sb("Fm", [128, 16, 64], F32)
        for t, n in ((Mk, "Mk"), (Ad, "Ad"), (Fm, "Fm")):
            P.dma("sp", t[:, :, :], io[n], [], [n])
        Gn = C.sb("Gn", [128, 16, 48], F32)
        P.dma("sp", Gn[:, :, :], sc["Gn"].rearrange("(t p) n -> p t n", p=128), [], ["Gn"])
        O = C.sb("O", [128, 16, 256], F32)
        Ob = [C.sb(f"Ob{i}", [128, 256], BF16) for i in range(2)]
        Os = [C.sb(f"Os{i}", [128, 2, 128], BF16) for i in range(2)]
        Pt = [C.sb(f"Pt{i}", [128, 512], BF16) for i in range(4)]
        Pc = [C.sb(f"Pc{i}", [128, 2, 512], BF16) for i in range(2)]
        Pw = [C.sb(f"Pw{i}", [128, 5, 128], BF16) for i in range(3)]
        hb = C.sb("hb", [128, 256], F32)
        sq = C.sb("sq", [128, 256], F32)
        uu = C.sb("uu", [128, 256], F32)
        sg = C.sb("sg", [128, 256], F32)
        gT = C.sb("gT", [128, 2, 2, 256], BF16)
        cb = C.sb("cb", [128, 4], F32)
        impb = C.sb("impb", [128, 4, 64], F32)
        impm = C.sb("impm", [128, 64], F32)
        imp2 = C.sb("imp2", [128, 64], F32)
        m8 = C.sb("m8", [128, 16], F32)
        nsel = C.sb("nsel", [128, 64], F32)
        NegT = C.sb("NegT", [128, 4, 128], BF16)
        P.add("pool", lambda e: e.memset(NegT[:, :, :], 0.0), [], [f"NegT{i}" for i in range(4)])
        sm = C.sb("sm", [128, 16, 4], F32)
        nsm = [0]
        t1 = C.sb("t1", [64, 256], F32)
        t2 = C.sb("t2", [64, 256], F32)
        pS = [C.ps(f"pS{i}", [128, 512], F32) for i in range(4)]
        pA = [C.ps(f"pA{i}", [128, 512], F32) for i in range(2)]
        pX = C.ps("pX", [128, 512], F32)
        pTb = C.ps("pTb", [128, 8, 128], BF16)

        zeros = C.sb("zeros", [128, 2048], BF16)
        P.add("pool", lambda e: e.memset(zeros[:, :], 0.0), [], ["zeros"])
        for e_ in range(NEXP):
            P.dma("pool", sc["Xg"][e_ * 128:(e_ + 1) * 128, :], zeros[:, :], ["zeros"], ["Xg"])
        for kv in range(2):
            for jt in range(2):
                for l in range(32):
                    P.mm(pX[:, 0:1], w1[kv][:, l, jt * 128:(jt + 1) * 128], posT[:, kv, l:l + 1], l == 0, l == 31,
                         [f"w1_{kv}", "posT"], ["pX"])
                i = kv * 2 + jt
                P.add("dve", lambda e, i=i: e.tensor_copy(cb[:, i:i + 1], pX[:, 0:1]), ["pX"], ["cb"])

        npt = [0]
        npc = [0]
        npw = [0]
        nps = [0]

        for g in (range(4) if GROUPS is None else GROUPS):
            pi, hf = g // 2, g % 2
            rows = slice(hf * 64, hf * 64 + 64)
            P.dma("sp", Ksp[0:64, :], sc["KT_A1"][pi, rows, :], ["KT_A1"], ["Ksp_k"])
            P.dma("sp", KcT[:, :], sc["KT_A1"][2 + pi, rows, :], ["KT_A1"], ["KcT"])
            P.dma("sp", VcT[:, :], sc["KT_A1"][4 + pi, rows, :], ["KT_A1"], ["VcT"])
            P.dma("sp", Vsp[:, :, 0:64], sc["Vs"][:, g * 64:(g + 1) * 64].rearrange("(t p) d -> p t d", p=128),
                  ["Vs"], ["Vsp_v"])
            for lc in range(4):
                P.dma("sp", Kwp[:, lc, 0:4, :], sc["KwT_prev"][pi, rows, lc * 512:(lc + 1) * 512], ["KwT_prev"], ["Kwp"])
                P.dma("sp", Kwp[:, lc, 4:8, :], sc["KwT_own"][pi, rows, lc * 512:(lc + 1) * 512], ["KwT_own"], ["Kwp"])
                P.dma("sp", Vwp[:, lc, 0:4, 0:64],
                      sc["Vw_prev"][lc * 512:(lc + 1) * 512, g * 64:(g + 1) * 64].rearrange("(t p) d -> p t d", p=128),
                      ["Vw_prev"], ["Vwp_v"])
                P.dma("sp", Vwp[:, lc, 4:8, 0:64],
                      sc["Vw_own"][lc * 512:(lc + 1) * 512, g * 64:(g + 1) * 64].rearrange("(t p) d -> p t d", p=128),
                      ["Vw_own"], ["Vwp_v"])
            for hh in range(4):
                h = 4 * g + hh
                P.dma("sp", Qp[0:64, hh, :], sc["QT"][h // 2, (h % 2) * 64:(h % 2) * 64 + 64, :], ["QT"], ["Qp_q"])

            for kv, src, skey in ((0, KcT, "KcT"), (1, VcT, "VcT")):
                for jt in range(2):
                    for l in range(32):
                        P.mm(pX[:, 0:255], w1[kv][:, l, jt * 128:(jt + 1) * 128], src[:, l:l + 16 * 254 + 1:16],
                             l == 0, l == 31, [f"w1_{kv}", skey], ["pX"])
                    i = kv * 2 + jt
                    _act(P, sq[:, 0:255], pX[:, 0:255], AF.Square, ["pX", "cb"], ["sq"], bias=cb[:, i:i + 1])
                    _ts(P, hb[:, 0:255], pX[:, 0:255], cb[:, i:i + 1], None, ALU.add, None, ["pX", "cb"], ["hb"])
                    _ts(P, uu[:, 0:255], sq[:, 0:255], 0.044715, 1.0, ALU.mult, ALU.add, ["sq"], ["uu"])
                    _tt(P, uu[:, 0:255], uu[:, 0:255], hb[:, 0:255], ALU.mult, ["uu", "hb"], ["uu"])
                    _act(P, sg[:, 0:255], uu[:, 0:255], AF.Sigmoid, ["uu"], ["sg"], scale=1.5957691216057308)
                    _tt(P, gT[:, kv, jt, 0:255], hb[:, 0:255], sg[:, 0:255], ALU.mult, ["hb", "sg"], ["gT"])
            for jt in range(2):
                P.mm(pX[0:64, 0:255], w2[:, 0, jt, :], gT[:, 0, jt, 0:255], jt == 0, jt == 1, ["w2", "gT"], ["pX"])
            _tt(P, t1[:, 0:255], pX[0:64, 0:255], ccos[:, 0:255], ALU.mult, ["pX", "ccos"], ["t1"])
            for jt in range(2):
                P.mm(pX[0:64, 0:255], w2[:, 1, jt, :], gT[:, 0, jt, 0:255], jt == 0, jt == 1, ["w2", "gT"], ["pX"])
            _tt(P, t2[:, 0:255], pX[0:64, 0:255], csin[:, 0:255], ALU.mult, ["pX", "csin"], ["t2"])
            _tt(P, KcmpT[:, 0:255], t1[:, 0:255], t2[:, 0:255], ALU.add, ["t1", "t2"], ["KcmpT"])
            for ct in range(2):
                n = 128 if ct == 0 else 127
                for jt in range(2):
                    P.mm(pX[0:n, 0:64], gT[:, 1, jt, ct * 128:ct * 128 + n], w2[:, 2, jt, :], jt == 0, jt == 1,
                         ["gT", "w2"], ["pX"])
                P.add("dve", lambda e, ct=ct, n=n: e.tensor_copy(Vcp[0:n, ct, 0:64], pX[0:n, 0:64]), ["pX"], ["Vcp"])

            for lc in range(4):
                cbi = lc % 2
                P.dma("sp", cbias[:, :, cbi, :], io["cmpbias"][:, :, lc * 512:(lc + 1) * 512], [], [f"cbias{cbi}"])
                qsl = slice(lc * 512, (lc + 1) * 512)

                def norm(acc_ap, acc_key, tt, hh, gcol, first):
                    si = nsm[0] % 16
                    nsm[0] += 1
                    sk = f"sm{si}"
                    _ts(P, sm[:, si, 0:1], acc_ap[:, 64:65], 1e-30, None, ALU.max, None, [acc_key], [sk])
                    P.add("dve", lambda e: e.reciprocal(sm[:, si, 1:2], sm[:, si, 0:1]), [sk], [sk])
                    _tt(P, sm[:, si, 2:3], sm[:, si, 1:2], Gn[:, tt, gcol:gcol + 1], ALU.mult, [sk, "Gn"], [sk])
                    osl = O[:, tt, hh * 64:(hh + 1) * 64]
                    if first:
                        _ts(P, osl, acc_ap[:, 0:64], sm[:, si, 2:3], None, ALU.mult, None, [acc_key, sk], [f"O{tt}"])
                    else:
                        _stt(P, osl, acc_ap[:, 0:64], sm[:, si, 2:3], osl, ALU.mult, ALU.add, [acc_key, sk, f"O{tt}"], [f"O{tt}"])
                    return sm[:, si, 1:2], sk

                b1banks = {}

                def b1_qk(hh):
                    bs = []
                    for ct in range(2):
                        b = nps[0] % 2
                        nps[0] += 1
                        P.mm(pS[b][:, :], KcmpT[:, ct * 128:(ct + 1) * 128], Qp[0:64, hh, qsl], True, False,
                             ["KcmpT", "Qp_q"], [f"pS{b}"])
                        P.mm(pS[b][:, :], ident[:, :], cbias[:, ct, cbi, :], False, True,
                             ["ident", f"cbias{cbi}"], [f"pS{b}"])
                        bs.append(b)
                    b1banks[hh] = bs

                def b1_rest(hh):
                    h = 4 * g + hh
                    pci = npc[0] % 2
                    npc[0] += 1
                    for ct in range(2):
                        b = b1banks[hh][ct]
                        _act(P, Pc[pci][:, ct, :], pS[b][:, :], AF.Exp, [f"pS{b}"], [f"Pc{pci}"])
                    accs = (pA if hh % 2 == 0 else pS[2:4])
                    akeys = (["pA0", "pA1"] if hh % 2 == 0 else ["pS2", "pS3"])
                    for qs in range(4):
                        acc = accs[qs // 2][:, (qs % 2) * 256:(qs % 2) * 256 + 129]
                        ak = akeys[qs // 2]
                        for ct in range(2):
                            P.mm(acc, Pc[pci][:, ct, qs * 128:(qs + 1) * 128], Vcp[:, ct, :], ct == 0, ct == 1,
                                 [f"Pc{pci}", "Vcp", "Vcp_o"], [ak])
                    for qs in range(4):
                        tt = lc * 4 + qs
                        acc = accs[qs // 2][:, (qs % 2) * 256:(qs % 2) * 256 + 129]
                        ak = akeys[qs // 2]
                        rz, sk = norm(acc, ak, tt, hh, h, True)
                        if hh == 0:
                            _ts(P, impb[:, qs, :], acc[:, 65:129], rz, None, ALU.mult, None, [ak, sk], [f"impb{qs}"])
                        else:
                            _stt(P, impb[:, qs, :], acc[:, 65:129], rz, impb[:, qs, :], ALU.mult, ALU.add,
                                 [ak, sk, f"impb{qs}"], [f"impb{qs}"])

                for hh in range(4):
                    b1_qk(hh)
                    b1_rest(hh)

                for qs in range(4):
                    tt = lc * 4 + qs
                    iq = impb[:, qs, :]
                    _tt(P, impm[:, :], iq, Mk[:, tt, :], ALU.mult, [f"impb{qs}", "Mk"], ["impm"])
                    _tt(P, impm[:, :], impm[:, :], Ad[:, tt, :], ALU.add, ["impm", "Ad"], ["impm"])
                    P.add("dve", lambda e: e.max(m8[:, 0:8], impm[:, :]), ["impm"], ["m8"])
                    P.add("dve", lambda e: e.match_replace(imp2[:, :], m8[:, 0:8], impm[:, :], -3.0e6), ["impm", "m8"], ["imp2"])
                    P.add("dve", lambda e: e.max(m8[:, 8:16], imp2[:, :]), ["imp2"], ["m8"])
                    _ts(P, nsel[:, :], impm[:, :], m8[:, 15:16], -NEG, ALU.is_ge, ALU.mult, ["impm", "m8"], ["nsel"])
                    _stt(P, NegT[:, qs, 64:128], nsel[:, :], NEG, Fm[:, tt, :], ALU.add, ALU.add, ["nsel", "Fm"], [f"NegT{qs}"])

                wunits = [(hh, qs) for hh in range(4) for qs in range(4)]
                wbanks = {}

                def b3_qk(u):
                    hh, qs = wunits[u]
                    tt = lc * 4 + qs
                    b = nps[0] % 4
                    b2 = (nps[0] + 1) % 4
                    nps[0] += 2
                    qap = Qp[0:64, hh, tt * 128:(tt + 1) * 128]
                    for r in range(qs, qs + 4):
                        o = pS[b][:, (r - qs) * 128:(r - qs + 1) * 128]
                        P.mm(o, Kwp[:, lc, r, :], qap, True, r != qs, ["Kwp", "Qp_q"], [f"pS{b}"])
                        if r == qs:
                            P.mm(o, ident[:, :], tri2[:, 0, :], False, True, ["ident", "tri2"], [f"pS{b}"])
                    P.mm(pS[b2][:, 0:128], Kwp[:, lc, qs + 4, :], qap, True, False, ["Kwp", "Qp_q"], [f"pS{b2}"])
                    P.mm(pS[b2][:, 0:128], ident[:, :], tri2[:, 1, :], False, True, ["ident", "tri2"], [f"pS{b2}"])
                    wbanks[u] = (b, b2)

                def b3_rest(u):
                    hh, qs = wunits[u]
                    h = 4 * g + hh
                    tt = lc * 4 + qs
                    b, b2 = wbanks[u]
                    pwi = npw[0] % 3
                    npw[0] += 1
                    _act(P, Pw[pwi][:, 0:4, :], pS[b][:, :].rearrange("p (a b) -> p a b", a=4), AF.Exp, [f"pS{b}"], [f"Pw{pwi}"])
                    _act(P, Pw[pwi][:, 4, :], pS[b2][:, 0:128], AF.Exp, [f"pS{b2}"], [f"Pw{pwi}"])
                    ai = u % 2
                    acc = pA[ai][:, 0:65]
                    for r in range(5):
                        P.mm(acc, Pw[pwi][:, r, :], Vwp[:, lc, qs + r, :], r == 0, r == 4,
                             [f"Pw{pwi}", "Vwp_v", "Vwp_1"], [f"pA{ai}"])
                    norm(acc, f"pA{ai}", tt, hh, 32 + h, False)

                b3_qk(0)
                for u in range(16):
                    if u + 1 < 16:
                        b3_qk(u + 1)
                    b3_rest(u)

                for qs in range(4):
                    tt = lc * 4 + qs
                    P.tr(pTb[:, 4 + qs, :], NegT[:, qs, :], ident[:, :], [f"NegT{qs}", "ident"], ["pTb"])
                    P.add("act", lambda e, tt=tt, qs=qs: e.copy(
                        Qp[64:128, :, tt * 128:(tt + 1) * 128],
                        pTb[64:128, 4 + qs:5 + qs, :].broadcast_to([64, 4, 128])), ["pTb"], ["Qp_m"])

                E = 8 * (lc + 1)
                sunits = [(hh, kt) for hh in range(4) for kt in range(E)]
                sbanks = {}

                def b2_qk(u):
                    hh, kt = sunits[u]
                    b = nps[0] % 4
                    nps[0] += 1
                    s = kt - (E - 8)
                    P.mm(pS[b][:, :], Ksp[:, kt * 128:(kt + 1) * 128], Qp[:, hh, qsl], True, s < 0,
                         ["Ksp_k", "Ksp_e", "Qp_q", "Qp_m"], [f"pS{b}"])
                    if s >= 0:
                        qd = s % 4
                        P.mm(pS[b][:, qd * 128:(qd + 1) * 128], ident[:, :], tritab[:, lc, s, :], False, True,
                             ["ident", "tritab"], [f"pS{b}"])
                    sbanks[u] = b

                def b2_rest(u):
                    hh, kt = sunits[u]
                    h = 4 * g + hh
                    b = sbanks[u]
                    pti = npt[0] % 4
                    npt[0] += 1
                    _act(P, Pt[pti][:, :], pS[b][:, :], AF.Exp, [f"pS{b}"], [f"Pt{pti}"])
                    ai = hh % 2
                    for qs in range(4):
                        P.mm(pA[ai][:, qs * 128:qs * 128 + 65], Pt[pti][:, qs * 128:(qs + 1) * 128], Vsp[:, kt, :],
                             kt == 0 and qs == 0, kt == E - 1, [f"Pt{pti}", "Vsp_v", "Vsp_1"], [f"pA{ai}"], skip=True)
                    if kt == E - 1:
                        for qs in range(4):
                            norm(pA[ai][:, qs * 128:qs * 128 + 65], f"pA{ai}", lc * 4 + qs, hh, 16 + h, False)

                LA = 2
                nsu = len(sunits)
                for u in range(min(LA, nsu)):
                    b2_qk(u)
                for u in range(nsu):
                    if u + LA < nsu:
                        b2_qk(u + LA)
                    b2_rest(u)

            for tt in range(16):
                oi = tt % 2
                P.add("act", lambda e, oi=oi, tt=tt: e.copy(Ob[oi][:, :], O[:, tt, :]), [f"O{tt}"], [f"Ob{oi}"])
                for i in range(2):
                    P.tr(pTb[:, 2 + i, :], Ob[oi][:, i * 128:(i + 1) * 128], ident[:, :], [f"Ob{oi}", "ident"], ["pTb"])
                P.add("dve", lambda e, oi=oi: e.tensor_copy(Os[oi][:, :, :], pTb[:, 2:4, :]), ["pTb"], [f"Os{oi}"])
                P.dma("sp", sc["OT"][2 * g:2 * g + 2].rearrange("i p t -> p i t")[:, :, tt * 128:(tt + 1) * 128],
                      Os[oi][:, :, :], [f"Os{oi}"], ["OT"])
        P.emit(st)


def _core_tables_B(p, t):
    own_pos = t["own_pos"]
    c = np.arange(256)
    cend = 16 * c + 31
    valid = (c[:, None] <= 254) & (cend[:, None] <= own_pos[None, :])
    cb = np.where(valid, 0.0, NEG).astype(np.float32).reshape(2, 128, NT).transpose(1, 0, 2)
    t["cmpbias"] = _bf16(np.ascontiguousarray(cb))
    k = np.arange(128)
    tri = np.where(k[:, None] > k[None, :], NEG, 0.0).astype(np.float32)
    tt = np.zeros((128, 4, 8, 128), np.float32)
    for lc, gc in enumerate(OWN[p]):
        E = 8 * (lc + 1)
        for s in range(8):
            kt = E - 8 + s
            if 4 * gc <= kt < 4 * gc + 4:
                assert (kt - 4 * gc) == s % 4
                tt[:, lc, s, :] = tri
    t["tritab"] = _bf16(tt)
    tri2 = np.zeros((128, 2, 128), np.float32)
    tri2[:, 0, :] = np.where(k[:, None] <= k[None, :], NEG, 0.0)
    tri2[:, 1, :] = np.where(k[:, None] > k[None, :], NEG, 0.0)
    t["tri2"] = _bf16(tri2)
    cs = np.arange(256) * 16
    ss = np.arange(64) * 64
    ov = ((cs[:, None] + 31 >= ss[None, :]) & (cs[:, None] <= ss[None, :] + 63) & (c[:, None] <= 254)).astype(np.float32)
    t["ovl"] = _bf16(np.ascontiguousarray(ov.reshape(2, 128, 64).transpose(1, 0, 2)))
    cur = own_pos // 64
    j = np.arange(64)
    forced = (j[None, :] == 0) | (j[None, :] == cur[:, None]) | (j[None, :] == cur[:, None] - 1)
    future = j[None, :] > cur[:, None]
    mk = (~(forced | future)).astype(np.float32)
    ad = np.where(forced, 1e6, np.where(future, -1e6, 0.0)).astype(np.float32)
    fm = np.where(future, NEG, 0.0).astype(np.float32)
    lay = lambda a: np.ascontiguousarray(a.reshape(16, 128, 64).transpose(1, 0, 2))
    t["Mk"], t["Ad"], t["Fm"] = lay(mk), lay(ad), lay(fm)
    t["pvones"] = _bf16(np.ascontiguousarray(t["prev_valid"].reshape(16, 128).T))
    t["Eoh"] = _bf16((np.arange(S)[None, :] // 64 == j[:, None]).astype(np.float32))
    half = HD // 2
    inv = (10000.0 ** (-2.0 * np.arange(half, dtype=np.float32) / HD)).astype(np.float32)
    ang = cend.astype(np.float32)[None, :] * np.concatenate([inv, inv])[:, None]
    t["ccos"] = np.cos(ang).astype(np.float32)
    sn = np.sin(ang).astype(np.float32)
    sn[:half] *= -1.0
    t["csin"] = sn
    return t


_IN_B = {
    "Eoh": ([64, S], BF16), "pvones": ([128, 16], BF16), "cmp_k_w1": ([2048, 256], F32), "cmp_v_w1": ([2048, 256], F32),
    "cmp_k_w2": ([256, 64], F32), "cmp_k_w2s": ([256, 64], F32), "cmp_v_w2": ([256, 64], F32),
    "cmp_pos_kT": ([64, 32], F32), "cmp_pos_vT": ([64, 32], F32), "ccos": ([64, 256], F32), "csin": ([64, 256], F32),
    "tritab": ([128, 4, 8, 128], BF16), "tri2": ([128, 2, 128], BF16), "ovl": ([128, 2, 64], BF16),
    "cmpbias": ([128, 2, NT], BF16), "Mk": ([128, 16, 64], F32), "Ad": ([128, 16, 64], F32), "Fm": ([128, 16, 64], F32),
}


def phase_C1(nc, io, sc):
    with ExitStack() as st:
        C = Ctx(nc, st, "C1")
        P = C.P
        ident = C.sb("ident", [128, 128], BF16)
        P.dma("sp", ident[:, :], io["ident"], [], ["ident"])
        wn = C.sb("wn", [128, 8, 2048], BF16)
        wp = C.sb("wp", [128, 8, 2048], BF16)
        for c in range(4):
            cs = slice(c * 512, (c + 1) * 512)
            P.dma("pool", wn[:, :, cs], io["w_nsa_proj"][:, cs].rearrange("(k p) n -> p k n", p=128), [], ["wn"])
            P.dma("pool", wp[:, :, cs], io["w_pool_proj"][:, cs].rearrange("(k p) n -> p k n", p=128), [], ["wp"])
        oT = [C.sb(f"oT{i}", [128, 8, 128], BF16) for i in range(2)]
        mT = [C.sb(f"mT{i}", [128, 8, 128], BF16) for i in range(2)]
        gm = [C.sb(f"gm{i}", [128, 4096], BF16) for i in range(2)]
        ta = [C.sb(f"ta{i}", [128, 512], F32) for i in range(2)]
        tb = [C.sb(f"tb{i}", [128, 512], F32) for i in range(2)]
        z = [C.sb(f"z{i}", [128, 2048], BF16) for i in range(2)]
        zT = [C.sb(f"zT{i}", [128, 16, 128], BF16) for i in range(2)]
        pa = [C.ps(f"pa{i}", [128, 512], F32) for i in range(2)]
        pb = [C.ps(f"pb{i}", [128, 512], F32) for i in range(2)]
        pT = [C.ps(f"pT{i}", [128, 8, 128], BF16) for i in range(2)]
        n = [0]

        def c1_stage1(tt):
            i = tt % 2
            ts_ = slice(tt * 128, (tt + 1) * 128)
            P.dma("sp", oT[i][:, :, :], sc["OT"].rearrange("k p t -> p k t")[:, :, ts_], ["OT"], [f"oT{i}"])
            P.dma("sp", mT[i][:, :, :], sc["MixT"].rearrange("(k p) t -> p k t", p=128)[:, :, ts_], ["MixT"], [f"mT{i}"])
            P.dma("sp", gm[i][:, :], sc["Gm"][ts_, :], ["Gm"], [f"gm{i}"])
            for cc in range(4):
                j = n[0] % 2
                n[0] += 1
                cs = slice(cc * 512, (cc + 1) * 512)
                for k in range(8):
                    P.mm(pa[j][:, :], oT[i][:, k, :], wn[:, k, cs], k == 0, k == 7, [f"oT{i}", "wn"], [f"pa{j}"])
                for k in range(8):
                    P.mm(pb[j][:, :], mT[i][:, k, :], wp[:, k, cs], k == 0, k == 7, [f"mT{i}", "wp"], [f"pb{j}"])
                _tt(P, ta[j][:, :], pa[j][:, :], gm[i][:, 2048 + cc * 512:2048 + (cc + 1) * 512], ALU.mult,
                    [f"pa{j}", f"gm{i}"], [f"ta{j}"])
                _tt(P, tb[j][:, :], pb[j][:, :], gm[i][:, cs], ALU.mult, [f"pb{j}", f"gm{i}"], [f"tb{j}"])
                _tt(P, z[i][:, cs], ta[j][:, :], tb[j][:, :], ALU.add, [f"ta{j}", f"tb{j}"], [f"z{i}"], eng="pool")

        def c1_stage2(tt):
            i = tt % 2
            ts_ = slice(tt * 128, (tt + 1) * 128)
            for hh in range(2):
                for k in range(8):
                    kk = hh * 8 + k
                    P.tr(pT[hh][:, k, :], z[i][:, kk * 128:(kk + 1) * 128], ident[:, :], [f"z{i}", "ident"], [f"pT{hh}"])
                P.add("act", lambda e, hh=hh: e.copy(zT[i][:, hh * 8:(hh + 1) * 8, :], pT[hh][:, :, :]),
                      [f"pT{hh}"], [f"zT{i}"])
            P.dma("sp", sc["ZT"].rearrange("k p t -> p k t")[:, :, ts_], zT[i][:, :, :], [f"zT{i}"], ["ZT"])

        c1_stage1(0)
        for tt in range(16):
            if tt + 1 < 16:
                c1_stage1(tt + 1)
            c1_stage2(tt)
        P.emit(st)


def _layer_norm(P, dst, src, skey, dkey, g_bc, b_bc, gkeys, st6, mv, tmp, tkeys):
    for c in range(4):
        P.add("dve", lambda e, c=c: e.bn_stats(st6[:, c * 6:(c + 1) * 6], src[:, c * 512:(c + 1) * 512]), [skey], [tkeys[0]])
    P.add("dve", lambda e: e.bn_aggr(mv[:, 0:2], st6[:, 0:24]), [tkeys[0]], [tkeys[1]])
    _act(P, mv[:, 2:3], mv[:, 1:2], AF.Sqrt, [tkeys[1]], [tkeys[1]], bias=mv[:, 4:5])
    P.add("dve", lambda e: e.reciprocal(mv[:, 3:4], mv[:, 2:3]), [tkeys[1]], [tkeys[1]])
    _ts(P, tmp[:, :], src[:, :], mv[:, 0:1], mv[:, 3:4], ALU.subtract, ALU.mult, [skey, tkeys[1]], [tkeys[2]])
    _tt(P, tmp[:, :], tmp[:, :], g_bc[:, :], ALU.mult, [tkeys[2], gkeys[0]], [tkeys[2]], eng="pool")
    _tt(P, dst[:, :], tmp[:, :], b_bc[:, :], ALU.add, [tkeys[2], gkeys[1]], [dkey])


def _breg(eng, cache):
    if "r" not in cache:
        cache["r"] = eng.to_reg(NEXP * CAP - 1)
    return cache["r"]


def phase_C2(nc, io, sc):
    with ExitStack() as st:
        C = Ctx(nc, st, "C2")
        P = C.P
        breg = {}
        identf = C.sb("identf", [128, 128], F32)
        P.dma("sp", identf[:, :], io["identf"], [], ["identf"])
        wo = C.sb("wo", [128, 16, 2048], BF16)
        for c in range(4):
            cs = slice(c * 512, (c + 1) * 512)
            P.dma("pool", wo[:, :, cs], io["w_out"][:, cs].rearrange("(k p) n -> p k n", p=128), [], ["wo"])
        wr = C.sb("wr", [128, 16, 72], F32)
        P.dma("sp", wr[:, :, :], io["w_router"].rearrange("(k p) n -> p k n", p=128), [], ["wr"])
        br = C.sb("br", [128, 72], F32)
        P.dma("sp", br[:, :], io["b_router"], [], ["br"])
        eid64 = C.sb("eid64", [128, 64], F32)
        P.dma("sp", eid64[:, :], io["eid64"], [], ["eid64"])
        g1 = C.sb("g1", [128, 2048], F32)
        b1 = C.sb("b1", [128, 2048], F32)
        P.dma("sp", g1[:, :], io["ln1_g"], [], ["g1"])
        P.dma("sp", b1[:, :], io["ln1_b"], [], ["b1"])
        Ut = C.sb("Ut", [128, 128], BF16)
        P.dma("sp", Ut[:, :], io["utri"], [], ["Ut"])
        ones = C.sb("ones", [128, 128], BF16)
        P.add("pool", lambda e: e.memset(ones[:, :], 1.0), [], ["ones"])
        accind = C.sb("accind", [128, 64], F32)
        P.add("pool", lambda e: e.memset(accind[:, :], 0.0), [], ["accind"])
        zT = [C.sb(f"zT{i}", [128, 16, 128], BF16) for i in range(2)]
        xt = [C.sb(f"xt{i}", [128, 2048], F32) for i in range(2)]
        r = [C.sb(f"r{i}", [128, 2048], F32) for i in range(2)]
        tmp = C.sb("tmp", [128, 2048], F32)
        h1 = [C.sb(f"h1_{i}", [128, 2048], F32) for i in range(2)]
        h1b = [C.sb(f"h1b{i}", [128, 2048], BF16) for i in range(2)]
        h1T = C.sb("h1T", [128, 16, 128], F32)
        st6 = C.sb("st6", [128, 24], F32)
        mv = C.sb("mv", [128, 8], F32)
        P.add("pool", lambda e: e.memset(mv[:, 4:5], LN_EPS), [], ["mv"])
        lg = C.sb("lg", [128, 72], F32)
        rt = [C.sb(f"rt{i}", [128, 64], F32) for i in range(2)]
        e3 = C.sb("e3", [128, 8, 8], F32)
        E1 = [C.sb(f"E1_{i}", [128, 8, 8], F32) for i in range(2)]
        E2 = [C.sb(f"E2_{i}", [128, 8, 8], F32) for i in range(2)]
        indb = [C.sb(f"indb{i}", [128, 64], BF16) for i in range(2)]
        accb = [C.sb(f"accb{i}", [128, 64], BF16) for i in range(2)]
        posf = C.sb("posf", [128, 64], F32)
        indf = C.sb("indf", [128, 64], F32)
        ridx = [C.sb(f"ridx{i}", [128, 2], I32) for i in range(2)]
        rw = [C.sb(f"rw{i}", [128, 2], F32) for i in range(2)]
        py = [C.ps(f"py{i}", [128, 512], F32) for i in range(4)]
        pt = [C.ps(f"pt{i}", [128, 4, 128], F32) for i in range(2)]
        pl = C.ps("pl", [128, 72], F32)
        pp = C.ps("pp", [128, 64], F32)
        npt = [0]

        def stage1(tt):
            i = tt % 2
            ts_ = slice(tt * 128, (tt + 1) * 128)
            P.dma("sp", zT[i][:, :, :], sc["ZT"].rearrange("k p t -> p k t")[:, :, ts_], ["ZT"], [f"zT{i}"])
            P.dma("sp", xt[i][:, :], io["x_own"][ts_, :], [], [f"xt{i}"])
            for cc in range(4):
                cs = slice(cc * 512, (cc + 1) * 512)
                for k in range(16):
                    P.mm(py[cc][:, :], zT[i][:, k, :], wo[:, k, cs], k == 0, k == 15, [f"zT{i}", "wo"], [f"py{cc}"])

        def stage1b(tt):
            i = tt % 2
            for cc in range(4):
                cs = slice(cc * 512, (cc + 1) * 512)
                _stt(P, r[i][:, cs], xt[i][:, cs], DN_ALPHA, py[cc][:, :], ALU.mult, ALU.add, [f"xt{i}", f"py{cc}"], [f"r{i}"])

        def s_ln1(tt):
            i = tt % 2
            for c in range(4):
                P.add("dve", lambda e, c=c: e.bn_stats(st6[:, c * 6:(c + 1) * 6], r[i][:, c * 512:(c + 1) * 512]), [f"r{i}"], ["st6"])
            P.add("dve", lambda e: e.bn_aggr(mv[:, 0:2], st6[:, 0:24]), ["st6"], ["mv"])
            _act(P, mv[:, 2:3], mv[:, 1:2], AF.Sqrt, ["mv"], ["mv"], bias=mv[:, 4:5])

        def s_ln2(tt):
            i = tt % 2
            ts_ = slice(tt * 128, (tt + 1) * 128)
            P.add("dve", lambda e: e.reciprocal(mv[:, 3:4], mv[:, 2:3]), ["mv"], ["mv"])
            _ts(P, tmp[:, :], r[i][:, :], mv[:, 0:1], mv[:, 3:4], ALU.subtract, ALU.mult, [f"r{i}", "mv"], ["tmp"])
            _tt(P, tmp[:, :], tmp[:, :], g1[:, :], ALU.mult, ["tmp", "g1"], ["tmp"])
            _tt(P, h1[i][:, :], tmp[:, :], b1[:, :], ALU.add, ["tmp", "b1"], [f"h1_{i}"])
            P.dma("sp", sc["H1"][ts_, :], h1[i][:, :], [f"h1_{i}"], ["H1"])
            P.add("act", lambda e: e.copy(h1b[i][:, :], h1[i][:, :]), [f"h1_{i}"], [f"h1b{i}"])

        def stage2b(tt):
            i = tt % 2
            R = [f"rt{i}"]
            rt_ = rt[i]
            for k4 in range(4):
                j = npt[0] % 2
                npt[0] += 1
                for k in range(4):
                    kk = k4 * 4 + k
                    P.tr(pt[j][:, k, :], h1[i][:, kk * 128:(kk + 1) * 128], identf[:, :], [f"h1_{i}", "identf"], [f"pt{j}"])
                P.add("act", lambda e, j=j, k4=k4: e.copy(h1T[:, k4 * 4:(k4 + 1) * 4, :], pt[j][:, :, :]), [f"pt{j}"], ["h1T"])
            for k in range(16):
                P.mm(pl[:, :], h1T[:, k, :], wr[:, k, :], k == 0, k == 15, ["h1T", "wr"], ["pl"])
            _tt(P, lg[:, :], pl[:, :], br[:, :], ALU.add, ["pl", "br"], ["lg"])
            P.add("dve", lambda e: e.tensor_reduce(rt_[:, 0:1], lg[:, 0:8], AX.X, ALU.max), ["lg"], R)
            _ts(P, rt_[:, 8:16], lg[:, 0:8], rt_[:, 0:1], None, ALU.is_equal, None, ["lg"] + R, R)
            _ts(P, rt_[:, 1:2], rt_[:, 0:1], -1.0, None, ALU.mult, None, R, R)
            P.add("act", lambda e: e.activation(rt_[:, 16:24], lg[:, 0:8], AF.Exp, bias=rt_[:, 1:2], accum_out=rt_[:, 2:3]), ["lg"] + R, R)
            P.add("dve", lambda e: e.reciprocal(rt_[:, 3:4], rt_[:, 2:3]), R, R)
            _tt(P, e3[:, :, :], lg[:, 8:72].rearrange("p (g e) -> p g e", g=8),
                rt_[:, 8:16].unsqueeze(2).broadcast_to([128, 8, 8]), ALU.mult, ["lg"] + R, ["e3"])
            P.add("dve", lambda e: e.tensor_reduce(rt_[:, 24:32], e3[:, :, :].rearrange("p g e -> p e g"), AX.X, ALU.add), ["e3"], R)
            P.add("dve", lambda e: e.max(rt_[:, 32:40], rt_[:, 24:32]), R, R)
            _ts(P, rt_[:, 40:48], rt_[:, 24:32], rt_[:, 32:33], None, ALU.is_equal, None, R, R)
            _ts(P, rt_[:, 48:56], rt_[:, 24:32], rt_[:, 33:34], None, ALU.is_equal, None, R, R)
            _tt(P, rt_[:, 4:5], rt_[:, 32:33], rt_[:, 33:34], ALU.subtract, R, R)
            _act(P, rt_[:, 5:6], rt_[:, 4:5], AF.Sigmoid, R, R)
            _tt(P, rw[i][:, 0:1], rt_[:, 5:6], rt_[:, 3:4], ALU.mult, R, [f"rw{i}"])
            _tt(P, rw[i][:, 1:2], rt_[:, 3:4], rw[i][:, 0:1], ALU.subtract, R + [f"rw{i}"], [f"rw{i}"])
            gb = rt_[:, 8:16].unsqueeze(2).broadcast_to([128, 8, 8])
            _tt(P, E1[i][:, :, :], gb, rt_[:, 40:48].unsqueeze(1).broadcast_to([128, 8, 8]), ALU.mult, R, [f"E1_{i}"])
            _tt(P, E2[i][:, :, :], gb, rt_[:, 48:56].unsqueeze(1).broadcast_to([128, 8, 8]), ALU.mult, R, [f"E2_{i}"])
            E1f = E1[i][:, :, :].rearrange("p g e -> p (g e)")
            E2f = E2[i][:, :, :].rearrange("p g e -> p (g e)")
            _tt(P, indf[:, :], E1f, E2f, ALU.add, [f"E1_{i}", f"E2_{i}"], ["indf"])
            P.add("dve", lambda e: e.tensor_copy(indb[i][:, :], indf[:, :]), ["indf"], [f"indb{i}"])
            P.add("dve", lambda e: e.tensor_copy(accb[i][:, :], accind[:, :]), ["accind"], [f"accb{i}"])
            _tt(P, accind[:, :], accind[:, :], indf[:, :], ALU.add, ["accind", "indf"], ["accind"])

        def s_pos_pe(tt):
            i = tt % 2
            P.mm(pp[:, :], Ut[:, :], indb[i][:, :], True, False, ["Ut", f"indb{i}"], ["pp"])
            P.mm(pp[:, :], ones[:, :], accb[i][:, :], False, True, ["ones", f"accb{i}"], ["pp"])

        def stage3(tt):
            i = tt % 2
            ts_ = slice(tt * 128, (tt + 1) * 128)
            R = [f"rt{i}"]
            rt_ = rt[i]
            P.add("dve", lambda e: e.tensor_copy(posf[:, :], pp[:, :]), ["pp"], ["posf"])
            e3f = e3[:, :, :].rearrange("p g e -> p (g e)")
            for kk, (Eb, ek) in enumerate(((E1[i], f"E1_{i}"), (E2[i], f"E2_{i}"))):
                Ef = Eb[:, :, :].rearrange("p g e -> p (g e)")
                o0 = 56 + kk * 4
                _tt(P, e3f, Ef, posf[:, :], ALU.mult, [ek, "posf"], ["e3"])
                P.add("dve", lambda e, o0=o0: e.tensor_reduce(rt_[:, o0:o0 + 1], e3f, AX.X, ALU.add), ["e3"], R)
                _tt(P, e3f, Ef, eid64[:, :], ALU.mult, [ek, "eid64"], ["e3"])
                P.add("dve", lambda e, o0=o0: e.tensor_reduce(rt_[:, o0 + 1:o0 + 2], e3f, AX.X, ALU.add), ["e3"], R)
                _ts(P, rt_[:, o0 + 2:o0 + 3], rt_[:, o0:o0 + 1], float(CAP), 1.0e6, ALU.is_ge, ALU.mult, R, R)
                _stt(P, rt_[:, o0 + 3:o0 + 4], rt_[:, o0 + 1:o0 + 2], float(CAP), rt_[:, o0:o0 + 1], ALU.mult, ALU.add, R, R)
                _tt(P, rt_[:, o0 + 3:o0 + 4], rt_[:, o0 + 3:o0 + 4], rt_[:, o0 + 2:o0 + 3], ALU.add, R, R)
                P.add("dve", lambda e, kk=kk, o0=o0: e.tensor_copy(ridx[i][:, kk:kk + 1], rt_[:, o0 + 3:o0 + 4]), R, [f"ridx{i}"])
            P.dma("sp", sc["Ridx"][ts_, :], ridx[i][:, :], [f"ridx{i}"], ["Ridx"])
            P.dma("sp", sc["Rw"][ts_, :], rw[i][:, :], [f"rw{i}"], ["Rw"])
            for kk in range(2):
                P.add("pool", lambda e, kk=kk: e.indirect_dma_start(
                    out=sc["Xg"][:, :], out_offset=bass.IndirectOffsetOnAxis(ap=ridx[i][:, kk:kk + 1], axis=0),
                    in_=h1b[i][:, :], in_offset=None, bounds_check=_breg(e, breg), oob_is_err=False),
                    [f"h1b{i}", f"ridx{i}"], ["Xg_s"], dma=True)

        stage1(0)
        stage1b(0)
        for tt in range(16):
            if tt >= 1:
                s_pos_pe(tt - 1)
            if tt + 1 < 16:
                stage1(tt + 1)
            s_ln1(tt)
            if tt >= 1:
                stage3(tt - 1)
            s_ln2(tt)
            stage2b(tt)
            if tt + 1 < 16:
                stage1b(tt + 1)
        s_pos_pe(15)
        stage3(15)
        P.emit(st)


def phase_D(nc, io, sc):
    with ExitStack() as st:
        C = Ctx(nc, st, "D")
        P = C.P
        ident = C.sb("ident", [128, 128], BF16)
        P.dma("sp", ident[:, :], io["ident"], [], ["ident"])
        wg = [C.sb(f"wg{i}", [128, 16, 512], BF16) for i in range(2)]
        wu = [C.sb(f"wu{i}", [128, 16, 512], BF16) for i in range(2)]
        wd = [C.sb(f"wd{i}", [128, 4, 2048], BF16) for i in range(2)]
        xe = [C.sb(f"xe{i}", [128, 2048], BF16) for i in range(2)]
        xT = [C.sb(f"xT{i}", [128, 16, 128], BF16) for i in range(2)]
        sg = C.sb("sg", [128, 512], F32)
        hm = C.sb("hm", [128, 512], BF16)
        hT = C.sb("hT", [128, 4, 128], BF16)
        ye = [C.sb(f"ye{i}", [128, 2048], F32) for i in range(2)]
        pg = C.ps("pg", [128, 512], F32)
        pu = C.ps("pu", [128, 512], F32)
        py = [C.ps(f"py{i}", [128, 512], F32) for i in range(2)]
        pT = [C.ps(f"pT{i}", [128, 8, 128], BF16) for i in range(2)]
        n = 0
        for e_ in (range(NEXP) if EXPERTS is None else EXPERTS):
            i = e_ % 2
            P.dma("pool", wg[i][:, :, :], io["w_gate"][e_].rearrange("(k p) n -> p k n", p=128), [], [f"wg{i}"])
            P.dma("pool", wu[i][:, :, :], io["w_up"][e_].rearrange("(k p) n -> p k n", p=128), [], [f"wu{i}"])
            for c in range(4):
                P.dma("pool", wd[i][:, :, c * 512:(c + 1) * 512],
                      io["w_down"][e_][:, c * 512:(c + 1) * 512].rearrange("(k p) n -> p k n", p=128), [], [f"wd{i}"])
            P.dma("sp", xe[i][:, :], sc["Xg"][e_ * 128:(e_ + 1) * 128, :], ["Xg"], [f"xe{i}"])
            for hh in range(2):
                for k in range(8):
                    kk = hh * 8 + k
                    P.tr(pT[hh][:, k, :], xe[i][:, kk * 128:(kk + 1) * 128], ident[:, :], [f"xe{i}", "ident"], [f"pT{hh}"])
                if hh == 0:
                    P.add("act", lambda e, i=i: e.copy(xT[i][:, 0:8, :], pT[0][:, :, :]), ["pT0"], [f"xT{i}"])
                else:
                    P.add("dve", lambda e, i=i: e.tensor_copy(xT[i][:, 8:16, :], pT[1][:, :, :]), ["pT1"], [f"xT{i}"])
            for k in range(16):
                P.mm(pg[:, :], xT[i][:, k, :], wg[i][:, k, :], k == 0, k == 15, [f"xT{i}", f"wg{i}"], ["pg"])
            for k in range(16):
                P.mm(pu[:, :], xT[i][:, k, :], wu[i][:, k, :], k == 0, k == 15, [f"xT{i}", f"wu{i}"], ["pu"])
            _act(P, sg[:, :], pg[:, :], AF.Silu, ["pg"], ["sg"])
            _tt(P, hm[:, :], sg[:, :], pu[:, :], ALU.mult, ["sg", "pu"], ["hm"])
            for k in range(4):
                P.tr(pT[0][:, k, :], hm[:, k * 128:(k + 1) * 128], ident[:, :], ["hm", "ident"], ["pT0"])
            P.add("act", lambda e: e.copy(hT[:, :, :], pT[0][:, 0:4, :]), ["pT0"], ["hT"])
            for cc in range(4):
                j = n % 2
                n += 1
                cs = slice(cc * 512, (cc + 1) * 512)
                for k in range(4):
                    P.mm(py[j][:, :], hT[:, k, :], wd[i][:, k, cs], k == 0, k == 3, ["hT", f"wd{i}"], [f"py{j}"])
                if cc % 2 == 0:
                    P.add("act", lambda e, i=i, j=j, cs=cs: e.copy(ye[i][:, cs], py[j][:, :]), [f"py{j}"], [f"ye{i}"])
                else:
                    P.add("dve", lambda e, i=i, j=j, cs=cs: e.tensor_copy(ye[i][:, cs], py[j][:, :]), [f"py{j}"], [f"ye{i}"])
            P.dma("sp", sc["Yg"][e_ * 128:(e_ + 1) * 128, :], ye[i][:, :], [f"ye{i}"], ["Yg"])
        P.emit(st)


def phase_E(nc, io, sc):
    with ExitStack() as st:
        C = Ctx(nc, st, "E")
        P = C.P
        breg = {}
        g2 = C.sb("g2", [128, 2048], F32)
        b2 = C.sb("b2", [128, 2048], F32)
        P.dma("sp", g2[:, :], io["ln2_g"], [], ["g2"])
        P.dma("sp", b2[:, :], io["ln2_b"], [], ["b2"])
        y1 = [C.sb(f"y1_{i}", [128, 2048], F32) for i in range(2)]
        y2 = [C.sb(f"y2_{i}", [128, 2048], F32) for i in range(2)]
        h1 = [C.sb(f"h1_{i}", [128, 2048], F32) for i in range(2)]
        ridx = [C.sb(f"ridx{i}", [128, 2], I32) for i in range(2)]
        rw = [C.sb(f"rw{i}", [128, 2], F32) for i in range(2)]
        st6 = [C.sb(f"st6_{i}", [128, 24], F32) for i in range(2)]
        mv = [C.sb(f"mv{i}", [128, 8], F32) for i in range(2)]
        for i in range(2):
            P.add("pool", lambda e, i=i: e.memset(mv[i][:, 4:5], LN_EPS), [], [f"mv{i}"])

        def part1(tt):
            i = tt % 2
            ts_ = slice(tt * 128, (tt + 1) * 128)
            P.dma("sp", ridx[i][:, :], sc["Ridx"][ts_, :], ["Ridx"], [f"ridx{i}"])
            P.dma("sp", rw[i][:, :], sc["Rw"][ts_, :], ["Rw"], [f"rw{i}"])
            P.dma("sp", h1[i][:, :], sc["H1"][ts_, :], ["H1"], [f"h1_{i}"])
            P.add("pool", lambda e: e.memset(y1[i][:, :], 0.0), [], [f"y1_{i}"])
            P.add("pool", lambda e: e.memset(y2[i][:, :], 0.0), [], [f"y2_{i}"])
            for kk, yb, yk in ((0, y1[i], f"y1_{i}"), (1, y2[i], f"y2_{i}")):
                P.add("pool", lambda e, kk=kk, yb=yb: e.indirect_dma_start(
                    out=yb[:, :], out_offset=None, in_=sc["Yg"][:, :],
                    in_offset=bass.IndirectOffsetOnAxis(ap=ridx[i][:, kk:kk + 1], axis=0),
                    bounds_check=_breg(e, breg), oob_is_err=False), [f"ridx{i}", "Yg", yk], [yk + "g"], dma=True)
            y1k = [f"y1_{i}", f"y1_{i}g"]
            y2k = [f"y2_{i}", f"y2_{i}g"]
            _ts(P, y1[i][:, :], y1[i][:, :], rw[i][:, 0:1], None, ALU.mult, None, y1k + [f"rw{i}"], [f"y1_{i}"])
            _stt(P, y1[i][:, :], y2[i][:, :], rw[i][:, 1:2], y1[i][:, :], ALU.mult, ALU.add, y1k + y2k + [f"rw{i}"], [f"y1_{i}"])
            _stt(P, h1[i][:, :], h1[i][:, :], DN_ALPHA, y1[i][:, :], ALU.mult, ALU.add, [f"h1_{i}"] + y1k, [f"h1_{i}"])
            for c in range(4):
                P.add("dve", lambda e, c=c: e.bn_stats(st6[i][:, c * 6:(c + 1) * 6], h1[i][:, c * 512:(c + 1) * 512]),
                      [f"h1_{i}"], [f"st6_{i}"])
            P.add("dve", lambda e: e.bn_aggr(mv[i][:, 0:2], st6[i][:, 0:24]), [f"st6_{i}"], [f"mv{i}"])
            _act(P, mv[i][:, 2:3], mv[i][:, 1:2], AF.Sqrt, [f"mv{i}"], [f"mv{i}"], bias=mv[i][:, 4:5])
            P.add("dve", lambda e: e.reciprocal(mv[i][:, 3:4], mv[i][:, 2:3]), [f"mv{i}"], [f"mv{i}"])

        def part2(tt):
            i = tt % 2
            ts_ = slice(tt * 128, (tt + 1) * 128)
            _ts(P, y2[i][:, :], h1[i][:, :], mv[i][:, 0:1], mv[i][:, 3:4], ALU.subtract, ALU.mult,
                [f"h1_{i}", f"mv{i}", f"y2_{i}g"], [f"y2_{i}"])
            _tt(P, y2[i][:, :], y2[i][:, :], g2[:, :], ALU.mult, [f"y2_{i}", "g2"], [f"y2_{i}"])
            _tt(P, y1[i][:, :], y2[i][:, :], b2[:, :], ALU.add, [f"y2_{i}", "b2", f"y1_{i}g"], [f"y1_{i}"])
            P.dma("sp", sc["out"][ts_, :], y1[i][:, :], [f"y1_{i}"], ["out"])

        part1(0)
        for tt in range(16):
            if tt + 1 < 16:
                part1(tt + 1)
            part2(tt)
        P.emit(st)


EXPERTS = None
_IN_C = {
    "identf": ([128, 128], F32), "w_nsa_proj": ([1024, 2048], F32), "w_pool_proj": ([1024, 2048], F32),
    "w_out": ([2048, 2048], F32), "w_router": ([2048, 72], F32), "b_router": ([128, 72], F32),
    "eid64": ([128, 64], F32), "utri": ([128, 128], BF16), "ln1_g": ([128, 2048], F32), "ln1_b": ([128, 2048], F32),
    "ln2_g": ([128, 2048], F32), "ln2_b": ([128, 2048], F32), "x_own": ([NT, 2048], F32),
    "w_gate": ([NEXP, 2048, 512], F32), "w_up": ([NEXP, 2048, 512], F32), "w_down": ([NEXP, 512, 2048], F32),
}
_SC_C = {
    "ZT": ([16, 128, NT], BF16), "H1": ([NT, 2048], F32), "Xg": ([NEXP * CAP, 2048], BF16),
    "Yg": ([NEXP * CAP, 2048], F32), "Ridx": ([NT, 2], I32), "Rw": ([NT, 2], F32),
}


def _shared_inputs(inp):
    w_in = inp["w_in"][0]
    ca = np.ascontiguousarray
    m = {
        "ident": _bf16(np.eye(128)), "identf": np.eye(128, dtype=np.float32),
        "w_A1": ca(np.concatenate([w_in[:, 2560:3072], w_in[:, 2048:2560]], 1)),
        "w_A2": ca(w_in[:, 3072:3584]), "w_q": ca(w_in[:, 1024:2048]), "w_gn": ca(w_in[:, 3584:3632]),
        "w_pool": ca(w_in[:, 0:1024]), "w_gm": ca(w_in[:, 3632:7728]),
        "pool_mix": ca(inp["pool_mix"][0]), "pool_scale": ca(inp["pool_scale"][0].reshape(8, 128).T),
        "cmp_k_w1": ca(inp["cmp_k_w1"][0]), "cmp_v_w1": ca(inp["cmp_v_w1"][0]),
        "cmp_k_w2": ca(inp["cmp_k_w2"][0]), "cmp_v_w2": ca(inp["cmp_v_w2"][0]),
        "cmp_k_w2s": ca(np.concatenate([inp["cmp_k_w2"][0][:, 32:], inp["cmp_k_w2"][0][:, :32]], 1)),
        "cmp_pos_kT": ca(inp["cmp_pos_k"][0].T), "cmp_pos_vT": ca(inp["cmp_pos_v"][0].T),
        "w_nsa_proj": ca(inp["w_nsa_proj"][0]), "w_pool_proj": ca(inp["w_pool_proj"][0]), "w_out": ca(inp["w_out"][0]),
        "w_router": ca(np.concatenate([inp["router_group_w"][0],
                                       inp["router_expert_w"][0].transpose(1, 0, 2).reshape(D, 64)], 1)),
        "b_router": ca(np.broadcast_to(np.concatenate([inp["router_group_b"][0], inp["router_expert_b"][0].reshape(64)])[None, :], (128, 72))),
        "eid64": ca(np.broadcast_to(np.arange(64, dtype=np.float32)[None, :], (128, 64))),
        "utri": _bf16((np.arange(128)[:, None] < np.arange(128)[None, :]).astype(np.float32)),
        "w_gate": ca(inp["w_gate"][0]), "w_up": ca(inp["w_up"][0]), "w_down": ca(inp["w_down"][0]),
    }
    for n in ("ln1_g", "ln1_b", "ln2_g", "ln2_b"):
        m[n] = ca(np.broadcast_to(inp[n][0][None, :], (128, D)))
    return m


def _core_inputs(inp, shared, core, tabs):
    b, p = core // 2, core % 2
    t = tabs[p]
    x0 = inp["x"][b]
    xo = x0[t["own_pos"]]
    xp = x0[t["prev_pos"]] * t["prev_valid"][:, None]
    m = dict(shared)
    m["xTg"] = np.ascontiguousarray(x0.T)
    m["xTo"] = np.ascontiguousarray(xo.T)
    m["xTp"] = np.ascontiguousarray(xp.T)
    m["x_own"] = np.ascontiguousarray(xo)
    for k in ALL_IN:
        if k not in m:
            m[k] = t[k]
    return m


ALL_IN = {}
ALL_IN.update(_IN_A)
ALL_IN.update(_IN_B)
ALL_IN.update(_IN_C)
ALL_SC = {}
ALL_SC.update(_SC_A)
ALL_SC.update(_SC_C)
PHASES = [phase_A, phase_B, phase_C1, phase_C2, phase_D, phase_E]


def build_full():
    return build_nc(ALL_IN, ALL_SC, PHASES, final_out=("out", [NT, D], F32))


def kernel(**inputs):
    inp = {k: np.asarray(v) for k, v in inputs.items()}
    tabs = []
    for p in range(2):
        t = _core_tables(p)
        tabs.append(_core_tables_B(p, t))
    shared = _shared_inputs(inp)
    in_maps = [_core_inputs(inp, shared, c, tabs) for c in range(8)]
    nc = build_full()
    res = run_bass_kernel_spmd(nc, in_maps, core_ids=list(range(8)))
    out = np.zeros((4, S, D), np.float32)
    for c in range(8):
        b, p = c // 2, c % 2
        out[b, tabs[p]["own_pos"]] = np.asarray(res.results[c]["out"]).astype(np.float32)
    return out
```

```python
import numpy as np
import concourse.bass as bass
import concourse.mybir as mybir
from concourse.bass_utils import run_bass_kernel_spmd
from contextlib import ExitStack

F32 = mybir.dt.float32
BF16 = mybir.dt.bfloat16
I32 = mybir.dt.int32
U32 = mybir.dt.uint32
AF = mybir.ActivationFunctionType
ALU = mybir.AluOpType
AX = mybir.AxisListType

D = 2048
S = 4096
NT = 2048
HD = 64
NH = 16
NG = 4
NCMP = 255
NEG = -30000.0
OWN = ([0, 3, 4, 7], [1, 2, 5, 6])
DN_ALPHA = 2.0 ** 0.25
LN_EPS = 1e-5
NEXP = 64
CAP = 128
NROW = NEXP * 2 * CAP
NCORES = 8

SHARED = ("Xg", "Yg")
DEBUG = []
STOP_AFTER = None
GROUPS = None
PARTS = None


def _on(name):
    return PARTS is None or name in PARTS


class _Op:
    __slots__ = ("eng", "fn", "dma", "deps", "idx", "marked", "count", "sem", "semval", "gid")


class Phase:
    ENGS = ("pe", "act", "dve", "pool", "sp")
    NDMA = 6

    def __init__(self, nc, name):
        self.nc = nc
        self.name = name
        self.ops = {e: [] for e in self.ENGS}
        self.bufs = {}
        self.excl = set()
        self.nops = 0

    def _buf(self, k):
        b = self.bufs.get(k)
        if b is None:
            b = [[], []]
            self.bufs[k] = b
        return b

    def add(self, eng, fn, reads=(), writes=(), dma=False):
        op = _Op()
        op.eng, op.fn, op.dma = eng, fn, dma
        op.idx = len(self.ops[eng])
        op.marked = False
        op.gid = self.nops
        self.nops += 1
        deps = set()
        for k in reads:
            b = self._buf(k)
            deps.update(b[0])
            if k in self.excl:
                for r in b[1]:
                    if r.eng != eng:
                        deps.add(r)
            b[1].append(op)
        for k in writes:
            b = self._buf(k)
            if dma and b[0] and not b[1] and all(w.dma for w in b[0]):
                b[0].append(op)
            else:
                deps.update(b[0])
                deps.update(b[1])
                b[1] = []
                b[0] = [op]
        deps.discard(op)
        op.deps = deps
        self.ops[eng].append(op)
        return op

    def mm(self, out, lhsT, rhs, start, stop, reads, writes, skip=False):
        if skip:
            return self.add("pe", lambda e: e.matmul(out, lhsT, rhs, start=start, stop=stop, skip_group_check=True), reads, writes)
        return self.add("pe", lambda e: e.matmul(out, lhsT, rhs, start=start, stop=stop), reads, writes)

    def tr(self, out, in_, ident, reads, writes):
        return self.add("pe", lambda e: e.transpose(out, in_, ident), reads, writes)

    def dma(self, q, out, in_, reads, writes):
        return self.add(q, lambda e: e.dma_start(out=out, in_=in_), reads, writes, dma=True)

    def emit(self, stack):
        nc = self.nc
        engs = {"pe": nc.tensor, "act": nc.scalar, "dve": nc.vector, "pool": nc.gpsimd, "sp": nc.sync}
        for e in self.ENGS:
            for op in self.ops[e]:
                for d in op.deps:
                    if d.dma:
                        continue
                    if d.eng == "pe" and op.eng == "pe" and not op.dma:
                        continue
                    d.marked = True
        esem = {e: nc.alloc_semaphore(name=f"{self.name}_{e}") for e in self.ENGS}
        dsem = {}
        for e in self.ENGS:
            cnt = 0
            nd = 0
            for op in self.ops[e]:
                if op.dma:
                    if e not in dsem:
                        dsem[e] = [nc.alloc_semaphore(name=f"{self.name}_{e}_d{i}") for i in range(self.NDMA)]
                    op.sem = dsem[e][nd % self.NDMA]
                    op.semval = 16 * (nd // self.NDMA + 1)
                    nd += 1
                elif op.marked:
                    cnt += 1
                    op.count = cnt
            assert cnt < 60000, (self.name, e, cnt)
        block = stack.enter_context(nc.Block())

        def body(ename):
            def _(eng):
                seen = {}

                def wait(sem, val):
                    k = id(sem)
                    if seen.get(k, 0) >= val:
                        return
                    seen[k] = val
                    eng.wait_ge(sem, val)

                for op in self.ops[ename]:
                    for d in sorted(op.deps, key=lambda o: o.gid):
                        if d.dma:
                            wait(d.sem, d.semval)
                        elif d.eng == "pe" and ename == "pe" and not op.dma:
                            continue
                        else:
                            wait(esem[d.eng], d.count)
                    if op.dma:
                        if op.semval > 16:
                            wait(op.sem, op.semval - 16)
                        op.fn(eng).then_inc(op.sem, 16)
                    else:
                        ins = op.fn(eng)
                        if op.marked:
                            ins.then_inc(esem[ename], 1)
                last = {}
                for op in self.ops[ename]:
                    if op.dma:
                        last[id(op.sem)] = (op.sem, op.semval)
                for sem, val in last.values():
                    wait(sem, val)
            return _

        for ename, reg in (("pe", block.tensor), ("act", block.scalar), ("dve", block.vector),
                           ("pool", block.gpsimd), ("sp", block.sync)):
            if self.ops[ename]:
                reg(body(ename))


class Ctx:
    def __init__(self, nc, stack, name):
        self.nc, self.stack, self.name = nc, stack, name
        self.P = Phase(nc, name)

    def sb(self, name, shape, dt):
        return self.stack.enter_context(self.nc.sbuf_tensor(f"{self.name}_{name}", list(shape), dt))

    def ps(self, name, shape, dt):
        self.P.excl.add(name)
        return self.stack.enter_context(self.nc.psum_tensor(f"{self.name}_{name}", list(shape), dt))


def _dram(nc, name, shape, dt, kind):
    return nc.dram_tensor(name, list(shape), dt, kind=kind).ap()


def _scratch(nc, name, shape, dt):
    kind = "ExternalOutput" if name in DEBUG else "Internal"
    if name in SHARED:
        return nc.dram_tensor(name, list(shape), dt, kind="Internal", addr_space="Shared").ap()
    return _dram(nc, name, shape, dt, kind)


def _rope(P, dst, src, cos, sin, nh, tmp, rkeys, wkeys, tag):
    s3 = src.rearrange("p (h d) -> p h d", h=nh)
    d3 = dst.rearrange("p (h d) -> p h d", h=nh)
    cb = cos.unsqueeze(1).broadcast_to([128, nh, 32])
    sn = sin.unsqueeze(1).broadcast_to([128, nh, 32])
    t1 = tmp[:, 0, 0:nh, :]
    t2 = tmp[:, 1, 0:nh, :]
    q1, q2 = s3[:, :, 0:32], s3[:, :, 32:64]
    tk = [tag + "_t1", tag + "_t2"]
    P.add("dve", lambda e: e.tensor_tensor(t1, q1, cb, ALU.mult), rkeys, [tk[0]])
    P.add("dve", lambda e: e.tensor_tensor(t2, q2, sn, ALU.mult), rkeys, [tk[1]])
    P.add("dve", lambda e: e.tensor_tensor(d3[:, :, 0:32], t1, t2, ALU.subtract), tk, wkeys)
    P.add("dve", lambda e: e.tensor_tensor(t1, q2, cb, ALU.mult), rkeys, [tk[0]])
    P.add("dve", lambda e: e.tensor_tensor(t2, q1, sn, ALU.mult), rkeys, [tk[1]])
    P.add("dve", lambda e: e.tensor_tensor(d3[:, :, 32:64], t1, t2, ALU.add), tk, wkeys)


def phase_A(nc, io, sc):
    with ExitStack() as st:
        C = Ctx(nc, st, "A")
        P = C.P
        ident = C.sb("ident", [128, 128], BF16)
        P.dma("sp", ident[:, :], io["ident"], [], ["ident"])
        wk = [C.sb(f"w{i}", [128, 16, 512], BF16) for i in range(2)]
        xt = [C.sb(f"xt{i}", [128, 16, 512], BF16) for i in range(2)]
        xo = C.sb("xo", [128, 16, NT], BF16)
        xh = C.sb("xh", [128, 16, 64], BF16)
        cosg = C.sb("cosg", [128, 32, 32], F32)
        sing = C.sb("sing", [128, 32, 32], F32)
        cosp = C.sb("cosp", [128, 16, 32], F32)
        sinp = C.sb("sinp", [128, 16, 32], F32)
        coso = C.sb("coso", [128, 16, 32], F32)
        sino = C.sb("sino", [128, 16, 32], F32)
        cosq = C.sb("cosq", [128, 16, 32], F32)
        sinq = C.sb("sinq", [128, 16, 32], F32)
        for t, n in ((cosg, "cosg"), (sing, "sing"), (cosp, "cosp"), (sinp, "sinp"), (coso, "coso"),
                     (sino, "sino"), (cosq, "cosq"), (sinq, "sinq")):
            P.dma("sp", t[:, :, :], io[n], [], [n])
        rtmp = C.sb("rtmp", [128, 2, 8, 32], F32)
        ksr = [C.sb(f"ksr{i}", [128, 512], BF16) for i in range(2)]
        kcv = [C.sb(f"kcv{i}", [128, 512], BF16) for i in range(2)]
        stT = [C.sb(f"stT{i}", [128, 6, 512], BF16) for i in range(2)]
        vst = [C.sb(f"vst{i}", [128, 4, 256], BF16) for i in range(2)]
        pm = [C.ps(f"pm{i}", [128, 512], F32) for i in range(4)]
        pT = [C.ps(f"pT{i}", [128, 8, 128], BF16) for i in range(2)]

        zeros = C.sb("zeros", [128, 2048], BF16)
        P.add("pool", lambda e: e.memset(zeros[:, :], 0.0), [], ["zeros"])
        for e_ in range(NROW // 128):
            P.dma("sp", sc["Xg"][e_ * 128:(e_ + 1) * 128, :], zeros[:, :], ["zeros"], ["Xg"])
        wq = ["pool", "sp"]
        nw = [0]
        nx = [0]
        npm = [0]
        npt = [0]

        def load_w(src_ap, ncols=512):
            i = nw[0] % 2
            nw[0] += 1
            P.dma("pool", wk[i][:, :, 0:ncols], src_ap.rearrange("(k p) n -> p k n", p=128), [], [f"w{i}"])
            return i

        def load_x(src_ap):
            i = nx[0] % 2
            nx[0] += 1
            P.dma("pool", xt[i][:, :, :], src_ap.rearrange("(k p) n -> p k n", p=128), [], [f"xt{i}"])
            return i

        def proj(xtile_ap, xkey, wi, ncols=512, c0=0):
            b = npm[0] % 4
            npm[0] += 1
            for k in range(16):
                P.mm(pm[b][:, 0:ncols], xtile_ap(k), wk[wi][:, k, c0:c0 + ncols], k == 0, k == 15,
                     [xkey, f"w{wi}"], [f"pm{b}"])
            return b

        def transposes(srcs, dst, dstkey, tcol):
            b = npt[0] % 2
            npt[0] += 1
            n = len(srcs)
            for i, (ap, key) in enumerate(srcs):
                P.tr(pT[b][:, i, :], ap, ident[:, :], [key, "ident"], [f"pT{b}"])
            P.add("act", lambda e: e.copy(dst[:, 0:n, tcol * 128:(tcol + 1) * 128], pT[b][:, 0:n, :]),
                  [f"pT{b}"], [dstkey])

        pending = []

        def flush():
            for f in pending:
                f()
            del pending[:]

        w_ks = load_w(io["w_A1"][:, 0:512])
        w_kc = load_w(io["w_A1"][:, 512:1024])
        for ch in (range(8) if _on("A1") else []):
            xi = load_x(io["xTg"][:, ch * 512:(ch + 1) * 512])
            si = ch % 2
            for j in range(4):
                tt = ch * 4 + j
                xa = (lambda xi, j: (lambda k: xt[xi][:, k, j * 128:(j + 1) * 128]))(xi, j)
                b0 = proj(xa, f"xt{xi}", w_ks)
                b1 = proj(xa, f"xt{xi}", w_kc)
                flush()
                r = tt % 2
                _rope(P, ksr[r][:, 0:256], pm[b0][:, 0:256], cosg[:, tt, :], sing[:, tt, :], 4, rtmp,
                      [f"pm{b0}", "cosg", "sing"], [f"ksr{r}"], "rA")
                P.add("act", lambda e, si=si, j=j, b0=b0: e.copy(vst[si][:, j, :], pm[b0][:, 256:512]),
                      [f"pm{b0}"], [f"vst{si}"])
                P.add("act", lambda e, r=r, b1=b1: e.copy(kcv[r][:, :], pm[b1][:, :]), [f"pm{b1}"], [f"kcv{r}"])
                srcs = [(ksr[r][:, 0:128], f"ksr{r}"), (ksr[r][:, 128:256], f"ksr{r}")]
                srcs += [(kcv[r][:, i * 128:(i + 1) * 128], f"kcv{r}") for i in range(4)]
                pending.append(lambda srcs=srcs, si=si, j=j: transposes(srcs, stT[si], f"stT{si}", j))
            pending.append(lambda si=si, ch=ch: P.dma(
                "sp", sc["KT_A1"].rearrange("i p t -> p i t")[:, :, ch * 512:(ch + 1) * 512], stT[si][:, :, :],
                [f"stT{si}"], ["KT_A1"]))
            pending.append(lambda si=si, ch=ch: P.dma(
                "sp", sc["Vs"].rearrange("(c j p) n -> c p j n", j=4, p=128)[ch], vst[si][:, :, :],
                [f"vst{si}"], ["Vs"]))
        flush()

        w_kw = load_w(io["w_A2"])
        def load_own():
            for ch in range(4):
                P.dma("pool", xo[:, :, ch * 512:(ch + 1) * 512],
                      io["xTo"][:, ch * 512:(ch + 1) * 512].rearrange("(k p) n -> p k n", p=128), [], ["xo"])
            for lc in range(4):
                P.dma("pool", xh[:, :, lc * 16:(lc + 1) * 16],
                      io["xTp"][:, lc * 512 + 496:lc * 512 + 512].rearrange("(k p) t -> p k t", p=128), [], ["xh"])

        for which in (("prev", "own") if _on("A2") else ()):
            cosw, sinw = (cosp, sinp) if which == "prev" else (coso, sino)
            cn, sn_ = ("cosp", "sinp") if which == "prev" else ("coso", "sino")
            for ch in range(4):
                si = ch % 2
                if which == "prev":
                    xi = load_x(io["xTp"][:, ch * 512:(ch + 1) * 512])
                    if ch == 0:
                        load_own()
                for j in range(4):
                    tt = ch * 4 + j
                    if which == "prev":
                        xa = (lambda xi, j: (lambda k: xt[xi][:, k, j * 128:(j + 1) * 128]))(xi, j)
                        xkey = f"xt{xi}"
                    else:
                        xa = (lambda tt: (lambda k: xo[:, k, tt * 128:(tt + 1) * 128]))(tt)
                        xkey = "xo"
                    b0 = proj(xa, xkey, w_kw)
                    flush()
                    r = tt % 2
                    _rope(P, ksr[r][:, 0:256], pm[b0][:, 0:256], cosw[:, tt, :], sinw[:, tt, :], 4, rtmp,
                          [f"pm{b0}", cn, sn_], [f"ksr{r}"], "rA")
                    P.add("act", lambda e, si=si, j=j, b0=b0: e.copy(vst[si][:, j, :], pm[b0][:, 256:512]),
                          [f"pm{b0}"], [f"vst{si}"])
                    srcs = [(ksr[r][:, 0:128], f"ksr{r}"), (ksr[r][:, 128:256], f"ksr{r}")]
                    pending.append(lambda srcs=srcs, si=si, j=j: transposes(srcs, stT[si], f"stT{si}", j))
                pending.append(lambda si=si, ch=ch, which=which: P.dma(
                    "sp", sc["KwT_" + which].rearrange("i p t -> p i t")[:, :, ch * 512:(ch + 1) * 512],
                    stT[si][:, 0:2, :], [f"stT{si}"], ["KwT_" + which]))
                pending.append(lambda si=si, ch=ch, which=which: P.dma(
                    "sp", sc["Vw_" + which].rearrange("(c j p) n -> c p j n", j=4, p=128)[ch], vst[si][:, :, :],
                    [f"vst{si}"], ["Vw_" + which]))
        flush()

        for qc in (range(2) if _on("Q") else []):
            wi = load_w(io["w_q"][:, qc * 512:(qc + 1) * 512])
            for ch in range(4):
                si = ch % 2
                for j in range(4):
                    tt = ch * 4 + j
                    xa = (lambda tt: (lambda k: xo[:, k, tt * 128:(tt + 1) * 128]))(tt)
                    b0 = proj(xa, "xo", wi)
                    flush()
                    r = tt % 2
                    _rope(P, ksr[r][:, :], pm[b0][:, :], cosq[:, tt, :], sinq[:, tt, :], 8, rtmp,
                          [f"pm{b0}", "cosq", "sinq"], [f"ksr{r}"], "rA")
                    srcs = [(ksr[r][:, i * 128:(i + 1) * 128], f"ksr{r}") for i in range(4)]
                    pending.append(lambda srcs=srcs, si=si, j=j: transposes(srcs, stT[si], f"stT{si}", j))
                pending.append(lambda si=si, ch=ch, qc=qc: P.dma(
                    "sp", sc["QT"][qc * 4:(qc + 1) * 4].rearrange("i p t -> p i t")[:, :, ch * 512:(ch + 1) * 512],
                    stT[si][:, 0:4, :], [f"stT{si}"], ["QT"]))
        flush()

        gst = C.sb("gst", [128, 16, 48], F32)
        wi = load_w(io["w_gn"], 48)
        for tt in (range(16) if _on("GN") else []):
            xa = (lambda tt: (lambda k: xo[:, k, tt * 128:(tt + 1) * 128]))(tt)
            b0 = proj(xa, "xo", wi, 48)
            P.add("act", lambda e, tt=tt, b0=b0: e.activation(gst[:, tt, :], pm[b0][:, 0:48], AF.Sigmoid),
                  [f"pm{b0}"], ["gst"])
        P.dma("sp", sc["Gn"].rearrange("(t p) n -> p t n", p=128), gst[:, :, :], ["gst"], ["Gn"])

        gms = [C.sb(f"gms{i}", [128, 512], BF16) for i in range(2)]
        ng = 0
        for gc in (range(8) if _on("GM") else []):
            wi = load_w(io["w_gm"][:, gc * 512:(gc + 1) * 512])
            for tt in range(16):
                xa = (lambda tt: (lambda k: xo[:, k, tt * 128:(tt + 1) * 128]))(tt)
                b0 = proj(xa, "xo", wi)
                gi = ng % 2
                ng += 1
                P.add("act", lambda e, gi=gi, b0=b0: e.activation(gms[gi][:, :], pm[b0][:, :], AF.Sigmoid),
                      [f"pm{b0}"], [f"gms{gi}"])
                P.dma("sp", sc["Gm"][tt * 128:(tt + 1) * 128, gc * 512:(gc + 1) * 512], gms[gi][:, :],
                      [f"gms{gi}"], ["Gm"])

        pmix = C.sb("pmix", [128, 4, 2, 256], BF16)
        P.dma("pool", pmix[:, :, :, :], io["pool_mix"].rearrange("g (c p) d -> p g c d", p=128), [], ["pmix"])
        pscale = C.sb("pscale", [128, 8], F32)
        P.dma("sp", pscale[:, :], io["pool_scale"], [], ["pscale"])
        rc16 = C.sb("rc16", [128, 4, 4, 16], F32)
        P.dma("sp", rc16[:, :, :, :], io["rc16"], [], ["rc16"])
        U = [C.sb(f"U{i}", [128, 528], F32) for i in range(2)]
        Wa = C.sb("Wa", [128, 528], F32)
        Wb = C.sb("Wb", [128, 528], F32)
        pld = [C.sb(f"pld{i}", [128, 2, 512], BF16) for i in range(2)]
        mxs = [C.sb(f"mxs{i}", [128, 512], BF16) for i in range(2)]
        phalo = C.ps("phalo", [128, 16], F32)
        nu = 0
        nmx = 0
        for g in (range(4) if _on("POOL") else []):
            win = (2, 4, 8, 16)[g]
            wis = [load_w(io["w_pool"][:, (2 * g + c2) * 128:(2 * g + c2 + 1) * 128], 128) for c2 in range(2)]
            for lc in range(4):
                pi = (g * 4 + lc) % 2
                for c2 in range(2):
                    wi = wis[c2]
                    b = npm[0] % 4
                    npm[0] += 1
                    for k in range(16):
                        P.mm(pm[b][:, :], wk[wi][:, k, 0:128], xo[:, k, lc * 512:(lc + 1) * 512], k == 0, k == 15,
                             ["xo", f"w{wi}"], [f"pm{b}"])
                    for k in range(16):
                        P.mm(phalo[:, :], wk[wi][:, k, 0:128], xh[:, k, lc * 16:(lc + 1) * 16], k == 0, k == 15,
                             ["xh", f"w{wi}"], ["phalo"])
                    ui = nu % 2
                    nu += 1
                    Ut = U[ui]
                    uk = f"U{ui}"
                    P.add("act", lambda e, Ut=Ut, b=b: e.copy(Ut[:, 16:528], pm[b][:, :]), [f"pm{b}"], [uk])
                    P.add("act", lambda e, Ut=Ut: e.copy(Ut[:, 0:16], phalo[:, :]), ["phalo"], [uk])
                    src, sk = Ut, uk
                    step = 1
                    dsts = [(Wa, "Wa"), (Wb, "Wb")]
                    di = 0
                    while step < win:
                        dt_, dk = dsts[di % 2]
                        di += 1
                        lo = 2 * step - 1
                        P.add("dve", lambda e, dt_=dt_, src=src, lo=lo, step=step: e.tensor_tensor(
                            dt_[:, lo:528], src[:, lo:528], src[:, lo - step:528 - step], ALU.add), [sk], [dk])
                        src, sk = dt_, dk
                        step *= 2
                    P.add("dve", lambda e, src=src, Ut=Ut, pi=pi, c2=c2, win=win: e.scalar_tensor_tensor(
                        pld[pi][:, c2, 16:512], src[:, 32:528], 1.0 / win, Ut[:, 32:528], ALU.mult, ALU.subtract),
                        [sk, uk], [f"pld{pi}"])
                    P.add("dve", lambda e, src=src, lc=lc, g=g: e.tensor_tensor(
                        Wa[:, 0:16] if src is not Wa else Wb[:, 0:16], src[:, 16:32], rc16[:, lc, g, :], ALU.mult),
                        [sk, "rc16"], ["Wa" if src is not Wa else "Wb"])
                    P.add("dve", lambda e, src=src, Ut=Ut, pi=pi, c2=c2: e.tensor_tensor(
                        pld[pi][:, c2, 0:16], Wa[:, 0:16] if src is not Wa else Wb[:, 0:16], Ut[:, 16:32], ALU.subtract),
                        ["Wa" if src is not Wa else "Wb", uk], [f"pld{pi}"])
                for d2 in range(2):
                    b = npm[0] % 4
                    npm[0] += 1
                    for c2 in range(2):
                        P.mm(pm[b][:, :], pmix[:, g, c2, d2 * 128:(d2 + 1) * 128], pld[pi][:, c2, :], c2 == 0, c2 == 1,
                             ["pmix", f"pld{pi}"], [f"pm{b}"])
                    mi = nmx % 2
                    nmx += 1
                    ct = 2 * g + d2
                    P.add("dve", lambda e, mi=mi, b=b, ct=ct: e.tensor_scalar(
                        mxs[mi][:, :], pm[b][:, :], pscale[:, ct:ct + 1], None, ALU.mult), [f"pm{b}", "pscale"], [f"mxs{mi}"])
                    P.dma("sp", sc["MixT"][ct * 128:(ct + 1) * 128, lc * 512:(lc + 1) * 512], mxs[mi][:, :],
                          [f"mxs{mi}"], ["MixT"])
        P.emit(st)


def _bf16(a):
    import ml_dtypes
    return np.asarray(a, dtype=np.float32).astype(ml_dtypes.bfloat16)


def _rope_tab(pos, scale=1.0):
    half = HD // 2
    inv = (10000.0 ** (-2.0 * np.arange(half, dtype=np.float32) / HD)).astype(np.float32)
    ang = pos.astype(np.float32)[:, None] * inv[None, :]
    c = (np.cos(ang).astype(np.float32) * np.float32(scale)).astype(np.float32)
    s = (np.sin(ang).astype(np.float32) * np.float32(scale)).astype(np.float32)
    n = pos.shape[0] // 128
    c = np.ascontiguousarray(c.reshape(n, 128, half).transpose(1, 0, 2))
    s = np.ascontiguousarray(s.reshape(n, 128, half).transpose(1, 0, 2))
    return c, s


def _core_tables(p):
    t = {}
    own_pos = np.concatenate([np.arange(512 * g, 512 * g + 512) for g in OWN[p]])
    prev_pos = np.concatenate([np.arange(512 * (g - 1), 512 * g) if g > 0 else np.zeros(512, np.int64) for g in OWN[p]])
    prev_valid = np.concatenate([np.full(512, 1.0 if g > 0 else 0.0, np.float32) for g in OWN[p]])
    t["own_pos"], t["prev_pos"], t["prev_valid"] = own_pos, prev_pos, prev_valid
    t["cosg"], t["sing"] = _rope_tab(np.arange(S))
    t["cosp"], t["sinp"] = _rope_tab(prev_pos)
    t["coso"], t["sino"] = _rope_tab(own_pos)
    t["cosq"], t["sinq"] = _rope_tab(own_pos, HD ** -0.5)
    rc = np.zeros((128, 4, 4, 16), np.float32)
    for lc, g in enumerate(OWN[p]):
        for gi, w in enumerate((2, 4, 8, 16)):
            for tt in range(16):
                rc[:, lc, gi, tt] = 1.0 / (min(tt + 1, w) if g == 0 else w)
    t["rc16"] = rc
    return t


_IN_A = {
    "ident": ([128, 128], BF16), "xTg": ([D, S], F32), "xTo": ([D, NT], F32), "xTp": ([D, NT], F32),
    "w_A1": ([D, 1024], F32), "w_A2": ([D, 512], F32), "w_q": ([D, 1024], F32), "w_gn": ([D, 48], F32),
    "w_pool": ([D, 1024], F32), "w_gm": ([D, 4096], F32), "pool_mix": ([4, 256, 256], F32),
    "pool_scale": ([128, 8], F32), "rc16": ([128, 4, 4, 16], F32),
    "cosg": ([128, 32, 32], F32), "sing": ([128, 32, 32], F32), "cosp": ([128, 16, 32], F32),
    "sinp": ([128, 16, 32], F32), "coso": ([128, 16, 32], F32), "sino": ([128, 16, 32], F32),
    "cosq": ([128, 16, 32], F32), "sinq": ([128, 16, 32], F32),
}
_SC_A = {
    "KT_A1": ([6, 128, S], BF16), "Vs": ([S, 256], BF16), "KwT_prev": ([2, 128, NT], BF16),
    "KwT_own": ([2, 128, NT], BF16), "Vw_prev": ([NT, 256], BF16), "Vw_own": ([NT, 256], BF16),
    "QT": ([8, 128, NT], BF16), "Gn": ([NT, 48], F32), "Gm": ([NT, 4096], BF16), "MixT": ([1024, NT], BF16),
    "OT": ([8, 128, NT], BF16),
}


def build_nc(in_specs, sc_specs, phases, final_out=None, ncores=NCORES):
    nc = bass.Bass("TRN2", target_bir_lowering=False, num_devices=ncores)
    io = {n: _dram(nc, n, shp, dt, "ExternalInput") for n, (shp, dt) in in_specs.items()}
    sc = {n: _scratch(nc, n, shp, dt) for n, (shp, dt) in sc_specs.items()}
    if final_out is not None:
        n, shp, dt = final_out
        sc[n] = _dram(nc, n, shp, dt, "ExternalOutput")
    for ph in phases:
        if ph == "core_barrier":
            nc.all_core_barrier()
            continue
        snap = nc.snapshot_sems()
        ph(nc, io, sc)
        nc.clear_and_free_semaphores(nc.allocated_since(snap))
        nc.all_engine_barrier()
    return nc


def _ts(P, out, in0, s1, s2, op0, op1, reads, writes, eng="dve"):
    if op1 is None:
        return P.add(eng, lambda e: e.tensor_scalar(out, in0, s1, None, op0), reads, writes)
    return P.add(eng, lambda e: e.tensor_scalar(out, in0, s1, s2, op0, op1), reads, writes)


def _tt(P, out, in0, in1, op, reads, writes, eng="dve"):
    return P.add(eng, lambda e: e.tensor_tensor(out, in0, in1, op), reads, writes)


def _stt(P, out, in0, sc_, in1, op0, op1, reads, writes):
    return P.add("dve", lambda e: e.scalar_tensor_tensor(out, in0, sc_, in1, op0, op1), reads, writes)


def _act(P, out, in_, func, reads, writes, bias=None, scale=None):
    kw = {}
    if bias is not None:
        kw["bias"] = bias
    if scale is not None:
        kw["scale"] = scale
    return P.add("act", lambda e: e.activation(out, in_, func, **kw), reads, writes)


def phase_B(nc, io, sc):
    with ExitStack() as st:
        C = Ctx(nc, st, "B")
        P = C.P
        ident = C.sb("ident", [128, 128], BF16)
        P.dma("sp", ident[:, :], io["ident"], [], ["ident"])
        Ksp = C.sb("Ksp", [128, S], BF16)
        P.dma("sp", Ksp[64:128, :], io["Eoh"], [], ["Ksp_e"])
        Qp = C.sb("Qp", [128, 4, NT], BF16)
        Vsp = C.sb("Vsp", [128, 32, 65], BF16)
        Kwp = C.sb("Kwp", [64, 4, 8, 128], BF16)
        Vwp = C.sb("Vwp", [128, 4, 8, 65], BF16)
        pvo = C.sb("pvo", [128, 16], BF16)
        P.dma("sp", pvo[:, :], io["pvones"], [], ["pvo"])
        P.add("pool", lambda e: e.memset(Vsp[:, :, 64:65], 1.0), [], ["Vsp_1"])
        P.add("pool", lambda e: e.memset(Vwp[:, :, 4:8, 64:65], 1.0), [], ["Vwp_1"])
        P.add("pool", lambda e: e.tensor_copy(Vwp[:, :, 0:4, 64:65].rearrange("p a b c -> p a (b c)"),
                                              pvo[:, :].rearrange("p (a b) -> p a b", a=4)), ["pvo"], ["Vwp_1"])
        KcT = C.sb("KcT", [64, S], BF16)
        VcT = C.sb("VcT", [64, S], BF16)
        w1 = [C.sb(f"w1_{i}", [64, 32, 256], BF16) for i in range(2)]
        w2 = C.sb("w2", [128, 3, 2, 64], BF16)
        posT = C.sb("posT", [64, 2, 32], BF16)
        P.dma("pool", w1[0][:, :, :], io["cmp_k_w1"].rearrange("(l d) j -> d l j", d=64), [], ["w1_0"])
        P.dma("pool", w1[1][:, :, :], io["cmp_v_w1"].rearrange("(l d) j -> d l j", d=64), [], ["w1_1"])
        for i, n in enumerate(("cmp_k_w2", "cmp_k_w2s", "cmp_v_w2")):
            P.dma("pool", w2[:, i, :, :], io[n].rearrange("(t p) d -> p t d", p=128), [], ["w2"])
        P.dma("pool", posT[:, 0, :], io["cmp_pos_kT"], [], ["posT"])
        P.dma("pool", posT[:, 1, :], io["cmp_pos_vT"], [], ["posT"])
        ccos = C.sb("ccos", [64, 256], F32)
        csin = C.sb("csin", [64, 256], F32)
        P.dma("sp", ccos[:, :], io["ccos"], [], ["ccos"])
        P.dma("sp", csin[:, :], io["csin"], [], ["csin"])
        cbias = C.sb("cbias", [128, 2, 2, 512], BF16)
        tritab = C.sb("tritab", [128, 4, 8, 128], BF16)
        P.dma("sp", tritab[:, :, :, :], io["tritab"], [], ["tritab"])
        tri2 = C.sb("tri2", [128, 2, 128], BF16)
        P.dma("sp", tri2[:, :, :], io["tri2"], [], ["tri2"])
        Vcp = C.sb("Vcp", [128, 2, 129], BF16)
        P.add("pool", lambda e: e.memset(Vcp[:, :, 0:65], 0.0), [], ["Vcp"])
        P.add("pool", lambda e: e.memset(Vcp[:, :, 64:65], 1.0), [], ["Vcp"])
        P.dma("sp", Vcp[:, :, 65:129], io["ovl"], [], ["Vcp_o"])
        KcmpT = C.sb("KcmpT", [64, 256], BF16)
        P.add("pool", lambda e: e.memset(KcmpT[:, :], 0.0), [], ["KcmpT"])
        Mk = C.sb("Mk", [128, 16, 64], F32)
        Ad = C.sb("Ad", [128, 16, 64], F32)
        Fm = C.sb("Fm", [128, 16, 64], F32)
        for t, n in ((Mk, "Mk"), (Ad, "Ad"), (Fm, "Fm")):
            P.dma("sp", t[:, :, :], io[n], [], [n])
        Gn = C.sb("Gn", [128, 16, 48], F32)
        P.dma("sp", Gn[:, :, :], sc["Gn"].rearrange("(t p) n -> p t n", p=128), [], ["Gn"])
        O = C.sb("O", [128, 16, 256], F32)
        Ob = [C.sb(f"Ob{i}", [128, 256], BF16) for i in range(2)]
        Os = [C.sb(f"Os{i}", [128, 2, 128], BF16) for i in range(2)]
        Pt = [C.sb(f"Pt{i}", [128, 512], BF16) for i in range(4)]
        Pc = [C.sb(f"Pc{i}", [128, 2, 512], BF16) for i in range(2)]
        Pw = [C.sb(f"Pw{i}", [128, 5, 128], BF16) for i in range(3)]
        hb = C.sb("hb", [128, 256], F32)
        sq = C.sb("sq", [128, 256], F32)
        uu = C.sb("uu", [128, 256], F32)
        sg = C.sb("sg", [128, 256], F32)
        gT = C.sb("gT", [128, 2, 2, 256], BF16)
        cb = C.sb("cb", [128, 4], F32)
        impb = C.sb("impb", [128, 4, 64], F32)
        impm = C.sb("impm", [128, 64], F32)
        imp2 = C.sb("imp2", [128, 64], F32)
        m8 = C.sb("m8", [128, 16], F32)
        nsel = C.sb("nsel", [128, 64], F32)
        NegT = C.sb("NegT", [128, 4, 128], BF16)
        P.add("pool", lambda e: e.memset(NegT[:, :, :], 0.0), [], [f"NegT{i}" for i in range(4)])
        sm = C.sb("sm", [128, 16, 4], F32)
        nsm = [0]
        t1 = C.sb("t1", [64, 256], F32)
        t2 = C.sb("t2", [64, 256], F32)
        pS = [C.ps(f"pS{i}", [128, 512], F32) for i in range(4)]
        pA = [C.ps(f"pA{i}", [128, 512], F32) for i in range(2)]
        pX = C.ps("pX", [128, 512], F32)
        pTb = C.ps("pTb", [128, 8, 128], BF16)

        for kv in range(2):
            for jt in range(2):
                for l in range(32):
                    P.mm(pX[:, 0:1], w1[kv][:, l, jt * 128:(jt + 1) * 128], posT[:, kv, l:l + 1], l == 0, l == 31,
                         [f"w1_{kv}", "posT"], ["pX"])
                i = kv * 2 + jt
                P.add("dve", lambda e, i=i: e.tensor_copy(cb[:, i:i + 1], pX[:, 0:1]), ["pX"], ["cb"])

        npt = [0]
        npc = [0]
        npw = [0]
        nps = [0]

        for g in (range(4) if GROUPS is None else GROUPS):
            pi, hf = g // 2, g % 2
            rows = slice(hf * 64, hf * 64 + 64)
            P.dma("sp", Ksp[0:64, :], sc["KT_A1"][pi, rows, :], ["KT_A1"], ["Ksp_k"])
            P.dma("sp", KcT[:, :], sc["KT_A1"][2 + pi, rows, :], ["KT_A1"], ["KcT"])
            P.dma("sp", VcT[:, :], sc["KT_A1"][4 + pi, rows, :], ["KT_A1"], ["VcT"])
            P.dma("sp", Vsp[:, :, 0:64], sc["Vs"][:, g * 64:(g + 1) * 64].rearrange("(t p) d -> p t d", p=128),
                  ["Vs"], ["Vsp_v"])
            for lc in range(4):
                P.dma("sp", Kwp[:, lc, 0:4, :], sc["KwT_prev"][pi, rows, lc * 512:(lc + 1) * 512], ["KwT_prev"], ["Kwp"])
                P.dma("sp", Kwp[:, lc, 4:8, :], sc["KwT_own"][pi, rows, lc * 512:(lc + 1) * 512], ["KwT_own"], ["Kwp"])
                P.dma("sp", Vwp[:, lc, 0:4, 0:64],
                      sc["Vw_prev"][lc * 512:(lc + 1) * 512, g * 64:(g + 1) * 64].rearrange("(t p) d -> p t d", p=128),
                      ["Vw_prev"], ["Vwp_v"])
                P.dma("sp", Vwp[:, lc, 4:8, 0:64],
                      sc["Vw_own"][lc * 512:(lc + 1) * 512, g * 64:(g + 1) * 64].rearrange("(t p) d -> p t d", p=128),
                      ["Vw_own"], ["Vwp_v"])
            for hh in range(4):
                h = 4 * g + hh
                P.dma("sp", Qp[0:64, hh, :], sc["QT"][h // 2, (h % 2) * 64:(h % 2) * 64 + 64, :], ["QT"], ["Qp_q"])

            for kv, src, skey in ((0, KcT, "KcT"), (1, VcT, "VcT")):
                for jt in range(2):
                    for l in range(32):
                        P.mm(pX[:, 0:255], w1[kv][:, l, jt * 128:(jt + 1) * 128], src[:, l:l + 16 * 254 + 1:16],
                             l == 0, l == 31, [f"w1_{kv}", skey], ["pX"])
                    i = kv * 2 + jt
                    _act(P, sq[:, 0:255], pX[:, 0:255], AF.Square, ["pX", "cb"], ["sq"], bias=cb[:, i:i + 1])
                    _ts(P, hb[:, 0:255], pX[:, 0:255], cb[:, i:i + 1], None, ALU.add, None, ["pX", "cb"], ["hb"])
                    _ts(P, uu[:, 0:255], sq[:, 0:255], 0.044715, 1.0, ALU.mult, ALU.add, ["sq"], ["uu"])
                    _tt(P, uu[:, 0:255], uu[:, 0:255], hb[:, 0:255], ALU.mult, ["uu", "hb"], ["uu"])
                    _act(P, sg[:, 0:255], uu[:, 0:255], AF.Sigmoid, ["uu"], ["sg"], scale=1.5957691216057308)
                    _tt(P, gT[:, kv, jt, 0:255], hb[:, 0:255], sg[:, 0:255], ALU.mult, ["hb", "sg"], ["gT"])
            for jt in range(2):
                P.mm(pX[0:64, 0:255], w2[:, 0, jt, :], gT[:, 0, jt, 0:255], jt == 0, jt == 1, ["w2", "gT"], ["pX"])
            _tt(P, t1[:, 0:255], pX[0:64, 0:255], ccos[:, 0:255], ALU.mult, ["pX", "ccos"], ["t1"])
            for jt in range(2):
                P.mm(pX[0:64, 0:255], w2[:, 1, jt, :], gT[:, 0, jt, 0:255], jt == 0, jt == 1, ["w2", "gT"], ["pX"])
            _tt(P, t2[:, 0:255], pX[0:64, 0:255], csin[:, 0:255], ALU.mult, ["pX", "csin"], ["t2"])
            _tt(P, KcmpT[:, 0:255], t1[:, 0:255], t2[:, 0:255], ALU.add, ["t1", "t2"], ["KcmpT"])
            for ct in range(2):
                n = 128 if ct == 0 else 127
                for jt in range(2):
                    P.mm(pX[0:n, 0:64], gT[:, 1, jt, ct * 128:ct * 128 + n], w2[:, 2, jt, :], jt == 0, jt == 1,
                         ["gT", "w2"], ["pX"])
                P.add("dve", lambda e, ct=ct, n=n: e.tensor_copy(Vcp[0:n, ct, 0:64], pX[0:n, 0:64]), ["pX"], ["Vcp"])

            for lc in range(4):
                cbi = lc % 2
                P.dma("sp", cbias[:, :, cbi, :], io["cmpbias"][:, :, lc * 512:(lc + 1) * 512], [], [f"cbias{cbi}"])
                qsl = slice(lc * 512, (lc + 1) * 512)

                def norm(acc_ap, acc_key, tt, hh, gcol, first):
                    si = nsm[0] % 16
                    nsm[0] += 1
                    sk = f"sm{si}"
                    _ts(P, sm[:, si, 0:1], acc_ap[:, 64:65], 1e-30, None, ALU.max, None, [acc_key], [sk])
                    P.add("dve", lambda e: e.reciprocal(sm[:, si, 1:2], sm[:, si, 0:1]), [sk], [sk])
                    _tt(P, sm[:, si, 2:3], sm[:, si, 1:2], Gn[:, tt, gcol:gcol + 1], ALU.mult, [sk, "Gn"], [sk])
                    osl = O[:, tt, hh * 64:(hh + 1) * 64]
                    if first:
                        _ts(P, osl, acc_ap[:, 0:64], sm[:, si, 2:3], None, ALU.mult, None, [acc_key, sk], [f"O{tt}"])
                    else:
                        _stt(P, osl, acc_ap[:, 0:64], sm[:, si, 2:3], osl, ALU.mult, ALU.add, [acc_key, sk, f"O{tt}"], [f"O{tt}"])
                    return sm[:, si, 1:2], sk

                b1banks = {}

                def b1_qk(hh):
                    bs = []
                    for ct in range(2):
                        b = nps[0] % 2
                        nps[0] += 1
                        P.mm(pS[b][:, :], KcmpT[:, ct * 128:(ct + 1) * 128], Qp[0:64, hh, qsl], True, False,
                             ["KcmpT", "Qp_q"], [f"pS{b}"])
                        P.mm(pS[b][:, :], ident[:, :], cbias[:, ct, cbi, :], False, True,
                             ["ident", f"cbias{cbi}"], [f"pS{b}"])
                        bs.append(b)
                    b1banks[hh] = bs

                def b1_rest(hh):
                    h = 4 * g + hh
                    pci = npc[0] % 2
                    npc[0] += 1
                    for ct in range(2):
                        b = b1banks[hh][ct]
                        _act(P, Pc[pci][:, ct, :], pS[b][:, :], AF.Exp, [f"pS{b}"], [f"Pc{pci}"])
                    accs = (pA if hh % 2 == 0 else pS[2:4])
                    akeys = (["pA0", "pA1"] if hh % 2 == 0 else ["pS2", "pS3"])
                    for qs in range(4):
                        acc = accs[qs // 2][:, (qs % 2) * 256:(qs % 2) * 256 + 129]
                        ak = akeys[qs // 2]
                        for ct in range(2):
                            P.mm(acc, Pc[pci][:, ct, qs * 128:(qs + 1) * 128], Vcp[:, ct, :], ct == 0, ct == 1,
                                 [f"Pc{pci}", "Vcp", "Vcp_o"], [ak])
                    for qs in range(4):
                        tt = lc * 4 + qs
                        acc = accs[qs // 2][:, (qs % 2) * 256:(qs % 2) * 256 + 129]
                        ak = akeys[qs // 2]
                        rz, sk = norm(acc, ak, tt, hh, h, True)
                        if hh == 0:
                            _ts(P, impb[:, qs, :], acc[:, 65:129], rz, None, ALU.mult, None, [ak, sk], [f"impb{qs}"])
                        else:
                            _stt(P, impb[:, qs, :], acc[:, 65:129], rz, impb[:, qs, :], ALU.mult, ALU.add,
                                 [ak, sk, f"impb{qs}"], [f"impb{qs}"])

                for hh in range(4):
                    b1_qk(hh)
                    b1_rest(hh)

                for qs in range(4):
                    tt = lc * 4 + qs
                    iq = impb[:, qs, :]
                    _tt(P, impm[:, :], iq, Mk[:, tt, :], ALU.mult, [f"impb{qs}", "Mk"], ["impm"])
                    _tt(P, impm[:, :], impm[:, :], Ad[:, tt, :], ALU.add, ["impm", "Ad"], ["impm"])
                    P.add("dve", lambda e: e.max(m8[:, 0:8], impm[:, :]), ["impm"], ["m8"])
                    P.add("dve", lambda e: e.match_replace(imp2[:, :], m8[:, 0:8], impm[:, :], -3.0e6), ["impm", "m8"], ["imp2"])
                    P.add("dve", lambda e: e.max(m8[:, 8:16], imp2[:, :]), ["imp2"], ["m8"])
                    _ts(P, nsel[:, :], impm[:, :], m8[:, 15:16], -NEG, ALU.is_ge, ALU.mult, ["impm", "m8"], ["nsel"])
                    _stt(P, NegT[:, qs, 64:128], nsel[:, :], NEG, Fm[:, tt, :], ALU.add, ALU.add, ["nsel", "Fm"], [f"NegT{qs}"])

                wunits = [(hh, qs) for hh in range(4) for qs in range(4)]
                wbanks = {}

                def b3_qk(u):
                    hh, qs = wunits[u]
                    tt = lc * 4 + qs
                    b = nps[0] % 4
                    b2 = (nps[0] + 1) % 4
                    nps[0] += 2
                    qap = Qp[0:64, hh, tt * 128:(tt + 1) * 128]
                    for r in range(qs, qs + 4):
                        o = pS[b][:, (r - qs) * 128:(r - qs + 1) * 128]
                        P.mm(o, Kwp[:, lc, r, :], qap, True, r != qs, ["Kwp", "Qp_q"], [f"pS{b}"])
                        if r == qs:
                            P.mm(o, ident[:, :], tri2[:, 0, :], False, True, ["ident", "tri2"], [f"pS{b}"])
                    P.mm(pS[b2][:, 0:128], Kwp[:, lc, qs + 4, :], qap, True, False, ["Kwp", "Qp_q"], [f"pS{b2}"])
                    P.mm(pS[b2][:, 0:128], ident[:, :], tri2[:, 1, :], False, True, ["ident", "tri2"], [f"pS{b2}"])
                    wbanks[u] = (b, b2)

                def b3_rest(u):
                    hh, qs = wunits[u]
                    h = 4 * g + hh
                    tt = lc * 4 + qs
                    b, b2 = wbanks[u]
                    pwi = npw[0] % 3
                    npw[0] += 1
                    _act(P, Pw[pwi][:, 0:4, :], pS[b][:, :].rearrange("p (a b) -> p a b", a=4), AF.Exp, [f"pS{b}"], [f"Pw{pwi}"])
                    _act(P, Pw[pwi][:, 4, :], pS[b2][:, 0:128], AF.Exp, [f"pS{b2}"], [f"Pw{pwi}"])
                    ai = u % 2
                    acc = pA[ai][:, 0:65]
                    for r in range(5):
                        P.mm(acc, Pw[pwi][:, r, :], Vwp[:, lc, qs + r, :], r == 0, r == 4,
                             [f"Pw{pwi}", "Vwp_v", "Vwp_1"], [f"pA{ai}"])
                    norm(acc, f"pA{ai}", tt, hh, 32 + h, False)

                b3_qk(0)
                for u in range(16):
                    if u + 1 < 16:
                        b3_qk(u + 1)
                    b3_rest(u)

                for qs in range(4):
                    tt = lc * 4 + qs
                    P.tr(pTb[:, 4 + qs, :], NegT[:, qs, :], ident[:, :], [f"NegT{qs}", "ident"], ["pTb"])
                    P.add("act", lambda e, tt=tt, qs=qs: e.copy(
                        Qp[64:128, :, tt * 128:(tt + 1) * 128],
                        pTb[64:128, 4 + qs:5 + qs, :].broadcast_to([64, 4, 128])), ["pTb"], ["Qp_m"])

                E = 8 * (lc + 1)
                sunits = [(hh, kt) for hh in range(4) for kt in range(E)]
                sbanks = {}

                def b2_qk(u):
                    hh, kt = sunits[u]
                    b = nps[0] % 4
                    nps[0] += 1
                    s = kt - (E - 8)
                    P.mm(pS[b][:, :], Ksp[:, kt * 128:(kt + 1) * 128], Qp[:, hh, qsl], True, s < 0,
                         ["Ksp_k", "Ksp_e", "Qp_q", "Qp_m"], [f"pS{b}"])
                    if s >= 0:
                        qd = s % 4
                        P.mm(pS[b][:, qd * 128:(qd + 1) * 128], ident[:, :], tritab[:, lc, s, :], False, True,
                             ["ident", "tritab"], [f"pS{b}"])
                    sbanks[u] = b

                def b2_rest(u):
                    hh, kt = sunits[u]
                    h = 4 * g + hh
                    b = sbanks[u]
                    pti = npt[0] % 4
                    npt[0] += 1
                    _act(P, Pt[pti][:, :], pS[b][:, :], AF.Exp, [f"pS{b}"], [f"Pt{pti}"])
                    ai = hh % 2
                    for qs in range(4):
                        P.mm(pA[ai][:, qs * 128:qs * 128 + 65], Pt[pti][:, qs * 128:(qs + 1) * 128], Vsp[:, kt, :],
                             kt == 0 and qs == 0, kt == E - 1, [f"Pt{pti}", "Vsp_v", "Vsp_1"], [f"pA{ai}"], skip=True)
                    if kt == E - 1:
                        for qs in range(4):
                            norm(pA[ai][:, qs * 128:qs * 128 + 65], f"pA{ai}", lc * 4 + qs, hh, 16 + h, False)

                LA = 2
                nsu = len(sunits)
                for u in range(min(LA, nsu)):
                    b2_qk(u)
                for u in range(nsu):
                    if u + LA < nsu:
                        b2_qk(u + LA)
                    b2_rest(u)

            for tt in range(16):
                oi = tt % 2
                P.add("act", lambda e, oi=oi, tt=tt: e.copy(Ob[oi][:, :], O[:, tt, :]), [f"O{tt}"], [f"Ob{oi}"])
                for i in range(2):
                    P.tr(pTb[:, 2 + i, :], Ob[oi][:, i * 128:(i + 1) * 128], ident[:, :], [f"Ob{oi}", "ident"], ["pTb"])
                P.add("dve", lambda e, oi=oi: e.tensor_copy(Os[oi][:, :, :], pTb[:, 2:4, :]), ["pTb"], [f"Os{oi}"])
                P.dma("sp", sc["OT"][2 * g:2 * g + 2].rearrange("i p t -> p i t")[:, :, tt * 128:(tt + 1) * 128],
                      Os[oi][:, :, :], [f"Os{oi}"], ["OT"])
        P.emit(st)


def _core_tables_B(p, t):
    own_pos = t["own_pos"]
    c = np.arange(256)
    cend = 16 * c + 31
    valid = (c[:, None] <= 254) & (cend[:, None] <= own_pos[None, :])
    cb = np.where(valid, 0.0, NEG).astype(np.float32).reshape(2, 128, NT).transpose(1, 0, 2)
    t["cmpbias"] = _bf16(np.ascontiguousarray(cb))
    k = np.arange(128)
    tri = np.where(k[:, None] > k[None, :], NEG, 0.0).astype(np.float32)
    tt = np.zeros((128, 4, 8, 128), np.float32)
    for lc, gc in enumerate(OWN[p]):
        E = 8 * (lc + 1)
        for s in range(8):
            kt = E - 8 + s
            if 4 * gc <= kt < 4 * gc + 4:
                assert (kt - 4 * gc) == s % 4
                tt[:, lc, s, :] = tri
    t["tritab"] = _bf16(tt)
    tri2 = np.zeros((128, 2, 128), np.float32)
    tri2[:, 0, :] = np.where(k[:, None] <= k[None, :], NEG, 0.0)
    tri2[:, 1, :] = np.where(k[:, None] > k[None, :], NEG, 0.0)
    t["tri2"] = _bf16(tri2)
    cs = np.arange(256) * 16
    ss = np.arange(64) * 64
    ov = ((cs[:, None] + 31 >= ss[None, :]) & (cs[:, None] <= ss[None, :] + 63) & (c[:, None] <= 254)).astype(np.float32)
    t["ovl"] = _bf16(np.ascontiguousarray(ov.reshape(2, 128, 64).transpose(1, 0, 2)))
    cur = own_pos // 64
    j = np.arange(64)
    forced = (j[None, :] == 0) | (j[None, :] == cur[:, None]) | (j[None, :] == cur[:, None] - 1)
    future = j[None, :] > cur[:, None]
    mk = (~(forced | future)).astype(np.float32)
    ad = np.where(forced, 1e6, np.where(future, -1e6, 0.0)).astype(np.float32)
    fm = np.where(future, NEG, 0.0).astype(np.float32)
    lay = lambda a: np.ascontiguousarray(a.reshape(16, 128, 64).transpose(1, 0, 2))
    t["Mk"], t["Ad"], t["Fm"] = lay(mk), lay(ad), lay(fm)
    t["pvones"] = _bf16(np.ascontiguousarray(t["prev_valid"].reshape(16, 128).T))
    t["Eoh"] = _bf16((np.arange(S)[None, :] // 64 == j[:, None]).astype(np.float32))
    half = HD // 2
    inv = (10000.0 ** (-2.0 * np.arange(half, dtype=np.float32) / HD)).astype(np.float32)
    ang = cend.astype(np.float32)[None, :] * np.concatenate([inv, inv])[:, None]
    t["ccos"] = np.cos(ang).astype(np.float32)
    sn = np.sin(ang).astype(np.float32)
    sn[:half] *= -1.0
    t["csin"] = sn
    return t


_IN_B = {
    "Eoh": ([64, S], BF16), "pvones": ([128, 16], BF16), "cmp_k_w1": ([2048, 256], F32), "cmp_v_w1": ([2048, 256], F32),
    "cmp_k_w2": ([256, 64], F32), "cmp_k_w2s": ([256, 64], F32), "cmp_v_w2": ([256, 64], F32),
    "cmp_pos_kT": ([64, 32], F32), "cmp_pos_vT": ([64, 32], F32), "ccos": ([64, 256], F32), "csin": ([64, 256], F32),
    "tritab": ([128, 4, 8, 128], BF16), "tri2": ([128, 2, 128], BF16), "ovl": ([128, 2, 64], BF16),
    "cmpbias": ([128, 2, NT], BF16), "Mk": ([128, 16, 64], F32), "Ad": ([128, 16, 64], F32), "Fm": ([128, 16, 64], F32),
}


def phase_C1(nc, io, sc):
    with ExitStack() as st:
        C = Ctx(nc, st, "C1")
        P = C.P
        ident = C.sb("ident", [128, 128], BF16)
        P.dma("sp", ident[:, :], io["ident"], [], ["ident"])
        wn = C.sb("wn", [128, 8, 2048], BF16)
        wp = C.sb("wp", [128, 8, 2048], BF16)
        for c in range(4):
            cs = slice(c * 512, (c + 1) * 512)
            P.dma("pool", wn[:, :, cs], io["w_nsa_proj"][:, cs].rearrange("(k p) n -> p k n", p=128), [], ["wn"])
            P.dma("pool", wp[:, :, cs], io["w_pool_proj"][:, cs].rearrange("(k p) n -> p k n", p=128), [], ["wp"])
        oT = [C.sb(f"oT{i}", [128, 8, 128], BF16) for i in range(2)]
        mT = [C.sb(f"mT{i}", [128, 8, 128], BF16) for i in range(2)]
        gm = [C.sb(f"gm{i}", [128, 4096], BF16) for i in range(2)]
        ta = [C.sb(f"ta{i}", [128, 512], F32) for i in range(2)]
        tb = [C.sb(f"tb{i}", [128, 512], F32) for i in range(2)]
        z = [C.sb(f"z{i}", [128, 2048], BF16) for i in range(2)]
        zT = [C.sb(f"zT{i}", [128, 16, 128], BF16) for i in range(2)]
        pa = [C.ps(f"pa{i}", [128, 512], F32) for i in range(2)]
        pb = [C.ps(f"pb{i}", [128, 512], F32) for i in range(2)]
        pT = [C.ps(f"pT{i}", [128, 8, 128], BF16) for i in range(2)]
        n = [0]

        def c1_stage1(tt):
            i = tt % 2
            ts_ = slice(tt * 128, (tt + 1) * 128)
            P.dma("sp", oT[i][:, :, :], sc["OT"].rearrange("k p t -> p k t")[:, :, ts_], ["OT"], [f"oT{i}"])
            P.dma("sp", mT[i][:, :, :], sc["MixT"].rearrange("(k p) t -> p k t", p=128)[:, :, ts_], ["MixT"], [f"mT{i}"])
            P.dma("sp", gm[i][:, :], sc["Gm"][ts_, :], ["Gm"], [f"gm{i}"])
            for cc in range(4):
                j = n[0] % 2
                n[0] += 1
                cs = slice(cc * 512, (cc + 1) * 512)
                for k in range(8):
                    P.mm(pa[j][:, :], oT[i][:, k, :], wn[:, k, cs], k == 0, k == 7, [f"oT{i}", "wn"], [f"pa{j}"])
                for k in range(8):
                    P.mm(pb[j][:, :], mT[i][:, k, :], wp[:, k, cs], k == 0, k == 7, [f"mT{i}", "wp"], [f"pb{j}"])
                _tt(P, ta[j][:, :], pa[j][:, :], gm[i][:, 2048 + cc * 512:2048 + (cc + 1) * 512], ALU.mult,
                    [f"pa{j}", f"gm{i}"], [f"ta{j}"])
                _tt(P, tb[j][:, :], pb[j][:, :], gm[i][:, cs], ALU.mult, [f"pb{j}", f"gm{i}"], [f"tb{j}"])
                _tt(P, z[i][:, cs], ta[j][:, :], tb[j][:, :], ALU.add, [f"ta{j}", f"tb{j}"], [f"z{i}"], eng="pool")

        def c1_stage2(tt):
            i = tt % 2
            ts_ = slice(tt * 128, (tt + 1) * 128)
            for hh in range(2):
                for k in range(8):
                    kk = hh * 8 + k
                    P.tr(pT[hh][:, k, :], z[i][:, kk * 128:(kk + 1) * 128], ident[:, :], [f"z{i}", "ident"], [f"pT{hh}"])
                P.add("act", lambda e, hh=hh: e.copy(zT[i][:, hh * 8:(hh + 1) * 8, :], pT[hh][:, :, :]),
                      [f"pT{hh}"], [f"zT{i}"])
            P.dma("sp", sc["ZT"].rearrange("k p t -> p k t")[:, :, ts_], zT[i][:, :, :], [f"zT{i}"], ["ZT"])

        c1_stage1(0)
        for tt in range(16):
            if tt + 1 < 16:
                c1_stage1(tt + 1)
            c1_stage2(tt)
        P.emit(st)


def _layer_norm(P, dst, src, skey, dkey, g_bc, b_bc, gkeys, st6, mv, tmp, tkeys):
    for c in range(4):
        P.add("dve", lambda e, c=c: e.bn_stats(st6[:, c * 6:(c + 1) * 6], src[:, c * 512:(c + 1) * 512]), [skey], [tkeys[0]])
    P.add("dve", lambda e: e.bn_aggr(mv[:, 0:2], st6[:, 0:24]), [tkeys[0]], [tkeys[1]])
    _act(P, mv[:, 2:3], mv[:, 1:2], AF.Sqrt, [tkeys[1]], [tkeys[1]], bias=mv[:, 4:5])
    P.add("dve", lambda e: e.reciprocal(mv[:, 3:4], mv[:, 2:3]), [tkeys[1]], [tkeys[1]])
    _ts(P, tmp[:, :], src[:, :], mv[:, 0:1], mv[:, 3:4], ALU.subtract, ALU.mult, [skey, tkeys[1]], [tkeys[2]])
    _tt(P, tmp[:, :], tmp[:, :], g_bc[:, :], ALU.mult, [tkeys[2], gkeys[0]], [tkeys[2]], eng="pool")
    _tt(P, dst[:, :], tmp[:, :], b_bc[:, :], ALU.add, [tkeys[2], gkeys[1]], [dkey])


def _breg(eng, cache):
    if "r" not in cache:
        cache["r"] = eng.to_reg(NROW - 1)
    return cache["r"]


def phase_C2(nc, io, sc):
    with ExitStack() as st:
        C = Ctx(nc, st, "C2")
        P = C.P
        breg = {}
        identf = C.sb("identf", [128, 128], F32)
        P.dma("sp", identf[:, :], io["identf"], [], ["identf"])
        wo = C.sb("wo", [128, 16, 2048], BF16)
        for c in range(4):
            cs = slice(c * 512, (c + 1) * 512)
            P.dma("pool", wo[:, :, cs], io["w_out"][:, cs].rearrange("(k p) n -> p k n", p=128), [], ["wo"])
        wr = C.sb("wr", [128, 16, 72], F32)
        P.dma("sp", wr[:, :, :], io["w_router"].rearrange("(k p) n -> p k n", p=128), [], ["wr"])
        br = C.sb("br", [128, 72], F32)
        P.dma("sp", br[:, :], io["b_router"], [], ["br"])
        eid64 = C.sb("eid64", [128, 64], F32)
        P.dma("sp", eid64[:, :], io["eid64"], [], ["eid64"])
        pbase = C.sb("pbase", [128, 1], F32)
        P.dma("sp", pbase[:, :], io["pbase"], [], ["pbase"])
        g1 = C.sb("g1", [128, 2048], F32)
        b1 = C.sb("b1", [128, 2048], F32)
        P.dma("sp", g1[:, :], io["ln1_g"], [], ["g1"])
        P.dma("sp", b1[:, :], io["ln1_b"], [], ["b1"])
        Ut = C.sb("Ut", [128, 128], BF16)
        P.dma("sp", Ut[:, :], io["utri"], [], ["Ut"])
        ones = C.sb("ones", [128, 128], BF16)
        P.add("pool", lambda e: e.memset(ones[:, :], 1.0), [], ["ones"])
        accind = C.sb("accind", [128, 64], F32)
        P.add("pool", lambda e: e.memset(accind[:, :], 0.0), [], ["accind"])
        zT = [C.sb(f"zT{i}", [128, 16, 128], BF16) for i in range(2)]
        xt = [C.sb(f"xt{i}", [128, 2048], F32) for i in range(2)]
        r = [C.sb(f"r{i}", [128, 2048], F32) for i in range(2)]
        tmp = C.sb("tmp", [128, 2048], F32)
        h1 = [C.sb(f"h1_{i}", [128, 2048], F32) for i in range(2)]
        h1b = [C.sb(f"h1b{i}", [128, 2048], BF16) for i in range(2)]
        h1T = C.sb("h1T", [128, 16, 128], F32)
        st6 = C.sb("st6", [128, 24], F32)
        mv = C.sb("mv", [128, 8], F32)
        P.add("pool", lambda e: e.memset(mv[:, 4:5], LN_EPS), [], ["mv"])
        lg = C.sb("lg", [128, 72], F32)
        rt = [C.sb(f"rt{i}", [128, 64], F32) for i in range(2)]
        e3 = C.sb("e3", [128, 8, 8], F32)
        E1 = [C.sb(f"E1_{i}", [128, 8, 8], F32) for i in range(2)]
        E2 = [C.sb(f"E2_{i}", [128, 8, 8], F32) for i in range(2)]
        indb = [C.sb(f"indb{i}", [128, 64], BF16) for i in range(2)]
        accb = [C.sb(f"accb{i}", [128, 64], BF16) for i in range(2)]
        posf = C.sb("posf", [128, 64], F32)
        indf = C.sb("indf", [128, 64], F32)
        ridx = [C.sb(f"ridx{i}", [128, 2], I32) for i in range(2)]
        rw = [C.sb(f"rw{i}", [128, 2], F32) for i in range(2)]
        py = [C.ps(f"py{i}", [128, 512], F32) for i in range(4)]
        pt = [C.ps(f"pt{i}", [128, 4, 128], F32) for i in range(2)]
        pl = C.ps("pl", [128, 72], F32)
        pp = C.ps("pp", [128, 64], F32)
        npt = [0]

        def stage1(tt):
            i = tt % 2
            ts_ = slice(tt * 128, (tt + 1) * 128)
            P.dma("sp", zT[i][:, :, :], sc["ZT"].rearrange("k p t -> p k t")[:, :, ts_], ["ZT"], [f"zT{i}"])
            P.dma("sp", xt[i][:, :], io["x_own"][ts_, :], [], [f"xt{i}"])
            for cc in range(4):
                cs = slice(cc * 512, (cc + 1) * 512)
                for k in range(16):
                    P.mm(py[cc][:, :], zT[i][:, k, :], wo[:, k, cs], k == 0, k == 15, [f"zT{i}", "wo"], [f"py{cc}"])

        def stage1b(tt):
            i = tt % 2
            for cc in range(4):
                cs = slice(cc * 512, (cc + 1) * 512)
                _stt(P, r[i][:, cs], xt[i][:, cs], DN_ALPHA, py[cc][:, :], ALU.mult, ALU.add, [f"xt{i}", f"py{cc}"], [f"r{i}"])

        def s_ln1(tt):
            i = tt % 2
            for c in range(4):
                P.add("dve", lambda e, c=c: e.bn_stats(st6[:, c * 6:(c + 1) * 6], r[i][:, c * 512:(c + 1) * 512]), [f"r{i}"], ["st6"])
            P.add("dve", lambda e: e.bn_aggr(mv[:, 0:2], st6[:, 0:24]), ["st6"], ["mv"])
            _act(P, mv[:, 2:3], mv[:, 1:2], AF.Sqrt, ["mv"], ["mv"], bias=mv[:, 4:5])

        def s_ln2(tt):
            i = tt % 2
            ts_ = slice(tt * 128, (tt + 1) * 128)
            P.add("dve", lambda e: e.reciprocal(mv[:, 3:4], mv[:, 2:3]), ["mv"], ["mv"])
            _ts(P, tmp[:, :], r[i][:, :], mv[:, 0:1], mv[:, 3:4], ALU.subtract, ALU.mult, [f"r{i}", "mv"], ["tmp"])
            _tt(P, tmp[:, :], tmp[:, :], g1[:, :], ALU.mult, ["tmp", "g1"], ["tmp"])
            _tt(P, h1[i][:, :], tmp[:, :], b1[:, :], ALU.add, ["tmp", "b1"], [f"h1_{i}"])
            P.dma("sp", sc["H1"][ts_, :], h1[i][:, :], [f"h1_{i}"], ["H1"])
            P.add("act", lambda e: e.copy(h1b[i][:, :], h1[i][:, :]), [f"h1_{i}"], [f"h1b{i}"])

        def stage2b(tt):
            i = tt % 2
            R = [f"rt{i}"]
            rt_ = rt[i]
            for k4 in range(4):
                j = npt[0] % 2
                npt[0] += 1
                for k in range(4):
                    kk = k4 * 4 + k
                    P.tr(pt[j][:, k, :], h1[i][:, kk * 128:(kk + 1) * 128], identf[:, :], [f"h1_{i}", "identf"], [f"pt{j}"])
                P.add("act", lambda e, j=j, k4=k4: e.copy(h1T[:, k4 * 4:(k4 + 1) * 4, :], pt[j][:, :, :]), [f"pt{j}"], ["h1T"])
            for k in range(16):
                P.mm(pl[:, :], h1T[:, k, :], wr[:, k, :], k == 0, k == 15, ["h1T", "wr"], ["pl"])
            _tt(P, lg[:, :], pl[:, :], br[:, :], ALU.add, ["pl", "br"], ["lg"])
            P.add("dve", lambda e: e.tensor_reduce(rt_[:, 0:1], lg[:, 0:8], AX.X, ALU.max), ["lg"], R)
            _ts(P, rt_[:, 8:16], lg[:, 0:8], rt_[:, 0:1], None, ALU.is_equal, None, ["lg"] + R, R)
            _ts(P, rt_[:, 1:2], rt_[:, 0:1], -1.0, None, ALU.mult, None, R, R)
            P.add("act", lambda e: e.activation(rt_[:, 16:24], lg[:, 0:8], AF.Exp, bias=rt_[:, 1:2], accum_out=rt_[:, 2:3]), ["lg"] + R, R)
            P.add("dve", lambda e: e.reciprocal(rt_[:, 3:4], rt_[:, 2:3]), R, R)
            _tt(P, e3[:, :, :], lg[:, 8:72].rearrange("p (g e) -> p g e", g=8),
                rt_[:, 8:16].unsqueeze(2).broadcast_to([128, 8, 8]), ALU.mult, ["lg"] + R, ["e3"])
            P.add("dve", lambda e: e.tensor_reduce(rt_[:, 24:32], e3[:, :, :].rearrange("p g e -> p e g"), AX.X, ALU.add), ["e3"], R)
            P.add("dve", lambda e: e.max(rt_[:, 32:40], rt_[:, 24:32]), R, R)
            _ts(P, rt_[:, 40:48], rt_[:, 24:32], rt_[:, 32:33], None, ALU.is_equal, None, R, R)
            _ts(P, rt_[:, 48:56], rt_[:, 24:32], rt_[:, 33:34], None, ALU.is_equal, None, R, R)
            _tt(P, rt_[:, 4:5], rt_[:, 32:33], rt_[:, 33:34], ALU.subtract, R, R)
            _act(P, rt_[:, 5:6], rt_[:, 4:5], AF.Sigmoid, R, R)
            _tt(P, rw[i][:, 0:1], rt_[:, 5:6], rt_[:, 3:4], ALU.mult, R, [f"rw{i}"])
            _tt(P, rw[i][:, 1:2], rt_[:, 3:4], rw[i][:, 0:1], ALU.subtract, R + [f"rw{i}"], [f"rw{i}"])
            gb = rt_[:, 8:16].unsqueeze(2).broadcast_to([128, 8, 8])
            _tt(P, E1[i][:, :, :], gb, rt_[:, 40:48].unsqueeze(1).broadcast_to([128, 8, 8]), ALU.mult, R, [f"E1_{i}"])
            _tt(P, E2[i][:, :, :], gb, rt_[:, 48:56].unsqueeze(1).broadcast_to([128, 8, 8]), ALU.mult, R, [f"E2_{i}"])
            E1f = E1[i][:, :, :].rearrange("p g e -> p (g e)")
            E2f = E2[i][:, :, :].rearrange("p g e -> p (g e)")
            _tt(P, indf[:, :], E1f, E2f, ALU.add, [f"E1_{i}", f"E2_{i}"], ["indf"])
            P.add("dve", lambda e: e.tensor_copy(indb[i][:, :], indf[:, :]), ["indf"], [f"indb{i}"])
            P.add("dve", lambda e: e.tensor_copy(accb[i][:, :], accind[:, :]), ["accind"], [f"accb{i}"])
            _tt(P, accind[:, :], accind[:, :], indf[:, :], ALU.add, ["accind", "indf"], ["accind"])

        def s_pos_pe(tt):
            i = tt % 2
            P.mm(pp[:, :], Ut[:, :], indb[i][:, :], True, False, ["Ut", f"indb{i}"], ["pp"])
            P.mm(pp[:, :], ones[:, :], accb[i][:, :], False, True, ["ones", f"accb{i}"], ["pp"])

        def stage3(tt):
            i = tt % 2
            ts_ = slice(tt * 128, (tt + 1) * 128)
            R = [f"rt{i}"]
            rt_ = rt[i]
            P.add("dve", lambda e: e.tensor_copy(posf[:, :], pp[:, :]), ["pp"], ["posf"])
            e3f = e3[:, :, :].rearrange("p g e -> p (g e)")
            for kk, (Eb, ek) in enumerate(((E1[i], f"E1_{i}"), (E2[i], f"E2_{i}"))):
                Ef = Eb[:, :, :].rearrange("p g e -> p (g e)")
                o0 = 56 + kk * 4
                _tt(P, e3f, Ef, posf[:, :], ALU.mult, [ek, "posf"], ["e3"])
                P.add("dve", lambda e, o0=o0: e.tensor_reduce(rt_[:, o0:o0 + 1], e3f, AX.X, ALU.add), ["e3"], R)
                _tt(P, e3f, Ef, eid64[:, :], ALU.mult, [ek, "eid64"], ["e3"])
                P.add("dve", lambda e, o0=o0: e.tensor_reduce(rt_[:, o0 + 1:o0 + 2], e3f, AX.X, ALU.add), ["e3"], R)
                _ts(P, rt_[:, o0 + 2:o0 + 3], rt_[:, o0:o0 + 1], float(CAP), 1.0e6, ALU.is_ge, ALU.mult, R, R)
                _stt(P, rt_[:, o0 + 3:o0 + 4], rt_[:, o0 + 1:o0 + 2], float(2 * CAP), rt_[:, o0:o0 + 1], ALU.mult, ALU.add, R, R)
                _tt(P, rt_[:, o0 + 3:o0 + 4], rt_[:, o0 + 3:o0 + 4], rt_[:, o0 + 2:o0 + 3], ALU.add, R, R)
                _tt(P, rt_[:, o0 + 3:o0 + 4], rt_[:, o0 + 3:o0 + 4], pbase[:, :], ALU.add, R + ["pbase"], R)
                P.add("dve", lambda e, kk=kk, o0=o0: e.tensor_copy(ridx[i][:, kk:kk + 1], rt_[:, o0 + 3:o0 + 4]), R, [f"ridx{i}"])
            P.dma("sp", sc["Ridx"][ts_, :], ridx[i][:, :], [f"ridx{i}"], ["Ridx"])
            P.dma("sp", sc["Rw"][ts_, :], rw[i][:, :], [f"rw{i}"], ["Rw"])
            for kk in range(2):
                P.add("pool", lambda e, kk=kk: e.indirect_dma_start(
                    out=sc["Xg"][:, :], out_offset=bass.IndirectOffsetOnAxis(ap=ridx[i][:, kk:kk + 1], axis=0),
                    in_=h1b[i][:, :], in_offset=None, bounds_check=_breg(e, breg), oob_is_err=False),
                    [f"h1b{i}", f"ridx{i}"], ["Xg_s"], dma=True)

        stage1(0)
        stage1b(0)
        for tt in range(16):
            if tt >= 1:
                s_pos_pe(tt - 1)
            if tt + 1 < 16:
                stage1(tt + 1)
            s_ln1(tt)
            if tt >= 1:
                stage3(tt - 1)
            s_ln2(tt)
            stage2b(tt)
            if tt + 1 < 16:
                stage1b(tt + 1)
        s_pos_pe(15)
        stage3(15)
        P.emit(st)


def phase_D(nc, io, sc):
    with ExitStack() as st:
        C = Ctx(nc, st, "D")
        P = C.P
        breg = {}
        ident = C.sb("ident", [128, 128], BF16)
        P.dma("sp", ident[:, :], io["ident"], [], ["ident"])
        idxd = C.sb("idxd", [128, 64], I32)
        P.dma("sp", idxd[:, :], io["idxD"], [], ["idxd"])
        wg = [C.sb(f"wg{i}", [128, 16, 512], BF16) for i in range(2)]
        wu = [C.sb(f"wu{i}", [128, 16, 512], BF16) for i in range(2)]
        wd = [C.sb(f"wd{i}", [128, 4, 2048], BF16) for i in range(2)]
        xe = [C.sb(f"xe{i}", [128, 2048], BF16) for i in range(4)]
        xT = [C.sb(f"xT{i}", [128, 16, 128], BF16) for i in range(2)]
        sg = C.sb("sg", [128, 512], F32)
        hm = C.sb("hm", [128, 512], BF16)
        hT = C.sb("hT", [128, 4, 128], BF16)
        ye = [C.sb(f"ye{i}", [128, 2048], F32) for i in range(2)]
        pg = C.ps("pg", [128, 512], F32)
        pu = C.ps("pu", [128, 512], F32)
        py = [C.ps(f"py{i}", [128, 512], F32) for i in range(2)]
        pT = [C.ps(f"pT{i}", [128, 8, 128], BF16) for i in range(2)]
        n = [0]
        NE = NEXP // 2

        def loads(el):
            i = el % 2
            P.dma("pool", wg[i][:, :, :], io["w_gate"][el].rearrange("(k p) n -> p k n", p=128), [], [f"wg{i}"])
            P.dma("pool", wu[i][:, :, :], io["w_up"][el].rearrange("(k p) n -> p k n", p=128), [], [f"wu{i}"])
            for c in range(4):
                P.dma("pool", wd[i][:, :, c * 512:(c + 1) * 512],
                      io["w_down"][el][:, c * 512:(c + 1) * 512].rearrange("(k p) n -> p k n", p=128), [], [f"wd{i}"])
            for s_ in range(2):
                u = el * 2 + s_
                xi = u % 4
                P.add("pool", lambda e, u=u, xi=xi: e.indirect_dma_start(
                    out=xe[xi][:, :], out_offset=None, in_=sc["Xg"][:, :],
                    in_offset=bass.IndirectOffsetOnAxis(ap=idxd[:, u:u + 1], axis=0),
                    bounds_check=_breg(e, breg), oob_is_err=False), ["idxd", "Xg"], [f"xe{xi}"], dma=True)

        def compute(el):
            i = el % 2
            for s_ in range(2):
                u = el * 2 + s_
                xi = u % 4
                ti = u % 2
                for hh in range(2):
                    for k in range(8):
                        kk = hh * 8 + k
                        P.tr(pT[hh][:, k, :], xe[xi][:, kk * 128:(kk + 1) * 128], ident[:, :], [f"xe{xi}", "ident"], [f"pT{hh}"])
                    if hh == 0:
                        P.add("act", lambda e, ti=ti: e.copy(xT[ti][:, 0:8, :], pT[0][:, :, :]), ["pT0"], [f"xT{ti}"])
                    else:
                        P.add("dve", lambda e, ti=ti: e.tensor_copy(xT[ti][:, 8:16, :], pT[1][:, :, :]), ["pT1"], [f"xT{ti}"])
                for k in range(16):
                    P.mm(pg[:, :], xT[ti][:, k, :], wg[i][:, k, :], k == 0, k == 15, [f"xT{ti}", f"wg{i}"], ["pg"])
                for k in range(16):
                    P.mm(pu[:, :], xT[ti][:, k, :], wu[i][:, k, :], k == 0, k == 15, [f"xT{ti}", f"wu{i}"], ["pu"])
                _act(P, sg[:, :], pg[:, :], AF.Silu, ["pg"], ["sg"])
                _tt(P, hm[:, :], sg[:, :], pu[:, :], ALU.mult, ["sg", "pu"], ["hm"])
                for k in range(4):
                    P.tr(pT[0][:, k, :], hm[:, k * 128:(k + 1) * 128], ident[:, :], ["hm", "ident"], ["pT0"])
                P.add("act", lambda e: e.copy(hT[:, :, :], pT[0][:, 0:4, :]), ["pT0"], ["hT"])
                for cc in range(4):
                    j = n[0] % 2
                    n[0] += 1
                    cs = slice(cc * 512, (cc + 1) * 512)
                    for k in range(4):
                        P.mm(py[j][:, :], hT[:, k, :], wd[i][:, k, cs], k == 0, k == 3, ["hT", f"wd{i}"], [f"py{j}"])
                    if cc % 2 == 0:
                        P.add("act", lambda e, ti=ti, j=j, cs=cs: e.copy(ye[ti][:, cs], py[j][:, :]), [f"py{j}"], [f"ye{ti}"])
                    else:
                        P.add("dve", lambda e, ti=ti, j=j, cs=cs: e.tensor_copy(ye[ti][:, cs], py[j][:, :]), [f"py{j}"], [f"ye{ti}"])
                P.add("pool", lambda e, u=u, ti=ti: e.indirect_dma_start(
                    out=sc["Yg"][:, :], out_offset=bass.IndirectOffsetOnAxis(ap=idxd[:, u:u + 1], axis=0),
                    in_=ye[ti][:, :], in_offset=None, bounds_check=_breg(e, breg), oob_is_err=False),
                    [f"ye{ti}", "idxd"], ["Yg_s"], dma=True)

        loads(0)
        for el in range(NE):
            if el + 1 < NE:
                loads(el + 1)
            compute(el)
        P.emit(st)


def phase_E(nc, io, sc):
    with ExitStack() as st:
        C = Ctx(nc, st, "E")
        P = C.P
        breg = {}
        g2 = C.sb("g2", [128, 2048], F32)
        b2 = C.sb("b2", [128, 2048], F32)
        P.dma("sp", g2[:, :], io["ln2_g"], [], ["g2"])
        P.dma("sp", b2[:, :], io["ln2_b"], [], ["b2"])
        y1 = [C.sb(f"y1_{i}", [128, 2048], F32) for i in range(2)]
        y2 = [C.sb(f"y2_{i}", [128, 2048], F32) for i in range(2)]
        h1 = [C.sb(f"h1_{i}", [128, 2048], F32) for i in range(2)]
        ridx = [C.sb(f"ridx{i}", [128, 2], I32) for i in range(2)]
        rw = [C.sb(f"rw{i}", [128, 2], F32) for i in range(2)]
        st6 = [C.sb(f"st6_{i}", [128, 24], F32) for i in range(2)]
        mv = [C.sb(f"mv{i}", [128, 8], F32) for i in range(2)]
        for i in range(2):
            P.add("pool", lambda e, i=i: e.memset(mv[i][:, 4:5], LN_EPS), [], [f"mv{i}"])

        def part1(tt):
            i = tt % 2
            ts_ = slice(tt * 128, (tt + 1) * 128)
            P.dma("sp", ridx[i][:, :], sc["Ridx"][ts_, :], ["Ridx"], [f"ridx{i}"])
            P.dma("sp", rw[i][:, :], sc["Rw"][ts_, :], ["Rw"], [f"rw{i}"])
            P.dma("sp", h1[i][:, :], sc["H1"][ts_, :], ["H1"], [f"h1_{i}"])
            P.add("pool", lambda e: e.memset(y1[i][:, :], 0.0), [], [f"y1_{i}"])
            P.add("pool", lambda e: e.memset(y2[i][:, :], 0.0), [], [f"y2_{i}"])
            for kk, yb, yk in ((0, y1[i], f"y1_{i}"), (1, y2[i], f"y2_{i}")):
                P.add("pool", lambda e, kk=kk, yb=yb: e.indirect_dma_start(
                    out=yb[:, :], out_offset=None, in_=sc["Yg"][:, :],
                    in_offset=bass.IndirectOffsetOnAxis(ap=ridx[i][:, kk:kk + 1], axis=0),
                    bounds_check=_breg(e, breg), oob_is_err=False), [f"ridx{i}", "Yg", yk], [yk + "g"], dma=True)
            y1k = [f"y1_{i}", f"y1_{i}g"]
            y2k = [f"y2_{i}", f"y2_{i}g"]
            _ts(P, y1[i][:, :], y1[i][:, :], rw[i][:, 0:1], None, ALU.mult, None, y1k + [f"rw{i}"], [f"y1_{i}"])
            _stt(P, y1[i][:, :], y2[i][:, :], rw[i][:, 1:2], y1[i][:, :], ALU.mult, ALU.add, y1k + y2k + [f"rw{i}"], [f"y1_{i}"])
            _stt(P, h1[i][:, :], h1[i][:, :], DN_ALPHA, y1[i][:, :], ALU.mult, ALU.add, [f"h1_{i}"] + y1k, [f"h1_{i}"])
            for c in range(4):
                P.add("dve", lambda e, c=c: e.bn_stats(st6[i][:, c * 6:(c + 1) * 6], h1[i][:, c * 512:(c + 1) * 512]),
                      [f"h1_{i}"], [f"st6_{i}"])
            P.add("dve", lambda e: e.bn_aggr(mv[i][:, 0:2], st6[i][:, 0:24]), [f"st6_{i}"], [f"mv{i}"])
            _act(P, mv[i][:, 2:3], mv[i][:, 1:2], AF.Sqrt, [f"mv{i}"], [f"mv{i}"], bias=mv[i][:, 4:5])
            P.add("dve", lambda e: e.reciprocal(mv[i][:, 3:4], mv[i][:, 2:3]), [f"mv{i}"], [f"mv{i}"])

        def part2(tt):
            i = tt % 2
            ts_ = slice(tt * 128, (tt + 1) * 128)
            _ts(P, y2[i][:, :], h1[i][:, :], mv[i][:, 0:1], mv[i][:, 3:4], ALU.subtract, ALU.mult,
                [f"h1_{i}", f"mv{i}", f"y2_{i}g"], [f"y2_{i}"])
            _tt(P, y2[i][:, :], y2[i][:, :], g2[:, :], ALU.mult, [f"y2_{i}", "g2"], [f"y2_{i}"])
            _tt(P, y1[i][:, :], y2[i][:, :], b2[:, :], ALU.add, [f"y2_{i}", "b2", f"y1_{i}g"], [f"y1_{i}"])
            P.dma("sp", sc["out"][ts_, :], y1[i][:, :], [f"y1_{i}"], ["out"])

        part1(0)
        for tt in range(16):
            if tt + 1 < 16:
                part1(tt + 1)
            part2(tt)
        P.emit(st)


EXPERTS = None
_IN_C = {
    "identf": ([128, 128], F32), "w_nsa_proj": ([1024, 2048], F32), "w_pool_proj": ([1024, 2048], F32),
    "w_out": ([2048, 2048], F32), "w_router": ([2048, 72], F32), "b_router": ([128, 72], F32),
    "eid64": ([128, 64], F32), "utri": ([128, 128], BF16), "ln1_g": ([128, 2048], F32), "ln1_b": ([128, 2048], F32),
    "ln2_g": ([128, 2048], F32), "ln2_b": ([128, 2048], F32), "x_own": ([NT, 2048], F32),
    "w_gate": ([NEXP // 2, 2048, 512], F32), "w_up": ([NEXP // 2, 2048, 512], F32), "w_down": ([NEXP // 2, 512, 2048], F32),
    "pbase": ([128, 1], F32), "idxD": ([128, 64], I32),
}
_SC_C = {
    "ZT": ([16, 128, NT], BF16), "H1": ([NT, 2048], F32), "Xg": ([NROW, 2048], BF16),
    "Yg": ([NROW, 2048], F32), "Ridx": ([NT, 2], I32), "Rw": ([NT, 2], F32),
}


def _shared_inputs(inp):
    w_in = inp["w_in"][0]
    ca = np.ascontiguousarray
    m = {
        "ident": _bf16(np.eye(128)), "identf": np.eye(128, dtype=np.float32),
        "w_A1": ca(np.concatenate([w_in[:, 2560:3072], w_in[:, 2048:2560]], 1)),
        "w_A2": ca(w_in[:, 3072:3584]), "w_q": ca(w_in[:, 1024:2048]), "w_gn": ca(w_in[:, 3584:3632]),
        "w_pool": ca(w_in[:, 0:1024]), "w_gm": ca(w_in[:, 3632:7728]),
        "pool_mix": ca(inp["pool_mix"][0]), "pool_scale": ca(inp["pool_scale"][0].reshape(8, 128).T),
        "cmp_k_w1": ca(inp["cmp_k_w1"][0]), "cmp_v_w1": ca(inp["cmp_v_w1"][0]),
        "cmp_k_w2": ca(inp["cmp_k_w2"][0]), "cmp_v_w2": ca(inp["cmp_v_w2"][0]),
        "cmp_k_w2s": ca(np.concatenate([inp["cmp_k_w2"][0][:, 32:], inp["cmp_k_w2"][0][:, :32]], 1)),
        "cmp_pos_kT": ca(inp["cmp_pos_k"][0].T), "cmp_pos_vT": ca(inp["cmp_pos_v"][0].T),
        "w_nsa_proj": ca(inp["w_nsa_proj"][0]), "w_pool_proj": ca(inp["w_pool_proj"][0]), "w_out": ca(inp["w_out"][0]),
        "w_router": ca(np.concatenate([inp["router_group_w"][0],
                                       inp["router_expert_w"][0].transpose(1, 0, 2).reshape(D, 64)], 1)),
        "b_router": ca(np.broadcast_to(np.concatenate([inp["router_group_b"][0], inp["router_expert_b"][0].reshape(64)])[None, :], (128, 72))),
        "eid64": ca(np.broadcast_to(np.arange(64, dtype=np.float32)[None, :], (128, 64))),
        "utri": _bf16((np.arange(128)[:, None] < np.arange(128)[None, :]).astype(np.float32)),
    }
    for p in range(2):
        sl = slice(p * (NEXP // 2), (p + 1) * (NEXP // 2))
        m[f"w_gate{p}"] = ca(inp["w_gate"][0][sl])
        m[f"w_up{p}"] = ca(inp["w_up"][0][sl])
        m[f"w_down{p}"] = ca(inp["w_down"][0][sl])
    for n in ("ln1_g", "ln1_b", "ln2_g", "ln2_b"):
        m[n] = ca(np.broadcast_to(inp[n][0][None, :], (128, D)))
    return m


def _core_inputs(inp, shared, core, tabs):
    b, p = core // 2, core % 2
    t = tabs[p]
    x0 = inp["x"][b]
    xo = x0[t["own_pos"]]
    xp = x0[t["prev_pos"]] * t["prev_valid"][:, None]
    m = {k: v for k, v in shared.items() if not k.startswith(("w_gate", "w_up", "w_down"))}
    for n_ in ("w_gate", "w_up", "w_down"):
        m[n_] = shared[f"{n_}{p}"]
    m["pbase"] = np.full((128, 1), float(CAP * p), np.float32)
    el = np.arange(NEXP // 2)
    rows = ((p * (NEXP // 2) + el[None, :, None]) * 2 + np.arange(2)[None, None, :]) * CAP + np.arange(CAP)[:, None, None]
    m["idxD"] = np.ascontiguousarray(rows.reshape(CAP, NEXP).astype(np.int32))
    m["xTg"] = np.ascontiguousarray(x0.T)
    m["xTo"] = np.ascontiguousarray(xo.T)
    m["xTp"] = np.ascontiguousarray(xp.T)
    m["x_own"] = np.ascontiguousarray(xo)
    for k in ALL_IN:
        if k not in m:
            m[k] = t[k]
    return m


ALL_IN = {}
ALL_IN.update(_IN_A)
ALL_IN.update(_IN_B)
ALL_IN.update(_IN_C)
ALL_SC = {}
ALL_SC.update(_SC_A)
ALL_SC.update(_SC_C)
PHASES = [phase_A, phase_B, phase_C1, "core_barrier", phase_C2, "core_barrier", phase_D, "core_barrier", phase_E]


def build_full():
    return build_nc(ALL_IN, ALL_SC, PHASES, final_out=("out", [NT, D], F32))


def kernel(**inputs):
    inp = {k: np.asarray(v) for k, v in inputs.items()}
    tabs = []
    for p in range(2):
        t = _core_tables(p)
        tabs.append(_core_tables_B(p, t))
    shared = _shared_inputs(inp)
    in_maps = [_core_inputs(inp, shared, c, tabs) for c in range(8)]
    nc = build_full()
    res = run_bass_kernel_spmd(nc, in_maps, core_ids=list(range(8)))
    out = np.zeros((4, S, D), np.float32)
    for c in range(8):
        b, p = c // 2, c % 2
        out[b, tabs[p]["own_pos"]] = np.asarray(res.results[c]["out"]).astype(np.float32)
    return out
```

```python
import numpy as np
import concourse.bass as bass
import concourse.mybir as mybir
from concourse.bass_utils import run_bass_kernel_spmd
from contextlib import ExitStack

F32 = mybir.dt.float32
BF16 = mybir.dt.bfloat16
I32 = mybir.dt.int32
U32 = mybir.dt.uint32
AF = mybir.ActivationFunctionType
ALU = mybir.AluOpType
AX = mybir.AxisListType

D = 2048
S = 4096
NT = 2048
HD = 64
NH = 16
NG = 4
NCMP = 255
NEG = -30000.0
OWN = ([0, 3, 4, 7], [1, 2, 5, 6])
DN_ALPHA = 2.0 ** 0.25
LN_EPS = 1e-5
NEXP = 64
CAP = 128
NROW = NEXP * 2 * CAP
NCORES = 8

SHARED = ("Xg", "Yg")
DEBUG = []
STOP_AFTER = None
GROUPS = None
PARTS = None


def _on(name):
    return PARTS is None or name in PARTS


class _Op:
    __slots__ = ("eng", "fn", "dma", "deps", "idx", "marked", "count", "sem", "semval", "gid")


class Phase:
    ENGS = ("pe", "act", "dve", "pool", "sp")
    NDMA = 6

    def __init__(self, nc, name):
        self.nc = nc
        self.name = name
        self.ops = {e: [] for e in self.ENGS}
        self.bufs = {}
        self.excl = set()
        self.nops = 0

    def _buf(self, k):
        b = self.bufs.get(k)
        if b is None:
            b = [[], []]
            self.bufs[k] = b
        return b

    def add(self, eng, fn, reads=(), writes=(), dma=False):
        op = _Op()
        op.eng, op.fn, op.dma = eng, fn, dma
        op.idx = len(self.ops[eng])
        op.marked = False
        op.gid = self.nops
        self.nops += 1
        deps = set()
        for k in reads:
            b = self._buf(k)
            deps.update(b[0])
            if k in self.excl:
                for r in b[1]:
                    if r.eng != eng:
                        deps.add(r)
            b[1].append(op)
        for k in writes:
            b = self._buf(k)
            if dma and b[0] and not b[1] and all(w.dma for w in b[0]):
                b[0].append(op)
            else:
                deps.update(b[0])
                deps.update(b[1])
                b[1] = []
                b[0] = [op]
        deps.discard(op)
        op.deps = deps
        self.ops[eng].append(op)
        return op

    def mm(self, out, lhsT, rhs, start, stop, reads, writes, skip=False):
        if skip:
            return self.add("pe", lambda e: e.matmul(out, lhsT, rhs, start=start, stop=stop, skip_group_check=True), reads, writes)
        return self.add("pe", lambda e: e.matmul(out, lhsT, rhs, start=start, stop=stop), reads, writes)

    def tr(self, out, in_, ident, reads, writes):
        return self.add("pe", lambda e: e.transpose(out, in_, ident), reads, writes)

    def dma(self, q, out, in_, reads, writes):
        return self.add(q, lambda e: e.dma_start(out=out, in_=in_), reads, writes, dma=True)

    def emit(self, stack):
        nc = self.nc
        engs = {"pe": nc.tensor, "act": nc.scalar, "dve": nc.vector, "pool": nc.gpsimd, "sp": nc.sync}
        for e in self.ENGS:
            for op in self.ops[e]:
                for d in op.deps:
                    if d.dma:
                        continue
                    if d.eng == "pe" and op.eng == "pe" and not op.dma:
                        continue
                    d.marked = True
        esem = {e: nc.alloc_semaphore(name=f"{self.name}_{e}") for e in self.ENGS}
        dsem = {}
        for e in self.ENGS:
            cnt = 0
            nd = 0
            for op in self.ops[e]:
                if op.dma:
                    if e not in dsem:
                        dsem[e] = [nc.alloc_semaphore(name=f"{self.name}_{e}_d{i}") for i in range(self.NDMA)]
                    op.sem = dsem[e][nd % self.NDMA]
                    op.semval = 16 * (nd // self.NDMA + 1)
                    nd += 1
                elif op.marked:
                    cnt += 1
                    op.count = cnt
            assert cnt < 60000, (self.name, e, cnt)
        block = stack.enter_context(nc.Block())

        def body(ename):
            def _(eng):
                seen = {}

                def wait(sem, val):
                    k = id(sem)
                    if seen.get(k, 0) >= val:
                        return
                    seen[k] = val
                    eng.wait_ge(sem, val)

                for op in self.ops[ename]:
                    for d in sorted(op.deps, key=lambda o: o.gid):
                        if d.dma:
                            wait(d.sem, d.semval)
                        elif d.eng == "pe" and ename == "pe" and not op.dma:
                            continue
                        else:
                            wait(esem[d.eng], d.count)
                    if op.dma:
                        if op.semval > 16:
                            wait(op.sem, op.semval - 16)
                        op.fn(eng).then_inc(op.sem, 16)
                    else:
                        ins = op.fn(eng)
                        if op.marked:
                            ins.then_inc(esem[ename], 1)
                last = {}
                for op in self.ops[ename]:
                    if op.dma:
                        last[id(op.sem)] = (op.sem, op.semval)
                for sem, val in last.values():
                    wait(sem, val)
            return _

        for ename, reg in (("pe", block.tensor), ("act", block.scalar), ("dve", block.vector),
                           ("pool", block.gpsimd), ("sp", block.sync)):
            if self.ops[ename]:
                reg(body(ename))


class Ctx:
    def __init__(self, nc, stack, name):
        self.nc, self.stack, self.name = nc, stack, name
        self.P = Phase(nc, name)

    def sb(self, name, shape, dt):
        return self.stack.enter_context(self.nc.sbuf_tensor(f"{self.name}_{name}", list(shape), dt))

    def ps(self, name, shape, dt):
        self.P.excl.add(name)
        return self.stack.enter_context(self.nc.psum_tensor(f"{self.name}_{name}", list(shape), dt))


def _dram(nc, name, shape, dt, kind):
    return nc.dram_tensor(name, list(shape), dt, kind=kind).ap()


def _scratch(nc, name, shape, dt):
    kind = "ExternalOutput" if name in DEBUG else "Internal"
    if name in SHARED:
        return nc.dram_tensor(name, list(shape), dt, kind="Internal", addr_space="Shared").ap()
    return _dram(nc, name, shape, dt, kind)


def _rope(P, dst, src, cos, sin, nh, tmp, rkeys, wkeys, tag):
    s3 = src.rearrange("p (h d) -> p h d", h=nh)
    d3 = dst.rearrange("p (h d) -> p h d", h=nh)
    cb = cos.unsqueeze(1).broadcast_to([128, nh, 32])
    sn = sin.unsqueeze(1).broadcast_to([128, nh, 32])
    t1 = tmp[:, 0, 0:nh, :]
    t2 = tmp[:, 1, 0:nh, :]
    q1, q2 = s3[:, :, 0:32], s3[:, :, 32:64]
    tk = [tag + "_t1", tag + "_t2"]
    P.add("dve", lambda e: e.tensor_tensor(t1, q1, cb, ALU.mult), rkeys, [tk[0]])
    P.add("dve", lambda e: e.tensor_tensor(t2, q2, sn, ALU.mult), rkeys, [tk[1]])
    P.add("dve", lambda e: e.tensor_tensor(d3[:, :, 0:32], t1, t2, ALU.subtract), tk, wkeys)
    P.add("dve", lambda e: e.tensor_tensor(t1, q2, cb, ALU.mult), rkeys, [tk[0]])
    P.add("dve", lambda e: e.tensor_tensor(t2, q1, sn, ALU.mult), rkeys, [tk[1]])
    P.add("dve", lambda e: e.tensor_tensor(d3[:, :, 32:64], t1, t2, ALU.add), tk, wkeys)


def phase_A(nc, io, sc):
    with ExitStack() as st:
        C = Ctx(nc, st, "A")
        P = C.P
        ident = C.sb("ident", [128, 128], BF16)
        P.dma("sp", ident[:, :], io["ident"], [], ["ident"])
        wk = [C.sb(f"w{i}", [128, 16, 512], BF16) for i in range(2)]
        xt = [C.sb(f"xt{i}", [128, 16, 512], BF16) for i in range(2)]
        xo = C.sb("xo", [128, 16, NT], BF16)
        xh = C.sb("xh", [128, 16, 64], BF16)
        cosg = C.sb("cosg", [128, 32, 32], F32)
        sing = C.sb("sing", [128, 32, 32], F32)
        cosp = C.sb("cosp", [128, 16, 32], F32)
        sinp = C.sb("sinp", [128, 16, 32], F32)
        coso = C.sb("coso", [128, 16, 32], F32)
        sino = C.sb("sino", [128, 16, 32], F32)
        cosq = C.sb("cosq", [128, 16, 32], F32)
        sinq = C.sb("sinq", [128, 16, 32], F32)
        for t, n in ((cosg, "cosg"), (sing, "sing"), (cosp, "cosp"), (sinp, "sinp"), (coso, "coso"),
                     (sino, "sino"), (cosq, "cosq"), (sinq, "sinq")):
            P.dma("sp", t[:, :, :], io[n], [], [n])
        rtmp = C.sb("rtmp", [128, 2, 8, 32], F32)
        ksr = [C.sb(f"ksr{i}", [128, 512], BF16) for i in range(2)]
        kcv = [C.sb(f"kcv{i}", [128, 512], BF16) for i in range(2)]
        stT = [C.sb(f"stT{i}", [128, 6, 512], BF16) for i in range(2)]
        vst = [C.sb(f"vst{i}", [128, 4, 256], BF16) for i in range(2)]
        pm = [C.ps(f"pm{i}", [128, 512], F32) for i in range(4)]
        pT = [C.ps(f"pT{i}", [128, 8, 128], BF16) for i in range(2)]

        zeros = C.sb("zeros", [128, 2048], BF16)
        P.add("pool", lambda e: e.memset(zeros[:, :], 0.0), [], ["zeros"])
        for e_ in range(NROW // 128):
            P.dma("sp", sc["Xg"][e_ * 128:(e_ + 1) * 128, :], zeros[:, :], ["zeros"], ["Xg"])
        wq = ["pool", "sp"]
        nw = [0]
        nx = [0]
        npm = [0]
        npt = [0]

        def load_w(src_ap, ncols=512):
            i = nw[0] % 2
            nw[0] += 1
            P.dma("pool", wk[i][:, :, 0:ncols], src_ap.rearrange("(k p) n -> p k n", p=128), [], [f"w{i}"])
            return i

        def load_x(src_ap):
            i = nx[0] % 2
            nx[0] += 1
            P.dma("pool", xt[i][:, :, :], src_ap.rearrange("(k p) n -> p k n", p=128), [], [f"xt{i}"])
            return i

        def proj(xtile_ap, xkey, wi, ncols=512, c0=0):
            b = npm[0] % 4
            npm[0] += 1
            for k in range(16):
                P.mm(pm[b][:, 0:ncols], xtile_ap(k), wk[wi][:, k, c0:c0 + ncols], k == 0, k == 15,
                     [xkey, f"w{wi}"], [f"pm{b}"])
            return b

        def transposes(srcs, dst, dstkey, tcol):
            b = npt[0] % 2
            npt[0] += 1
            n = len(srcs)
            for i, (ap, key) in enumerate(srcs):
                P.tr(pT[b][:, i, :], ap, ident[:, :], [key, "ident"], [f"pT{b}"])
            P.add("act", lambda e: e.copy(dst[:, 0:n, tcol * 128:(tcol + 1) * 128], pT[b][:, 0:n, :]),
                  [f"pT{b}"], [dstkey])

        pending = []

        def flush():
            for f in pending:
                f()
            del pending[:]

        w_ks = load_w(io["w_A1"][:, 0:512])
        w_kc = load_w(io["w_A1"][:, 512:1024])
        for ch in (range(8) if _on("A1") else []):
            xi = load_x(io["xTg"][:, ch * 512:(ch + 1) * 512])
            si = ch % 2
            for j in range(4):
                tt = ch * 4 + j
                xa = (lambda xi, j: (lambda k: xt[xi][:, k, j * 128:(j + 1) * 128]))(xi, j)
                b0 = proj(xa, f"xt{xi}", w_ks)
                b1 = proj(xa, f"xt{xi}", w_kc)
                flush()
                r = tt % 2
                _rope(P, ksr[r][:, 0:256], pm[b0][:, 0:256], cosg[:, tt, :], sing[:, tt, :], 4, rtmp,
                      [f"pm{b0}", "cosg", "sing"], [f"ksr{r}"], "rA")
                P.add("act", lambda e, si=si, j=j, b0=b0: e.copy(vst[si][:, j, :], pm[b0][:, 256:512]),
                      [f"pm{b0}"], [f"vst{si}"])
                P.add("act", lambda e, r=r, b1=b1: e.copy(kcv[r][:, :], pm[b1][:, :]), [f"pm{b1}"], [f"kcv{r}"])
                srcs = [(ksr[r][:, 0:128], f"ksr{r}"), (ksr[r][:, 128:256], f"ksr{r}")]
                srcs += [(kcv[r][:, i * 128:(i + 1) * 128], f"kcv{r}") for i in range(4)]
                pending.append(lambda srcs=srcs, si=si, j=j: transposes(srcs, stT[si], f"stT{si}", j))
            pending.append(lambda si=si, ch=ch: P.dma(
                "sp", sc["KT_A1"].rearrange("i p t -> p i t")[:, :, ch * 512:(ch + 1) * 512], stT[si][:, :, :],
                [f"stT{si}"], ["KT_A1"]))
            pending.append(lambda si=si, ch=ch: P.dma(
                "sp", sc["Vs"].rearrange("(c j p) n -> c p j n", j=4, p=128)[ch], vst[si][:, :, :],
                [f"vst{si}"], ["Vs"]))
        flush()

        w_kw = load_w(io["w_A2"])
        def load_own():
            for ch in range(4):
                P.dma("pool", xo[:, :, ch * 512:(ch + 1) * 512],
                      io["xTo"][:, ch * 512:(ch + 1) * 512].rearrange("(k p) n -> p k n", p=128), [], ["xo"])
            for lc in range(4):
                P.dma("pool", xh[:, :, lc * 16:(lc + 1) * 16],
                      io["xTp"][:, lc * 512 + 496:lc * 512 + 512].rearrange("(k p) t -> p k t", p=128), [], ["xh"])

        for which in (("prev", "own") if _on("A2") else ()):
            cosw, sinw = (cosp, sinp) if which == "prev" else (coso, sino)
            cn, sn_ = ("cosp", "sinp") if which == "prev" else ("coso", "sino")
            for ch in range(4):
                si = ch % 2
                if which == "prev":
                    xi = load_x(io["xTp"][:, ch * 512:(ch + 1) * 512])
                    if ch == 0:
                        load_own()
                for j in range(4):
                    tt = ch * 4 + j
                    if which == "prev":
                        xa = (lambda xi, j: (lambda k: xt[xi][:, k, j * 128:(j + 1) * 128]))(xi, j)
                        xkey = f"xt{xi}"
                    else:
                        xa = (lambda tt: (lambda k: xo[:, k, tt * 128:(tt + 1) * 128]))(tt)
                        xkey = "xo"
                    b0 = proj(xa, xkey, w_kw)
                    flush()
                    r = tt % 2
                    _rope(P, ksr[r][:, 0:256], pm[b0][:, 0:256], cosw[:, tt, :], sinw[:, tt, :], 4, rtmp,
                          [f"pm{b0}", cn, sn_], [f"ksr{r}"], "rA")
                    P.add("act", lambda e, si=si, j=j, b0=b0: e.copy(vst[si][:, j, :], pm[b0][:, 256:512]),
                          [f"pm{b0}"], [f"vst{si}"])
                    srcs = [(ksr[r][:, 0:128], f"ksr{r}"), (ksr[r][:, 128:256], f"ksr{r}")]
                    pending.append(lambda srcs=srcs, si=si, j=j: transposes(srcs, stT[si], f"stT{si}", j))
                pending.append(lambda si=si, ch=ch, which=which: P.dma(
                    "sp", sc["KwT_" + which].rearrange("i p t -> p i t")[:, :, ch * 512:(ch + 1) * 512],
                    stT[si][:, 0:2, :], [f"stT{si}"], ["KwT_" + which]))
                pending.append(lambda si=si, ch=ch, which=which: P.dma(
                    "sp", sc["Vw_" + which].rearrange("(c j p) n -> c p j n", j=4, p=128)[ch], vst[si][:, :, :],
                    [f"vst{si}"], ["Vw_" + which]))
        flush()

        for qc in (range(2) if _on("Q") else []):
            wi = load_w(io["w_q"][:, qc * 512:(qc + 1) * 512])
            for ch in range(4):
                si = ch % 2
                for j in range(4):
                    tt = ch * 4 + j
                    xa = (lambda tt: (lambda k: xo[:, k, tt * 128:(tt + 1) * 128]))(tt)
                    b0 = proj(xa, "xo", wi)
                    flush()
                    r = tt % 2
                    _rope(P, ksr[r][:, :], pm[b0][:, :], cosq[:, tt, :], sinq[:, tt, :], 8, rtmp,
                          [f"pm{b0}", "cosq", "sinq"], [f"ksr{r}"], "rA")
                    srcs = [(ksr[r][:, i * 128:(i + 1) * 128], f"ksr{r}") for i in range(4)]
                    pending.append(lambda srcs=srcs, si=si, j=j: transposes(srcs, stT[si], f"stT{si}", j))
                pending.append(lambda si=si, ch=ch, qc=qc: P.dma(
                    "sp", sc["QT"][qc * 4:(qc + 1) * 4].rearrange("i p t -> p i t")[:, :, ch * 512:(ch + 1) * 512],
                    stT[si][:, 0:4, :], [f"stT{si}"], ["QT"]))
        flush()

        gst = C.sb("gst", [128, 16, 48], F32)
        wi = load_w(io["w_gn"], 48)
        for tt in (range(16) if _on("GN") else []):
            xa = (lambda tt: (lambda k: xo[:, k, tt * 128:(tt + 1) * 128]))(tt)
            b0 = proj(xa, "xo", wi, 48)
            P.add("act", lambda e, tt=tt, b0=b0: e.activation(gst[:, tt, :], pm[b0][:, 0:48], AF.Sigmoid),
                  [f"pm{b0}"], ["gst"])
        P.dma("sp", sc["Gn"].rearrange("(t p) n -> p t n", p=128), gst[:, :, :], ["gst"], ["Gn"])

        gms = [C.sb(f"gms{i}", [128, 512], BF16) for i in range(2)]
        ng = 0
        for gc in (range(8) if _on("GM") else []):
            wi = load_w(io["w_gm"][:, gc * 512:(gc + 1) * 512])
            for tt in range(16):
                xa = (lambda tt: (lambda k: xo[:, k, tt * 128:(tt + 1) * 128]))(tt)
                b0 = proj(xa, "xo", wi)
                gi = ng % 2
                ng += 1
                P.add("act", lambda e, gi=gi, b0=b0: e.activation(gms[gi][:, :], pm[b0][:, :], AF.Sigmoid),
                      [f"pm{b0}"], [f"gms{gi}"])
                P.dma("sp", sc["Gm"][tt * 128:(tt + 1) * 128, gc * 512:(gc + 1) * 512], gms[gi][:, :],
                      [f"gms{gi}"], ["Gm"])

        pmix = C.sb("pmix", [128, 4, 2, 256], BF16)
        P.dma("pool", pmix[:, :, :, :], io["pool_mix"].rearrange("g (c p) d -> p g c d", p=128), [], ["pmix"])
        pscale = C.sb("pscale", [128, 8], F32)
        P.dma("sp", pscale[:, :], io["pool_scale"], [], ["pscale"])
        rc16 = C.sb("rc16", [128, 4, 4, 16], F32)
        P.dma("sp", rc16[:, :, :, :], io["rc16"], [], ["rc16"])
        U = [C.sb(f"U{i}", [128, 528], F32) for i in range(2)]
        Wa = C.sb("Wa", [128, 528], F32)
        Wb = C.sb("Wb", [128, 528], F32)
        pld = [C.sb(f"pld{i}", [128, 2, 512], BF16) for i in range(2)]
        mxs = [C.sb(f"mxs{i}", [128, 512], BF16) for i in range(2)]
        phalo = C.ps("phalo", [128, 16], F32)
        nu = 0
        nmx = 0
        for g in (range(4) if _on("POOL") else []):
            win = (2, 4, 8, 16)[g]
            wis = [load_w(io["w_pool"][:, (2 * g + c2) * 128:(2 * g + c2 + 1) * 128], 128) for c2 in range(2)]
            for lc in range(4):
                pi = (g * 4 + lc) % 2
                for c2 in range(2):
                    wi = wis[c2]
                    b = npm[0] % 4
                    npm[0] += 1
                    for k in range(16):
                        P.mm(pm[b][:, :], wk[wi][:, k, 0:128], xo[:, k, lc * 512:(lc + 1) * 512], k == 0, k == 15,
                             ["xo", f"w{wi}"], [f"pm{b}"])
                    for k in range(16):
                        P.mm(phalo[:, :], wk[wi][:, k, 0:128], xh[:, k, lc * 16:(lc + 1) * 16], k == 0, k == 15,
                             ["xh", f"w{wi}"], ["phalo"])
                    ui = nu % 2
                    nu += 1
                    Ut = U[ui]
                    uk = f"U{ui}"
                    P.add("act", lambda e, Ut=Ut, b=b: e.copy(Ut[:, 16:528], pm[b][:, :]), [f"pm{b}"], [uk])
                    P.add("act", lambda e, Ut=Ut: e.copy(Ut[:, 0:16], phalo[:, :]), ["phalo"], [uk])
                    src, sk = Ut, uk
                    step = 1
                    dsts = [(Wa, "Wa"), (Wb, "Wb")]
                    di = 0
                    while step < win:
                        dt_, dk = dsts[di % 2]
                        di += 1
                        lo = 2 * step - 1
                        P.add("dve", lambda e, dt_=dt_, src=src, lo=lo, step=step: e.tensor_tensor(
                            dt_[:, lo:528], src[:, lo:528], src[:, lo - step:528 - step], ALU.add), [sk], [dk])
                        src, sk = dt_, dk
                        step *= 2
                    P.add("dve", lambda e, src=src, Ut=Ut, pi=pi, c2=c2, win=win: e.scalar_tensor_tensor(
                        pld[pi][:, c2, 16:512], src[:, 32:528], 1.0 / win, Ut[:, 32:528], ALU.mult, ALU.subtract),
                        [sk, uk], [f"pld{pi}"])
                    P.add("dve", lambda e, src=src, lc=lc, g=g: e.tensor_tensor(
                        Wa[:, 0:16] if src is not Wa else Wb[:, 0:16], src[:, 16:32], rc16[:, lc, g, :], ALU.mult),
                        [sk, "rc16"], ["Wa" if src is not Wa else "Wb"])
                    P.add("dve", lambda e, src=src, Ut=Ut, pi=pi, c2=c2: e.tensor_tensor(
                        pld[pi][:, c2, 0:16], Wa[:, 0:16] if src is not Wa else Wb[:, 0:16], Ut[:, 16:32], ALU.subtract),
                        ["Wa" if src is not Wa else "Wb", uk], [f"pld{pi}"])
                for d2 in range(2):
                    b = npm[0] % 4
                    npm[0] += 1
                    for c2 in range(2):
                        P.mm(pm[b][:, :], pmix[:, g, c2, d2 * 128:(d2 + 1) * 128], pld[pi][:, c2, :], c2 == 0, c2 == 1,
                             ["pmix", f"pld{pi}"], [f"pm{b}"])
                    mi = nmx % 2
                    nmx += 1
                    ct = 2 * g + d2
                    P.add("dve", lambda e, mi=mi, b=b, ct=ct: e.tensor_scalar(
                        mxs[mi][:, :], pm[b][:, :], pscale[:, ct:ct + 1], None, ALU.mult), [f"pm{b}", "pscale"], [f"mxs{mi}"])
                    P.dma("sp", sc["MixT"][ct * 128:(ct + 1) * 128, lc * 512:(lc + 1) * 512], mxs[mi][:, :],
                          [f"mxs{mi}"], ["MixT"])
        P.emit(st)


def _bf16(a):
    import ml_dtypes
    return np.asarray(a, dtype=np.float32).astype(ml_dtypes.bfloat16)


def _rope_tab(pos, scale=1.0):
    half = HD // 2
    inv = (10000.0 ** (-2.0 * np.arange(half, dtype=np.float32) / HD)).astype(np.float32)
    ang = pos.astype(np.float32)[:, None] * inv[None, :]
    c = (np.cos(ang).astype(np.float32) * np.float32(scale)).astype(np.float32)
    s = (np.sin(ang).astype(np.float32) * np.float32(scale)).astype(np.float32)
    n = pos.shape[0] // 128
    c = np.ascontiguousarray(c.reshape(n, 128, half).transpose(1, 0, 2))
    s = np.ascontiguousarray(s.reshape(n, 128, half).transpose(1, 0, 2))
    return c, s


def _core_tables(p):
    t = {}
    own_pos = np.concatenate([np.arange(512 * g, 512 * g + 512) for g in OWN[p]])
    prev_pos = np.concatenate([np.arange(512 * (g - 1), 512 * g) if g > 0 else np.zeros(512, np.int64) for g in OWN[p]])
    prev_valid = np.concatenate([np.full(512, 1.0 if g > 0 else 0.0, np.float32) for g in OWN[p]])
    t["own_pos"], t["prev_pos"], t["prev_valid"] = own_pos, prev_pos, prev_valid
    t["cosg"], t["sing"] = _rope_tab(np.arange(S))
    t["cosp"], t["sinp"] = _rope_tab(prev_pos)
    t["coso"], t["sino"] = _rope_tab(own_pos)
    t["cosq"], t["sinq"] = _rope_tab(own_pos, HD ** -0.5)
    rc = np.zeros((128, 4, 4, 16), np.float32)
    for lc, g in enumerate(OWN[p]):
        for gi, w in enumerate((2, 4, 8, 16)):
            for tt in range(16):
                rc[:, lc, gi, tt] = 1.0 / (min(tt + 1, w) if g == 0 else w)
    t["rc16"] = rc
    return t


_IN_A = {
    "ident": ([128, 128], BF16), "xTg": ([D, S], F32), "xTo": ([D, NT], F32), "xTp": ([D, NT], F32),
    "w_A1": ([D, 1024], F32), "w_A2": ([D, 512], F32), "w_q": ([D, 1024], F32), "w_gn": ([D, 48], F32),
    "w_pool": ([D, 1024], F32), "w_gm": ([D, 4096], F32), "pool_mix": ([4, 256, 256], F32),
    "pool_scale": ([128, 8], F32), "rc16": ([128, 4, 4, 16], F32),
    "cosg": ([128, 32, 32], F32), "sing": ([128, 32, 32], F32), "cosp": ([128, 16, 32], F32),
    "sinp": ([128, 16, 32], F32), "coso": ([128, 16, 32], F32), "sino": ([128, 16, 32], F32),
    "cosq": ([128, 16, 32], F32), "sinq": ([128, 16, 32], F32),
}
_SC_A = {
    "KT_A1": ([6, 128, S], BF16), "Vs": ([S, 256], BF16), "KwT_prev": ([2, 128, NT], BF16),
    "KwT_own": ([2, 128, NT], BF16), "Vw_prev": ([NT, 256], BF16), "Vw_own": ([NT, 256], BF16),
    "QT": ([8, 128, NT], BF16), "Gn": ([NT, 48], F32), "Gm": ([NT, 4096], BF16), "MixT": ([1024, NT], BF16),
    "OT": ([8, 128, NT], BF16),
}


def build_nc(in_specs, sc_specs, phases, final_out=None, ncores=NCORES):
    nc = bass.Bass("TRN2", target_bir_lowering=False, num_devices=ncores)
    io = {n: _dram(nc, n, shp, dt, "ExternalInput") for n, (shp, dt) in in_specs.items()}
    sc = {n: _scratch(nc, n, shp, dt) for n, (shp, dt) in sc_specs.items()}
    if final_out is not None:
        n, shp, dt = final_out
        sc[n] = _dram(nc, n, shp, dt, "ExternalOutput")
    for ph in phases:
        if ph == "core_barrier":
            nc.all_core_barrier()
            continue
        snap = nc.snapshot_sems()
        ph(nc, io, sc)
        nc.clear_and_free_semaphores(nc.allocated_since(snap))
        nc.all_engine_barrier()
    return nc


def _ts(P, out, in0, s1, s2, op0, op1, reads, writes, eng="dve"):
    if op1 is None:
        return P.add(eng, lambda e: e.tensor_scalar(out, in0, s1, None, op0), reads, writes)
    return P.add(eng, lambda e: e.tensor_scalar(out, in0, s1, s2, op0, op1), reads, writes)


def _tt(P, out, in0, in1, op, reads, writes, eng="dve"):
    return P.add(eng, lambda e: e.tensor_tensor(out, in0, in1, op), reads, writes)


def _stt(P, out, in0, sc_, in1, op0, op1, reads, writes):
    return P.add("dve", lambda e: e.scalar_tensor_tensor(out, in0, sc_, in1, op0, op1), reads, writes)


def _act(P, out, in_, func, reads, writes, bias=None, scale=None):
    kw = {}
    if bias is not None:
        kw["bias"] = bias
    if scale is not None:
        kw["scale"] = scale
    return P.add("act", lambda e: e.activation(out, in_, func, **kw), reads, writes)


def phase_B(nc, io, sc):
    with ExitStack() as st:
        C = Ctx(nc, st, "B")
        P = C.P
        ident = C.sb("ident", [128, 128], BF16)
        P.dma("sp", ident[:, :], io["ident"], [], ["ident"])
        Ksp = C.sb("Ksp", [128, S], BF16)
        P.dma("sp", Ksp[64:128, :], io["Eoh"], [], ["Ksp_e"])
        Qp = C.sb("Qp", [128, 4, NT], BF16)
        Vsp = C.sb("Vsp", [128, 32, 65], BF16)
        Kwp = C.sb("Kwp", [64, 4, 8, 128], BF16)
        Vwp = C.sb("Vwp", [128, 4, 8, 65], BF16)
        pvo = C.sb("pvo", [128, 16], BF16)
        P.dma("sp", pvo[:, :], io["pvones"], [], ["pvo"])
        P.add("pool", lambda e: e.memset(Vsp[:, :, 64:65], 1.0), [], ["Vsp_1"])
        P.add("pool", lambda e: e.memset(Vwp[:, :, 4:8, 64:65], 1.0), [], ["Vwp_1"])
        P.add("pool", lambda e: e.tensor_copy(Vwp[:, :, 0:4, 64:65].rearrange("p a b c -> p a (b c)"),
                                              pvo[:, :].rearrange("p (a b) -> p a b", a=4)), ["pvo"], ["Vwp_1"])
        KcT = C.sb("KcT", [64, S], BF16)
        VcT = C.sb("VcT", [64, S], BF16)
        w1 = [C.sb(f"w1_{i}", [64, 32, 256], BF16) for i in range(2)]
        w2 = C.sb("w2", [128, 3, 2, 64], BF16)
        posT = C.sb("posT", [64, 2, 32], BF16)
        P.dma("pool", w1[0][:, :, :], io["cmp_k_w1"].rearrange("(l d) j -> d l j", d=64), [], ["w1_0"])
        P.dma("pool", w1[1][:, :, :], io["cmp_v_w1"].rearrange("(l d) j -> d l j", d=64), [], ["w1_1"])
        for i, n in enumerate(("cmp_k_w2", "cmp_k_w2s", "cmp_v_w2")):
            P.dma("pool", w2[:, i, :, :], io[n].rearrange("(t p) d -> p t d", p=128), [], ["w2"])
        P.dma("pool", posT[:, 0, :], io["cmp_pos_kT"], [], ["posT"])
        P.dma("pool", posT[:, 1, :], io["cmp_pos_vT"], [], ["posT"])
        ccos = C.sb("ccos", [64, 256], F32)
        csin = C.sb("csin", [64, 256], F32)
        P.dma("sp", ccos[:, :], io["ccos"], [], ["ccos"])
        P.dma("sp", csin[:, :], io["csin"], [], ["csin"])
        cbias = C.sb("cbias", [128, 2, 2, 512], BF16)
        tritab = C.sb("tritab", [128, 4, 8, 128], BF16)
        P.dma("sp", tritab[:, :, :, :], io["tritab"], [], ["tritab"])
        tri2 = C.sb("tri2", [128, 2, 128], BF16)
        P.dma("sp", tri2[:, :, :], io["tri2"], [], ["tri2"])
        Vcp = C.sb("Vcp", [128, 2, 129], BF16)
        P.add("pool", lambda e: e.memset(Vcp[:, :, 0:65], 0.0), [], ["Vcp"])
        P.add("pool", lambda e: e.memset(Vcp[:, :, 64:65], 1.0), [], ["Vcp"])
        P.dma("sp", Vcp[:, :, 65:129], io["ovl"], [], ["Vcp_o"])
        KcmpT = C.sb("KcmpT", [64, 256], BF16)
        P.add("pool", lambda e: e.memset(KcmpT[:, :], 0.0), [], ["KcmpT"])
        Mk = C.sb("Mk", [128, 16, 64], F32)
        Ad = C.sb("Ad", [128, 16, 64], F32)
        Fm = C.sb("Fm", [128, 16, 64], F32)
        for t, n in ((Mk, "Mk"), (Ad, "Ad"), (Fm, "Fm")):
            P.dma("sp", t[:, :, :], io[n], [], [n])
        Gn = C.sb("Gn", [128, 16, 48], F32)
        P.dma("sp", Gn[:, :, :], sc["Gn"].rearrange("(t p) n -> p t n", p=128), [], ["Gn"])
        O = C.sb("O", [128, 16, 256], F32)
        Ob = [C.sb(f"Ob{i}", [128, 256], BF16) for i in range(2)]
        Os = [C.sb(f"Os{i}", [128, 2, 128], BF16) for i in range(2)]
        Pt = [C.sb(f"Pt{i}", [128, 512], BF16) for i in range(4)]
        Pc = [C.sb(f"Pc{i}", [128, 2, 512], BF16) for i in range(2)]
        Pw = [C.sb(f"Pw{i}", [128, 5, 128], BF16) for i in range(3)]
        hb = C.sb("hb", [128, 256], F32)
        sq = C.sb("sq", [128, 256], F32)
        uu = C.sb("uu", [128, 256], F32)
        sg = C.sb("sg", [128, 256], F32)
        gT = C.sb("gT", [128, 2, 2, 256], BF16)
        cb = C.sb("cb", [128, 4], F32)
        impb = C.sb("impb", [128, 4, 64], F32)
        impm = C.sb("impm", [128, 64], F32)
        imp2 = C.sb("imp2", [128, 64], F32)
        m8 = C.sb("m8", [128, 16], F32)
        nsel = C.sb("nsel", [128, 64], F32)
        NegT = C.sb("NegT", [128, 4, 128], BF16)
        P.add("pool", lambda e: e.memset(NegT[:, :, :], 0.0), [], [f"NegT{i}" for i in range(4)])
        sm = C.sb("sm", [128, 16, 4], F32)
        nsm = [0]
        t1 = C.sb("t1", [64, 256], F32)
        t2 = C.sb("t2", [64, 256], F32)
        pS = [C.ps(f"pS{i}", [128, 512], F32) for i in range(4)]
        pA = [C.ps(f"pA{i}", [128, 512], F32) for i in range(2)]
        pX = C.ps("pX", [128, 512], F32)
        pTb = C.ps("pTb", [128, 8, 128], BF16)

        for kv in range(2):
            for jt in range(2):
                for l in range(32):
                    P.mm(pX[:, 0:1], w1[kv][:, l, jt * 128:(jt + 1) * 128], posT[:, kv, l:l + 1], l == 0, l == 31,
                         [f"w1_{kv}", "posT"], ["pX"])
                i = kv * 2 + jt
                P.add("dve", lambda e, i=i: e.tensor_copy(cb[:, i:i + 1], pX[:, 0:1]), ["pX"], ["cb"])

        npt = [0]
        npc = [0]
        npw = [0]
        nps = [0]

        for g in (range(4) if GROUPS is None else GROUPS):
            pi, hf = g // 2, g % 2
            rows = slice(hf * 64, hf * 64 + 64)
            P.dma("sp", Ksp[0:64, :], sc["KT_A1"][pi, rows, :], ["KT_A1"], ["Ksp_k"])
            P.dma("sp", KcT[:, :], sc["KT_A1"][2 + pi, rows, :], ["KT_A1"], ["KcT"])
            P.dma("sp", VcT[:, :], sc["KT_A1"][4 + pi, rows, :], ["KT_A1"], ["VcT"])
            P.dma("sp", Vsp[:, :, 0:64], sc["Vs"][:, g * 64:(g + 1) * 64].rearrange("(t p) d -> p t d", p=128),
                  ["Vs"], ["Vsp_v"])
            for lc in range(4):
                P.dma("sp", Kwp[:, lc, 0:4, :], sc["KwT_prev"][pi, rows, lc * 512:(lc + 1) * 512], ["KwT_prev"], ["Kwp"])
                P.dma("sp", Kwp[:, lc, 4:8, :], sc["KwT_own"][pi, rows, lc * 512:(lc + 1) * 512], ["KwT_own"], ["Kwp"])
                P.dma("sp", Vwp[:, lc, 0:4, 0:64],
                      sc["Vw_prev"][lc * 512:(lc + 1) * 512, g * 64:(g + 1) * 64].rearrange("(t p) d -> p t d", p=128),
                      ["Vw_prev"], ["Vwp_v"])
                P.dma("sp", Vwp[:, lc, 4:8, 0:64],
                      sc["Vw_own"][lc * 512:(lc + 1) * 512, g * 64:(g + 1) * 64].rearrange("(t p) d -> p t d", p=128),
                      ["Vw_own"], ["Vwp_v"])
            for hh in range(4):
                h = 4 * g + hh
                P.dma("sp", Qp[0:64, hh, :], sc["QT"][h // 2, (h % 2) * 64:(h % 2) * 64 + 64, :], ["QT"], ["Qp_q"])

            for kv, src, skey in ((0, KcT, "KcT"), (1, VcT, "VcT")):
                for jt in range(2):
                    for l in range(32):
                        P.mm(pX[:, 0:255], w1[kv][:, l, jt * 128:(jt + 1) * 128], src[:, l:l + 16 * 254 + 1:16],
                             l == 0, l == 31, [f"w1_{kv}", skey], ["pX"])
                    i = kv * 2 + jt
                    _act(P, sq[:, 0:255], pX[:, 0:255], AF.Square, ["pX", "cb"], ["sq"], bias=cb[:, i:i + 1])
                    _ts(P, hb[:, 0:255], pX[:, 0:255], cb[:, i:i + 1], None, ALU.add, None, ["pX", "cb"], ["hb"])
                    _ts(P, uu[:, 0:255], sq[:, 0:255], 0.044715, 1.0, ALU.mult, ALU.add, ["sq"], ["uu"])
                    _tt(P, uu[:, 0:255], uu[:, 0:255], hb[:, 0:255], ALU.mult, ["uu", "hb"], ["uu"])
                    _act(P, sg[:, 0:255], uu[:, 0:255], AF.Sigmoid, ["uu"], ["sg"], scale=1.5957691216057308)
                    _tt(P, gT[:, kv, jt, 0:255], hb[:, 0:255], sg[:, 0:255], ALU.mult, ["hb", "sg"], ["gT"])
            for jt in range(2):
                P.mm(pX[0:64, 0:255], w2[:, 0, jt, :], gT[:, 0, jt, 0:255], jt == 0, jt == 1, ["w2", "gT"], ["pX"])
            _tt(P, t1[:, 0:255], pX[0:64, 0:255], ccos[:, 0:255], ALU.mult, ["pX", "ccos"], ["t1"])
            for jt in range(2):
                P.mm(pX[0:64, 0:255], w2[:, 1, jt, :], gT[:, 0, jt, 0:255], jt == 0, jt == 1, ["w2", "gT"], ["pX"])
            _tt(P, t2[:, 0:255], pX[0:64, 0:255], csin[:, 0:255], ALU.mult, ["pX", "csin"], ["t2"])
            _tt(P, KcmpT[:, 0:255], t1[:, 0:255], t2[:, 0:255], ALU.add, ["t1", "t2"], ["KcmpT"])
            for ct in range(2):
                n = 128 if ct == 0 else 127
                for jt in range(2):
                    P.mm(pX[0:n, 0:64], gT[:, 1, jt, ct * 128:ct * 128 + n], w2[:, 2, jt, :], jt == 0, jt == 1,
                         ["gT", "w2"], ["pX"])
                P.add("dve", lambda e, ct=ct, n=n: e.tensor_copy(Vcp[0:n, ct, 0:64], pX[0:n, 0:64]), ["pX"], ["Vcp"])

            for lc in range(4):
                cbi = lc % 2
                P.dma("sp", cbias[:, :, cbi, :], io["cmpbias"][:, :, lc * 512:(lc + 1) * 512], [], [f"cbias{cbi}"])
                qsl = slice(lc * 512, (lc + 1) * 512)

                def norm(acc_ap, acc_key, tt, hh, gcol, first):
                    si = nsm[0] % 16
                    nsm[0] += 1
                    sk = f"sm{si}"
                    _ts(P, sm[:, si, 0:1], acc_ap[:, 64:65], 1e-30, None, ALU.max, None, [acc_key], [sk])
                    P.add("dve", lambda e: e.reciprocal(sm[:, si, 1:2], sm[:, si, 0:1]), [sk], [sk])
                    _tt(P, sm[:, si, 2:3], sm[:, si, 1:2], Gn[:, tt, gcol:gcol + 1], ALU.mult, [sk, "Gn"], [sk])
                    osl = O[:, tt, hh * 64:(hh + 1) * 64]
                    if first:
                        _ts(P, osl, acc_ap[:, 0:64], sm[:, si, 2:3], None, ALU.mult, None, [acc_key, sk], [f"O{tt}"])
                    else:
                        _stt(P, osl, acc_ap[:, 0:64], sm[:, si, 2:3], osl, ALU.mult, ALU.add, [acc_key, sk, f"O{tt}"], [f"O{tt}"])
                    return sm[:, si, 1:2], sk

                b1banks = {}

                def b1_qk(hh):
                    bs = []
                    for ct in range(2):
                        b = nps[0] % 2
                        nps[0] += 1
                        P.mm(pS[b][:, :], KcmpT[:, ct * 128:(ct + 1) * 128], Qp[0:64, hh, qsl], True, False,
                             ["KcmpT", "Qp_q"], [f"pS{b}"])
                        P.mm(pS[b][:, :], ident[:, :], cbias[:, ct, cbi, :], False, True,
                             ["ident", f"cbias{cbi}"], [f"pS{b}"])
                        bs.append(b)
                    b1banks[hh] = bs

                def b1_rest(hh):
                    h = 4 * g + hh
                    pci = npc[0] % 2
                    npc[0] += 1
                    for ct in range(2):
                        b = b1banks[hh][ct]
                        _act(P, Pc[pci][:, ct, :], pS[b][:, :], AF.Exp, [f"pS{b}"], [f"Pc{pci}"])
                    accs = (pA if hh % 2 == 0 else pS[2:4])
                    akeys = (["pA0", "pA1"] if hh % 2 == 0 else ["pS2", "pS3"])
                    for qs in range(4):
                        acc = accs[qs // 2][:, (qs % 2) * 256:(qs % 2) * 256 + 129]
                        ak = akeys[qs // 2]
                        for ct in range(2):
                            P.mm(acc, Pc[pci][:, ct, qs * 128:(qs + 1) * 128], Vcp[:, ct, :], ct == 0, ct == 1,
                                 [f"Pc{pci}", "Vcp", "Vcp_o"], [ak])
                    for qs in range(4):
                        tt = lc * 4 + qs
                        acc = accs[qs // 2][:, (qs % 2) * 256:(qs % 2) * 256 + 129]
                        ak = akeys[qs // 2]
                        rz, sk = norm(acc, ak, tt, hh, h, True)
                        if hh == 0:
                            _ts(P, impb[:, qs, :], acc[:, 65:129], rz, None, ALU.mult, None, [ak, sk], [f"impb{qs}"])
                        else:
                            _stt(P, impb[:, qs, :], acc[:, 65:129], rz, impb[:, qs, :], ALU.mult, ALU.add,
                                 [ak, sk, f"impb{qs}"], [f"impb{qs}"])

                for hh in range(4):
                    b1_qk(hh)
                    b1_rest(hh)

                for qs in range(4):
                    tt = lc * 4 + qs
                    iq = impb[:, qs, :]
                    _tt(P, impm[:, :], iq, Mk[:, tt, :], ALU.mult, [f"impb{qs}", "Mk"], ["impm"])
                    _tt(P, impm[:, :], impm[:, :], Ad[:, tt, :], ALU.add, ["impm", "Ad"], ["impm"])
                    P.add("dve", lambda e: e.max(m8[:, 0:8], impm[:, :]), ["impm"], ["m8"])
                    P.add("dve", lambda e: e.match_replace(imp2[:, :], m8[:, 0:8], impm[:, :], -3.0e6), ["impm", "m8"], ["imp2"])
                    P.add("dve", lambda e: e.max(m8[:, 8:16], imp2[:, :]), ["imp2"], ["m8"])
                    _ts(P, nsel[:, :], impm[:, :], m8[:, 15:16], -NEG, ALU.is_ge, ALU.mult, ["impm", "m8"], ["nsel"])
                    _stt(P, NegT[:, qs, 64:128], nsel[:, :], NEG, Fm[:, tt, :], ALU.add, ALU.add, ["nsel", "Fm"], [f"NegT{qs}"])

                wunits = [(hh, qs) for hh in range(4) for qs in range(4)]
                wbanks = {}

                def b3_qk(u):
                    hh, qs = wunits[u]
                    tt = lc * 4 + qs
                    b = nps[0] % 4
                    b2 = (nps[0] + 1) % 4
                    nps[0] += 2
                    qap = Qp[0:64, hh, tt * 128:(tt + 1) * 128]
                    for r in range(qs, qs + 4):
                        o = pS[b][:, (r - qs) * 128:(r - qs + 1) * 128]
                        P.mm(o, Kwp[:, lc, r, :], qap, True, r != qs, ["Kwp", "Qp_q"], [f"pS{b}"])
                        if r == qs:
                            P.mm(o, ident[:, :], tri2[:, 0, :], False, True, ["ident", "tri2"], [f"pS{b}"])
                    P.mm(pS[b2][:, 0:128], Kwp[:, lc, qs + 4, :], qap, True, False, ["Kwp", "Qp_q"], [f"pS{b2}"])
                    P.mm(pS[b2][:, 0:128], ident[:, :], tri2[:, 1, :], False, True, ["ident", "tri2"], [f"pS{b2}"])
                    wbanks[u] = (b, b2)

                def b3_rest(u):
                    hh, qs = wunits[u]
                    h = 4 * g + hh
                    tt = lc * 4 + qs
                    b, b2 = wbanks[u]
                    pwi = npw[0] % 3
                    npw[0] += 1
                    _act(P, Pw[pwi][:, 0:4, :], pS[b][:, :].rearrange("p (a b) -> p a b", a=4), AF.Exp, [f"pS{b}"], [f"Pw{pwi}"])
                    _act(P, Pw[pwi][:, 4, :], pS[b2][:, 0:128], AF.Exp, [f"pS{b2}"], [f"Pw{pwi}"])
                    ai = u % 2
                    acc = pA[ai][:, 0:65]
                    for r in range(5):
                        P.mm(acc, Pw[pwi][:, r, :], Vwp[:, lc, qs + r, :], r == 0, r == 4,
                             [f"Pw{pwi}", "Vwp_v", "Vwp_1"], [f"pA{ai}"])
                    norm(acc, f"pA{ai}", tt, hh, 32 + h, False)

                b3_qk(0)
                for u in range(16):
                    if u + 1 < 16:
                        b3_qk(u + 1)
                    b3_rest(u)

                for qs in range(4):
                    tt = lc * 4 + qs
                    P.tr(pTb[:, 4 + qs, :], NegT[:, qs, :], ident[:, :], [f"NegT{qs}", "ident"], ["pTb"])
                    P.add("act", lambda e, tt=tt, qs=qs: e.copy(
                        Qp[64:128, :, tt * 128:(tt + 1) * 128],
                        pTb[64:128, 4 + qs:5 + qs, :].broadcast_to([64, 4, 128])), ["pTb"], ["Qp_m"])

                E = 8 * (lc + 1)
                sunits = [(hh, kt) for hh in range(4) for kt in range(E)]
                sbanks = {}

                def b2_qk(u):
                    hh, kt = sunits[u]
                    b = nps[0] % 4
                    nps[0] += 1
                    s = kt - (E - 8)
                    P.mm(pS[b][:, :], Ksp[:, kt * 128:(kt + 1) * 128], Qp[:, hh, qsl], True, s < 0,
                         ["Ksp_k", "Ksp_e", "Qp_q", "Qp_m"], [f"pS{b}"])
                    if s >= 0:
                        qd = s % 4
                        P.mm(pS[b][:, qd * 128:(qd + 1) * 128], ident[:, :], tritab[:, lc, s, :], False, True,
                             ["ident", "tritab"], [f"pS{b}"])
                    sbanks[u] = b

                def b2_rest(u):
                    hh, kt = sunits[u]
                    h = 4 * g + hh
                    b = sbanks[u]
                    pti = npt[0] % 4
                    npt[0] += 1
                    _act(P, Pt[pti][:, :], pS[b][:, :], AF.Exp, [f"pS{b}"], [f"Pt{pti}"])
                    ai = hh % 2
                    for qs in range(4):
                        P.mm(pA[ai][:, qs * 128:qs * 128 + 65], Pt[pti][:, qs * 128:(qs + 1) * 128], Vsp[:, kt, :],
                             kt == 0 and qs == 0, kt == E - 1, [f"Pt{pti}", "Vsp_v", "Vsp_1"], [f"pA{ai}"], skip=True)
                    if kt == E - 1:
                        for qs in range(4):
                            norm(pA[ai][:, qs * 128:qs * 128 + 65], f"pA{ai}", lc * 4 + qs, hh, 16 + h, False)

                LA = 2
                nsu = len(sunits)
                for u in range(min(LA, nsu)):
                    b2_qk(u)
                for u in range(nsu):
                    if u + LA < nsu:
                        b2_qk(u + LA)
                    b2_rest(u)

            for tt in range(16):
                oi = tt % 2
                P.add("act", lambda e, oi=oi, tt=tt: e.copy(Ob[oi][:, :], O[:, tt, :]), [f"O{tt}"], [f"Ob{oi}"])
                for i in range(2):
                    P.tr(pTb[:, 2 + i, :], Ob[oi][:, i * 128:(i + 1) * 128], ident[:, :], [f"Ob{oi}", "ident"], ["pTb"])
                P.add("dve", lambda e, oi=oi: e.tensor_copy(Os[oi][:, :, :], pTb[:, 2:4, :]), ["pTb"], [f"Os{oi}"])
                P.dma("sp", sc["OT"][2 * g:2 * g + 2].rearrange("i p t -> p i t")[:, :, tt * 128:(tt + 1) * 128],
                      Os[oi][:, :, :], [f"Os{oi}"], ["OT"])
        P.emit(st)


def _core_tables_B(p, t):
    own_pos = t["own_pos"]
    c = np.arange(256)
    cend = 16 * c + 31
    valid = (c[:, None] <= 254) & (cend[:, None] <= own_pos[None, :])
    cb = np.where(valid, 0.0, NEG).astype(np.float32).reshape(2, 128, NT).transpose(1, 0, 2)
    t["cmpbias"] = _bf16(np.ascontiguousarray(cb))
    k = np.arange(128)
    tri = np.where(k[:, None] > k[None, :], NEG, 0.0).astype(np.float32)
    tt = np.zeros((128, 4, 8, 128), np.float32)
    for lc, gc in enumerate(OWN[p]):
        E = 8 * (lc + 1)
        for s in range(8):
            kt = E - 8 + s
            if 4 * gc <= kt < 4 * gc + 4:
                assert (kt - 4 * gc) == s % 4
                tt[:, lc, s, :] = tri
    t["tritab"] = _bf16(tt)
    tri2 = np.zeros((128, 2, 128), np.float32)
    tri2[:, 0, :] = np.where(k[:, None] <= k[None, :], NEG, 0.0)
    tri2[:, 1, :] = np.where(k[:, None] > k[None, :], NEG, 0.0)
    t["tri2"] = _bf16(tri2)
    cs = np.arange(256) * 16
    ss = np.arange(64) * 64
    ov = ((cs[:, None] + 31 >= ss[None, :]) & (cs[:, None] <= ss[None, :] + 63) & (c[:, None] <= 254)).astype(np.float32)
    t["ovl"] = _bf16(np.ascontiguousarray(ov.reshape(2, 128, 64).transpose(1, 0, 2)))
    cur = own_pos // 64
    j = np.arange(64)
    forced = (j[None, :] == 0) | (j[None, :] == cur[:, None]) | (j[None, :] == cur[:, None] - 1)
    future = j[None, :] > cur[:, None]
    mk = (~(forced | future)).astype(np.float32)
    ad = np.where(forced, 1e6, np.where(future, -1e6, 0.0)).astype(np.float32)
    fm = np.where(future, NEG, 0.0).astype(np.float32)
    lay = lambda a: np.ascontiguousarray(a.reshape(16, 128, 64).transpose(1, 0, 2))
    t["Mk"], t["Ad"], t["Fm"] = lay(mk), lay(ad), lay(fm)
    t["pvones"] = _bf16(np.ascontiguousarray(t["prev_valid"].reshape(16, 128).T))
    t["Eoh"] = _bf16((np.arange(S)[None, :] // 64 == j[:, None]).astype(np.float32))
    half = HD // 2
    inv = (10000.0 ** (-2.0 * np.arange(half, dtype=np.float32) / HD)).astype(np.float32)
    ang = cend.astype(np.float32)[None, :] * np.concatenate([inv, inv])[:, None]
    t["ccos"] = np.cos(ang).astype(np.float32)
    sn = np.sin(ang).astype(np.float32)
    sn[:half] *= -1.0
    t["csin"] = sn
    return t


_IN_B = {
    "Eoh": ([64, S], BF16), "pvones": ([128, 16], BF16), "cmp_k_w1": ([2048, 256], F32), "cmp_v_w1": ([2048, 256], F32),
    "cmp_k_w2": ([256, 64], F32), "cmp_k_w2s": ([256, 64], F32), "cmp_v_w2": ([256, 64], F32),
    "cmp_pos_kT": ([64, 32], F32), "cmp_pos_vT": ([64, 32], F32), "ccos": ([64, 256], F32), "csin": ([64, 256], F32),
    "tritab": ([128, 4, 8, 128], BF16), "tri2": ([128, 2, 128], BF16), "ovl": ([128, 2, 64], BF16),
    "cmpbias": ([128, 2, NT], BF16), "Mk": ([128, 16, 64], F32), "Ad": ([128, 16, 64], F32), "Fm": ([128, 16, 64], F32),
}


def phase_C1(nc, io, sc):
    with ExitStack() as st:
        C = Ctx(nc, st, "C1")
        P = C.P
        ident = C.sb("ident", [128, 128], BF16)
        P.dma("sp", ident[:, :], io["ident"], [], ["ident"])
        wn = C.sb("wn", [128, 8, 2048], BF16)
        wp = C.sb("wp", [128, 8, 2048], BF16)
        for c in range(4):
            cs = slice(c * 512, (c + 1) * 512)
            P.dma("pool", wn[:, :, cs], io["w_nsa_proj"][:, cs].rearrange("(k p) n -> p k n", p=128), [], ["wn"])
            P.dma("pool", wp[:, :, cs], io["w_pool_proj"][:, cs].rearrange("(k p) n -> p k n", p=128), [], ["wp"])
        oT = [C.sb(f"oT{i}", [128, 8, 128], BF16) for i in range(2)]
        mT = [C.sb(f"mT{i}", [128, 8, 128], BF16) for i in range(2)]
        gm = [C.sb(f"gm{i}", [128, 4096], BF16) for i in range(2)]
        ta = [C.sb(f"ta{i}", [128, 512], F32) for i in range(2)]
        tb = [C.sb(f"tb{i}", [128, 512], F32) for i in range(2)]
        z = [C.sb(f"z{i}", [128, 2048], BF16) for i in range(2)]
        zT = [C.sb(f"zT{i}", [128, 16, 128], BF16) for i in range(2)]
        pa = [C.ps(f"pa{i}", [128, 512], F32) for i in range(2)]
        pb = [C.ps(f"pb{i}", [128, 512], F32) for i in range(2)]
        pT = [C.ps(f"pT{i}", [128, 8, 128], BF16) for i in range(2)]
        n = [0]

        def c1_stage1(tt):
            i = tt % 2
            ts_ = slice(tt * 128, (tt + 1) * 128)
            P.dma("sp", oT[i][:, :, :], sc["OT"].rearrange("k p t -> p k t")[:, :, ts_], ["OT"], [f"oT{i}"])
            P.dma("sp", mT[i][:, :, :], sc["MixT"].rearrange("(k p) t -> p k t", p=128)[:, :, ts_], ["MixT"], [f"mT{i}"])
            P.dma("sp", gm[i][:, :], sc["Gm"][ts_, :], ["Gm"], [f"gm{i}"])
            for cc in range(4):
                j = n[0] % 2
                n[0] += 1
                cs = slice(cc * 512, (cc + 1) * 512)
                for k in range(8):
                    P.mm(pa[j][:, :], oT[i][:, k, :], wn[:, k, cs], k == 0, k == 7, [f"oT{i}", "wn"], [f"pa{j}"])
                for k in range(8):
                    P.mm(pb[j][:, :], mT[i][:, k, :], wp[:, k, cs], k == 0, k == 7, [f"mT{i}", "wp"], [f"pb{j}"])
                _tt(P, ta[j][:, :], pa[j][:, :], gm[i][:, 2048 + cc * 512:2048 + (cc + 1) * 512], ALU.mult,
                    [f"pa{j}", f"gm{i}"], [f"ta{j}"])
                _tt(P, tb[j][:, :], pb[j][:, :], gm[i][:, cs], ALU.mult, [f"pb{j}", f"gm{i}"], [f"tb{j}"])
                _tt(P, z[i][:, cs], ta[j][:, :], tb[j][:, :], ALU.add, [f"ta{j}", f"tb{j}"], [f"z{i}"], eng="pool")

        def c1_stage2(tt):
            i = tt % 2
            ts_ = slice(tt * 128, (tt + 1) * 128)
            for hh in range(2):
                for k in range(8):
                    kk = hh * 8 + k
                    P.tr(pT[hh][:, k, :], z[i][:, kk * 128:(kk + 1) * 128], ident[:, :], [f"z{i}", "ident"], [f"pT{hh}"])
                P.add("act", lambda e, hh=hh: e.copy(zT[i][:, hh * 8:(hh + 1) * 8, :], pT[hh][:, :, :]),
                      [f"pT{hh}"], [f"zT{i}"])
            P.dma("sp", sc["ZT"].rearrange("k p t -> p k t")[:, :, ts_], zT[i][:, :, :], [f"zT{i}"], ["ZT"])

        c1_stage1(0)
        for tt in range(16):
            if tt + 1 < 16:
                c1_stage1(tt + 1)
            c1_stage2(tt)
        P.emit(st)


def _layer_norm(P, dst, src, skey, dkey, g_bc, b_bc, gkeys, st6, mv, tmp, tkeys):
    for c in range(4):
        P.add("dve", lambda e, c=c: e.bn_stats(st6[:, c * 6:(c + 1) * 6], src[:, c * 512:(c + 1) * 512]), [skey], [tkeys[0]])
    P.add("dve", lambda e: e.bn_aggr(mv[:, 0:2], st6[:, 0:24]), [tkeys[0]], [tkeys[1]])
    _act(P, mv[:, 2:3], mv[:, 1:2], AF.Sqrt, [tkeys[1]], [tkeys[1]], bias=mv[:, 4:5])
    P.add("dve", lambda e: e.reciprocal(mv[:, 3:4], mv[:, 2:3]), [tkeys[1]], [tkeys[1]])
    _ts(P, tmp[:, :], src[:, :], mv[:, 0:1], mv[:, 3:4], ALU.subtract, ALU.mult, [skey, tkeys[1]], [tkeys[2]])
    _tt(P, tmp[:, :], tmp[:, :], g_bc[:, :], ALU.mult, [tkeys[2], gkeys[0]], [tkeys[2]], eng="pool")
    _tt(P, dst[:, :], tmp[:, :], b_bc[:, :], ALU.add, [tkeys[2], gkeys[1]], [dkey])


def _breg(eng, cache):
    if "r" not in cache:
        cache["r"] = eng.to_reg(NROW - 1)
    return cache["r"]


def phase_C2(nc, io, sc):
    with ExitStack() as st:
        C = Ctx(nc, st, "C2")
        P = C.P
        breg = {}
        identf = C.sb("identf", [128, 128], F32)
        P.dma("sp", identf[:, :], io["identf"], [], ["identf"])
        wo = C.sb("wo", [128, 16, 2048], BF16)
        for c in range(4):
            cs = slice(c * 512, (c + 1) * 512)
            P.dma("pool", wo[:, :, cs], io["w_out"][:, cs].rearrange("(k p) n -> p k n", p=128), [], ["wo"])
        wr = C.sb("wr", [128, 16, 72], F32)
        P.dma("sp", wr[:, :, :], io["w_router"].rearrange("(k p) n -> p k n", p=128), [], ["wr"])
        br = C.sb("br", [128, 72], F32)
        P.dma("sp", br[:, :], io["b_router"], [], ["br"])
        eid64 = C.sb("eid64", [128, 64], F32)
        P.dma("sp", eid64[:, :], io["eid64"], [], ["eid64"])
        pbase = C.sb("pbase", [128, 1], F32)
        P.dma("sp", pbase[:, :], io["pbase"], [], ["pbase"])
        g1 = C.sb("g1", [128, 2048], F32)
        b1 = C.sb("b1", [128, 2048], F32)
        P.dma("sp", g1[:, :], io["ln1_g"], [], ["g1"])
        P.dma("sp", b1[:, :], io["ln1_b"], [], ["b1"])
        Ut = C.sb("Ut", [128, 128], BF16)
        P.dma("sp", Ut[:, :], io["utri"], [], ["Ut"])
        ones = C.sb("ones", [128, 128], BF16)
        P.add("pool", lambda e: e.memset(ones[:, :], 1.0), [], ["ones"])
        accind = C.sb("accind", [128, 64], F32)
        P.add("pool", lambda e: e.memset(accind[:, :], 0.0), [], ["accind"])
        zT = [C.sb(f"zT{i}", [128, 16, 128], BF16) for i in range(2)]
        xt = [C.sb(f"xt{i}", [128, 2048], F32) for i in range(2)]
        r = [C.sb(f"r{i}", [128, 2048], F32) for i in range(2)]
        tmp = C.sb("tmp", [128, 2048], F32)
        h1 = [C.sb(f"h1_{i}", [128, 2048], F32) for i in range(2)]
        h1b = [C.sb(f"h1b{i}", [128, 2048], BF16) for i in range(2)]
        h1T = C.sb("h1T", [128, 16, 128], F32)
        st6 = C.sb("st6", [128, 24], F32)
        mv = C.sb("mv", [128, 8], F32)
        P.add("pool", lambda e: e.memset(mv[:, 4:5], LN_EPS), [], ["mv"])
        lg = C.sb("lg", [128, 72], F32)
        rt = [C.sb(f"rt{i}", [128, 64], F32) for i in range(2)]
        e3 = C.sb("e3", [128, 8, 8], F32)
        E1 = [C.sb(f"E1_{i}", [128, 8, 8], F32) for i in range(2)]
        E2 = [C.sb(f"E2_{i}", [128, 8, 8], F32) for i in range(2)]
        indb = [C.sb(f"indb{i}", [128, 64], BF16) for i in range(2)]
        accb = [C.sb(f"accb{i}", [128, 64], BF16) for i in range(2)]
        posf = C.sb("posf", [128, 64], F32)
        indf = C.sb("indf", [128, 64], F32)
        ridx = [C.sb(f"ridx{i}", [128, 2], I32) for i in range(2)]
        rw = [C.sb(f"rw{i}", [128, 2], F32) for i in range(2)]
        py = [C.ps(f"py{i}", [128, 512], F32) for i in range(4)]
        pt = [C.ps(f"pt{i}", [128, 4, 128], F32) for i in range(2)]
        pl = C.ps("pl", [128, 72], F32)
        pp = C.ps("pp", [128, 64], F32)
        npt = [0]

        def stage1(tt):
            i = tt % 2
            ts_ = slice(tt * 128, (tt + 1) * 128)
            P.dma("sp", zT[i][:, :, :], sc["ZT"].rearrange("k p t -> p k t")[:, :, ts_], ["ZT"], [f"zT{i}"])
            P.dma("sp", xt[i][:, :], io["x_own"][ts_, :], [], [f"xt{i}"])
            for cc in range(4):
                cs = slice(cc * 512, (cc + 1) * 512)
                for k in range(16):
                    P.mm(py[cc][:, :], zT[i][:, k, :], wo[:, k, cs], k == 0, k == 15, [f"zT{i}", "wo"], [f"py{cc}"])

        def stage1b(tt):
            i = tt % 2
            for cc in range(4):
                cs = slice(cc * 512, (cc + 1) * 512)
                _stt(P, r[i][:, cs], xt[i][:, cs], DN_ALPHA, py[cc][:, :], ALU.mult, ALU.add, [f"xt{i}", f"py{cc}"], [f"r{i}"])

        def s_ln1(tt):
            i = tt % 2
            for c in range(4):
                P.add("dve", lambda e, c=c: e.bn_stats(st6[:, c * 6:(c + 1) * 6], r[i][:, c * 512:(c + 1) * 512]), [f"r{i}"], ["st6"])
            P.add("dve", lambda e: e.bn_aggr(mv[:, 0:2], st6[:, 0:24]), ["st6"], ["mv"])
            _act(P, mv[:, 2:3], mv[:, 1:2], AF.Sqrt, ["mv"], ["mv"], bias=mv[:, 4:5])

        def s_ln2(tt):
            i = tt % 2
            ts_ = slice(tt * 128, (tt + 1) * 128)
            P.add("dve", lambda e: e.reciprocal(mv[:, 3:4], mv[:, 2:3]), ["mv"], ["mv"])
            _ts(P, tmp[:, :], r[i][:, :], mv[:, 0:1], mv[:, 3:4], ALU.subtract, ALU.mult, [f"r{i}", "mv"], ["tmp"])
            _tt(P, tmp[:, :], tmp[:, :], g1[:, :], ALU.mult, ["tmp", "g1"], ["tmp"])
            _tt(P, h1[i][:, :], tmp[:, :], b1[:, :], ALU.add, ["tmp", "b1"], [f"h1_{i}"])
            P.dma("sp", sc["H1"][ts_, :], h1[i][:, :], [f"h1_{i}"], ["H1"])
            P.add("act", lambda e: e.copy(h1b[i][:, :], h1[i][:, :]), [f"h1_{i}"], [f"h1b{i}"])

        def stage2b(tt):
            i = tt % 2
            R = [f"rt{i}"]
            rt_ = rt[i]
            for k4 in range(4):
                j = npt[0] % 2
                npt[0] += 1
                for k in range(4):
                    kk = k4 * 4 + k
                    P.tr(pt[j][:, k, :], h1[i][:, kk * 128:(kk + 1) * 128], identf[:, :], [f"h1_{i}", "identf"], [f"pt{j}"])
                P.add("act", lambda e, j=j, k4=k4: e.copy(h1T[:, k4 * 4:(k4 + 1) * 4, :], pt[j][:, :, :]), [f"pt{j}"], ["h1T"])
            for k in range(16):
                P.mm(pl[:, :], h1T[:, k, :], wr[:, k, :], k == 0, k == 15, ["h1T", "wr"], ["pl"])
            _tt(P, lg[:, :], pl[:, :], br[:, :], ALU.add, ["pl", "br"], ["lg"])
            P.add("dve", lambda e: e.tensor_reduce(rt_[:, 0:1], lg[:, 0:8], AX.X, ALU.max), ["lg"], R)
            _ts(P, rt_[:, 8:16], lg[:, 0:8], rt_[:, 0:1], None, ALU.is_equal, None, ["lg"] + R, R)
            _ts(P, rt_[:, 1:2], rt_[:, 0:1], -1.0, None, ALU.mult, None, R, R)
            P.add("act", lambda e: e.activation(rt_[:, 16:24], lg[:, 0:8], AF.Exp, bias=rt_[:, 1:2], accum_out=rt_[:, 2:3]), ["lg"] + R, R)
            P.add("dve", lambda e: e.reciprocal(rt_[:, 3:4], rt_[:, 2:3]), R, R)
            _tt(P, e3[:, :, :], lg[:, 8:72].rearrange("p (g e) -> p g e", g=8),
                rt_[:, 8:16].unsqueeze(2).broadcast_to([128, 8, 8]), ALU.mult, ["lg"] + R, ["e3"])
            P.add("dve", lambda e: e.tensor_reduce(rt_[:, 24:32], e3[:, :, :].rearrange("p g e -> p e g"), AX.X, ALU.add), ["e3"], R)
            P.add("dve", lambda e: e.max(rt_[:, 32:40], rt_[:, 24:32]), R, R)
            _ts(P, rt_[:, 40:48], rt_[:, 24:32], rt_[:, 32:33], None, ALU.is_equal, None, R, R)
            _ts(P, rt_[:, 48:56], rt_[:, 24:32], rt_[:, 33:34], None, ALU.is_equal, None, R, R)
            _tt(P, rt_[:, 4:5], rt_[:, 32:33], rt_[:, 33:34], ALU.subtract, R, R)
            _act(P, rt_[:, 5:6], rt_[:, 4:5], AF.Sigmoid, R, R)
            _tt(P, rw[i][:, 0:1], rt_[:, 5:6], rt_[:, 3:4], ALU.mult, R, [f"rw{i}"])
            _tt(P, rw[i][:, 1:2], rt_[:, 3:4], rw[i][:, 0:1], ALU.subtract, R + [f"rw{i}"], [f"rw{i}"])
            gb = rt_[:, 8:16].unsqueeze(2).broadcast_to([128, 8, 8])
            _tt(P, E1[i][:, :, :], gb, rt_[:, 40:48].unsqueeze(1).broadcast_to([128, 8, 8]), ALU.mult, R, [f"E1_{i}"])
            _tt(P, E2[i][:, :, :], gb, rt_[:, 48:56].unsqueeze(1).broadcast_to([128, 8, 8]), ALU.mult, R, [f"E2_{i}"])
            E1f = E1[i][:, :, :].rearrange("p g e -> p (g e)")
            E2f = E2[i][:, :, :].rearrange("p g e -> p (g e)")
            _tt(P, indf[:, :], E1f, E2f, ALU.add, [f"E1_{i}", f"E2_{i}"], ["indf"])
            P.add("dve", lambda e: e.tensor_copy(indb[i][:, :], indf[:, :]), ["indf"], [f"indb{i}"])
            P.add("dve", lambda e: e.tensor_copy(accb[i][:, :], accind[:, :]), ["accind"], [f"accb{i}"])
            _tt(P, accind[:, :], accind[:, :], indf[:, :], ALU.add, ["accind", "indf"], ["accind"])

        def s_pos_pe(tt):
            i = tt % 2
            P.mm(pp[:, :], Ut[:, :], indb[i][:, :], True, False, ["Ut", f"indb{i}"], ["pp"])
            P.mm(pp[:, :], ones[:, :], accb[i][:, :], False, True, ["ones", f"accb{i}"], ["pp"])

        def stage3(tt):
            i = tt % 2
            ts_ = slice(tt * 128, (tt + 1) * 128)
            R = [f"rt{i}"]
            rt_ = rt[i]
            P.add("dve", lambda e: e.tensor_copy(posf[:, :], pp[:, :]), ["pp"], ["posf"])
            e3f = e3[:, :, :].rearrange("p g e -> p (g e)")
            for kk, (Eb, ek) in enumerate(((E1[i], f"E1_{i}"), (E2[i], f"E2_{i}"))):
                Ef = Eb[:, :, :].rearrange("p g e -> p (g e)")
                o0 = 56 + kk * 4
                _tt(P, e3f, Ef, posf[:, :], ALU.mult, [ek, "posf"], ["e3"])
                P.add("dve", lambda e, o0=o0: e.tensor_reduce(rt_[:, o0:o0 + 1], e3f, AX.X, ALU.add), ["e3"], R)
                _tt(P, e3f, Ef, eid64[:, :], ALU.mult, [ek, "eid64"], ["e3"])
                P.add("dve", lambda e, o0=o0: e.tensor_reduce(rt_[:, o0 + 1:o0 + 2], e3f, AX.X, ALU.add), ["e3"], R)
                _ts(P, rt_[:, o0 + 2:o0 + 3], rt_[:, o0:o0 + 1], float(CAP), 1.0e6, ALU.is_ge, ALU.mult, R, R)
                _stt(P, rt_[:, o0 + 3:o0 + 4], rt_[:, o0 + 1:o0 + 2], float(2 * CAP), rt_[:, o0:o0 + 1], ALU.mult, ALU.add, R, R)
                _tt(P, rt_[:, o0 + 3:o0 + 4], rt_[:, o0 + 3:o0 + 4], rt_[:, o0 + 2:o0 + 3], ALU.add, R, R)
                _tt(P, rt_[:, o0 + 3:o0 + 4], rt_[:, o0 + 3:o0 + 4], pbase[:, :], ALU.add, R + ["pbase"], R)
                P.add("dve", lambda e, kk=kk, o0=o0: e.tensor_copy(ridx[i][:, kk:kk + 1], rt_[:, o0 + 3:o0 + 4]), R, [f"ridx{i}"])
            P.dma("sp", sc["Ridx"][ts_, :], ridx[i][:, :], [f"ridx{i}"], ["Ridx"])
            P.dma("sp", sc["Rw"][ts_, :], rw[i][:, :], [f"rw{i}"], ["Rw"])
            for kk in range(2):
                P.add("pool", lambda e, kk=kk: e.indirect_dma_start(
                    out=sc["Xg"][:, :], out_offset=bass.IndirectOffsetOnAxis(ap=ridx[i][:, kk:kk + 1], axis=0),
                    in_=h1b[i][:, :], in_offset=None, bounds_check=_breg(e, breg), oob_is_err=False),
                    [f"h1b{i}", f"ridx{i}"], ["Xg_s"], dma=True)

        stage1(0)
        stage1b(0)
        for tt in range(16):
            if tt >= 1:
                s_pos_pe(tt - 1)
            if tt + 1 < 16:
                stage1(tt + 1)
            s_ln1(tt)
            if tt >= 1:
                stage3(tt - 1)
            s_ln2(tt)
            stage2b(tt)
            if tt + 1 < 16:
                stage1b(tt + 1)
        s_pos_pe(15)
        stage3(15)
        P.emit(st)


def phase_D(nc, io, sc):
    with ExitStack() as st:
        C = Ctx(nc, st, "D")
        P = C.P
        breg = {}
        ident = C.sb("ident", [128, 128], BF16)
        P.dma("sp", ident[:, :], io["ident"], [], ["ident"])
        idxd = C.sb("idxd", [128, 64], I32)
        P.dma("sp", idxd[:, :], io["idxD"], [], ["idxd"])
        wg = [C.sb(f"wg{i}", [128, 16, 512], BF16) for i in range(2)]
        wu = [C.sb(f"wu{i}", [128, 16, 512], BF16) for i in range(2)]
        wd = [C.sb(f"wd{i}", [128, 4, 2048], BF16) for i in range(2)]
        xe = [C.sb(f"xe{i}", [128, 2048], BF16) for i in range(4)]
        xT = [C.sb(f"xT{i}", [128, 16, 128], BF16) for i in range(2)]
        sg = C.sb("sg", [128, 512], F32)
        hm = C.sb("hm", [128, 512], BF16)
        hT = C.sb("hT", [128, 4, 128], BF16)
        ye = [C.sb(f"ye{i}", [128, 2048], F32) for i in range(2)]
        pg = C.ps("pg", [128, 512], F32)
        pu = C.ps("pu", [128, 512], F32)
        py = [C.ps(f"py{i}", [128, 512], F32) for i in range(2)]
        pT = [C.ps(f"pT{i}", [128, 8, 128], BF16) for i in range(2)]
        n = [0]
        NE = NEXP // 2

        def loads(el):
            i = el % 2
            P.dma("pool", wg[i][:, :, :].rearrange("p (a b) n -> p a (b n)", b=4),
                  io["w_gate"][el].rearrange("(p a b) n -> p a (b n)", p=128, b=4), [], [f"wg{i}"])
            P.dma("pool", wu[i][:, :, :].rearrange("p (a b) n -> p a (b n)", b=4),
                  io["w_up"][el].rearrange("(p a b) n -> p a (b n)", p=128, b=4), [], [f"wu{i}"])
            P.dma("pool", wd[i][:, :, :], io["w_down"][el].rearrange("(p k) n -> p k n", p=128), [], [f"wd{i}"])
            for s_ in range(2):
                u = el * 2 + s_
                xi = u % 4
                P.add("pool", lambda e, u=u, xi=xi: e.indirect_dma_start(
                    out=xe[xi][:, :], out_offset=None, in_=sc["Xg"][:, :],
                    in_offset=bass.IndirectOffsetOnAxis(ap=idxd[:, u:u + 1], axis=0),
                    bounds_check=_breg(e, breg), oob_is_err=False), ["idxd", "Xg"], [f"xe{xi}"], dma=True)

        def compute(el):
            i = el % 2
            for s_ in range(2):
                u = el * 2 + s_
                xi = u % 4
                ti = u % 2
                for hh in range(2):
                    for k in range(8):
                        kk = hh * 8 + k
                        P.tr(pT[hh][:, k, :], xe[xi][:, kk:2048:16], ident[:, :], [f"xe{xi}", "ident"], [f"pT{hh}"])
                    if hh == 0:
                        P.add("act", lambda e, ti=ti: e.copy(xT[ti][:, 0:8, :], pT[0][:, :, :]), ["pT0"], [f"xT{ti}"])
                    else:
                        P.add("dve", lambda e, ti=ti: e.tensor_copy(xT[ti][:, 8:16, :], pT[1][:, :, :]), ["pT1"], [f"xT{ti}"])
                for k in range(16):
                    P.mm(pg[:, :], xT[ti][:, k, :], wg[i][:, k, :], k == 0, k == 15, [f"xT{ti}", f"wg{i}"], ["pg"])
                for k in range(16):
                    P.mm(pu[:, :], xT[ti][:, k, :], wu[i][:, k, :], k == 0, k == 15, [f"xT{ti}", f"wu{i}"], ["pu"])
                _act(P, sg[:, :], pg[:, :], AF.Silu, ["pg"], ["sg"])
                _tt(P, hm[:, :], sg[:, :], pu[:, :], ALU.mult, ["sg", "pu"], ["hm"])
                for k in range(4):
                    P.tr(pT[0][:, k, :], hm[:, k:512:4], ident[:, :], ["hm", "ident"], ["pT0"])
                P.add("act", lambda e: e.copy(hT[:, :, :], pT[0][:, 0:4, :]), ["pT0"], ["hT"])
                for cc in range(4):
                    j = n[0] % 2
                    n[0] += 1
                    cs = slice(cc * 512, (cc + 1) * 512)
                    for k in range(4):
                        P.mm(py[j][:, :], hT[:, k, :], wd[i][:, k, cs], k == 0, k == 3, ["hT", f"wd{i}"], [f"py{j}"])
                    if cc % 2 == 0:
                        P.add("act", lambda e, ti=ti, j=j, cs=cs: e.copy(ye[ti][:, cs], py[j][:, :]), [f"py{j}"], [f"ye{ti}"])
                    else:
                        P.add("dve", lambda e, ti=ti, j=j, cs=cs: e.tensor_copy(ye[ti][:, cs], py[j][:, :]), [f"py{j}"], [f"ye{ti}"])
                P.add("pool", lambda e, u=u, ti=ti: e.indirect_dma_start(
                    out=sc["Yg"][:, :], out_offset=bass.IndirectOffsetOnAxis(ap=idxd[:, u:u + 1], axis=0),
                    in_=ye[ti][:, :], in_offset=None, bounds_check=_breg(e, breg), oob_is_err=False),
                    [f"ye{ti}", "idxd"], ["Yg_s"], dma=True)

        loads(0)
        for el in range(NE):
            if el + 1 < NE:
                loads(el + 1)
            compute(el)
        P.emit(st)


def phase_E(nc, io, sc):
    with ExitStack() as st:
        C = Ctx(nc, st, "E")
        P = C.P
        breg = {}
        g2 = C.sb("g2", [128, 2048], F32)
        b2 = C.sb("b2", [128, 2048], F32)
        P.dma("sp", g2[:, :], io["ln2_g"], [], ["g2"])
        P.dma("sp", b2[:, :], io["ln2_b"], [], ["b2"])
        y1 = [C.sb(f"y1_{i}", [128, 2048], F32) for i in range(2)]
        y2 = [C.sb(f"y2_{i}", [128, 2048], F32) for i in range(2)]
        h1 = [C.sb(f"h1_{i}", [128, 2048], F32) for i in range(2)]
        ridx = [C.sb(f"ridx{i}", [128, 2], I32) for i in range(2)]
        rw = [C.sb(f"rw{i}", [128, 2], F32) for i in range(2)]
        st6 = [C.sb(f"st6_{i}", [128, 24], F32) for i in range(2)]
        mv = [C.sb(f"mv{i}", [128, 8], F32) for i in range(2)]
        for i in range(2):
            P.add("pool", lambda e, i=i: e.memset(mv[i][:, 4:5], LN_EPS), [], [f"mv{i}"])

        def part1(tt):
            i = tt % 2
            ts_ = slice(tt * 128, (tt + 1) * 128)
            P.dma("sp", ridx[i][:, :], sc["Ridx"][ts_, :], ["Ridx"], [f"ridx{i}"])
            P.dma("sp", rw[i][:, :], sc["Rw"][ts_, :], ["Rw"], [f"rw{i}"])
            P.dma("sp", h1[i][:, :], sc["H1"][ts_, :], ["H1"], [f"h1_{i}"])
            P.add("pool", lambda e: e.memset(y1[i][:, :], 0.0), [], [f"y1_{i}"])
            P.add("pool", lambda e: e.memset(y2[i][:, :], 0.0), [], [f"y2_{i}"])
            for kk, yb, yk in ((0, y1[i], f"y1_{i}"), (1, y2[i], f"y2_{i}")):
                P.add("pool", lambda e, kk=kk, yb=yb: e.indirect_dma_start(
                    out=yb[:, :], out_offset=None, in_=sc["Yg"][:, :],
                    in_offset=bass.IndirectOffsetOnAxis(ap=ridx[i][:, kk:kk + 1], axis=0),
                    bounds_check=_breg(e, breg), oob_is_err=False), [f"ridx{i}", "Yg", yk], [yk + "g"], dma=True)
            y1k = [f"y1_{i}", f"y1_{i}g"]
            y2k = [f"y2_{i}", f"y2_{i}g"]
            _ts(P, y1[i][:, :], y1[i][:, :], rw[i][:, 0:1], None, ALU.mult, None, y1k + [f"rw{i}"], [f"y1_{i}"])
            _stt(P, y1[i][:, :], y2[i][:, :], rw[i][:, 1:2], y1[i][:, :], ALU.mult, ALU.add, y1k + y2k + [f"rw{i}"], [f"y1_{i}"])
            _stt(P, h1[i][:, :], h1[i][:, :], DN_ALPHA, y1[i][:, :], ALU.mult, ALU.add, [f"h1_{i}"] + y1k, [f"h1_{i}"])
            for c in range(4):
                P.add("dve", lambda e, c=c: e.bn_stats(st6[i][:, c * 6:(c + 1) * 6], h1[i][:, c * 512:(c + 1) * 512]),
                      [f"h1_{i}"], [f"st6_{i}"])
            P.add("dve", lambda e: e.bn_aggr(mv[i][:, 0:2], st6[i][:, 0:24]), [f"st6_{i}"], [f"mv{i}"])
            _act(P, mv[i][:, 2:3], mv[i][:, 1:2], AF.Sqrt, [f"mv{i}"], [f"mv{i}"], bias=mv[i][:, 4:5])
            P.add("dve", lambda e: e.reciprocal(mv[i][:, 3:4], mv[i][:, 2:3]), [f"mv{i}"], [f"mv{i}"])

        def part2(tt):
            i = tt % 2
            ts_ = slice(tt * 128, (tt + 1) * 128)
            _ts(P, y2[i][:, :], h1[i][:, :], mv[i][:, 0:1], mv[i][:, 3:4], ALU.subtract, ALU.mult,
                [f"h1_{i}", f"mv{i}", f"y2_{i}g"], [f"y2_{i}"])
            _tt(P, y2[i][:, :], y2[i][:, :], g2[:, :], ALU.mult, [f"y2_{i}", "g2"], [f"y2_{i}"])
            _tt(P, y1[i][:, :], y2[i][:, :], b2[:, :], ALU.add, [f"y2_{i}", "b2", f"y1_{i}g"], [f"y1_{i}"])
            P.dma("sp", sc["out"][ts_, :], y1[i][:, :], [f"y1_{i}"], ["out"])

        part1(0)
        for tt in range(16):
            if tt + 1 < 16:
                part1(tt + 1)
            part2(tt)
        P.emit(st)


EXPERTS = None
_IN_C = {
    "identf": ([128, 128], F32), "w_nsa_proj": ([1024, 2048], F32), "w_pool_proj": ([1024, 2048], F32),
    "w_out": ([2048, 2048], F32), "w_router": ([2048, 72], F32), "b_router": ([128, 72], F32),
    "eid64": ([128, 64], F32), "utri": ([128, 128], BF16), "ln1_g": ([128, 2048], F32), "ln1_b": ([128, 2048], F32),
    "ln2_g": ([128, 2048], F32), "ln2_b": ([128, 2048], F32), "x_own": ([NT, 2048], F32),
    "w_gate": ([NEXP // 2, 2048, 512], F32), "w_up": ([NEXP // 2, 2048, 512], F32), "w_down": ([NEXP // 2, 512, 2048], F32),
    "pbase": ([128, 1], F32), "idxD": ([128, 64], I32),
}
_SC_C = {
    "ZT": ([16, 128, NT], BF16), "H1": ([NT, 2048], F32), "Xg": ([NROW, 2048], BF16),
    "Yg": ([NROW, 2048], F32), "Ridx": ([NT, 2], I32), "Rw": ([NT, 2], F32),
}


def _shared_inputs(inp):
    w_in = inp["w_in"][0]
    ca = np.ascontiguousarray
    m = {
        "ident": _bf16(np.eye(128)), "identf": np.eye(128, dtype=np.float32),
        "w_A1": ca(np.concatenate([w_in[:, 2560:3072], w_in[:, 2048:2560]], 1)),
        "w_A2": ca(w_in[:, 3072:3584]), "w_q": ca(w_in[:, 1024:2048]), "w_gn": ca(w_in[:, 3584:3632]),
        "w_pool": ca(w_in[:, 0:1024]), "w_gm": ca(w_in[:, 3632:7728]),
        "pool_mix": ca(inp["pool_mix"][0]), "pool_scale": ca(inp["pool_scale"][0].reshape(8, 128).T),
        "cmp_k_w1": ca(inp["cmp_k_w1"][0]), "cmp_v_w1": ca(inp["cmp_v_w1"][0]),
        "cmp_k_w2": ca(inp["cmp_k_w2"][0]), "cmp_v_w2": ca(inp["cmp_v_w2"][0]),
        "cmp_k_w2s": ca(np.concatenate([inp["cmp_k_w2"][0][:, 32:], inp["cmp_k_w2"][0][:, :32]], 1)),
        "cmp_pos_kT": ca(inp["cmp_pos_k"][0].T), "cmp_pos_vT": ca(inp["cmp_pos_v"][0].T),
        "w_nsa_proj": ca(inp["w_nsa_proj"][0]), "w_pool_proj": ca(inp["w_pool_proj"][0]), "w_out": ca(inp["w_out"][0]),
        "w_router": ca(np.concatenate([inp["router_group_w"][0],
                                       inp["router_expert_w"][0].transpose(1, 0, 2).reshape(D, 64)], 1)),
        "b_router": ca(np.broadcast_to(np.concatenate([inp["router_group_b"][0], inp["router_expert_b"][0].reshape(64)])[None, :], (128, 72))),
        "eid64": ca(np.broadcast_to(np.arange(64, dtype=np.float32)[None, :], (128, 64))),
        "utri": _bf16((np.arange(128)[:, None] < np.arange(128)[None, :]).astype(np.float32)),
    }
    for p in range(2):
        sl = slice(p * (NEXP // 2), (p + 1) * (NEXP // 2))
        m[f"w_gate{p}"] = ca(inp["w_gate"][0][sl])
        m[f"w_up{p}"] = ca(inp["w_up"][0][sl])
        m[f"w_down{p}"] = ca(inp["w_down"][0][sl])
    for n in ("ln1_g", "ln1_b", "ln2_g", "ln2_b"):
        m[n] = ca(np.broadcast_to(inp[n][0][None, :], (128, D)))
    return m


def _core_inputs(inp, shared, core, tabs):
    b, p = core // 2, core % 2
    t = tabs[p]
    x0 = inp["x"][b]
    xo = x0[t["own_pos"]]
    xp = x0[t["prev_pos"]] * t["prev_valid"][:, None]
    m = {k: v for k, v in shared.items() if not k.startswith(("w_gate", "w_up", "w_down"))}
    for n_ in ("w_gate", "w_up", "w_down"):
        m[n_] = shared[f"{n_}{p}"]
    m["pbase"] = np.full((128, 1), float(CAP * p), np.float32)
    el = np.arange(NEXP // 2)
    rows = ((p * (NEXP // 2) + el[None, :, None]) * 2 + np.arange(2)[None, None, :]) * CAP + np.arange(CAP)[:, None, None]
    m["idxD"] = np.ascontiguousarray(rows.reshape(CAP, NEXP).astype(np.int32))
    m["xTg"] = np.ascontiguousarray(x0.T)
    m["xTo"] = np.ascontiguousarray(xo.T)
    m["xTp"] = np.ascontiguousarray(xp.T)
    m["x_own"] = np.ascontiguousarray(xo)
    for k in ALL_IN:
        if k not in m:
            m[k] = t[k]
    return m


ALL_IN = {}
ALL_IN.update(_IN_A)
ALL_IN.update(_IN_B)
ALL_IN.update(_IN_C)
ALL_SC = {}
ALL_SC.update(_SC_A)
ALL_SC.update(_SC_C)
PHASES = [phase_A, phase_B, phase_C1, "core_barrier", phase_C2, "core_barrier", phase_D, "core_barrier", phase_E]


def build_full():
    return build_nc(ALL_IN, ALL_SC, PHASES, final_out=("out", [NT, D], F32))


def kernel(**inputs):
    inp = {k: np.asarray(v) for k, v in inputs.items()}
    tabs = []
    for p in range(2):
        t = _core_tables(p)
        tabs.append(_core_tables_B(p, t))
    shared = _shared_inputs(inp)
    in_maps = [_core_inputs(inp, shared, c, tabs) for c in range(8)]
    nc = build_full()
    res = run_bass_kernel_spmd(nc, in_maps, core_ids=list(range(8)))
    out = np.zeros((4, S, D), np.float32)
    for c in range(8):
        b, p = c // 2, c % 2
        out[b, tabs[p]["own_pos"]] = np.asarray(res.results[c]["out"]).astype(np.float32)
    return out
```
